# Optimizing a Trainium2 kernel written in Bass

```python
import math
import jax
import jax.numpy as jnp
from jax import lax
import numpy as np


D_MODEL = 1024
BATCH = 2
SEQ = 8192
DEPTH = 2

GRID_W = 64
CTX_LEN = 256
N_MIXERS = 2
RMS_EPS = 1e-6

S5_GROUP = 16
S5_GROUPS = D_MODEL // S5_GROUP
S5_STATE = 64
S5_DT_MIN = 1e-3
S5_DT_MAX = 1e-1

SSD_EXPAND = 2
SSD_D_INNER = SSD_EXPAND * D_MODEL
SSD_HEAD_DIM = 64
SSD_HEADS = SSD_D_INNER // SSD_HEAD_DIM
SSD_GROUPS = 8
SSD_STATE = 128
SSD_CONV = 5
SSD_CHUNK = 128
SSD_CONV_CH = SSD_D_INNER + 2 * SSD_GROUPS * SSD_STATE
SSD_PROJ = SSD_D_INNER + SSD_CONV_CH + 2 * SSD_HEADS
SSD_DT_MIN = 1e-3
SSD_DT_MAX = 1e-1

N_EXPERTS = 16
N_EXPERT_GROUPS = 4
EXPERTS_PER_GROUP = N_EXPERTS // N_EXPERT_GROUPS
TOP_K = 2
D_EXPERT = 512

F32 = jnp.float32

kernel_name = 'hybrid_s5_ssd_moe_prefix_dit'


def rmsnorm(x, g):
    xf = x.astype(F32)
    y = xf * lax.rsqrt(jnp.mean(xf * xf, axis=-1, keepdims=True) + RMS_EPS)
    return y.astype(x.dtype) * g


def adaln(xn, shift, scale):
    return xn * (1.0 + scale) + shift


def _cmul(ar, ai, br, bi):
    return ar * br - ai * bi, ar * bi + ai * br


def _s5_combine(e1, e2):
    a1r, a1i, b1r, b1i = e1
    a2r, a2i, b2r, b2i = e2
    ar, ai = _cmul(a2r, a2i, a1r, a1i)
    br, bi = _cmul(a2r, a2i, b1r, b1i)
    return ar, ai, br + b2r, bi + b2i


def s5_scan(u, lam_re, lam_im, log_dt, b_re, b_im, c_re, c_im, h0_re, h0_im, with_y):
    bsz, length, d = u.shape
    uf = u.astype(F32).reshape(bsz, length, S5_GROUPS, S5_GROUP)
    lr = lam_re.astype(F32)
    li = lam_im.astype(F32)
    step = jnp.exp(log_dt.astype(F32))[:, None]
    mag = jnp.exp(lr * step)
    abar_r = mag * jnp.cos(li * step)
    abar_i = mag * jnp.sin(li * step)
    den = lr * lr + li * li
    q_r = ((abar_r - 1.0) * lr + abar_i * li) / den
    q_i = (abar_i * lr - (abar_r - 1.0) * li) / den
    bbar_r, bbar_i = _cmul(q_r[..., None], q_i[..., None], b_re.astype(F32), b_im.astype(F32))
    bu_r = jnp.einsum('blgk,gpk->lbgp', uf, bbar_r)
    bu_i = jnp.einsum('blgk,gpk->lbgp', uf, bbar_i)
    i_r, i_i = _cmul(abar_r, abar_i, h0_re.astype(F32), h0_im.astype(F32))
    bu_r = bu_r.at[0].add(i_r)
    bu_i = bu_i.at[0].add(i_i)
    a_r = jnp.broadcast_to(abar_r, (length, 1) + abar_r.shape)
    a_i = jnp.broadcast_to(abar_i, (length, 1) + abar_i.shape)
    _, _, h_r, h_i = lax.associative_scan(_s5_combine, (a_r, a_i, bu_r, bu_i), axis=0)
    final = (h_r[-1], h_i[-1])
    if not with_y:
        return None, final
    y = (jnp.einsum('lbgp,gkp->blgk', h_r, c_re.astype(F32))
         - jnp.einsum('lbgp,gkp->blgk', h_i, c_im.astype(F32)))
    return y.reshape(bsz, length, d).astype(u.dtype), final


def s5_mixer(hn, cn, lam_re, lam_im, log_dt, b_re, b_im, c_re, c_im, d_skip, glu_w, glu_b, need_ctx):
    bsz, _, d = hn.shape
    zero = jnp.zeros((bsz, S5_GROUPS, S5_STATE), F32)

    def run(u, k, h0, with_y):
        return s5_scan(u, lam_re[k], lam_im[k], log_dt[k], b_re, b_im, c_re, c_im, h0[0], h0[1], with_y)

    ycf, hf = run(cn, 0, (zero, zero), need_ctx)
    ycb, hb = run(cn[:, ::-1], 1, (zero, zero), need_ctx)
    ylf, _ = run(hn, 0, hf, True)
    ylb, _ = run(hn[:, ::-1], 1, hb, True)

    def head(yf, yb_rev, u):
        g = jax.nn.gelu(yf + yb_rev[:, ::-1] + d_skip * u)
        z = g @ glu_w + glu_b
        return z[..., :d] * jax.nn.sigmoid(z[..., d:])

    out_lat = head(ylf, ylb, hn)
    out_ctx = head(ycf, ycb, cn) if need_ctx else None
    return out_lat, out_ctx


def dwconv(u, w, b):
    out = lax.conv_general_dilated(
        u, w[:, None, :].astype(u.dtype), (1,), [(SSD_CONV // 2, SSD_CONV // 2)],
        dimension_numbers=('NWC', 'WIO', 'NWC'), feature_group_count=u.shape[-1])
    return out + b


def segsum(v):
    t = v.shape[-1]
    cs = jnp.cumsum(v, axis=-1)
    diff = cs[..., :, None] - cs[..., None, :]
    mask = jnp.tril(jnp.ones((t, t), dtype=bool))
    return jnp.where(mask, diff, -jnp.inf)


def ssd_chunked(xdt, da, bm, cm, h0, with_y):
    bsz, length, nh, p = xdt.shape
    g, n = bm.shape[2], bm.shape[3]
    r = nh // g
    nc = length // SSD_CHUNK
    xc = xdt.reshape(bsz, nc, SSD_CHUNK, g, r, p)
    a = da.reshape(bsz, nc, SSD_CHUNK, g, r).transpose(0, 3, 4, 1, 2)
    bc = bm.reshape(bsz, nc, SSD_CHUNK, g, n)
    cc = cm.reshape(bsz, nc, SSD_CHUNK, g, n)
    a_cs = jnp.cumsum(a, axis=-1)
    decay_states = jnp.exp(a_cs[..., -1:] - a_cs)
    states = jnp.einsum('bcsgn,bgrcs,bcsgrp->bcgrpn', bc, decay_states, xc)
    states = jnp.concatenate([h0.reshape(bsz, 1, g, r, p, n), states], axis=1)
    chunk_tot = jnp.pad(a_cs[..., -1], ((0, 0), (0, 0), (0, 0), (1, 0)))
    decay_chunk = jnp.exp(segsum(chunk_tot))
    new_states = jnp.einsum('bgrzc,bcgrpn->bzgrpn', decay_chunk, states)
    final = new_states[:, -1].reshape(bsz, nh, p, n)
    if not with_y:
        return None, final
    prev = new_states[:, :-1]
    lmat = jnp.exp(segsum(a))
    cb = jnp.einsum('bcqgn,bcsgn->bcgqs', cc, bc)
    y_diag = jnp.einsum('bcgqs,bgrcqs,bcsgrp->bcqgrp', cb, lmat, xc)
    y_off = jnp.einsum('bcqgn,bcgrpn,bgrcq->bcqgrp', cc, prev, jnp.exp(a_cs))
    return (y_diag + y_off).reshape(bsz, length, nh, p), final


def ssd_mixer(hn, cn, in_w, conv_w, conv_b, dt_bias, a_log, d_skip, norm_g, out_w, need_ctx):
    bsz, seq, d = hn.shape
    rows = seq // GRID_W
    a = -jnp.exp(a_log.astype(F32))

    def project(t):
        length = t.shape[1]
        zxbcdt = t @ in_w
        z, xbc, dt_raw = jnp.split(zxbcdt, [SSD_D_INNER, SSD_D_INNER + SSD_CONV_CH], axis=-1)
        xbc = jax.nn.silu(dwconv(xbc, conv_w, conv_b))
        xs, bm, cm = jnp.split(xbc, [SSD_D_INNER, SSD_D_INNER + SSD_GROUPS * SSD_STATE], axis=-1)
        dt = jax.nn.softplus(dt_raw.astype(F32).reshape(bsz, length, 2, SSD_HEADS) + dt_bias.astype(F32))
        return (z, xs.reshape(bsz, length, SSD_HEADS, SSD_HEAD_DIM),
                bm.reshape(bsz, length, SSD_GROUPS, SSD_STATE),
                cm.reshape(bsz, length, SSD_GROUPS, SSD_STATE), dt)

    def scan(xs, bm, cm, dt, k, h0, with_y):
        if k == 1:
            xs, bm, cm, dt = xs[:, ::-1], bm[:, ::-1], cm[:, ::-1], dt[:, ::-1]
        dtk = dt[:, :, k]
        y, hl = ssd_chunked(xs.astype(F32) * dtk[..., None], dtk * a[k],
                            bm.astype(F32), cm.astype(F32), h0, with_y)
        if with_y and k == 1:
            y = y[:, ::-1]
        return y, hl

    def finish(z, xs, yf, yb):
        y = yf + yb + d_skip.astype(F32)[:, None] * xs.astype(F32)
        y = y.reshape(bsz, -1, SSD_D_INNER).astype(z.dtype)
        return rmsnorm(y * jax.nn.silu(z), norm_g) @ out_w

    zero = jnp.zeros((bsz, SSD_HEADS, SSD_HEAD_DIM, SSD_STATE), F32)
    zc, xc, bcx, ccx, dtc = project(cn)
    ycf, hf = scan(xc, bcx, ccx, dtc, 0, zero, need_ctx)
    ycb, hb = scan(xc, bcx, ccx, dtc, 1, zero, need_ctx)
    hp = hn.reshape(bsz, rows, GRID_W, d).transpose(0, 2, 1, 3).reshape(bsz, seq, d)
    zl, xl, bl, cl, dtl = project(hp)
    ylf, _ = scan(xl, bl, cl, dtl, 0, hf, True)
    ylb, _ = scan(xl, bl, cl, dtl, 1, hb, True)
    out = finish(zl, xl, ylf, ylb)
    out = out.reshape(bsz, GRID_W, rows, d).transpose(0, 2, 1, 3).reshape(bsz, seq, d)
    out_ctx = finish(zc, xc, ycf, ycb) if need_ctx else None
    return out, out_ctx


def moe(t, router_w, router_b, w1, w3, w2):
    n = t.shape[0]
    s = jax.nn.sigmoid((t @ router_w).astype(F32))
    sel = (s + router_b.astype(F32)).reshape(n, N_EXPERT_GROUPS, EXPERTS_PER_GROUP)
    group_score = lax.top_k(sel, TOP_K)[0].sum(-1)
    gidx = jnp.argmax(group_score, axis=-1)
    in_group = jnp.take_along_axis(sel, gidx[:, None, None], axis=1)[:, 0]
    _, loc = lax.top_k(in_group, TOP_K)
    eidx = gidx[:, None] * EXPERTS_PER_GROUP + loc
    w = jnp.take_along_axis(s, eidx, axis=1)
    w = w / jnp.sum(w, axis=-1, keepdims=True)
    gates = jnp.sum(jax.nn.one_hot(eidx, N_EXPERTS, dtype=F32) * w[..., None], axis=1)
    y = jnp.zeros_like(t)
    for e in range(N_EXPERTS):
        h = jax.nn.silu(t @ w1[e]) * (t @ w3[e])
        y = y + gates[:, e:e + 1].astype(t.dtype) * (h @ w2[e])
    return y


def setup_inputs(seed: int = 0) -> dict:
    key = jax.random.key(seed)
    ks = iter(jax.random.split(key, 48))

    def nrm(shape, scale):
        return jax.random.normal(next(ks), shape, F32) * scale

    def unif(shape, lo, hi):
        return jax.random.uniform(next(ks), shape, F32, lo, hi)

    d = D_MODEL
    n_a = (DEPTH + N_MIXERS - 1) // N_MIXERS
    n_b = DEPTH // N_MIXERS
    n_idx = jnp.arange(S5_STATE, dtype=F32)
    ssd_dt0 = jnp.exp(unif((n_b, 2, SSD_HEADS), math.log(SSD_DT_MIN), math.log(SSD_DT_MAX)))
    return {
        'x': nrm((BATCH, SEQ, d), 1.0),
        'c': nrm((BATCH, d), 1.0),
        'ctx': nrm((BATCH, CTX_LEN, d), 1.0),
        'c_ctx': nrm((d,), 1.0),
        'mod_w': nrm((DEPTH, d, 6 * d), 0.5 * d ** -0.5),
        'mod_b': nrm((DEPTH, 6 * d), 0.01),
        'norm1_g': 1.0 + nrm((DEPTH, d), 0.1),
        'norm2_g': 1.0 + nrm((DEPTH, d), 0.1),
        'final_g': 1.0 + nrm((d,), 0.1),
        's5_lam_re': -0.5 + nrm((n_a, 2, S5_GROUPS, S5_STATE), 0.01),
        's5_lam_im': jnp.pi * n_idx + nrm((n_a, 2, S5_GROUPS, S5_STATE), 0.01),
        's5_log_dt': unif((n_a, 2, S5_GROUPS), math.log(S5_DT_MIN), math.log(S5_DT_MAX)),
        's5_b_re': nrm((n_a, S5_GROUPS, S5_STATE, S5_GROUP), (2 * S5_GROUP) ** -0.5),
        's5_b_im': nrm((n_a, S5_GROUPS, S5_STATE, S5_GROUP), (2 * S5_GROUP) ** -0.5),
        's5_c_re': nrm((n_a, S5_GROUPS, S5_GROUP, S5_STATE), (2 * S5_STATE) ** -0.5),
        's5_c_im': nrm((n_a, S5_GROUPS, S5_GROUP, S5_STATE), (2 * S5_STATE) ** -0.5),
        's5_d': 1.0 + nrm((n_a, d), 0.1),
        's5_glu_w': nrm((n_a, d, 2 * d), d ** -0.5),
        's5_glu_b': nrm((n_a, 2 * d), 0.01),
        'ssd_in_w': nrm((n_b, d, SSD_PROJ), d ** -0.5),
        'ssd_conv_w': nrm((n_b, SSD_CONV, SSD_CONV_CH), SSD_CONV ** -0.5),
        'ssd_conv_b': nrm((n_b, SSD_CONV_CH), 0.01),
        'ssd_dt_bias': ssd_dt0 + jnp.log(-jnp.expm1(-ssd_dt0)),
        'ssd_a_log': jnp.log(unif((n_b, 2, SSD_HEADS), 1.0, 16.0)),
        'ssd_d': 1.0 + nrm((n_b, SSD_HEADS), 0.1),
        'ssd_norm_g': 1.0 + nrm((n_b, SSD_D_INNER), 0.1),
        'ssd_out_w': nrm((n_b, SSD_D_INNER, d), SSD_D_INNER ** -0.5),
        'router_w': nrm((d, N_EXPERTS), d ** -0.5),
        'router_b': nrm((N_EXPERTS,), 0.01),
        'moe_w1': nrm((DEPTH, N_EXPERTS, d, D_EXPERT), d ** -0.5),
        'moe_w3': nrm((DEPTH, N_EXPERTS, d, D_EXPERT), d ** -0.5),
        'moe_w2': nrm((DEPTH, N_EXPERTS, D_EXPERT, d), D_EXPERT ** -0.5),
    }


def reference(x, c, ctx, c_ctx, mod_w, mod_b, norm1_g, norm2_g, final_g,
              s5_lam_re, s5_lam_im, s5_log_dt, s5_b_re, s5_b_im, s5_c_re, s5_c_im, s5_d, s5_glu_w, s5_glu_b,
              ssd_in_w, ssd_conv_w, ssd_conv_b, ssd_dt_bias, ssd_a_log, ssd_d, ssd_norm_g, ssd_out_w,
              router_w, router_b, moe_w1, moe_w3, moe_w2):
    d = x.shape[-1]
    n_ctx = ctx.shape[1]
    for i in range(DEPTH):
        need_ctx = i < DEPTH - 1
        m_lat = jax.nn.silu(c) @ mod_w[i] + mod_b[i]
        m_ctx = jax.nn.silu(c_ctx) @ mod_w[i] + mod_b[i]
        sh1, sc1, g1, sh2, sc2, g2 = jnp.split(m_lat[:, None, :], 6, axis=-1)
        sh1c, sc1c, g1c, sh2c, sc2c, g2c = jnp.split(m_ctx, 6)

        hn = adaln(rmsnorm(x, norm1_g[i]), sh1, sc1)
        cn = adaln(rmsnorm(ctx, norm1_g[i]), sh1c, sc1c)
        j = i // N_MIXERS
        if i % N_MIXERS == 0:
            y, yc = s5_mixer(hn, cn, s5_lam_re[j], s5_lam_im[j], s5_log_dt[j], s5_b_re[j], s5_b_im[j],
                             s5_c_re[j], s5_c_im[j], s5_d[j], s5_glu_w[j], s5_glu_b[j], need_ctx)
        else:
            y, yc = ssd_mixer(hn, cn, ssd_in_w[j], ssd_conv_w[j], ssd_conv_b[j], ssd_dt_bias[j],
                              ssd_a_log[j], ssd_d[j], ssd_norm_g[j], ssd_out_w[j], need_ctx)
        x = x + g1 * y

        hn2 = adaln(rmsnorm(x, norm2_g[i]), sh2, sc2)
        if need_ctx:
            ctx = ctx + g1c * yc
            cn2 = adaln(rmsnorm(ctx, norm2_g[i]), sh2c, sc2c)
            tokens = jnp.concatenate([cn2, hn2], axis=1)
            out = moe(tokens.reshape(-1, d), router_w, router_b,
                      moe_w1[i], moe_w3[i], moe_w2[i]).reshape(tokens.shape)
            ctx = ctx + g2c * out[:, :n_ctx]
            x = x + g2 * out[:, n_ctx:]
        else:
            out = moe(hn2.reshape(-1, d), router_w, router_b,
                      moe_w1[i], moe_w3[i], moe_w2[i]).reshape(hn2.shape)
            x = x + g2 * out
    return rmsnorm(x, final_g)
```

```python
import math, time, sys
import numpy as np
import contextlib
import concourse.bass as bass
import concourse.mybir as mybir
from concourse.bass_utils import run_bass_kernel_spmd

F32 = mybir.dt.float32
BF16 = mybir.dt.bfloat16
AF = mybir.ActivationFunctionType
ALU = mybir.AluOpType
AX = mybir.AxisListType

ENGS = ("pe", "dve", "act", "pool", "sp")


class Buf:
    def __init__(self, t, name, excl=False):
        self.t = t
        self.name = name
        self.excl = excl
        self.w = None
        self.r = []

    def __getitem__(self, k):
        return self.t[k]


class Prog:
    def __init__(self, name="k"):
        self.nc = bass.Bass("TRN2", target_bir_lowering=False)
        self.es = contextlib.ExitStack()
        self.q = {e: [] for e in ENGS}
        self.cnt = {e: 0 for e in ENGS}
        self.known = {e: {} for e in ENGS}
        self.sems = {}
        self.dcnt = {}
        self.mult = {}
        self.nbuf = 0
        self.sb_bytes = 0
        for e in ENGS:
            self.sems[e] = self.es.enter_context(self.nc.semaphore("s_" + e))
        self.NDS = 48
        self.rr = 0
        for i in range(self.NDS):
            nm = f"dq{i}"
            self.sems[nm] = self.es.enter_context(self.nc.semaphore(nm))
            self.dcnt[nm] = 0
            self.mult[nm] = 16

    def dram(self, name, shape, dtype=F32, kind="ExternalInput"):
        t = self.nc.dram_tensor(name, list(shape), dtype, kind=kind)
        return Buf(t.ap(), name)

    def sb(self, shape, dtype=F32, name=None):
        self.nbuf += 1
        name = name or f"sb{self.nbuf}"
        t = self.es.enter_context(self.nc.sbuf_tensor(name, list(shape), dtype))
        sz = int(np.prod(shape[1:])) * (4 if dtype == F32 else 2)
        self.sb_bytes += sz
        return Buf(t, name)

    def init_pool(self, words=48 * 1024):
        self.big = self.es.enter_context(self.nc.sbuf_tensor("big", [128, words], F32))
        self.words = words
        self.top = 0
        self.hi = words
        self.peak = 0
        self.banks = [self.es.enter_context(self.nc.psum_tensor(f"bank{i}", [128, 512], F32)) for i in range(8)]
        self.pb = [Buf(self.banks[i][:], f"bank{i}", excl=True) for i in range(8)]

    def alloc(self, shape, dtype=F32, name=None, hi=False):
        n = int(np.prod(shape[1:]))
        w = n if dtype == F32 else (n + 1) // 2
        assert self.top + w <= self.hi, f"SBUF overflow {self.top}+{w} > {self.hi} ({name})"
        if hi:
            self.hi -= w
            ap = self.big[:, self.hi:self.hi + w]
        else:
            ap = self.big[:, self.top:self.top + w]
            self.top += w
        self.peak = max(self.peak, self.top + (self.words - self.hi))
        if dtype != F32:
            ap = ap.bitcast(dtype)[:, 0:n]
        if len(shape) > 2:
            names = " ".join(f"d{i}" for i in range(1, len(shape)))
            kw = {f"d{i}": shape[i] for i in range(1, len(shape))}
            ap = ap.rearrange(f"p ({names}) -> p {names}", **kw)
        if shape[0] < 128:
            ap = ap[0:shape[0]]
        self.nbuf += 1
        return Buf(ap, name or f"a{self.nbuf}")


    def barrier(self):
        snap_c = dict(self.cnt)
        snap_d = dict(self.dcnt)
        for e in ENGS:
            for src, n in snap_c.items():
                if src != e and n > self.known[e].get(src, 0):
                    self.q[e].append(("wait", src, n))
                    self.known[e][src] = n
            for s_, n in snap_d.items():
                if n > self.known[e].get(s_, 0):
                    self.q[e].append(("wait", s_, n * self.mult[s_]))
                    self.known[e][s_] = n

    def mark(self):
        return self.top

    def release(self, m):
        self.barrier()
        self.top = m

    def ps(self, shape, dtype=F32, name=None):
        self.nbuf += 1
        name = name or f"ps{self.nbuf}"
        t = self.es.enter_context(self.nc.psum_tensor(name, list(shape), dtype))
        return Buf(t, name)

    def stream(self, s):
        if s not in self.sems:
            self.sems[s] = self.es.enter_context(self.nc.semaphore("d_" + s))
            self.dcnt[s] = 0
            self.mult[s] = 16
        return s

    def _deps(self, eng, reads, writes):
        deps = {}

        def add(x):
            if x is None:
                return
            src, n = x
            if deps.get(src, 0) < n:
                deps[src] = n

        for b in reads:
            add(b.w)
            if b.excl:
                for x in b.r:
                    if x[0] != eng:
                        add(x)
        for b in writes:
            add(b.w)
            for x in b.r:
                add(x)
        for src, n in deps.items():
            if src == eng and eng == "pe":
                continue
            if self.known[eng].get(src, 0) >= n:
                continue
            self.known[eng][src] = n
            mult = self.mult[src] if src in self.dcnt else 1
            self.q[eng].append(("wait", src, n * mult))

    def _mark(self, tag, reads, writes):
        for b in reads:
            b.r.append(tag)
        for b in writes:
            b.w = tag
            b.r = []

    def op(self, eng, fn, reads=(), writes=()):
        self._deps(eng, reads, writes)
        self.cnt[eng] += 1
        self.q[eng].append(("op", fn))
        self._mark((eng, self.cnt[eng]), reads, writes)

    def dma(self, out, in_, reads=(), writes=(), eng="sp", stream=None, **kw):
        s_ = f"dq{self.rr % self.NDS}"
        self.rr += 1
        self._deps(eng, reads, writes)
        prev = self.dcnt[s_]
        if prev and self.known[eng].get(s_, 0) < prev:
            self.q[eng].append(("wait", s_, prev * 16))
            self.known[eng][s_] = prev
        self.dcnt[s_] += 1
        self.q[eng].append(("dma", s_, out, in_, kw))
        self._mark((s_, self.dcnt[s_]), reads, writes)

    def cc(self, fn, reads=(), writes=(), eng="pool", stream="cc"):
        self.stream(stream)
        self.mult[stream] = 1
        self._deps(eng, reads, writes)
        prev = self.dcnt[stream]
        if prev and self.known[eng].get(stream, 0) < prev:
            self.q[eng].append(("wait", stream, prev))
            self.known[eng][stream] = prev
        self.dcnt[stream] += 1
        self.q[eng].append(("cc", stream, fn))
        self._mark((stream, self.dcnt[stream]), reads, writes)

    def wait_all(self, eng="sp"):
        for s, n in self.dcnt.items():
            if n:
                self.q[eng].append(("wait", s, self.mult[s] * n))
        for e in ENGS:
            if e != eng and self.cnt[e]:
                self.q[eng].append(("wait", e, self.cnt[e]))

    def finish(self):
        nc = self.nc
        sems = self.sems

        def replay(e):
            def body(h):
                for item in self.q[e]:
                    if item[0] == "wait":
                        h.wait_ge(sems[item[1]], item[2])
                    elif item[0] == "op":
                        item[1](h).then_inc(sems[e], 1)
                    elif item[0] == "cc":
                        item[2](h).then_inc(sems[item[1]], 1)
                    else:
                        _, s, out, in_, kw = item
                        h.dma_start(out=out, in_=in_, **kw).then_inc(sems[s], 16)
            return body

        with nc.Block() as block:
            block.tensor(replay("pe"))
            block.vector(replay("dve"))
            block.scalar(replay("act"))
            block.gpsimd(replay("pool"))
            block.sync(replay("sp"))
        self.es.close()
        return nc


def run(prog, in_maps):
    nc = prog.finish()
    res = run_bass_kernel_spmd(nc, in_maps, core_ids=list(range(len(in_maps))))
    return res.results


def _bufs(xs):
    return [x for x in xs if isinstance(x, Buf)]


def e_act(p, out, in_, func, r, w, eng="act", **kw):
    p.op(eng, lambda h: h.activation(out=out, in_=in_, func=func, **kw), reads=r, writes=w)


def e_tt(p, eng, out, in0, in1, op, r, w):
    p.op(eng, lambda h: h.tensor_tensor(out=out, in0=in0, in1=in1, op=op), reads=r, writes=w)


def e_ts(p, eng, out, in0, s1, s2, op0, op1, r, w):
    if op1 is None:
        if op0 == ALU.add:
            op1, s2 = ALU.mult, 1.0
        else:
            op1, s2 = ALU.add, 0.0
    if True:
        p.op(eng, lambda h: h.tensor_scalar(out=out, in0=in0, scalar1=s1, scalar2=s2, op0=op0, op1=op1), reads=r, writes=w)


def e_stt(p, out, in0, scalar, in1, op0, op1, r, w):
    p.op("dve", lambda h: h.scalar_tensor_tensor(out=out, in0=in0, scalar=scalar, in1=in1, op0=op0, op1=op1),
         reads=r, writes=w)


def e_copy(p, eng, out, in_, r, w):
    if eng == "act":
        p.op(eng, lambda h: h.activation(out=out, in_=in_, func=AF.Copy), reads=r, writes=w)
    else:
        p.op(eng, lambda h: h.tensor_copy(out=out, in_=in_), reads=r, writes=w)


def e_mm(p, out, lhsT, rhs, start, stop, r, w):
    p.op("pe", lambda h: h.matmul(out, lhsT=lhsT, rhs=rhs, start=start, stop=stop), reads=r, writes=w)


def e_tr(p, out, in_, ident, r, w):
    p.op("pe", lambda h: h.transpose(out, in_, ident), reads=r, writes=w)


def rev_ap(ap):
    (pst, pn), (st, n) = ap.ap
    return bass.AP(ap.tensor, ap.offset + (n - 1) * st, [[pst, pn], [-st, n]])


D = 1024
NL = 2048
NCX = 256
NT = NL + NCX
PI = math.pi


def phase_mod(p, cT, modw, modb, modsc, ident):
    m = p.mark()
    c_sb = p.alloc([128, 8, 2]); s_sb = p.alloc([128, 8, 2])
    p.dma(c_sb[:], cT[:], writes=[c_sb])
    e_act(p, s_sb[:], c_sb[:], AF.Silu, [c_sb], [s_sb])
    b_sb = p.alloc([1, 6144]); ones = p.alloc([1, 2])
    p.dma(b_sb[:], modb[:], writes=[b_sb])
    p.op("pool", lambda h: h.memset(ones[:], 1.0), writes=[ones])
    wb = [p.alloc([128, 8, 512]) for _ in range(2)]
    ob = [p.alloc([2, 512]) for _ in range(2)]
    for nb in range(12):
        w = wb[nb % 2]; o = ob[nb % 2]; ps = p.pb[nb % 2]
        p.dma(w[:], modw[:, nb * 512:(nb + 1) * 512].rearrange("(k q) n -> q k n", q=128), writes=[w])
        for k in range(8):
            e_mm(p, ps[0:2, :], s_sb[:, k, :], w[:, k, :], k == 0, False, [s_sb, w], [ps])
        e_mm(p, ps[0:2, :], ones[:], b_sb[:, nb * 512:(nb + 1) * 512], False, True, [ones, b_sb], [ps])
        e_copy(p, "act", o[:], ps[0:2, :], [ps], [o])
        p.dma(modsc[:, nb * 512:(nb + 1) * 512], o[:], reads=[o], writes=[modsc], stream="st")
    p.release(m)


def load_bcast(p, dst, src_row_ap, r, w):
    p.dma(dst, src_row_ap.to_broadcast([128, src_row_ap.shape[-1]]), reads=r, writes=w)


def phase_hn(p, xl, xc, modsc, gvec, ident, uT, part_sh, part_sc):
    m = p.mark()
    g_sb = p.alloc([128, D]); A = [p.alloc([128, D]) for _ in range(2)]; SH = [p.alloc([128, D]) for _ in range(2)]
    load_bcast(p, g_sb[:], gvec[0:1, :], [], [g_sb])
    for which in range(2):
        load_bcast(p, A[which][:], modsc[which:which + 1, part_sc * D:(part_sc + 1) * D], [modsc], [A[which]])
        load_bcast(p, SH[which][:], modsc[which:which + 1, part_sh * D:(part_sh + 1) * D], [modsc], [SH[which]])
        e_stt(p, A[which][:], A[which][:], 1.0, g_sb[:], ALU.add, ALU.mult, [A[which], g_sb], [A[which]])
    nbuf = 2
    xs = [p.alloc([128, D]) for _ in range(nbuf)]; ys = [p.alloc([128, D]) for _ in range(nbuf)]
    junk = p.alloc([128, D]); ss = [p.alloc([128, 1]) for _ in range(nbuf)]; rs = [p.alloc([128, 1]) for _ in range(nbuf)]
    for t in range(18):
        which = 1 if t < 2 else 0
        src = xc[t * 128:(t + 1) * 128, :] if t < 2 else xl[(t - 2) * 128:(t - 1) * 128, :]
        x = xs[t % nbuf]; y = ys[t % nbuf]; s = ss[t % nbuf]; r = rs[t % nbuf]
        p.dma(x[:], src, writes=[x])
        e_act(p, junk[:], x[:], AF.Square, [x], [junk, s], accum_out=s[:])
        e_ts(p, "dve", r[:], s[:], 1.0 / D, 1e-6, ALU.mult, ALU.add, [s], [r])
        e_act(p, r[:], r[:], AF.Sqrt, [r], [r])
        p.op("dve", lambda h, r=r: h.reciprocal(out=r[:], in_=r[:]), reads=[r], writes=[r])
        e_stt(p, y[:], x[:], r[:], A[which][:], ALU.mult, ALU.mult, [x, r, A[which]], [y])
        e_tt(p, "pool", y[:], y[:], SH[which][:], ALU.add, [y, SH[which]], [y])
        for half in range(2):
            ps = p.pb[(2 * t + half) % 4]
            for c4 in range(4):
                ct = half * 4 + c4
                e_tr(p, ps[:, c4 * 128:(c4 + 1) * 128], y[:, ct * 128:(ct + 1) * 128], ident[:], [y, ident], [ps])
            e_copy(p, "act", uT[:, half * 4:(half + 1) * 4, t * 128:(t + 1) * 128],
                   ps[:].rearrange("q (c n) -> q c n", c=4), [ps], [uT])
    p.release(m)


def cmul(p, eng, outr, outi, ar, ai, br, bi, t1, t2, bufs_r, bufs_w):
    e_tt(p, eng, t1, ar, br, ALU.mult, bufs_r, bufs_w)
    e_tt(p, eng, t2, ai, bi, ALU.mult, bufs_r, bufs_w)
    e_tt(p, eng, outr, t1, t2, ALU.subtract, bufs_r, bufs_w)
    e_tt(p, eng, t1, ar, bi, ALU.mult, bufs_r, bufs_w)
    e_tt(p, eng, t2, ai, br, ALU.mult, bufs_r, bufs_w)
    e_tt(p, eng, outi, t1, t2, ALU.add, bufs_r, bufs_w)


class S5:
    pass


def s5_params(p, io, ident):
    S = S5()
    lam = p.alloc([128, 2, 64]); ldt = p.alloc([128, 64])
    p.dma(lam[:], io["lamP"][:], writes=[lam]); p.dma(ldt[:], io["ldtP"][:], writes=[ldt])
    lr = lam[:, 0, :]; li = lam[:, 1, :]
    W = p.alloc([128, 10, 64], name="s5w")
    sl = lambda i: W[:, i, :]
    R = [W]
    step, th, mag, t1, t2, cs, sn, den, am1 = (sl(i) for i in range(9))
    S.ar = p.alloc([128, 64]); S.ai = p.alloc([128, 64]); S.cth = p.alloc([128, 64]); S.sth = p.alloc([128, 64])
    S.mag = p.alloc([128, 64]); S.qr = p.alloc([128, 64]); S.qi = p.alloc([128, 64])
    e_act(p, step, ldt[:], AF.Exp, [ldt], R)
    e_tt(p, "dve", th, li, step, ALU.mult, [lam, W], R)
    e_tt(p, "dve", t1, lr, step, ALU.mult, [lam, W], R)
    e_act(p, S.mag[:], t1, AF.Exp, R, [S.mag])
    e_act(p, sn, th, AF.Sin, R, R, scale=1.0 / 16)
    e_ts(p, "dve", t1, th, 1.0 / 16, PI / 2, ALU.mult, ALU.add, R, R)
    e_act(p, cs, t1, AF.Sin, R, R)
    for _ in range(4):
        e_tt(p, "dve", t1, cs, cs, ALU.mult, R, R)
        e_tt(p, "dve", t2, sn, sn, ALU.mult, R, R)
        e_tt(p, "dve", sn, sn, cs, ALU.mult, R, R)
        e_ts(p, "dve", sn, sn, 2.0, None, ALU.mult, None, R, R)
        e_tt(p, "dve", cs, t1, t2, ALU.subtract, R, R)
    e_copy(p, "dve", S.cth[:], cs, R, [S.cth]); e_copy(p, "dve", S.sth[:], sn, R, [S.sth])
    e_tt(p, "dve", S.ar[:], S.mag[:], cs, ALU.mult, R + [S.mag], [S.ar])
    e_tt(p, "dve", S.ai[:], S.mag[:], sn, ALU.mult, R + [S.mag], [S.ai])
    e_tt(p, "dve", t1, lr, lr, ALU.mult, [lam], R)
    e_tt(p, "dve", t2, li, li, ALU.mult, [lam], R)
    e_tt(p, "dve", den, t1, t2, ALU.add, R, R)
    p.op("dve", lambda h: h.reciprocal(out=den, in_=den), reads=R, writes=R)
    e_ts(p, "dve", am1, S.ar[:], -1.0, None, ALU.add, None, [S.ar], R)
    e_tt(p, "dve", t1, am1, lr, ALU.mult, R + [lam], R)
    e_tt(p, "dve", t2, S.ai[:], li, ALU.mult, [S.ai, lam], R)
    e_tt(p, "dve", t1, t1, t2, ALU.add, R, R)
    e_tt(p, "dve", S.qr[:], t1, den, ALU.mult, R, [S.qr])
    e_tt(p, "dve", t1, S.ai[:], lr, ALU.mult, [S.ai, lam], R)
    e_tt(p, "dve", t2, am1, li, ALU.mult, R + [lam], R)
    e_tt(p, "dve", t1, t1, t2, ALU.subtract, R, R)
    e_tt(p, "dve", S.qi[:], t1, den, ALU.mult, R, [S.qi])
    S.wc = p.alloc([128, 11, 64]); S.ws = p.alloc([128, 11, 64])
    e_copy(p, "dve", S.wc[:, 0, :], S.cth[:], [S.cth], [S.wc]); e_copy(p, "dve", S.ws[:, 0, :], S.sth[:], [S.sth], [S.ws])
    for k in range(10):
        e_tt(p, "dve", t1, S.wc[:, k, :], S.wc[:, k, :], ALU.mult, [S.wc], R)
        e_tt(p, "dve", t2, S.ws[:, k, :], S.ws[:, k, :], ALU.mult, [S.ws], R)
        e_tt(p, "dve", S.wc[:, k + 1, :], t1, t2, ALU.subtract, R, [S.wc])
        e_tt(p, "dve", t1, S.wc[:, k, :], S.ws[:, k, :], ALU.mult, [S.wc, S.ws], R)
        e_ts(p, "dve", S.ws[:, k + 1, :], t1, 2.0, None, ALU.mult, None, R, [S.ws])
    S.nws = p.alloc([128, 11, 64])
    e_ts(p, "dve", S.nws[:], S.ws[:], -1.0, None, ALU.mult, None, [S.ws], [S.nws])
    S.Ar = p.alloc([128, 64]); S.Ai = p.alloc([128, 64])
    e_copy(p, "dve", S.Ar[:], S.ar[:], [S.ar], [S.Ar]); e_copy(p, "dve", S.Ai[:], S.ai[:], [S.ai], [S.Ai])
    for k in range(11):
        e_tt(p, "dve", t1, S.Ar[:], S.Ar[:], ALU.mult, [S.Ar], R)
        e_tt(p, "dve", t2, S.Ai[:], S.Ai[:], ALU.mult, [S.Ai], R)
        e_tt(p, "dve", den, S.Ar[:], S.Ai[:], ALU.mult, [S.Ar, S.Ai], R)
        e_tt(p, "dve", S.Ar[:], t1, t2, ALU.subtract, R, [S.Ar])
        e_ts(p, "dve", S.Ai[:], den, 2.0, None, ALU.mult, None, R, [S.Ai])
    S.cP = p.alloc([128, 2, 32, 16]); p.dma(S.cP[:], io["cP"][:], writes=[S.cP])
    S.Bb = p.alloc([128, 2, 2, 32, 16])
    m = p.mark()
    bP = p.alloc([128, 2, 32, 16]); p.dma(bP[:], io["bP"][:], writes=[bP])
    Bb = S.Bb
    tA = p.alloc([128, 32, 16]); tB = p.alloc([128, 32, 16])
    for d in range(2):
        qr_b = S.qr[:, d * 32:(d + 1) * 32].unsqueeze(2).to_broadcast([128, 32, 16])
        qi_b = S.qi[:, d * 32:(d + 1) * 32].unsqueeze(2).to_broadcast([128, 32, 16])
        e_tt(p, "dve", tA[:], bP[:, 0], qr_b, ALU.mult, [bP, S.qr], [tA])
        e_tt(p, "dve", tB[:], bP[:, 1], qi_b, ALU.mult, [bP, S.qi], [tB])
        e_tt(p, "dve", Bb[:, d, 0], tA[:], tB[:], ALU.subtract, [tA, tB], [Bb])
        e_tt(p, "dve", tA[:], bP[:, 1], qr_b, ALU.mult, [bP, S.qr], [tA])
        e_tt(p, "dve", tB[:], bP[:, 0], qi_b, ALU.mult, [bP, S.qi], [tB])
        e_tt(p, "dve", Bb[:, d, 1], tA[:], tB[:], ALU.add, [tA, tB], [Bb])
    p.release(m)
    return S


def s5_build_ct(p, S, ct, ident, BbT, Cp, src):
    i = 0
    for gpl in range(4):
        gp = ct * 4 + gpl
        for d in range(2):
            for ri in range(2):
                s_ = src[i % 2]; ps = p.pb[7]
                for g2 in range(2):
                    e_copy(p, "dve", s_[g2 * 64:(g2 + 1) * 64, gpl, g2, :], S.Bb[g2 * 64:(g2 + 1) * 64, d, ri, gp, :], [S.Bb], [s_])
                e_tr(p, ps[:, (i % 4) * 128:(i % 4 + 1) * 128], s_[:].rearrange("q a b c -> q (a b c)"), ident[:], [s_, ident], [ps])
                e_copy(p, "act", BbT[:, gpl, d, ri, :], ps[:, (i % 4) * 128:(i % 4 + 1) * 128], [ps], [BbT])
                for g2 in range(2):
                    p.op("pool", lambda h, s_=s_, g2=g2, gpl=gpl: h.memset(s_[g2 * 64:(g2 + 1) * 64, gpl, g2, :], 0.0),
                         reads=[], writes=[s_])
                i += 1
    p.op("pool", lambda h: h.memset(Cp[:], 0.0), writes=[Cp])
    Cv = Cp[:].rearrange("q g r (a b k) -> q g r a b k", a=4, b=2)
    for gpl in range(4):
        gp = ct * 4 + gpl
        for g2 in range(2):
            rows = slice(g2 * 64, (g2 + 1) * 64)
            e_copy(p, "dve", Cv[rows, gpl, 0, gpl, g2, :], S.cP[rows, 0, gp, :], [S.cP], [Cp])
            e_ts(p, "dve", Cv[rows, gpl, 1, gpl, g2, :], S.cP[rows, 1, gp, :], -1.0, None, ALU.mult, None, [S.cP], [Cp])


def s5_tables(p, S, col, cosT, sinT):
    p.op("pool", lambda h: h.memset(cosT[:, 0:1], 1.0), writes=[cosT])
    p.op("pool", lambda h: h.memset(sinT[:, 0:1], 0.0), writes=[sinT])
    R = [cosT, sinT, S.wc, S.ws, S.nws]
    for k in range(11):
        n = 1 << k
        wc = S.wc[:, k, col:col + 1]; ws = S.ws[:, k, col:col + 1]; nws = S.nws[:, k, col:col + 1]
        lo = slice(0, n); hi = slice(n, 2 * n)
        e_ts(p, "dve", cosT[:, hi], cosT[:, lo], wc, None, ALU.mult, None, R, [cosT])
        e_stt(p, cosT[:, hi], sinT[:, lo], nws, cosT[:, hi], ALU.mult, ALU.add, R, [cosT])
        e_ts(p, "dve", sinT[:, hi], sinT[:, lo], wc, None, ALU.mult, None, R, [sinT])
        e_stt(p, sinT[:, hi], cosT[:, lo], ws, sinT[:, hi], ALU.mult, ALU.add, R, [sinT])


def s5_pass(p, S, uT, tabsc, full, Fsum, kin, ident, yacc_evac=None):
    m = p.mark()
    cos2 = [p.alloc([128, 2048]) for _ in range(2)]; sin2 = [p.alloc([128, 2048]) for _ in range(2)]
    TL = 2048
    CH = 512
    nb = 2
    bur = [p.alloc([128, CH]) for _ in range(nb)]; bui = [p.alloc([128, CH]) for _ in range(nb)]
    m1 = [p.alloc([128, CH]) for _ in range(nb)]; m2 = [p.alloc([128, CH]) for _ in range(nb)]
    cr = [p.alloc([128, CH]) for _ in range(nb)]; ci = [p.alloc([128, CH]) for _ in range(nb)]
    kr = [p.alloc([128, CH]) for _ in range(nb)]; ki = [p.alloc([128, CH]) for _ in range(nb)]
    hr = [p.alloc([128, CH], BF16) for _ in range(nb)]; hi = [p.alloc([128, CH], BF16) for _ in range(nb)]
    tiny = p.alloc([128, 4])
    BbT2 = [p.alloc([128, 4, 2, 2, 128], BF16) for _ in range(2)]
    Cp2 = [p.alloc([128, 4, 2, 128], BF16) for _ in range(2)]
    srcs = [p.alloc([128, 4, 2, 16]) for _ in range(2)]
    for s_ in srcs:
        p.op("pool", lambda h, s_=s_: h.memset(s_[:], 0.0), writes=[s_])
    subs = [(0, 0, NCX), (1, NCX, NL)]
    it = 0
    ci_ = 0
    for ct in range(8):
        BbT = BbT2[ct % 2]; Cp = Cp2[ct % 2]
        s5_build_ct(p, S, ct, ident, BbT, Cp, srcs)
        for gpl in range(4):
            gp = ct * 4 + gpl
            for d in range(2):
                col = d * 32 + gp
                if not full:
                    s5_tables(p, S, col, cos2[0], sin2[0])
                    if d == 0:
                        cosT = cos2[0]; sinT = sin2[0]
                    else:
                        cosT = cos2[1]; sinT = sin2[1]
                        e_copy(p, "dve", rev_ap(cosT[:]), cos2[0][:], [cos2[0]], [cosT])
                        e_copy(p, "dve", rev_ap(sinT[:]), sin2[0][:], [sin2[0]], [sinT])
                    p.dma(tabsc[col, 0], cosT[:], reads=[cosT], writes=[tabsc], stream="tb")
                    p.dma(tabsc[col, 1], sinT[:], reads=[sinT], writes=[tabsc], stream="tb")
                else:
                    cosT = cos2[it % 2]; sinT = sin2[it % 2]
                    it += 1
                    p.dma(cosT[:], tabsc[col, 0], reads=[tabsc], writes=[cosT], stream="tbl")
                    p.dma(sinT[:], tabsc[col, 1], reads=[tabsc], writes=[sinT], stream="tbl")
                mcol = S.mag[:, col:col + 1]
                for (which, c0, L) in subs:
                    nch = (L + CH - 1) // CH
                    carry_r = None
                    for cj in range(nch):
                        n = min(CH, L)
                        a = cj * n if d == 0 else L - (cj + 1) * n
                        tau0 = cj * n
                        b_ = ci_ % nb
                        ci_ += 1
                        tok = slice(c0 + a, c0 + a + n)
                        pr = p.pb[5]; pi_ = p.pb[6]
                        e_mm(p, pr[:, 0:n], BbT[:, gpl, d, 0, :], uT[:, ct, tok], True, True, [BbT, uT], [pr])
                        e_mm(p, pi_[:, 0:n], BbT[:, gpl, d, 1, :], uT[:, ct, tok], True, True, [BbT, uT], [pi_])
                        e_copy(p, "act", bur[b_][:, 0:n], pr[:, 0:n], [pr], [bur[b_]])
                        e_copy(p, "act", bui[b_][:, 0:n], pi_[:, 0:n], [pi_], [bui[b_]])
                        br = bur[b_][:, 0:n]; bi = bui[b_][:, 0:n]
                        ts0 = tau0 if d == 0 else (TL - L) + a
                        cs = cosT[:, ts0:ts0 + n]; sn = sinT[:, ts0:ts0 + n]
                        R = [bur[b_], bui[b_], cosT, sinT]
                        e_tt(p, "dve", m1[b_][:, 0:n], cs, br, ALU.mult, R, [m1[b_]])
                        e_tt(p, "dve", m2[b_][:, 0:n], sn, bi, ALU.mult, R, [m2[b_]])
                        e_tt(p, "dve", m1[b_][:, 0:n], m1[b_][:, 0:n], m2[b_][:, 0:n], ALU.add, [m1[b_], m2[b_]], [m1[b_]])
                        e_tt(p, "dve", cr[b_][:, 0:n], cs, bi, ALU.mult, R, [cr[b_]])
                        e_tt(p, "dve", ci[b_][:, 0:n], sn, br, ALU.mult, R, [ci[b_]])
                        e_tt(p, "dve", cr[b_][:, 0:n], cr[b_][:, 0:n], ci[b_][:, 0:n], ALU.subtract, [cr[b_], ci[b_]], [cr[b_]])
                        if cj == 0:
                            if full and which == 1:
                                ini_r = kin[:, 0, col:col + 1]; ini_i = kin[:, 1, col:col + 1]; rd = [kin]
                            else:
                                ini_r = 0.0; ini_i = 0.0; rd = []
                        else:
                            ini_r = carry_r; ini_i = carry_i; rd = [carry_br, carry_bi]
                        mb = mcol.to_broadcast([128, n])
                        ko_r = kr[b_][:, 0:n]; ko_i = ki[b_][:, 0:n]; xi_r = m1[b_][:, 0:n]; xi_i = cr[b_][:, 0:n]
                        if d == 1:
                            ko_r = rev_ap(ko_r); ko_i = rev_ap(ko_i); xi_r = rev_ap(xi_r); xi_i = rev_ap(xi_i)
                        p.op("dve", lambda h, o=ko_r, x=xi_r, ini=ini_r, mb=mb: h.tensor_tensor_scan(
                            out=o, data0=mb, data1=x, initial=ini, op0=ALU.mult, op1=ALU.add),
                            reads=[m1[b_], S.mag] + rd, writes=[kr[b_]])
                        p.op("dve", lambda h, o=ko_i, x=xi_i, ini=ini_i, mb=mb: h.tensor_tensor_scan(
                            out=o, data0=mb, data1=x, initial=ini, op0=ALU.mult, op1=ALU.add),
                            reads=[cr[b_], S.mag] + rd, writes=[ki[b_]])
                        lastc = n - 1 if d == 0 else 0
                        carry_r = kr[b_][:, lastc:lastc + 1]; carry_i = ki[b_][:, lastc:lastc + 1]
                        carry_br = kr[b_]; carry_bi = ki[b_]
                        if full:
                            o_r = hr[b_][:, 0:n]; o_i = hi[b_][:, 0:n]
                            K = [kr[b_], ki[b_], cosT, sinT]
                            e_tt(p, "dve", m1[b_][:, 0:n], cs, kr[b_][:, 0:n], ALU.mult, K, [m1[b_]])
                            e_tt(p, "dve", m2[b_][:, 0:n], sn, ki[b_][:, 0:n], ALU.mult, K, [m2[b_]])
                            e_tt(p, "dve", o_r, m1[b_][:, 0:n], m2[b_][:, 0:n], ALU.subtract, [m1[b_], m2[b_]], [hr[b_]])
                            e_tt(p, "dve", cr[b_][:, 0:n], sn, kr[b_][:, 0:n], ALU.mult, K, [cr[b_]])
                            e_tt(p, "dve", ci[b_][:, 0:n], cs, ki[b_][:, 0:n], ALU.mult, K, [ci[b_]])
                            e_tt(p, "dve", o_i, cr[b_][:, 0:n], ci[b_][:, 0:n], ALU.add, [cr[b_], ci[b_]], [hi[b_]])
                            if which == 0:
                                bank = p.pb[0]; bsl = slice(0, n)
                            else:
                                bank = p.pb[1 + a // CH]; bsl = slice(0, n)
                            first = (gpl == 0 and d == 0)
                            last = (gpl == 3 and d == 1)
                            e_mm(p, bank[:, bsl], Cp[:, gpl, 0, :], hr[b_][:, 0:n], first, False, [Cp, hr[b_]], [bank])
                            e_mm(p, bank[:, bsl], Cp[:, gpl, 1, :], hi[b_][:, 0:n], False, last, [Cp, hi[b_]], [bank])
                    if not full:
                        fl = L - 1 if d == 0 else TL - L
                        csl = cosT[:, fl:fl + 1]; snl = sinT[:, fl:fl + 1]
                        Kt = [carry_br, carry_bi, cosT, sinT, tiny]
                        e_tt(p, "dve", tiny[:, 0:1], csl, carry_r, ALU.mult, Kt, [tiny])
                        e_tt(p, "dve", tiny[:, 1:2], snl, carry_i, ALU.mult, Kt, [tiny])
                        e_tt(p, "dve", Fsum[:, 0, which, col:col + 1], tiny[:, 0:1], tiny[:, 1:2], ALU.subtract, [tiny], [Fsum])
                        e_tt(p, "dve", tiny[:, 2:3], snl, carry_r, ALU.mult, Kt, [tiny])
                        e_tt(p, "dve", tiny[:, 3:4], csl, carry_i, ALU.mult, Kt, [tiny])
                        e_tt(p, "dve", Fsum[:, 1, which, col:col + 1], tiny[:, 2:3], tiny[:, 3:4], ALU.add, [tiny], [Fsum])
        if full:
            yacc_evac(ct)
    p.release(m)


def s5_incoming(p, S, Fsum, gath, onehot, kin):
    m = p.mark()
    I = p.alloc([128, 4, 2, 64])
    t1 = p.alloc([128, 32]); t2 = p.alloc([128, 32]); nr = p.alloc([128, 32]); ni = p.alloc([128, 32])
    f = slice(0, 32); b = slice(32, 64)
    R = [I, gath, Fsum, S.Ar, S.Ai, t1, t2, nr, ni]
    e_copy(p, "dve", I[:, 0, 0, f], Fsum[:, 0, 0, f], R, [I]); e_copy(p, "dve", I[:, 0, 1, f], Fsum[:, 1, 0, f], R, [I])
    for q in range(3):
        cmul(p, "dve", nr[:], ni[:], S.Ar[:, f], S.Ai[:, f], I[:, q, 0, f], I[:, q, 1, f], t1[:], t2[:], R, [t1, t2, nr, ni])
        e_tt(p, "dve", I[:, q + 1, 0, f], nr[:], gath[:, q, 0, f], ALU.add, R, [I])
        e_tt(p, "dve", I[:, q + 1, 1, f], ni[:], gath[:, q, 1, f], ALU.add, R, [I])
    e_copy(p, "dve", I[:, 3, 0, b], Fsum[:, 0, 0, b], R, [I]); e_copy(p, "dve", I[:, 3, 1, b], Fsum[:, 1, 0, b], R, [I])
    for q in (3, 2, 1):
        cmul(p, "dve", nr[:], ni[:], S.Ar[:, b], S.Ai[:, b], I[:, q, 0, b], I[:, q, 1, b], t1[:], t2[:], R, [t1, t2, nr, ni])
        e_tt(p, "dve", I[:, q - 1, 0, b], nr[:], gath[:, q, 0, b], ALU.add, R, [I])
        e_tt(p, "dve", I[:, q - 1, 1, b], ni[:], gath[:, q, 1, b], ALU.add, R, [I])
    own = p.alloc([128, 2, 64])
    e_ts(p, "dve", own[:], I[:, 0], onehot[:, 0:1], None, ALU.mult, None, [I, onehot], [own])
    for q in range(1, 4):
        e_stt(p, own[:], I[:, q], onehot[:, q:q + 1], own[:], ALU.mult, ALU.add, [I, onehot, own], [own])
    t3 = p.alloc([128, 64]); t4 = p.alloc([128, 64])
    cmul(p, "dve", kin[:, 0, :], kin[:, 1, :], S.cth[:], S.sth[:], own[:, 0, :], own[:, 1, :], t3[:], t4[:],
         [S.cth, S.sth, own, t3, t4, kin], [t3, t4, kin])
    p.release(m)


def build_s5_test():
    p = Prog(); p.init_pool()
    io = {}
    for name, shape in [("xl", [NL, D]), ("xc", [NCX, D]), ("cT", [128, 8, 2]), ("modw", [D, 6144]), ("modb", [1, 6144]),
                        ("n1g", [1, D]), ("ident", [128, 128]), ("lamP", [128, 2, 64]), ("ldtP", [128, 64]),
                        ("bP", [128, 2, 32, 16]), ("cP", [128, 2, 32, 16]), ("dP", [128, 8]), ("onehot", [128, 4])]:
        io[name] = p.dram(name, shape)
    dbg = p.dram("dbg", [128, 8, NT], kind="ExternalOutput")
    dbgF = p.dram("dbgF", [128, 2, 2, 64], kind="ExternalOutput")
    dbgK = p.dram("dbgK", [128, 2, 64], kind="ExternalOutput")
    modsc = p.dram("modsc", [2, 6144], kind="Internal")
    tabsc = p.dram("tabsc", [64, 2, 128, 2048], kind="Internal")
    fsrc = p.dram("fsrc", [128, 128], kind="Internal")
    fdst = p.dram("fdst", [512, 128], kind="Internal")
    ident = p.alloc([128, 128]); p.dma(ident[:], io["ident"][:], writes=[ident])
    onehot = p.alloc([128, 4]); p.dma(onehot[:], io["onehot"][:], writes=[onehot])
    dP = p.alloc([128, 8]); p.dma(dP[:], io["dP"][:], writes=[dP])
    phase_mod(p, io["cT"], io["modw"], io["modb"], modsc, ident)
    uT = p.alloc([128, 8, NT], BF16, name="uT")
    phase_hn(p, io["xl"], io["xc"], modsc, io["n1g"], ident, uT, 0, 1)
    S = s5_params(p, io, ident)
    Fsum = p.alloc([128, 2, 2, 64]); kin = p.alloc([128, 2, 64])
    s5_pass(p, S, uT, tabsc, False, Fsum, None, ident)
    p.dma(fsrc[:].rearrange("q (r c) -> q r c", r=2), Fsum[:, :, 1, :], reads=[Fsum], writes=[fsrc], stream="st")
    p.cc(lambda h: h.collective_compute("AllGather", ALU.bypass, replica_groups=[[0, 1, 2, 3], [4, 5, 6, 7]],
                                        ins=[fsrc.t.opt()], outs=[fdst.t.opt()]), reads=[fsrc], writes=[fdst])
    gath = p.alloc([128, 4, 2, 64])
    p.dma(gath[:], fdst[:].rearrange("(q x) (r c) -> x q r c", x=128, r=2), reads=[fdst], writes=[gath])
    s5_incoming(p, S, Fsum, gath, onehot, kin)
    p.dma(dbgF[:], Fsum[:], reads=[Fsum], stream="st"); p.dma(dbgK[:], kin[:], reads=[kin], stream="st")
    vbuf = [p.alloc([128, 512]) for _ in range(2)]

    def evac(ct):
        for bi_ in range(5):
            n = NCX if bi_ == 0 else 512
            c0 = 0 if bi_ == 0 else NCX + (bi_ - 1) * 512
            v = vbuf[bi_ % 2]
            e_stt(p, v[:, 0:n], uT[:, ct, c0:c0 + n], dP[:, ct:ct + 1], p.pb[bi_][:, 0:n], ALU.mult, ALU.add,
                  [uT, dP, p.pb[bi_]], [v])
            p.dma(dbg[:, ct, c0:c0 + n], v[:, 0:n], reads=[v], stream="st")

    s5_pass(p, S, uT, tabsc, True, None, kin, ident, evac)
    p.wait_all()
    return p


def host_inputs(z, layer=0):
    x = z["x"]; c = z["c"]; ctx = z["ctx"]; c_ctx = z["c_ctx"]
    lam = np.stack([z["s5_lam_re"][0], z["s5_lam_im"][0]], 0)
    lamP = lam.reshape(2, 2, 32, 2, 64).transpose(3, 4, 0, 1, 2).reshape(128, 2, 64)
    ldt = z["s5_log_dt"][0]
    ldtP = np.broadcast_to(ldt.reshape(2, 32, 2)[:, :, :, None], (2, 32, 2, 64)).transpose(2, 3, 0, 1).reshape(128, 64)
    b = np.stack([z["s5_b_re"][0], z["s5_b_im"][0]], 0)
    bP = b.reshape(2, 32, 2, 64, 16).transpose(2, 3, 0, 1, 4).reshape(128, 2, 32, 16)
    cc = np.stack([z["s5_c_re"][0], z["s5_c_im"][0]], 0)
    cP = cc.reshape(2, 32, 2, 16, 64).transpose(2, 4, 0, 1, 3).reshape(128, 2, 32, 16)
    dP = z["s5_d"][0].reshape(8, 128).T
    maps = []
    for k in range(8):
        bb = k // 4; q = k % 4
        cT = np.stack([c[bb], c_ctx], axis=-1).reshape(8, 128, 2).transpose(1, 0, 2)
        oh = np.zeros((128, 4), np.float32); oh[:, q] = 1
        maps.append(dict(xl=x[bb, q * NL:(q + 1) * NL], xc=ctx[bb], cT=np.ascontiguousarray(cT),
                         modw=z["mod_w"][layer], modb=z["mod_b"][layer][None], n1g=z["norm1_g"][layer][None],
                         ident=np.eye(128, dtype=np.float32), lamP=np.ascontiguousarray(lamP),
                         ldtP=np.ascontiguousarray(ldtP), bP=np.ascontiguousarray(bP), cP=np.ascontiguousarray(cP),
                         dP=np.ascontiguousarray(dP), onehot=oh))
    return maps


def ref_s5(z, bb, groups):
    f8 = np.float64
    x = z["x"][bb].astype(f8); ctx = z["ctx"][bb].astype(f8); c = z["c"][bb].astype(f8); c_ctx = z["c_ctx"].astype(f8)
    silu = lambda v: v / (1 + np.exp(-v))
    rms = lambda v, g: v / np.sqrt((v * v).mean(-1, keepdims=True) + 1e-6) * g
    mw = z["mod_w"][0].astype(f8); mb = z["mod_b"][0].astype(f8); g1 = z["norm1_g"][0].astype(f8)
    ml = silu(c) @ mw + mb; mc = silu(c_ctx) @ mw + mb
    hn = rms(x, g1) * (1 + ml[D:2 * D]) + ml[:D]
    cn = rms(ctx, g1) * (1 + mc[D:2 * D]) + mc[:D]
    out = {}
    for g in groups:
        ch = slice(g * 16, (g + 1) * 16)
        tot = np.zeros((NCX + 8192, 16))
        for d in range(2):
            lr = z["s5_lam_re"][0, d, g].astype(f8); li = z["s5_lam_im"][0, d, g].astype(f8)
            step = np.exp(z["s5_log_dt"][0, d, g].astype(f8))
            lamc = lr + 1j * li
            abar = np.exp(lamc * step)
            Bc = z["s5_b_re"][0, g].astype(f8) + 1j * z["s5_b_im"][0, g].astype(f8)
            Cc = z["s5_c_re"][0, g].astype(f8) + 1j * z["s5_c_im"][0, g].astype(f8)
            Bbar = ((abar - 1) / lamc)[:, None] * Bc
            seq = np.concatenate([cn[:, ch], hn[:, ch]], 0) if d == 0 else np.concatenate([cn[::-1, ch], hn[::-1, ch]], 0)
            bu = seq @ Bbar.T
            h = np.zeros(64, complex); ys = np.zeros((len(seq), 16))
            for t in range(len(seq)):
                h = abar * h + bu[t]
                ys[t] = (Cc @ h).real
            if d == 1:
                ys = np.concatenate([ys[:NCX][::-1], ys[NCX:][::-1]], 0)
            tot += ys
        u = np.concatenate([cn[:, ch], hn[:, ch]], 0)
        out[g] = tot + z["s5_d"][0, ch].astype(f8) * u
    return out


def gelu_evac(p, uT, dP, gT):
    vb = [p.alloc([128, 512]) for _ in range(2)]
    wb = [p.alloc([128, 512]) for _ in range(1)]
    cnt = [0]

    def evac(ct):
        for bi_ in range(5):
            n = NCX if bi_ == 0 else 512
            c0 = 0 if bi_ == 0 else NCX + (bi_ - 1) * 512
            v = vb[cnt[0] % 2]; w = wb[0]
            cnt[0] += 1
            e_stt(p, v[:, 0:n], uT[:, ct, c0:c0 + n], dP[:, ct:ct + 1], p.pb[bi_][:, 0:n], ALU.mult, ALU.add,
                  [uT, dP, p.pb[bi_]], [v])
            e_act(p, w[:, 0:n], v[:, 0:n], AF.Square, [v], [w])
            e_ts(p, "dve", w[:, 0:n], w[:, 0:n], 0.044715, 1.0, ALU.mult, ALU.add, [w], [w])
            e_tt(p, "dve", w[:, 0:n], w[:, 0:n], v[:, 0:n], ALU.mult, [w, v], [w])
            e_act(p, w[:, 0:n], w[:, 0:n], AF.Sigmoid, [w], [w], scale=1.5957691216057308)
            e_tt(p, "pool", gT[:, ct, c0:c0 + n], v[:, 0:n], w[:, 0:n], ALU.mult, [v, w], [gT])
    return evac


def load_w_bf16(p, dst, src_ap, w):
    p.dma(dst, src_ap, writes=w, eng="pool")


def phase_c(p, io, modsc, ident, gT, h2T, gates, x1sc, layer_glu=True):
    m = p.mark()
    W = p.alloc([128, 4, 8, 512], BF16)
    for nb in range(4):
        load_w_bf16(p, W[:, nb],
                    io["gluw"][:, nb * 512:(nb + 1) * 512].rearrange("(k q) n -> q k n", q=128), [W])
    gb = p.alloc([128, 2048]); load_bcast(p, gb[:], io["glub"][0:1, :], [], [gb])
    n2 = p.alloc([128, D]); load_bcast(p, n2[:], io["n2g"][0:1, :], [], [n2])
    G1 = []; A2 = []; SH2 = []
    for which in range(2):
        g1 = p.alloc([128, D]); a2 = p.alloc([128, D]); s2 = p.alloc([128, D])
        load_bcast(p, g1[:], modsc[which:which + 1, 2 * D:3 * D], [modsc], [g1])
        load_bcast(p, s2[:], modsc[which:which + 1, 3 * D:4 * D], [modsc], [s2])
        load_bcast(p, a2[:], modsc[which:which + 1, 4 * D:5 * D], [modsc], [a2])
        e_stt(p, a2[:], a2[:], 1.0, n2[:], ALU.add, ALU.mult, [a2, n2], [a2])
        G1.append(g1); A2.append(a2); SH2.append(s2)
    rw = p.alloc([128, 8, 16]); p.dma(rw[:], io["rw"][:].rearrange("(k q) n -> q k n", q=128), writes=[rw])
    rb = p.alloc([128, 16]); load_bcast(p, rb[:], io["rb"][0:1, :], [], [rb])
    xs = [p.alloc([128, D]) for _ in range(2)]
    val = p.alloc([128, D]); gat = p.alloc([128, D]); x1 = [p.alloc([128, D]) for _ in range(2)]
    hn2 = p.alloc([128, D]); junk = p.alloc([128, D]); hT32 = p.alloc([128, 8, 128])
    sm = p.alloc([128, 128])
    ss = p.alloc([128, 1]); rs = p.alloc([128, 1])
    for t in range(18):
        which = 1 if t < 2 else 0
        src = io["xc"][t * 128:(t + 1) * 128, :] if t < 2 else io["xl"][(t - 2) * 128:(t - 1) * 128, :]
        x = xs[t % 2]; xo = x1[t % 2]
        p.dma(x[:], src, writes=[x])
        tok = slice(t * 128, (t + 1) * 128)
        for nb in range(4):
            ps = p.pb[nb]
            for k in range(8):
                e_mm(p, ps[:], gT[:, k, tok], W[:, nb, k, :], k == 0, k == 7, [gT, W], [ps])
        for nb in range(2):
            e_tt(p, "dve", val[:, nb * 512:(nb + 1) * 512], p.pb[nb][:], gb[:, nb * 512:(nb + 1) * 512], ALU.add,
                 [p.pb[nb], gb], [val])
            e_tt(p, "dve", gat[:, nb * 512:(nb + 1) * 512], p.pb[2 + nb][:], gb[:, D + nb * 512:D + (nb + 1) * 512],
                 ALU.add, [p.pb[2 + nb], gb], [gat])
        e_act(p, gat[:], gat[:], AF.Sigmoid, [gat], [gat])
        e_tt(p, "pool", val[:], val[:], gat[:], ALU.mult, [val, gat], [val])
        e_tt(p, "pool", val[:], val[:], G1[which][:], ALU.mult, [val, G1[which]], [val])
        e_tt(p, "dve", xo[:], val[:], x[:], ALU.add, [val, x], [xo])
        p.dma(x1sc[tok, :], xo[:], reads=[xo], writes=[x1sc])
        e_act(p, junk[:], xo[:], AF.Square, [xo], [junk, ss], accum_out=ss[:])
        e_ts(p, "dve", rs[:], ss[:], 1.0 / D, 1e-6, ALU.mult, ALU.add, [ss], [rs])
        e_act(p, rs[:], rs[:], AF.Sqrt, [rs], [rs])
        p.op("dve", lambda h: h.reciprocal(out=rs[:], in_=rs[:]), reads=[rs], writes=[rs])
        e_stt(p, hn2[:], xo[:], rs[:], A2[which][:], ALU.mult, ALU.mult, [xo, rs, A2[which]], [hn2])
        e_tt(p, "pool", hn2[:], hn2[:], SH2[which][:], ALU.add, [hn2, SH2[which]], [hn2])
        for half in range(2):
            ps = p.pb[4 + half]
            for c4 in range(4):
                ct = half * 4 + c4
                e_tr(p, ps[:, c4 * 128:(c4 + 1) * 128], hn2[:, ct * 128:(ct + 1) * 128], ident[:], [hn2, ident], [ps])
            pv = ps[:].rearrange("q (c n) -> q c n", c=4)
            e_copy(p, "act", h2T[:, half * 4:(half + 1) * 4, tok], pv, [ps], [h2T])
            e_copy(p, "dve", hT32[:, half * 4:(half + 1) * 4, :], pv, [ps], [hT32])
        pl = p.pb[6]
        for k in range(8):
            e_mm(p, pl[:, 0:16], hT32[:, k, :], rw[:, k, :], k == 0, k == 7, [hT32, rw], [pl])
        routing(p, pl, rb, sm, gates[:, t, :], gates)
    p.release(m)


def routing(p, pl, rb, sm, gout, gates_buf):
    R = [sm]
    s = sm[:, 0:16]; sel2 = sm[:, 16:48]; ps_ = sm[:, 48:72]; gs = sm[:, 72:76]; t2 = sm[:, 76:78]
    gmax = sm[:, 78:79]; Gm = sm[:, 80:84]; g1 = sm[:, 84:100]; cnt = sm[:, 100:116]; wsum = sm[:, 116:117]
    e_act(p, s, pl[:, 0:16], AF.Sigmoid, [pl], R)
    sel2v = sel2.rearrange("q (g e) -> q g e", g=4)
    sv = s.rearrange("q (g e) -> q g e", g=4)
    rbv = rb[:].rearrange("q (g e) -> q g e", g=4)
    e_tt(p, "dve", sel2v[:, :, 0:4], sv, rbv, ALU.add, R + [rb], R)
    e_copy(p, "dve", sel2v[:, :, 4:8], sel2v[:, :, 0:4], R, R)
    pv = ps_.rearrange("q (g e) -> q g e", g=4)
    pairs = [(0, 1), (0, 2), (0, 3), (1, 2), (1, 3), (2, 3)]
    for i, (a, b) in enumerate(pairs):
        e_tt(p, "dve", pv[:, :, i:i + 1], sel2v[:, :, a:a + 1], sel2v[:, :, b:b + 1], ALU.add, R, R)
    e_tt(p, "dve", pv[:, :, 0:3], pv[:, :, 0:3], pv[:, :, 3:6], ALU.max, R, R)
    e_tt(p, "dve", pv[:, :, 0:1], pv[:, :, 0:1], pv[:, :, 1:2], ALU.max, R, R)
    e_tt(p, "dve", gs.unsqueeze(2), pv[:, :, 0:1], pv[:, :, 2:3], ALU.max, R, R)
    e_tt(p, "dve", t2, gs[:, 0:2], gs[:, 2:4], ALU.max, R, R)
    e_tt(p, "dve", gmax, t2[:, 0:1], t2[:, 1:2], ALU.max, R, R)
    e_ts(p, "dve", Gm, gs, gmax, 1.0, ALU.is_ge, ALU.mult, R, R)
    g1v = g1.rearrange("q (g e) -> q g e", g=4); cv = cnt.rearrange("q (g e) -> q g e", g=4)
    e_tt(p, "dve", cv, sel2v[:, :, 1:5], sel2v[:, :, 0:4], ALU.is_gt, R, R)
    for r in (2, 3):
        e_tt(p, "dve", g1v, sel2v[:, :, r:r + 4], sel2v[:, :, 0:4], ALU.is_gt, R, R)
        e_tt(p, "dve", cv, cv, g1v, ALU.add, R, R)
    e_ts(p, "dve", cv, cv, 1.5, 1.0, ALU.is_lt, ALU.mult, R, R)
    e_tt(p, "dve", cv, cv, Gm.unsqueeze(2).to_broadcast([128, 4, 4]), ALU.mult, R, R)
    e_tt(p, "dve", cnt, cnt, s, ALU.mult, R, R)
    p.op("dve", lambda h: h.reduce_sum(out=wsum, in_=cnt, axis=AX.X), reads=R, writes=R)
    p.op("dve", lambda h: h.reciprocal(out=wsum, in_=wsum), reads=R, writes=R)
    e_ts(p, "dve", gout, cnt, wsum, 1.0, ALU.mult, ALU.mult, R, [gates_buf])


def phase_moe(p, io, layer_w, modsc, h2T, gates, x1sc, ntiles, out_cb, mod_which_of_tile):
    m = p.mark()
    w1d, w3d, w2d = layer_w
    ntok = ntiles * 128
    yacc = p.alloc([128, ntiles, D], name="yacc")
    for t0 in range(0, ntiles, 4):
        t1 = min(ntiles, t0 + 4)
        p.op("pool", lambda h, t0=t0, t1=t1: h.memset(yacc[:, t0:t1, :], 0.0), writes=[yacc])
    w1 = [p.alloc([128, 8, 512], BF16) for _ in range(2)]; w3 = [p.alloc([128, 8, 512], BF16) for _ in range(2)]
    w2 = [p.alloc([128, 4, D], BF16) for _ in range(2)]
    hT = [p.alloc([128, 4, 512], BF16) for _ in range(2)]
    s1 = [p.alloc([128, 512]) for _ in range(2)]
    blocks = [(b0, min(512, ntok - b0)) for b0 in range(0, ntok, 512)]
    ib = 0; iy = 0; ih = 0
    for e in range(16):
        a1 = w1[e % 2]; a3 = w3[e % 2]; a2 = w2[e % 2]
        load_w_bf16(p, a1[:], w1d[e].rearrange("(k q) n -> q k n", q=128), [a1])
        load_w_bf16(p, a3[:], w3d[e].rearrange("(k q) n -> q k n", q=128), [a3])
        load_w_bf16(p, a2[:], w2d[e].rearrange("(k q) n -> q k n", q=128), [a2])
        for (b0, n) in blocks:
            h = hT[ib % 2]; ib += 1
            for hc in range(4):
                p1 = p.pb[ih % 2]; p3 = p.pb[2 + ih % 2]; sb1 = s1[ih % 2]; ih += 1
                for k in range(8):
                    e_mm(p, p1[:, 0:n], a1[:, k, hc * 128:(hc + 1) * 128], h2T[:, k, b0:b0 + n], k == 0, k == 7, [a1, h2T], [p1])
                for k in range(8):
                    e_mm(p, p3[:, 0:n], a3[:, k, hc * 128:(hc + 1) * 128], h2T[:, k, b0:b0 + n], k == 0, k == 7, [a3, h2T], [p3])
                e_act(p, sb1[:, 0:n], p1[:, 0:n], AF.Silu, [p1], [sb1])
                e_tt(p, "dve", h[:, hc, 0:n], sb1[:, 0:n], p3[:, 0:n], ALU.mult, [sb1, p3], [h])
            for tt in range(n // 128):
                t = b0 // 128 + tt
                for dh in range(2):
                    py = p.pb[4 + iy % 4]; iy += 1
                    for hc in range(4):
                        e_mm(p, py[:], h[:, hc, tt * 128:(tt + 1) * 128], a2[:, hc, dh * 512:(dh + 1) * 512],
                             hc == 0, hc == 3, [h, a2], [py])
                    ya = yacc[:, t, dh * 512:(dh + 1) * 512]
                    e_stt(p, ya, py[:], gates[:, t, e:e + 1], ya, ALU.mult, ALU.add, [py, gates, yacc], [yacc])
    G2 = []
    for which in range(2):
        g2 = p.alloc([128, D]); load_bcast(p, g2[:], modsc[which:which + 1, 5 * D:6 * D], [modsc], [g2]); G2.append(g2)
    xb = [p.alloc([128, D]) for _ in range(2)]
    for t in range(ntiles):
        which = mod_which_of_tile(t)
        x = xb[t % 2]
        p.dma(x[:], x1sc[t * 128:(t + 1) * 128, :], reads=[x1sc], writes=[x])
        e_tt(p, "pool", yacc[:, t, :], yacc[:, t, :], G2[which][:], ALU.mult, [yacc, G2[which]], [yacc])
        e_tt(p, "dve", x[:], x[:], yacc[:, t, :], ALU.add, [x, yacc], [x])
        out_cb(t, x)
    p.release(m)


L0_INPUTS = [("xl", [NL, D]), ("xc", [NCX, D]), ("cT", [128, 8, 2]), ("modw", [D, 6144]), ("modb", [1, 6144]),
             ("n1g", [1, D]), ("ident", [128, 128]), ("lamP", [128, 2, 64]), ("ldtP", [128, 64]),
             ("bP", [128, 2, 32, 16]), ("cP", [128, 2, 32, 16]), ("dP", [128, 8]), ("onehot", [128, 4]),
             ("gluw", [D, 2048]), ("glub", [1, 2048]), ("n2g", [1, D]), ("rw", [D, 16]), ("rb", [1, 16]),
             ("w1", [16, D, 512]), ("w3", [16, D, 512]), ("w2", [16, 512, D])]


def layer0(p, io, sc, ident, onehot, out_cb, skip_s5=False, stop_after_c=False, dbg=None):
    dP = p.alloc([128, 8]); p.dma(dP[:], io["dP"][:], writes=[dP])
    phase_mod(p, io["cT"], io["modw"], io["modb"], sc["modsc"], ident)
    gates = p.alloc([128, 18, 16])
    hi0 = p.hi
    gT = p.alloc([128, 8, NT], BF16, name="gT", hi=True)
    m_g = p.mark()
    uT = p.alloc([128, 8, NT], BF16, name="uT")
    phase_hn(p, io["xl"], io["xc"], sc["modsc"], io["n1g"], ident, uT, 0, 1)
    if skip_s5:
        for ct in range(8):
            e_copy(p, "dve", gT[:, ct, :], uT[:, ct, :], [uT], [gT])
    S = None if skip_s5 else s5_params(p, io, ident)
    Fsum = p.alloc([128, 2, 2, 64]); kin = p.alloc([128, 2, 64])
    if not skip_s5:
      s5_pass(p, S, uT, sc["tabsc"], False, Fsum, None, ident)
    p.dma(sc["fsrc"][:].rearrange("q (r c) -> q r c", r=2), Fsum[:, :, 1, :], reads=[Fsum], writes=[sc["fsrc"]])
    p.cc(lambda h: h.collective_compute("AllGather", ALU.bypass, replica_groups=[[0, 1, 2, 3], [4, 5, 6, 7]],
                                        ins=[sc["fsrc"].t.opt()], outs=[sc["fdst"].t.opt()]),
         reads=[sc["fsrc"]], writes=[sc["fdst"]])
    gath = p.alloc([128, 4, 2, 64])
    p.dma(gath[:], sc["fdst"][:].rearrange("(q x) (r c) -> x q r c", x=128, r=2), reads=[sc["fdst"]], writes=[gath])
    if not skip_s5:
        s5_incoming(p, S, Fsum, gath, onehot, kin)
        evac = gelu_evac(p, uT, dP, gT)
        s5_pass(p, S, uT, sc["tabsc"], True, None, kin, ident, evac)
    p.release(m_g)
    h2T = p.alloc([128, 8, NT], BF16, name="h2T")
    phase_c(p, io, sc["modsc"], ident, gT, h2T, gates, sc["x1sc"])
    p.hi = hi0
    if dbg is not None:
        p.dma(dbg["gates"][:], gates[:], reads=[gates])
    if stop_after_c:
        return
    phase_moe(p, io, (io["w1"], io["w3"], io["w2"]), sc["modsc"], h2T, gates, sc["x1sc"], 18,
              out_cb, lambda t: 1 if t < 2 else 0)


def build_l0_test():
    p = Prog(); p.init_pool()
    io = {name: p.dram(name, shape) for name, shape in L0_INPUTS}
    xo = p.dram("xo", [NT, D], kind="ExternalOutput")
    sc = dict(modsc=p.dram("modsc", [2, 6144], kind="Internal"),
              tabsc=p.dram("tabsc", [64, 2, 128, 2048], kind="Internal"),
              fsrc=p.dram("fsrc", [128, 128], kind="Internal"), fdst=p.dram("fdst", [512, 128], kind="Internal"),
              x1sc=p.dram("x1sc", [NT, D], kind="Internal"))
    ident = p.alloc([128, 128]); p.dma(ident[:], io["ident"][:], writes=[ident])
    onehot = p.alloc([128, 4]); p.dma(onehot[:], io["onehot"][:], writes=[onehot])

    def out_cb(t, x):
        p.dma(xo[t * 128:(t + 1) * 128, :], x[:], reads=[x])

    layer0(p, io, sc, ident, onehot, out_cb)
    p.wait_all()
    return p


def host_inputs_l0(z):
    maps = host_inputs(z, 0)
    for k in range(8):
        maps[k].update(gluw=z["s5_glu_w"][0], glub=z["s5_glu_b"][0][None], n2g=z["norm2_g"][0][None],
                       rw=z["router_w"], rb=z["router_b"][None], w1=z["moe_w1"][0], w3=z["moe_w3"][0], w2=z["moe_w2"][0])
    return maps


NCH = 66
PADW = 8456
CTX0 = 2
LAT0 = 262
RSW = 1028

L1_INPUTS = [("cT", [128, 8, 2]), ("modw1", [D, 6144]), ("modb1", [1, 6144]), ("n1g1", [1, D]), ("n2g1", [1, D]),
             ("fing", [1, D]), ("wz", [D, 512]), ("wxbc", [D, 1024]), ("wdt", [D, 16]), ("convw", [128, 8, 5]),
             ("convb", [128, 8]), ("dtb", [1, 16]), ("alog", [1, 16]), ("dsk", [1, 8]), ("sng", [1, 512]),
             ("outw", [512, D]), ("triU", [128, 128]), ("triL", [128, 128]), ("ones", [128, 128]),
             ("rw", [D, 16]), ("rb", [1, 16]), ("w1b", [16, D, 512]), ("w3b", [16, D, 512]), ("w2b", [16, 512, D])]


def l1_scratch(p):
    sc = {}
    sc["modsc1"] = p.dram("modsc1", [2, 6144], kind="Internal")
    sc["pre"] = p.dram("pre", [8, 128, PADW], kind="Internal")
    sc["X"] = p.dram("Xs", [NCH, 128, 512], BF16, kind="Internal")
    sc["Bt"] = p.dram("Bts", [NCH, 128, 256], BF16, kind="Internal")
    sc["BT"] = p.dram("BTs", [NCH, 128, 256], BF16, kind="Internal")
    sc["CT"] = p.dram("CTs", [NCH, 128, 256], BF16, kind="Internal")
    sc["dts"] = p.dram("dts", [NCH, 128, 16], kind="Internal")
    sc["Z"] = p.dram("Zs", [NCH, 128, 512], BF16, kind="Internal")
    sc["yf"] = p.dram("yfs", [NCH, 128, 512], kind="Internal")
    sc["yb"] = p.dram("ybs", [NCH, 128, 512], kind="Internal")
    sc["rsrc"] = p.dram("rsrc", [8192, RSW], kind="Internal")
    sc["rdst"] = p.dram("rdst", [2048, RSW], kind="Internal")
    sc["x1b"] = p.dram("x1b", [2048, D], kind="Internal")
    return sc


def chunk_rows(xg, ctx1, c):
    if c < 2:
        return ctx1[c * 128:(c + 1) * 128, :]
    return xg[:].rearrange("(r w) d -> w r d", w=64)[c - 2]


def l1_proj(p, io, sc, xg, ctx1, ident):
    m = p.mark()
    modsc = sc["modsc1"]
    g_sb = p.alloc([128, D]); load_bcast(p, g_sb[:], io["n1g1"][0:1, :], [], [g_sb])
    A = []; SH = []
    for which in range(2):
        a = p.alloc([128, D]); s = p.alloc([128, D])
        load_bcast(p, a[:], modsc[which:which + 1, D:2 * D], [modsc], [a])
        load_bcast(p, s[:], modsc[which:which + 1, 0:D], [modsc], [s])
        e_stt(p, a[:], a[:], 1.0, g_sb[:], ALU.add, ALU.mult, [a, g_sb], [a])
        A.append(a); SH.append(s)
    Wx = p.alloc([128, 8, 1024], BF16); Wz = p.alloc([128, 8, 512], BF16); Wd = p.alloc([128, 8, 16])
    for half in range(2):
        load_w_bf16(p, Wx[:, :, half * 512:(half + 1) * 512] if False else Wx[:, half * 4:(half + 1) * 4, :],
                    io["wxbc"][half * 512:(half + 1) * 512, :].rearrange("(k q) n -> q k n", q=128), [Wx])
    load_w_bf16(p, Wz[:], io["wz"][:].rearrange("(k q) n -> q k n", q=128), [Wz])
    p.dma(Wd[:], io["wdt"][:].rearrange("(k q) n -> q k n", q=128), writes=[Wd])
    dtb = p.alloc([128, 16]); load_bcast(p, dtb[:], io["dtb"][0:1, :], [], [dtb])
    zero = p.alloc([128, 8]); p.op("pool", lambda h: h.memset(zero[:], 0.0), writes=[zero])
    for ft in range(8):
        for c0 in (0, CTX0 + 256, LAT0 - 2, LAT0 + 8192):
            p.dma(sc["pre"][ft, :, c0:c0 + 2], zero[:, 0:2], reads=[zero], writes=[sc["pre"]])
    xs = [p.alloc([128, D]) for _ in range(2)]; ys = [p.alloc([128, D]) for _ in range(2)]
    junk = p.alloc([128, D]); ss = p.alloc([128, 1]); rs = p.alloc([128, 1])
    hT = [p.alloc([128, 8, 512], BF16) for _ in range(2)]
    hT32 = p.alloc([128, 8, 128])
    zt = [p.alloc([128, 512], BF16) for _ in range(2)]
    dtt = [p.alloc([128, 16]) for _ in range(2)]
    pre_sb = [p.alloc([128, 512]) for _ in range(2)]
    blocks = [(0, [0, 1])] + [(1, [2 + 4 * b + i for i in range(4)]) for b in range(16)]
    ib = 0; it = 0; ip = 0
    for (islat, chunks) in blocks:
        which = 0 if islat else 1
        h = hT[ib % 2]; ib += 1
        n = 128 * len(chunks)
        for ti, c in enumerate(chunks):
            x = xs[it % 2]; y = ys[it % 2]; it += 1
            p.dma(x[:], chunk_rows(xg, ctx1, c), reads=[xg, ctx1], writes=[x])
            e_act(p, junk[:], x[:], AF.Square, [x], [junk, ss], accum_out=ss[:])
            e_ts(p, "dve", rs[:], ss[:], 1.0 / D, 1e-6, ALU.mult, ALU.add, [ss], [rs])
            e_act(p, rs[:], rs[:], AF.Sqrt, [rs], [rs])
            p.op("dve", lambda hh: hh.reciprocal(out=rs[:], in_=rs[:]), reads=[rs], writes=[rs])
            e_stt(p, y[:], x[:], rs[:], A[which][:], ALU.mult, ALU.mult, [x, rs, A[which]], [y])
            e_tt(p, "pool", y[:], y[:], SH[which][:], ALU.add, [y, SH[which]], [y])
            for half in range(2):
                ps = p.pb[half]
                for c4 in range(4):
                    ct = half * 4 + c4
                    e_tr(p, ps[:, c4 * 128:(c4 + 1) * 128], y[:, ct * 128:(ct + 1) * 128], ident[:], [y, ident], [ps])
                pv = ps[:].rearrange("q (c n) -> q c n", c=4)
                e_copy(p, "act", h[:, half * 4:(half + 1) * 4, ti * 128:(ti + 1) * 128], pv, [ps], [h])
                e_copy(p, "dve", hT32[:, half * 4:(half + 1) * 4, :], pv, [ps], [hT32])
            tokc = slice(ti * 128, (ti + 1) * 128)
            if islat:
                pz = p.pb[2]
                for k in range(8):
                    e_mm(p, pz[:], h[:, k, tokc], Wz[:, k, :], k == 0, k == 7, [h, Wz], [pz])
                z = zt[it % 2]
                e_act(p, z[:], pz[:], AF.Silu, [pz], [z])
                p.dma(sc["Z"][c], z[:], reads=[z], writes=[sc["Z"]])
            pd = p.pb[3]
            for k in range(8):
                e_mm(p, pd[:, 0:16], hT32[:, k, :], Wd[:, k, :], k == 0, k == 7, [hT32, Wd], [pd])
            dt_ = dtt[it % 2]
            e_tt(p, "dve", dt_[:], pd[:, 0:16], dtb[:], ALU.add, [pd, dtb], [dt_])
            e_act(p, dt_[:], dt_[:], AF.Exp, [dt_], [dt_])
            e_ts(p, "dve", dt_[:], dt_[:], 1.0, 1.0, ALU.add, ALU.mult, [dt_], [dt_])
            e_act(p, dt_[:], dt_[:], AF.Ln, [dt_], [dt_])
            p.dma(sc["dts"][c], dt_[:], reads=[dt_], writes=[sc["dts"]])
        col0 = (LAT0 + (chunks[0] - 2) * 128) if islat else CTX0
        for ft in range(8):
            px = p.pb[4 + ft % 4]
            for k in range(8):
                e_mm(p, px[:, 0:n], Wx[:, k, ft * 128:(ft + 1) * 128], h[:, k, 0:n], k == 0, k == 7, [Wx, h], [px])
            o = pre_sb[ip % 2]; ip += 1
            e_copy(p, "act", o[:, 0:n], px[:, 0:n], [px], [o])
            p.dma(sc["pre"][ft, :, col0:col0 + n], o[:, 0:n], reads=[o], writes=[sc["pre"]])
    p.release(m)


def l1_conv(p, io, sc, identb):
    m = p.mark()
    cw = p.alloc([128, 8, 5]); cb = p.alloc([128, 8])
    p.dma(cw[:], io["convw"][:], writes=[cw]); p.dma(cb[:], io["convb"][:], writes=[cb])
    inb = [p.alloc([128, 516]) for _ in range(3)]
    acc = [p.alloc([128, 512]) for _ in range(2)]
    act = [p.alloc([128, 8, 512], BF16) for _ in range(2)]
    xo = [p.alloc([128, 512], BF16) for _ in range(2)]; bo = [p.alloc([128, 256], BF16) for _ in range(2)]
    blocks = [(0, [0, 1])] + [(1, [2 + 4 * b + i for i in range(4)]) for b in range(16)]
    ii = 0; ia = 0; ib = 0; ix = 0
    for (islat, chunks) in blocks:
        n = 128 * len(chunks)
        col0 = (LAT0 + (chunks[0] - 2) * 128) if islat else CTX0
        a8 = act[ib % 2]; ib += 1
        for ft in range(8):
            xin = inb[ii % 3]; ii += 1
            p.dma(xin[:, 0:n + 4], sc["pre"][ft, :, col0 - 2:col0 + n + 2], reads=[sc["pre"]], writes=[xin])
            a = acc[ia % 2]; ia += 1
            e_ts(p, "dve", a[:, 0:n], xin[:, 0:n], cw[:, ft, 0:1], cb[:, ft:ft + 1], ALU.mult, ALU.add, [xin, cw, cb], [a])
            for k in range(1, 5):
                e_stt(p, a[:, 0:n], xin[:, k:k + n], cw[:, ft, k:k + 1], a[:, 0:n], ALU.mult, ALU.add, [xin, cw, a], [a])
            e_act(p, a8[:, ft, 0:n], a[:, 0:n], AF.Silu, [a], [a8])
        for ti, c in enumerate(chunks):
            tok = slice(ti * 128, (ti + 1) * 128)
            pt = p.pb[ix % 2]; ptb = pt.t.bitcast(BF16)
            x_o = xo[ix % 2]; b_o = bo[ix % 2]; ix += 1
            for ft in range(6):
                e_tr(p, ptb[:, ft * 128:(ft + 1) * 128], a8[:, ft, tok], identb[:], [a8, identb], [pt])
            e_copy(p, "act", x_o[:], ptb[:, 0:512], [pt], [x_o])
            e_copy(p, "dve", b_o[:], ptb[:, 512:768], [pt], [b_o])
            p.dma(sc["X"][c], x_o[:], reads=[x_o], writes=[sc["X"]])
            p.dma(sc["Bt"][c], b_o[:], reads=[b_o], writes=[sc["Bt"]])
            p.dma(sc["BT"][c].rearrange("q (g s) -> q g s", g=2), a8[:, 4:6, tok], reads=[a8], writes=[sc["BT"]])
            p.dma(sc["CT"][c].rearrange("q (g s) -> q g s", g=2), a8[:, 6:8, tok], reads=[a8], writes=[sc["CT"]])
    p.release(m)


def l1_ssd(p, io, sc):
    m = p.mark()
    triU = p.alloc([128, 128]); triL = p.alloc([128, 128]); ones = p.alloc([128, 128])
    p.dma(triU[:], io["triU"][:], writes=[triU]); p.dma(triL[:], io["triL"][:], writes=[triL])
    p.dma(ones[:], io["ones"][:], writes=[ones])
    Aneg = p.alloc([128, 16]); load_bcast(p, Aneg[:], io["alog"][0:1, :], [], [Aneg])
    e_act(p, Aneg[:], Aneg[:], AF.Exp, [Aneg], [Aneg])
    e_ts(p, "dve", Aneg[:], Aneg[:], -1.0, 0.0, ALU.mult, ALU.add, [Aneg], [Aneg])
    dsk = p.alloc([128, 8]); load_bcast(p, dsk[:], io["dsk"][0:1, :], [], [dsk])
    sng = p.alloc([128, 512]); load_bcast(p, sng[:], io["sng"][0:1, :], [], [sng])
    Wo = p.alloc([128, 4, D], BF16); load_w_bf16(p, Wo[:], io["outw"][:].rearrange("(k q) n -> q k n", q=128), [Wo])
    identb = p.identb
    nb = 2

    def mk():
        T = {}
        T["S"] = p.alloc([128, 512]); T["Sb"] = p.alloc([128, 512], BF16)
        T["X"] = [p.alloc([128, 512], BF16) for _ in range(nb)]; T["Bt"] = [p.alloc([128, 256], BF16) for _ in range(nb)]
        T["BT"] = [p.alloc([128, 2, 128], BF16) for _ in range(nb)]; T["CT"] = [p.alloc([128, 2, 128], BF16) for _ in range(nb)]
        T["dt"] = [p.alloc([128, 16]) for _ in range(nb)]
        T["da"] = p.alloc([128, 8]); T["dabc"] = p.alloc([128, 8, 128]); T["acs"] = p.alloc([128, 8]); T["ea"] = p.alloc([128, 8])
        T["wdec"] = p.alloc([128, 8]); T["dch"] = p.alloc([128, 8]); T["BCm"] = p.alloc([128, 2, 128], BF16)
        T["tmpH"] = [p.alloc([128, 4, 128]) for _ in range(2)]; T["exH"] = [p.alloc([128, 4, 128], BF16) for _ in range(2)]
        T["MH"] = [p.alloc([128, 4, 128], BF16) for _ in range(2)]
        T["xdt"] = p.alloc([128, 512], BF16); T["xw"] = p.alloc([128, 512], BF16); T["yt"] = [p.alloc([128, 512]) for _ in range(2)]
        return T

    TT = [mk(), mk()]
    orders = [list(range(NCH)), [1, 0] + list(range(NCH - 1, 1, -1))]
    ydst = [sc["yf"], sc["yb"]]

    def chunk(d, pos, c):
        T = TT[d]
        b_ = pos % nb
        islat = c >= 2
        tri = triU if d == 0 else triL
        S = T["S"]; Sb = T["Sb"]
        X = T["X"][b_]; Bt = T["Bt"][b_]; BT = T["BT"][b_]; CT = T["CT"][b_]; dt = T["dt"][b_]
        da = T["da"]; dabc = T["dabc"]; acs = T["acs"]; ea = T["ea"]; wdec = T["wdec"]; dch = T["dch"]; BCm = T["BCm"]
        xdt = T["xdt"]; xw = T["xw"]; yt = T["yt"][b_]
        p.dma(X[:], sc["X"][c], reads=[sc["X"]], writes=[X])
        p.dma(Bt[:], sc["Bt"][c], reads=[sc["Bt"]], writes=[Bt])
        p.dma(BT[:], sc["BT"][c].rearrange("q (g s) -> q g s", g=2), reads=[sc["BT"]], writes=[BT])
        p.dma(CT[:], sc["CT"][c].rearrange("q (g s) -> q g s", g=2), reads=[sc["CT"]], writes=[CT])
        p.dma(dt[:], sc["dts"][c], reads=[sc["dts"]], writes=[dt])
        dtd = dt[:, d * 8:(d + 1) * 8]
        if pos == 0:
            p.op("pool", lambda h: h.memset(S[:], 0.0), writes=[S])
            p.op("pool", lambda h: h.memset(Sb[:], 0.0), writes=[Sb])
        e_tt(p, "dve", da[:], dtd, Aneg[:, d * 8:(d + 1) * 8], ALU.mult, [dt, Aneg], [da])
        e_copy(p, "dve", dabc[:], da[:].unsqueeze(2).to_broadcast([128, 8, 128]), [da], [dabc])
        p0 = p.pb[0] if d == 0 else p.pb[7]
        e_mm(p, p0[:, 0:8], tri[:], da[:], True, True, [tri, da], [p0])
        e_mm(p, p0[:, 8:16], ones[:], da[:], True, True, [ones, da], [p0])
        e_copy(p, "dve", acs[:], p0[:, 0:8], [p0], [acs])
        e_act(p, ea[:], p0[:, 0:8], AF.Exp, [p0], [ea])
        e_tt(p, "dve", wdec[:], p0[:, 8:16], acs[:], ALU.subtract, [p0, acs], [wdec])
        e_act(p, wdec[:], wdec[:], AF.Exp, [wdec], [wdec])
        e_act(p, dch[:], p0[:, 8:16], AF.Exp, [p0], [dch])
        p1 = p.pb[1]
        for g in range(2):
            e_mm(p, p1[:, g * 128:(g + 1) * 128], BT[:, g, :], CT[:, g, :], True, True, [BT, CT], [p1])
        e_tt(p, "dve", BCm[:], p1[:, 0:256].rearrange("q (g s) -> q g s", g=2),
             tri[:].unsqueeze(1).to_broadcast([128, 2, 128]), ALU.mult, [p1, tri], [BCm])
        e_tt(p, "dve", xdt[:].rearrange("q (h e) -> q h e", h=8), X[:].rearrange("q (h e) -> q h e", h=8),
             dtd.unsqueeze(2).to_broadcast([128, 8, 64]), ALU.mult, [X, dt], [xdt])
        e_tt(p, "dve", xw[:].rearrange("q (h e) -> q h e", h=8), xdt[:].rearrange("q (h e) -> q h e", h=8),
             wdec[:].unsqueeze(2).to_broadcast([128, 8, 64]), ALU.mult, [xdt, wdec], [xw])
        if islat:
            pyd = p.pb[4]
            for hh in range(8):
                e_mm(p, p.pb[2 + hh // 4][:, (hh % 4) * 128:(hh % 4 + 1) * 128], dabc[:, hh, :], tri[:], True, True,
                     [dabc, tri], [p.pb[2 + hh // 4]])
            for half in range(2):
                pr = p.pb[2 + half]; t_ = T["tmpH"][half]; e_ = T["exH"][half]; M = T["MH"][half]
                e_tt(p, "dve", t_[:], pr[:].rearrange("q (h s) -> q h s", h=4),
                     acs[:, 4 * half:4 * half + 4].unsqueeze(2).to_broadcast([128, 4, 128]), ALU.subtract, [pr, acs], [t_])
                e_ts(p, "dve", t_[:], t_[:], 0.0, 0.0, ALU.min, ALU.add, [t_], [t_])
                e_act(p, e_[:], t_[:], AF.Exp, [t_], [e_])
                e_tt(p, "dve", M[:], e_[:], BCm[:, half, :].unsqueeze(1).to_broadcast([128, 4, 128]), ALU.mult, [e_, BCm], [M])
                for h4 in range(4):
                    hh = 4 * half + h4
                    e_mm(p, pyd[:, hh * 64:(hh + 1) * 64], M[:, h4, :], xdt[:, hh * 64:(hh + 1) * 64], True, True, [M, xdt], [pyd])
            pyo = p.pb[5]
            for g in range(2):
                e_mm(p, pyo[:, g * 256:(g + 1) * 256], CT[:, g, :], Sb[:, g * 256:(g + 1) * 256], True, True, [CT, Sb], [pyo])
            e_tt(p, "dve", yt[:].rearrange("q (h e) -> q h e", h=8), pyo[:].rearrange("q (h e) -> q h e", h=8),
                 ea[:].unsqueeze(2).to_broadcast([128, 8, 64]), ALU.mult, [pyo, ea], [yt])
            e_tt(p, "dve", yt[:], yt[:], pyd[:], ALU.add, [yt, pyd], [yt])
            p.dma(ydst[d][c], yt[:], reads=[yt], writes=[ydst[d]])
        pst = p.pb[6]
        for g in range(2):
            e_mm(p, pst[:, g * 256:(g + 1) * 256], Bt[:, g * 128:(g + 1) * 128], xw[:, g * 256:(g + 1) * 256],
                 True, True, [Bt, xw], [pst])
        e_tt(p, "dve", S[:].rearrange("q (h e) -> q h e", h=8), S[:].rearrange("q (h e) -> q h e", h=8),
             dch[:].unsqueeze(2).to_broadcast([128, 8, 64]), ALU.mult, [S, dch], [S])
        e_tt(p, "dve", S[:], S[:], pst[:], ALU.add, [S, pst], [S])
        e_copy(p, "act", Sb[:], S[:], [S], [Sb])

    for pos in range(NCH):
        chunk(0, pos, orders[0][pos])
        chunk(1, pos, orders[1][pos])

    Xc = [p.alloc([128, 512], BF16) for _ in range(nb)]; Zc = [p.alloc([128, 512], BF16) for _ in range(nb)]
    yfc = [p.alloc([128, 512]) for _ in range(nb)]; ybc = [p.alloc([128, 512]) for _ in range(nb)]
    v = [p.alloc([128, 512]) for _ in range(nb)]; vb = p.alloc([128, 512], BF16); vT = p.alloc([128, 4, 128], BF16)
    junk = p.alloc([128, 512]); orow = [p.alloc([128, RSW]) for _ in range(2)]
    for c in range(2, NCH):
        b_ = c % nb
        X = Xc[b_]; Z = Zc[b_]; yf = yfc[b_]; yb = ybc[b_]; vv = v[b_]; o = orow[b_]
        p.dma(X[:], sc["X"][c], reads=[sc["X"]], writes=[X])
        p.dma(Z[:], sc["Z"][c], reads=[sc["Z"]], writes=[Z])
        p.dma(yf[:], sc["yf"][c], reads=[sc["yf"]], writes=[yf])
        p.dma(yb[:], sc["yb"][c], reads=[sc["yb"]], writes=[yb])
        e_tt(p, "dve", vv[:].rearrange("q (h e) -> q h e", h=8), X[:].rearrange("q (h e) -> q h e", h=8),
             dsk[:].unsqueeze(2).to_broadcast([128, 8, 64]), ALU.mult, [X, dsk], [vv])
        e_tt(p, "dve", yf[:], yf[:], yb[:], ALU.add, [yf, yb], [yf])
        e_tt(p, "dve", vv[:], vv[:], yf[:], ALU.add, [vv, yf], [vv])
        e_tt(p, "dve", vv[:], vv[:], Z[:], ALU.mult, [vv, Z], [vv])
        e_act(p, junk[:], vv[:], AF.Square, [vv], [junk, o], accum_out=o[:, 1024:1025])
        e_tt(p, "dve", vb[:], vv[:], sng[:], ALU.mult, [vv, sng], [vb])
        pt = p.pb[c % 2]; ptb = pt.t.bitcast(BF16)
        for k_ in range(4):
            e_tr(p, ptb[:, k_ * 128:(k_ + 1) * 128], vb[:, k_ * 128:(k_ + 1) * 128], identb[:], [vb, identb], [pt])
        e_copy(p, "act", vT[:], ptb[:, 0:512].rearrange("q (k s) -> q k s", k=4), [pt], [vT])
        for dh in range(2):
            pp = p.pb[2 + (2 * c + dh) % 4]
            for k_ in range(4):
                e_mm(p, pp[:], vT[:, k_, :], Wo[:, k_, dh * 512:(dh + 1) * 512], k_ == 0, k_ == 3, [vT, Wo], [pp])
            e_copy(p, "act", o[:, dh * 512:(dh + 1) * 512], pp[:], [pp], [o])
        p.op("pool", lambda h, o=o: h.memset(o[:, 1025:RSW], 0.0), writes=[o])
        w = c - 2
        p.dma(sc["rsrc"][w * 128:(w + 1) * 128, :], o[:], reads=[o], writes=[sc["rsrc"]])
    p.release(m)


def l1_tail(p, io, sc, xg, onehot, ident, out_dram):
    modsc = sc["modsc1"]
    gates = p.alloc([128, 16, 16]); h2T = p.alloc([128, 8, NL], BF16)
    m = p.mark()
    n2 = p.alloc([128, D]); load_bcast(p, n2[:], io["n2g1"][0:1, :], [], [n2])
    g1 = p.alloc([128, D]); a2 = p.alloc([128, D]); s2 = p.alloc([128, D])
    load_bcast(p, g1[:], modsc[0:1, 2 * D:3 * D], [modsc], [g1])
    load_bcast(p, s2[:], modsc[0:1, 3 * D:4 * D], [modsc], [s2])
    load_bcast(p, a2[:], modsc[0:1, 4 * D:5 * D], [modsc], [a2])
    e_stt(p, a2[:], a2[:], 1.0, n2[:], ALU.add, ALU.mult, [a2, n2], [a2])
    rw = p.alloc([128, 8, 16]); p.dma(rw[:], io["rw"][:].rearrange("(k q) n -> q k n", q=128), writes=[rw])
    rb = p.alloc([128, 16]); load_bcast(p, rb[:], io["rb"][0:1, :], [], [rb])
    rt = [p.alloc([128, RSW]) for _ in range(2)]
    xq = [p.alloc([128, D]) for _ in range(2)]; xa = p.alloc([128, D]); x1 = [p.alloc([128, D]) for _ in range(2)]
    hn2 = p.alloc([128, D]); junk = p.alloc([128, D]); hT32 = p.alloc([128, 8, 128]); sm = p.alloc([128, 128])
    ss = p.alloc([128, 1]); rs = p.alloc([128, 1]); rsd = p.alloc([128, 1])
    xgv = xg[:].rearrange("(r w) d -> w r d", w=64)
    iq = 0
    for t in range(16):
        r_ = rt[t % 2]; xo = x1[t % 2]
        p.dma(r_[:], sc["rdst"][t * 128:(t + 1) * 128, :], reads=[sc["rdst"]], writes=[r_])
        for j in range(4):
            xj = xq[iq % 2]; iq += 1
            p.dma(xj[:], xgv[16 * j + t], reads=[xg], writes=[xj])
            if j == 0:
                e_ts(p, "dve", xa[:], xj[:], onehot[:, 0:1], 0.0, ALU.mult, ALU.add, [xj, onehot], [xa])
            else:
                e_stt(p, xa[:], xj[:], onehot[:, j:j + 1], xa[:], ALU.mult, ALU.add, [xj, onehot, xa], [xa])
        e_ts(p, "dve", rsd[:], r_[:, 1024:1025], 1.0 / 2048, 1e-6, ALU.mult, ALU.add, [r_], [rsd])
        e_act(p, rsd[:], rsd[:], AF.Sqrt, [rsd], [rsd])
        p.op("dve", lambda h: h.reciprocal(out=rsd[:], in_=rsd[:]), reads=[rsd], writes=[rsd])
        e_stt(p, xo[:], r_[:, 0:D], rsd[:], g1[:], ALU.mult, ALU.mult, [r_, rsd, g1], [xo])
        e_tt(p, "pool", xo[:], xo[:], xa[:], ALU.add, [xo, xa], [xo])
        tok = slice(t * 128, (t + 1) * 128)
        p.dma(sc["x1b"][tok, :], xo[:], reads=[xo], writes=[sc["x1b"]])
        e_act(p, junk[:], xo[:], AF.Square, [xo], [junk, ss], accum_out=ss[:])
        e_ts(p, "dve", rs[:], ss[:], 1.0 / D, 1e-6, ALU.mult, ALU.add, [ss], [rs])
        e_act(p, rs[:], rs[:], AF.Sqrt, [rs], [rs])
        p.op("dve", lambda h: h.reciprocal(out=rs[:], in_=rs[:]), reads=[rs], writes=[rs])
        e_stt(p, hn2[:], xo[:], rs[:], a2[:], ALU.mult, ALU.mult, [xo, rs, a2], [hn2])
        e_tt(p, "pool", hn2[:], hn2[:], s2[:], ALU.add, [hn2, s2], [hn2])
        for half in range(2):
            ps = p.pb[4 + half]
            for c4 in range(4):
                ct = half * 4 + c4
                e_tr(p, ps[:, c4 * 128:(c4 + 1) * 128], hn2[:, ct * 128:(ct + 1) * 128], ident[:], [hn2, ident], [ps])
            pv = ps[:].rearrange("q (c n) -> q c n", c=4)
            e_copy(p, "act", h2T[:, half * 4:(half + 1) * 4, tok], pv, [ps], [h2T])
            e_copy(p, "dve", hT32[:, half * 4:(half + 1) * 4, :], pv, [ps], [hT32])
        pl = p.pb[6]
        for k in range(8):
            e_mm(p, pl[:, 0:16], hT32[:, k, :], rw[:, k, :], k == 0, k == 7, [hT32, rw], [pl])
        routing(p, pl, rb, sm, gates[:, t, :], gates)
    p.release(m)
    fg = p.alloc([128, D]); load_bcast(p, fg[:], io["fing"][0:1, :], [], [fg])
    junk2 = p.alloc([128, D]); ss2 = p.alloc([128, 1]); rs2 = p.alloc([128, 1])

    def out_cb(t, x):
        e_act(p, junk2[:], x[:], AF.Square, [x], [junk2, ss2], accum_out=ss2[:])
        e_ts(p, "dve", rs2[:], ss2[:], 1.0 / D, 1e-6, ALU.mult, ALU.add, [ss2], [rs2])
        e_act(p, rs2[:], rs2[:], AF.Sqrt, [rs2], [rs2])
        p.op("dve", lambda h: h.reciprocal(out=rs2[:], in_=rs2[:]), reads=[rs2], writes=[rs2])
        e_stt(p, x[:], x[:], rs2[:], fg[:], ALU.mult, ALU.mult, [x, rs2, fg], [x])
        p.dma(out_dram[t * 128:(t + 1) * 128, :], x[:], reads=[x], writes=[out_dram])

    phase_moe(p, io, (io["w1b"], io["w3b"], io["w2b"]), modsc, h2T, gates, sc["x1b"], 16, out_cb, lambda t: 0)


def layer1(p, io, sc, xg, ctx1, ident, onehot, out_dram, dbg=None):
    p.identb = p.alloc([128, 128], BF16)
    e_copy(p, "dve", p.identb[:], ident[:], [ident], [p.identb])
    phase_mod(p, io["cT"], io["modw1"], io["modb1"], sc["modsc1"], ident)
    l1_proj(p, io, sc, xg, ctx1, ident)
    l1_conv(p, io, sc, p.identb)
    l1_ssd(p, io, sc)
    p.cc(lambda h: h.collective_compute("ReduceScatter", ALU.add, replica_groups=[[0, 1, 2, 3], [4, 5, 6, 7]],
                                        ins=[sc["rsrc"].t.opt()], outs=[sc["rdst"].t.opt()]),
         reads=[sc["rsrc"]], writes=[sc["rdst"]])
    l1_tail(p, io, sc, xg, onehot, ident, out_dram)


def host_inputs_l1(z):
    i = 1
    maps = []
    inw = z["ssd_in_w"][0]
    for k in range(8):
        bb = k // 4; j = k % 4
        cT = np.stack([z["c"][bb], z["c_ctx"]], axis=-1).reshape(8, 128, 2).transpose(1, 0, 2)
        wz = inw[:, 512 * j:512 * (j + 1)]
        wx = inw[:, 2048 + 512 * j:2048 + 512 * (j + 1)]
        wB = inw[:, 4096 + 256 * j:4096 + 256 * (j + 1)]
        wC = inw[:, 5120 + 256 * j:5120 + 256 * (j + 1)]
        wdt = np.concatenate([inw[:, 6144 + 8 * j:6144 + 8 * (j + 1)], inw[:, 6176 + 8 * j:6176 + 8 * (j + 1)]], 1)
        chs = np.concatenate([np.arange(512 * j, 512 * (j + 1)), 2048 + np.arange(256 * j, 256 * (j + 1)),
                              3072 + np.arange(256 * j, 256 * (j + 1))])
        cw = z["ssd_conv_w"][0][:, chs]
        convw = cw.T.reshape(8, 128, 5).transpose(1, 0, 2)
        convb = z["ssd_conv_b"][0][chs].reshape(8, 128).T
        hs = slice(8 * j, 8 * (j + 1))
        dtb = np.concatenate([z["ssd_dt_bias"][0, 0, hs], z["ssd_dt_bias"][0, 1, hs]])[None]
        alog = np.concatenate([z["ssd_a_log"][0, 0, hs], z["ssd_a_log"][0, 1, hs]])[None]
        oh = np.zeros((128, 4), np.float32); oh[:, j] = 1
        maps.append(dict(
            cT=np.ascontiguousarray(cT), modw1=z["mod_w"][i], modb1=z["mod_b"][i][None], n1g1=z["norm1_g"][i][None],
            n2g1=z["norm2_g"][i][None], fing=z["final_g"][None], wz=np.ascontiguousarray(wz),
            wxbc=np.ascontiguousarray(np.concatenate([wx, wB, wC], 1)), wdt=np.ascontiguousarray(wdt),
            convw=np.ascontiguousarray(convw), convb=np.ascontiguousarray(convb), dtb=np.ascontiguousarray(dtb),
            alog=np.ascontiguousarray(alog), dsk=z["ssd_d"][0][hs][None].copy(),
            sng=z["ssd_norm_g"][0][512 * j:512 * (j + 1)][None].copy(),
            outw=np.ascontiguousarray(z["ssd_out_w"][0][512 * j:512 * (j + 1), :]),
            triU=np.triu(np.ones((128, 128), np.float32)), triL=np.tril(np.ones((128, 128), np.float32)),
            ones=np.ones((128, 128), np.float32), rw=z["router_w"], rb=z["router_b"][None],
            w1b=z["moe_w1"][i], w3b=z["moe_w3"][i], w2b=z["moe_w2"][i],
            ident=np.eye(128, dtype=np.float32), onehot=oh))
    return maps


def colmajor(xb):
    return xb.reshape(128, 64, -1).transpose(1, 0, 2).reshape(8192, -1)


def build_full():
    p = Prog(); p.init_pool()
    io = {}
    for name, shape in L0_INPUTS + L1_INPUTS:
        if name not in io:
            io[name] = p.dram(name, shape)
    out = p.dram("out", [2048, D], kind="ExternalOutput")
    sc0 = dict(modsc=p.dram("modsc", [2, 6144], kind="Internal"),
               tabsc=p.dram("tabsc", [64, 2, 128, 2048], kind="Internal"),
               fsrc=p.dram("fsrc", [128, 128], kind="Internal"), fdst=p.dram("fdst", [512, 128], kind="Internal"),
               x1sc=p.dram("x1sc", [NT, D], kind="Internal"))
    xsrcs = [p.dram(f"xsrc{a}", [128, 2048], kind="Internal") for a in range(8)]
    xdsts = [p.dram(f"xdst{a}", [512, 2048], kind="Internal") for a in range(8)]
    xg = p.dram("xg", [8192, D], kind="Internal")
    ctx1 = p.dram("ctx1", [NCX, D], kind="Internal")
    sc1 = l1_scratch(p)
    ident = p.alloc([128, 128]); p.dma(ident[:], io["ident"][:], writes=[ident])
    onehot = p.alloc([128, 4]); p.dma(onehot[:], io["onehot"][:], writes=[onehot])
    m0 = p.mark()

    def out_cb(t, x):
        if t < 2:
            p.dma(ctx1[t * 128:(t + 1) * 128, :], x[:], reads=[x], writes=[ctx1])
        else:
            a = (t - 2) // 2; half = (t - 2) % 2
            dst = xsrcs[a][:].rearrange("i (b d) -> (i b) d", d=D)[half * 128:(half + 1) * 128, :]
            p.dma(dst, x[:], reads=[x], writes=[xsrcs[a]])

    layer0(p, io, sc0, ident, onehot, out_cb)
    p.release(m0)
    xgv = xg[:].rearrange("(r a t) d -> a r t d", r=4, a=8)
    for a in range(8):
        p.cc(lambda h, a=a: h.collective_compute("AllGather", ALU.bypass, replica_groups=[[0, 1, 2, 3], [4, 5, 6, 7]],
                                                 ins=[xsrcs[a].t.opt()], outs=[xdsts[a].t.opt()]),
             reads=[xsrcs[a]], writes=[xdsts[a]])
        p.dma(xgv[a], xdsts[a][:].rearrange("(r i) (b d) -> r (i b) d", r=4, d=D), reads=[xdsts[a]], writes=[xg])
    layer1(p, io, sc1, xg, ctx1, ident, onehot, out)
    p.wait_all()
    return p


def kernel(**inputs):
    z = {k: np.asarray(v) for k, v in inputs.items()}
    m0 = host_inputs_l0(z)
    m1 = host_inputs_l1(z)
    maps = []
    for k in range(8):
        d = dict(m0[k]); d.update(m1[k])
        maps.append({kk: np.ascontiguousarray(vv, dtype=np.float32) for kk, vv in d.items()})
    p = build_full()
    nc = p.finish()
    res = run_bass_kernel_spmd(nc, maps, core_ids=list(range(8))).results
    outp = np.zeros((2, 8192, D), np.float32)
    for bb in range(2):
        cm = np.concatenate([res[4 * bb + j]["out"] for j in range(4)], 0)
        outp[bb] = cm.reshape(64, 128, D).transpose(1, 0, 2).reshape(8192, D)
    return outp
```

```python
import math, time, sys
import numpy as np
import contextlib
import concourse.bass as bass
import concourse.mybir as mybir
from concourse.bass_utils import run_bass_kernel_spmd

F32 = mybir.dt.float32
BF16 = mybir.dt.bfloat16
AF = mybir.ActivationFunctionType
ALU = mybir.AluOpType
AX = mybir.AxisListType

ENGS = ("pe", "dve", "act", "pool", "sp")


class Buf:
    def __init__(self, t, name, excl=False):
        self.t = t
        self.name = name
        self.excl = excl
        self.w = None
        self.r = []

    def __getitem__(self, k):
        return self.t[k]


class Prog:
    def __init__(self, name="k"):
        self.nc = bass.Bass("TRN2", target_bir_lowering=False)
        self.es = contextlib.ExitStack()
        self.q = {e: [] for e in ENGS}
        self.cnt = {e: 0 for e in ENGS}
        self.known = {e: {} for e in ENGS}
        self.sems = {}
        self.dcnt = {}
        self.mult = {}
        self.nbuf = 0
        self.sb_bytes = 0
        for e in ENGS:
            self.sems[e] = self.es.enter_context(self.nc.semaphore("s_" + e))
        self.NDS = 48
        self.rr = 0
        for i in range(self.NDS):
            nm = f"dq{i}"
            self.sems[nm] = self.es.enter_context(self.nc.semaphore(nm))
            self.dcnt[nm] = 0
            self.mult[nm] = 16

    def dram(self, name, shape, dtype=F32, kind="ExternalInput"):
        t = self.nc.dram_tensor(name, list(shape), dtype, kind=kind)
        b = Buf(t.ap(), name)
        b.is_dram = True
        return b

    def sb(self, shape, dtype=F32, name=None):
        self.nbuf += 1
        name = name or f"sb{self.nbuf}"
        t = self.es.enter_context(self.nc.sbuf_tensor(name, list(shape), dtype))
        sz = int(np.prod(shape[1:])) * (4 if dtype == F32 else 2)
        self.sb_bytes += sz
        return Buf(t, name)

    def init_pool(self, words=48 * 1024):
        self.big = self.es.enter_context(self.nc.sbuf_tensor("big", [128, words], F32))
        self.words = words
        self.top = 0
        self.hi = words
        self.peak = 0
        self.banks = [self.es.enter_context(self.nc.psum_tensor(f"bank{i}", [128, 512], F32)) for i in range(8)]
        self.pb = [Buf(self.banks[i][:], f"bank{i}", excl=True) for i in range(8)]

    def alloc(self, shape, dtype=F32, name=None, hi=False):
        n = int(np.prod(shape[1:]))
        w = n if dtype == F32 else (n + 1) // 2
        assert self.top + w <= self.hi, f"SBUF overflow {self.top}+{w} > {self.hi} ({name})"
        if hi:
            self.hi -= w
            ap = self.big[:, self.hi:self.hi + w]
        else:
            ap = self.big[:, self.top:self.top + w]
            self.top += w
        self.peak = max(self.peak, self.top + (self.words - self.hi))
        if dtype != F32:
            ap = ap.bitcast(dtype)[:, 0:n]
        if len(shape) > 2:
            names = " ".join(f"d{i}" for i in range(1, len(shape)))
            kw = {f"d{i}": shape[i] for i in range(1, len(shape))}
            ap = ap.rearrange(f"p ({names}) -> p {names}", **kw)
        if shape[0] < 128:
            ap = ap[0:shape[0]]
        self.nbuf += 1
        return Buf(ap, name or f"a{self.nbuf}")


    def barrier(self):
        snap_c = dict(self.cnt)
        snap_d = dict(self.dcnt)
        for e in ENGS:
            for src, n in snap_c.items():
                if src != e and n > self.known[e].get(src, 0):
                    self.q[e].append(("wait", src, n))
                    self.known[e][src] = n
            for s_, n in snap_d.items():
                if n > self.known[e].get(s_, 0):
                    self.q[e].append(("wait", s_, n * self.mult[s_]))
                    self.known[e][s_] = n

    def mark(self):
        return self.top

    def release(self, m):
        self.barrier()
        self.top = m

    def ps(self, shape, dtype=F32, name=None):
        self.nbuf += 1
        name = name or f"ps{self.nbuf}"
        t = self.es.enter_context(self.nc.psum_tensor(name, list(shape), dtype))
        return Buf(t, name)

    def stream(self, s):
        if s not in self.sems:
            self.sems[s] = self.es.enter_context(self.nc.semaphore("d_" + s))
            self.dcnt[s] = 0
            self.mult[s] = 16
        return s

    def _deps(self, eng, reads, writes):
        deps = {}

        def add(x):
            if x is None:
                return
            src, n = x
            if deps.get(src, 0) < n:
                deps[src] = n

        for b in reads:
            add(b.w)
            if b.excl:
                for x in b.r:
                    if x[0] != eng:
                        add(x)
        for b in writes:
            add(b.w)
            for x in b.r:
                add(x)
        for src, n in deps.items():
            if src == eng and eng == "pe":
                continue
            if self.known[eng].get(src, 0) >= n:
                continue
            self.known[eng][src] = n
            mult = self.mult[src] if src in self.dcnt else 1
            self.q[eng].append(("wait", src, n * mult))

    def _mark(self, tag, reads, writes):
        for b in reads:
            b.r.append(tag)
        for b in writes:
            b.w = tag
            b.r = []

    def op(self, eng, fn, reads=(), writes=()):
        self._deps(eng, reads, writes)
        self.cnt[eng] += 1
        self.q[eng].append(("op", fn))
        self._mark((eng, self.cnt[eng]), reads, writes)

    def dma(self, out, in_, reads=(), writes=(), eng="sp", stream=None, **kw):
        s_ = f"dq{self.rr % self.NDS}"
        self.rr += 1
        if eng == "sp" and any(not getattr(b, "is_dram", False) for b in reads):
            eng = "pool"
        self._deps(eng, reads, writes)
        prev = self.dcnt[s_]
        if prev and self.known[eng].get(s_, 0) < prev:
            self.q[eng].append(("wait", s_, prev * 16))
            self.known[eng][s_] = prev
        self.dcnt[s_] += 1
        self.q[eng].append(("dma", s_, out, in_, kw))
        self._mark((s_, self.dcnt[s_]), reads, writes)

    def cc(self, fn, reads=(), writes=(), eng="pool", stream="cc"):
        self.stream(stream)
        self.mult[stream] = 1
        self._deps(eng, reads, writes)
        prev = self.dcnt[stream]
        if prev and self.known[eng].get(stream, 0) < prev:
            self.q[eng].append(("wait", stream, prev))
            self.known[eng][stream] = prev
        self.dcnt[stream] += 1
        self.q[eng].append(("cc", stream, fn))
        self._mark((stream, self.dcnt[stream]), reads, writes)

    def wait_all(self, eng="sp"):
        for s, n in self.dcnt.items():
            if n:
                self.q[eng].append(("wait", s, self.mult[s] * n))
        for e in ENGS:
            if e != eng and self.cnt[e]:
                self.q[eng].append(("wait", e, self.cnt[e]))

    def finish(self):
        nc = self.nc
        sems = self.sems

        def replay(e):
            def body(h):
                for item in self.q[e]:
                    if item[0] == "wait":
                        h.wait_ge(sems[item[1]], item[2])
                    elif item[0] == "op":
                        item[1](h).then_inc(sems[e], 1)
                    elif item[0] == "cc":
                        item[2](h).then_inc(sems[item[1]], 1)
                    else:
                        _, s, out, in_, kw = item
                        h.dma_start(out=out, in_=in_, **kw).then_inc(sems[s], 16)
            return body

        with nc.Block() as block:
            block.tensor(replay("pe"))
            block.vector(replay("dve"))
            block.scalar(replay("act"))
            block.gpsimd(replay("pool"))
            block.sync(replay("sp"))
        self.es.close()
        return nc


def run(prog, in_maps):
    nc = prog.finish()
    res = run_bass_kernel_spmd(nc, in_maps, core_ids=list(range(len(in_maps))))
    return res.results


def _bufs(xs):
    return [x for x in xs if isinstance(x, Buf)]


def e_act(p, out, in_, func, r, w, eng="act", **kw):
    p.op(eng, lambda h: h.activation(out=out, in_=in_, func=func, **kw), reads=r, writes=w)


def e_tt(p, eng, out, in0, in1, op, r, w):
    p.op(eng, lambda h: h.tensor_tensor(out=out, in0=in0, in1=in1, op=op), reads=r, writes=w)


def e_ts(p, eng, out, in0, s1, s2, op0, op1, r, w):
    if op1 is None:
        if op0 == ALU.add:
            op1, s2 = ALU.mult, 1.0
        else:
            op1, s2 = ALU.add, 0.0
    if True:
        p.op(eng, lambda h: h.tensor_scalar(out=out, in0=in0, scalar1=s1, scalar2=s2, op0=op0, op1=op1), reads=r, writes=w)


def e_stt(p, out, in0, scalar, in1, op0, op1, r, w):
    p.op("dve", lambda h: h.scalar_tensor_tensor(out=out, in0=in0, scalar=scalar, in1=in1, op0=op0, op1=op1),
         reads=r, writes=w)


def e_copy(p, eng, out, in_, r, w):
    if eng == "act":
        p.op(eng, lambda h: h.activation(out=out, in_=in_, func=AF.Copy), reads=r, writes=w)
    else:
        p.op(eng, lambda h: h.tensor_copy(out=out, in_=in_), reads=r, writes=w)


def e_mm(p, out, lhsT, rhs, start, stop, r, w):
    p.op("pe", lambda h: h.matmul(out, lhsT=lhsT, rhs=rhs, start=start, stop=stop), reads=r, writes=w)


def e_tr(p, out, in_, ident, r, w):
    p.op("pe", lambda h: h.transpose(out, in_, ident), reads=r, writes=w)


def rev_ap(ap):
    (pst, pn), (st, n) = ap.ap
    return bass.AP(ap.tensor, ap.offset + (n - 1) * st, [[pst, pn], [-st, n]])


D = 1024
NL = 2048
NCX = 256
NT = NL + NCX
PI = math.pi


def phase_mod(p, cT, modw, modb, modsc, ident):
    m = p.mark()
    c_sb = p.alloc([128, 8, 2]); s_sb = p.alloc([128, 8, 2])
    p.dma(c_sb[:], cT[:], writes=[c_sb])
    e_act(p, s_sb[:], c_sb[:], AF.Silu, [c_sb], [s_sb])
    b_sb = p.alloc([1, 6144]); ones = p.alloc([1, 2])
    p.dma(b_sb[:], modb[:], writes=[b_sb])
    p.op("pool", lambda h: h.memset(ones[:], 1.0), writes=[ones])
    wb = [p.alloc([128, 8, 512]) for _ in range(2)]
    ob = [p.alloc([2, 512]) for _ in range(2)]
    for nb in range(12):
        w = wb[nb % 2]; o = ob[nb % 2]; ps = p.pb[nb % 2]
        p.dma(w[:], modw[:, nb * 512:(nb + 1) * 512].rearrange("(k q) n -> q k n", q=128), writes=[w])
        for k in range(8):
            e_mm(p, ps[0:2, :], s_sb[:, k, :], w[:, k, :], k == 0, False, [s_sb, w], [ps])
        e_mm(p, ps[0:2, :], ones[:], b_sb[:, nb * 512:(nb + 1) * 512], False, True, [ones, b_sb], [ps])
        e_copy(p, "act", o[:], ps[0:2, :], [ps], [o])
        p.dma(modsc[:, nb * 512:(nb + 1) * 512], o[:], reads=[o], writes=[modsc], stream="st")
    p.release(m)


def load_bcast(p, dst, src_row_ap, r, w):
    p.dma(dst, src_row_ap.to_broadcast([128, src_row_ap.shape[-1]]), reads=r, writes=w)


def phase_hn(p, xl, xc, modsc, gvec, ident, uT, part_sh, part_sc):
    m = p.mark()
    g_sb = p.alloc([128, D]); A = [p.alloc([128, D]) for _ in range(2)]; SH = [p.alloc([128, D]) for _ in range(2)]
    load_bcast(p, g_sb[:], gvec[0:1, :], [], [g_sb])
    for which in range(2):
        load_bcast(p, A[which][:], modsc[which:which + 1, part_sc * D:(part_sc + 1) * D], [modsc], [A[which]])
        load_bcast(p, SH[which][:], modsc[which:which + 1, part_sh * D:(part_sh + 1) * D], [modsc], [SH[which]])
        e_stt(p, A[which][:], A[which][:], 1.0, g_sb[:], ALU.add, ALU.mult, [A[which], g_sb], [A[which]])
    nbuf = 2
    xs = [p.alloc([128, D]) for _ in range(nbuf)]; ys = [p.alloc([128, D]) for _ in range(nbuf)]
    junk = p.alloc([128, D]); ss = [p.alloc([128, 1]) for _ in range(nbuf)]; rs = [p.alloc([128, 1]) for _ in range(nbuf)]
    for t in range(18):
        which = 1 if t < 2 else 0
        src = xc[t * 128:(t + 1) * 128, :] if t < 2 else xl[(t - 2) * 128:(t - 1) * 128, :]
        x = xs[t % nbuf]; y = ys[t % nbuf]; s = ss[t % nbuf]; r = rs[t % nbuf]
        p.dma(x[:], src, writes=[x])
        e_act(p, junk[:], x[:], AF.Square, [x], [junk, s], accum_out=s[:])
        e_ts(p, "dve", r[:], s[:], 1.0 / D, 1e-6, ALU.mult, ALU.add, [s], [r])
        e_act(p, r[:], r[:], AF.Sqrt, [r], [r])
        p.op("dve", lambda h, r=r: h.reciprocal(out=r[:], in_=r[:]), reads=[r], writes=[r])
        e_stt(p, y[:], x[:], r[:], A[which][:], ALU.mult, ALU.mult, [x, r, A[which]], [y])
        e_tt(p, "pool", y[:], y[:], SH[which][:], ALU.add, [y, SH[which]], [y])
        for half in range(2):
            ps = p.pb[(2 * t + half) % 4]
            for c4 in range(4):
                ct = half * 4 + c4
                e_tr(p, ps[:, c4 * 128:(c4 + 1) * 128], y[:, ct * 128:(ct + 1) * 128], ident[:], [y, ident], [ps])
            e_copy(p, "act", uT[:, half * 4:(half + 1) * 4, t * 128:(t + 1) * 128],
                   ps[:].rearrange("q (c n) -> q c n", c=4), [ps], [uT])
    p.release(m)


def cmul(p, eng, outr, outi, ar, ai, br, bi, t1, t2, bufs_r, bufs_w):
    e_tt(p, eng, t1, ar, br, ALU.mult, bufs_r, bufs_w)
    e_tt(p, eng, t2, ai, bi, ALU.mult, bufs_r, bufs_w)
    e_tt(p, eng, outr, t1, t2, ALU.subtract, bufs_r, bufs_w)
    e_tt(p, eng, t1, ar, bi, ALU.mult, bufs_r, bufs_w)
    e_tt(p, eng, t2, ai, br, ALU.mult, bufs_r, bufs_w)
    e_tt(p, eng, outi, t1, t2, ALU.add, bufs_r, bufs_w)


class S5:
    pass


def s5_params(p, io, ident):
    S = S5()
    lam = p.alloc([128, 2, 64]); ldt = p.alloc([128, 64])
    p.dma(lam[:], io["lamP"][:], writes=[lam]); p.dma(ldt[:], io["ldtP"][:], writes=[ldt])
    lr = lam[:, 0, :]; li = lam[:, 1, :]
    W = p.alloc([128, 10, 64], name="s5w")
    sl = lambda i: W[:, i, :]
    R = [W]
    step, th, mag, t1, t2, cs, sn, den, am1 = (sl(i) for i in range(9))
    S.ar = p.alloc([128, 64]); S.ai = p.alloc([128, 64]); S.cth = p.alloc([128, 64]); S.sth = p.alloc([128, 64])
    S.mag = p.alloc([128, 64]); S.qr = p.alloc([128, 64]); S.qi = p.alloc([128, 64])
    e_act(p, step, ldt[:], AF.Exp, [ldt], R)
    e_tt(p, "dve", th, li, step, ALU.mult, [lam, W], R)
    e_tt(p, "dve", t1, lr, step, ALU.mult, [lam, W], R)
    e_act(p, S.mag[:], t1, AF.Exp, R, [S.mag])
    e_act(p, sn, th, AF.Sin, R, R, scale=1.0 / 16)
    e_ts(p, "dve", t1, th, 1.0 / 16, PI / 2, ALU.mult, ALU.add, R, R)
    e_act(p, cs, t1, AF.Sin, R, R)
    for _ in range(4):
        e_tt(p, "dve", t1, cs, cs, ALU.mult, R, R)
        e_tt(p, "dve", t2, sn, sn, ALU.mult, R, R)
        e_tt(p, "dve", sn, sn, cs, ALU.mult, R, R)
        e_ts(p, "dve", sn, sn, 2.0, None, ALU.mult, None, R, R)
        e_tt(p, "dve", cs, t1, t2, ALU.subtract, R, R)
    e_copy(p, "dve", S.cth[:], cs, R, [S.cth]); e_copy(p, "dve", S.sth[:], sn, R, [S.sth])
    e_tt(p, "dve", S.ar[:], S.mag[:], cs, ALU.mult, R + [S.mag], [S.ar])
    e_tt(p, "dve", S.ai[:], S.mag[:], sn, ALU.mult, R + [S.mag], [S.ai])
    e_tt(p, "dve", t1, lr, lr, ALU.mult, [lam], R)
    e_tt(p, "dve", t2, li, li, ALU.mult, [lam], R)
    e_tt(p, "dve", den, t1, t2, ALU.add, R, R)
    p.op("dve", lambda h: h.reciprocal(out=den, in_=den), reads=R, writes=R)
    e_ts(p, "dve", am1, S.ar[:], -1.0, None, ALU.add, None, [S.ar], R)
    e_tt(p, "dve", t1, am1, lr, ALU.mult, R + [lam], R)
    e_tt(p, "dve", t2, S.ai[:], li, ALU.mult, [S.ai, lam], R)
    e_tt(p, "dve", t1, t1, t2, ALU.add, R, R)
    e_tt(p, "dve", S.qr[:], t1, den, ALU.mult, R, [S.qr])
    e_tt(p, "dve", t1, S.ai[:], lr, ALU.mult, [S.ai, lam], R)
    e_tt(p, "dve", t2, am1, li, ALU.mult, R + [lam], R)
    e_tt(p, "dve", t1, t1, t2, ALU.subtract, R, R)
    e_tt(p, "dve", S.qi[:], t1, den, ALU.mult, R, [S.qi])
    S.wc = p.alloc([128, 11, 64]); S.ws = p.alloc([128, 11, 64])
    e_copy(p, "dve", S.wc[:, 0, :], S.cth[:], [S.cth], [S.wc]); e_copy(p, "dve", S.ws[:, 0, :], S.sth[:], [S.sth], [S.ws])
    for k in range(10):
        e_tt(p, "dve", t1, S.wc[:, k, :], S.wc[:, k, :], ALU.mult, [S.wc], R)
        e_tt(p, "dve", t2, S.ws[:, k, :], S.ws[:, k, :], ALU.mult, [S.ws], R)
        e_tt(p, "dve", S.wc[:, k + 1, :], t1, t2, ALU.subtract, R, [S.wc])
        e_tt(p, "dve", t1, S.wc[:, k, :], S.ws[:, k, :], ALU.mult, [S.wc, S.ws], R)
        e_ts(p, "dve", S.ws[:, k + 1, :], t1, 2.0, None, ALU.mult, None, R, [S.ws])
    S.nws = p.alloc([128, 11, 64])
    e_ts(p, "dve", S.nws[:], S.ws[:], -1.0, None, ALU.mult, None, [S.ws], [S.nws])
    S.Ar = p.alloc([128, 64]); S.Ai = p.alloc([128, 64])
    e_copy(p, "dve", S.Ar[:], S.ar[:], [S.ar], [S.Ar]); e_copy(p, "dve", S.Ai[:], S.ai[:], [S.ai], [S.Ai])
    for k in range(11):
        e_tt(p, "dve", t1, S.Ar[:], S.Ar[:], ALU.mult, [S.Ar], R)
        e_tt(p, "dve", t2, S.Ai[:], S.Ai[:], ALU.mult, [S.Ai], R)
        e_tt(p, "dve", den, S.Ar[:], S.Ai[:], ALU.mult, [S.Ar, S.Ai], R)
        e_tt(p, "dve", S.Ar[:], t1, t2, ALU.subtract, R, [S.Ar])
        e_ts(p, "dve", S.Ai[:], den, 2.0, None, ALU.mult, None, R, [S.Ai])
    S.cP = p.alloc([128, 2, 32, 16]); p.dma(S.cP[:], io["cP"][:], writes=[S.cP])
    S.Bb = p.alloc([128, 2, 2, 32, 16])
    m = p.mark()
    bP = p.alloc([128, 2, 32, 16]); p.dma(bP[:], io["bP"][:], writes=[bP])
    Bb = S.Bb
    tA = p.alloc([128, 32, 16]); tB = p.alloc([128, 32, 16])
    for d in range(2):
        qr_b = S.qr[:, d * 32:(d + 1) * 32].unsqueeze(2).to_broadcast([128, 32, 16])
        qi_b = S.qi[:, d * 32:(d + 1) * 32].unsqueeze(2).to_broadcast([128, 32, 16])
        e_tt(p, "dve", tA[:], bP[:, 0], qr_b, ALU.mult, [bP, S.qr], [tA])
        e_tt(p, "dve", tB[:], bP[:, 1], qi_b, ALU.mult, [bP, S.qi], [tB])
        e_tt(p, "dve", Bb[:, d, 0], tA[:], tB[:], ALU.subtract, [tA, tB], [Bb])
        e_tt(p, "dve", tA[:], bP[:, 1], qr_b, ALU.mult, [bP, S.qr], [tA])
        e_tt(p, "dve", tB[:], bP[:, 0], qi_b, ALU.mult, [bP, S.qi], [tB])
        e_tt(p, "dve", Bb[:, d, 1], tA[:], tB[:], ALU.add, [tA, tB], [Bb])
    p.release(m)
    return S


def s5_build_ct(p, S, ct, ident, BbT, Cp, src):
    i = 0
    for gpl in range(4):
        gp = ct * 4 + gpl
        for d in range(2):
            for ri in range(2):
                s_ = src[i % 2]; ps = p.pb[7]
                for g2 in range(2):
                    e_copy(p, "dve", s_[g2 * 64:(g2 + 1) * 64, gpl, g2, :], S.Bb[g2 * 64:(g2 + 1) * 64, d, ri, gp, :], [S.Bb], [s_])
                e_tr(p, ps[:, (i % 4) * 128:(i % 4 + 1) * 128], s_[:].rearrange("q a b c -> q (a b c)"), ident[:], [s_, ident], [ps])
                e_copy(p, "act", BbT[:, gpl, d, ri, :], ps[:, (i % 4) * 128:(i % 4 + 1) * 128], [ps], [BbT])
                for g2 in range(2):
                    p.op("pool", lambda h, s_=s_, g2=g2, gpl=gpl: h.memset(s_[g2 * 64:(g2 + 1) * 64, gpl, g2, :], 0.0),
                         reads=[], writes=[s_])
                i += 1
    p.op("pool", lambda h: h.memset(Cp[:], 0.0), writes=[Cp])
    Cv = Cp[:].rearrange("q g r (a b k) -> q g r a b k", a=4, b=2)
    for gpl in range(4):
        gp = ct * 4 + gpl
        for g2 in range(2):
            rows = slice(g2 * 64, (g2 + 1) * 64)
            e_copy(p, "dve", Cv[rows, gpl, 0, gpl, g2, :], S.cP[rows, 0, gp, :], [S.cP], [Cp])
            e_ts(p, "dve", Cv[rows, gpl, 1, gpl, g2, :], S.cP[rows, 1, gp, :], -1.0, None, ALU.mult, None, [S.cP], [Cp])


def s5_tables(p, S, col, cosT, sinT):
    p.op("pool", lambda h: h.memset(cosT[:, 0:1], 1.0), writes=[cosT])
    p.op("pool", lambda h: h.memset(sinT[:, 0:1], 0.0), writes=[sinT])
    R = [cosT, sinT, S.wc, S.ws, S.nws]
    for k in range(11):
        n = 1 << k
        wc = S.wc[:, k, col:col + 1]; ws = S.ws[:, k, col:col + 1]; nws = S.nws[:, k, col:col + 1]
        lo = slice(0, n); hi = slice(n, 2 * n)
        e_ts(p, "dve", cosT[:, hi], cosT[:, lo], wc, None, ALU.mult, None, R, [cosT])
        e_stt(p, cosT[:, hi], sinT[:, lo], nws, cosT[:, hi], ALU.mult, ALU.add, R, [cosT])
        e_ts(p, "dve", sinT[:, hi], sinT[:, lo], wc, None, ALU.mult, None, R, [sinT])
        e_stt(p, sinT[:, hi], cosT[:, lo], ws, sinT[:, hi], ALU.mult, ALU.add, R, [sinT])


def s5_pass(p, S, uT, tabsc, full, Fsum, kin, ident, yacc_evac=None):
    m = p.mark()
    cos2 = [p.alloc([128, 2048]) for _ in range(2)]; sin2 = [p.alloc([128, 2048]) for _ in range(2)]
    TL = 2048
    CH = 512
    nb = 2
    bur = [p.alloc([128, CH]) for _ in range(nb)]; bui = [p.alloc([128, CH]) for _ in range(nb)]
    m1 = [p.alloc([128, CH]) for _ in range(nb)]; m2 = [p.alloc([128, CH]) for _ in range(nb)]
    cr = [p.alloc([128, CH]) for _ in range(nb)]; ci = [p.alloc([128, CH]) for _ in range(nb)]
    kr = [p.alloc([128, CH]) for _ in range(nb)]; ki = [p.alloc([128, CH]) for _ in range(nb)]
    hr = [p.alloc([128, CH], BF16) for _ in range(nb)]; hi = [p.alloc([128, CH], BF16) for _ in range(nb)]
    tiny = p.alloc([128, 4])
    BbT2 = [p.alloc([128, 4, 2, 2, 128], BF16) for _ in range(2)]
    Cp2 = [p.alloc([128, 4, 2, 128], BF16) for _ in range(2)]
    srcs = [p.alloc([128, 4, 2, 16]) for _ in range(2)]
    for s_ in srcs:
        p.op("pool", lambda h, s_=s_: h.memset(s_[:], 0.0), writes=[s_])
    subs = [(0, 0, NCX), (1, NCX, NL)]
    it = 0
    ci_ = 0
    for ct in range(8):
        BbT = BbT2[ct % 2]; Cp = Cp2[ct % 2]
        s5_build_ct(p, S, ct, ident, BbT, Cp, srcs)
        for gpl in range(4):
            gp = ct * 4 + gpl
            for d in range(2):
                col = d * 32 + gp
                if not full:
                    s5_tables(p, S, col, cos2[0], sin2[0])
                    if d == 0:
                        cosT = cos2[0]; sinT = sin2[0]
                    else:
                        cosT = cos2[1]; sinT = sin2[1]
                        e_copy(p, "dve", rev_ap(cosT[:]), cos2[0][:], [cos2[0]], [cosT])
                        e_copy(p, "dve", rev_ap(sinT[:]), sin2[0][:], [sin2[0]], [sinT])
                    p.dma(tabsc[col, 0], cosT[:], reads=[cosT], writes=[tabsc], stream="tb")
                    p.dma(tabsc[col, 1], sinT[:], reads=[sinT], writes=[tabsc], stream="tb")
                else:
                    cosT = cos2[it % 2]; sinT = sin2[it % 2]
                    it += 1
                    p.dma(cosT[:], tabsc[col, 0], reads=[tabsc], writes=[cosT], stream="tbl")
                    p.dma(sinT[:], tabsc[col, 1], reads=[tabsc], writes=[sinT], stream="tbl")
                mcol = S.mag[:, col:col + 1]
                for (which, c0, L) in subs:
                    nch = (L + CH - 1) // CH
                    carry_r = None
                    for cj in range(nch):
                        n = min(CH, L)
                        a = cj * n if d == 0 else L - (cj + 1) * n
                        tau0 = cj * n
                        b_ = ci_ % nb
                        ci_ += 1
                        tok = slice(c0 + a, c0 + a + n)
                        pr = p.pb[5]; pi_ = p.pb[6]
                        e_mm(p, pr[:, 0:n], BbT[:, gpl, d, 0, :], uT[:, ct, tok], True, True, [BbT, uT], [pr])
                        e_mm(p, pi_[:, 0:n], BbT[:, gpl, d, 1, :], uT[:, ct, tok], True, True, [BbT, uT], [pi_])
                        e_copy(p, "act", bur[b_][:, 0:n], pr[:, 0:n], [pr], [bur[b_]])
                        e_copy(p, "act", bui[b_][:, 0:n], pi_[:, 0:n], [pi_], [bui[b_]])
                        br = bur[b_][:, 0:n]; bi = bui[b_][:, 0:n]
                        ts0 = tau0 if d == 0 else (TL - L) + a
                        cs = cosT[:, ts0:ts0 + n]; sn = sinT[:, ts0:ts0 + n]
                        R = [bur[b_], bui[b_], cosT, sinT]
                        e_tt(p, "dve", m1[b_][:, 0:n], cs, br, ALU.mult, R, [m1[b_]])
                        e_tt(p, "dve", m2[b_][:, 0:n], sn, bi, ALU.mult, R, [m2[b_]])
                        e_tt(p, "dve", m1[b_][:, 0:n], m1[b_][:, 0:n], m2[b_][:, 0:n], ALU.add, [m1[b_], m2[b_]], [m1[b_]])
                        e_tt(p, "dve", cr[b_][:, 0:n], cs, bi, ALU.mult, R, [cr[b_]])
                        e_tt(p, "dve", ci[b_][:, 0:n], sn, br, ALU.mult, R, [ci[b_]])
                        e_tt(p, "dve", cr[b_][:, 0:n], cr[b_][:, 0:n], ci[b_][:, 0:n], ALU.subtract, [cr[b_], ci[b_]], [cr[b_]])
                        if cj == 0:
                            if full and which == 1:
                                ini_r = kin[:, 0, col:col + 1]; ini_i = kin[:, 1, col:col + 1]; rd = [kin]
                            else:
                                ini_r = 0.0; ini_i = 0.0; rd = []
                        else:
                            ini_r = carry_r; ini_i = carry_i; rd = [carry_br, carry_bi]
                        mb = mcol.to_broadcast([128, n])
                        ko_r = kr[b_][:, 0:n]; ko_i = ki[b_][:, 0:n]; xi_r = m1[b_][:, 0:n]; xi_i = cr[b_][:, 0:n]
                        if d == 1:
                            ko_r = rev_ap(ko_r); ko_i = rev_ap(ko_i); xi_r = rev_ap(xi_r); xi_i = rev_ap(xi_i)
                        p.op("dve", lambda h, o=ko_r, x=xi_r, ini=ini_r, mb=mb: h.tensor_tensor_scan(
                            out=o, data0=mb, data1=x, initial=ini, op0=ALU.mult, op1=ALU.add),
                            reads=[m1[b_], S.mag] + rd, writes=[kr[b_]])
                        p.op("dve", lambda h, o=ko_i, x=xi_i, ini=ini_i, mb=mb: h.tensor_tensor_scan(
                            out=o, data0=mb, data1=x, initial=ini, op0=ALU.mult, op1=ALU.add),
                            reads=[cr[b_], S.mag] + rd, writes=[ki[b_]])
                        lastc = n - 1 if d == 0 else 0
                        carry_r = kr[b_][:, lastc:lastc + 1]; carry_i = ki[b_][:, lastc:lastc + 1]
                        carry_br = kr[b_]; carry_bi = ki[b_]
                        if full:
                            o_r = hr[b_][:, 0:n]; o_i = hi[b_][:, 0:n]
                            K = [kr[b_], ki[b_], cosT, sinT]
                            e_tt(p, "dve", m1[b_][:, 0:n], cs, kr[b_][:, 0:n], ALU.mult, K, [m1[b_]])
                            e_tt(p, "dve", m2[b_][:, 0:n], sn, ki[b_][:, 0:n], ALU.mult, K, [m2[b_]])
                            e_tt(p, "dve", o_r, m1[b_][:, 0:n], m2[b_][:, 0:n], ALU.subtract, [m1[b_], m2[b_]], [hr[b_]])
                            e_tt(p, "dve", cr[b_][:, 0:n], sn, kr[b_][:, 0:n], ALU.mult, K, [cr[b_]])
                            e_tt(p, "dve", ci[b_][:, 0:n], cs, ki[b_][:, 0:n], ALU.mult, K, [ci[b_]])
                            e_tt(p, "dve", o_i, cr[b_][:, 0:n], ci[b_][:, 0:n], ALU.add, [cr[b_], ci[b_]], [hi[b_]])
                            if which == 0:
                                bank = p.pb[0]; bsl = slice(0, n)
                            else:
                                bank = p.pb[1 + a // CH]; bsl = slice(0, n)
                            first = (gpl == 0 and d == 0)
                            last = (gpl == 3 and d == 1)
                            e_mm(p, bank[:, bsl], Cp[:, gpl, 0, :], hr[b_][:, 0:n], first, False, [Cp, hr[b_]], [bank])
                            e_mm(p, bank[:, bsl], Cp[:, gpl, 1, :], hi[b_][:, 0:n], False, last, [Cp, hi[b_]], [bank])
                    if not full:
                        fl = L - 1 if d == 0 else TL - L
                        csl = cosT[:, fl:fl + 1]; snl = sinT[:, fl:fl + 1]
                        Kt = [carry_br, carry_bi, cosT, sinT, tiny]
                        e_tt(p, "dve", tiny[:, 0:1], csl, carry_r, ALU.mult, Kt, [tiny])
                        e_tt(p, "dve", tiny[:, 1:2], snl, carry_i, ALU.mult, Kt, [tiny])
                        e_tt(p, "dve", Fsum[:, 0, which, col:col + 1], tiny[:, 0:1], tiny[:, 1:2], ALU.subtract, [tiny], [Fsum])
                        e_tt(p, "dve", tiny[:, 2:3], snl, carry_r, ALU.mult, Kt, [tiny])
                        e_tt(p, "dve", tiny[:, 3:4], csl, carry_i, ALU.mult, Kt, [tiny])
                        e_tt(p, "dve", Fsum[:, 1, which, col:col + 1], tiny[:, 2:3], tiny[:, 3:4], ALU.add, [tiny], [Fsum])
        if full:
            yacc_evac(ct)
    p.release(m)


def s5_incoming(p, S, Fsum, gath, onehot, kin):
    m = p.mark()
    I = p.alloc([128, 4, 2, 64])
    t1 = p.alloc([128, 32]); t2 = p.alloc([128, 32]); nr = p.alloc([128, 32]); ni = p.alloc([128, 32])
    f = slice(0, 32); b = slice(32, 64)
    R = [I, gath, Fsum, S.Ar, S.Ai, t1, t2, nr, ni]
    e_copy(p, "dve", I[:, 0, 0, f], Fsum[:, 0, 0, f], R, [I]); e_copy(p, "dve", I[:, 0, 1, f], Fsum[:, 1, 0, f], R, [I])
    for q in range(3):
        cmul(p, "dve", nr[:], ni[:], S.Ar[:, f], S.Ai[:, f], I[:, q, 0, f], I[:, q, 1, f], t1[:], t2[:], R, [t1, t2, nr, ni])
        e_tt(p, "dve", I[:, q + 1, 0, f], nr[:], gath[:, q, 0, f], ALU.add, R, [I])
        e_tt(p, "dve", I[:, q + 1, 1, f], ni[:], gath[:, q, 1, f], ALU.add, R, [I])
    e_copy(p, "dve", I[:, 3, 0, b], Fsum[:, 0, 0, b], R, [I]); e_copy(p, "dve", I[:, 3, 1, b], Fsum[:, 1, 0, b], R, [I])
    for q in (3, 2, 1):
        cmul(p, "dve", nr[:], ni[:], S.Ar[:, b], S.Ai[:, b], I[:, q, 0, b], I[:, q, 1, b], t1[:], t2[:], R, [t1, t2, nr, ni])
        e_tt(p, "dve", I[:, q - 1, 0, b], nr[:], gath[:, q, 0, b], ALU.add, R, [I])
        e_tt(p, "dve", I[:, q - 1, 1, b], ni[:], gath[:, q, 1, b], ALU.add, R, [I])
    own = p.alloc([128, 2, 64])
    e_ts(p, "dve", own[:], I[:, 0], onehot[:, 0:1], None, ALU.mult, None, [I, onehot], [own])
    for q in range(1, 4):
        e_stt(p, own[:], I[:, q], onehot[:, q:q + 1], own[:], ALU.mult, ALU.add, [I, onehot, own], [own])
    t3 = p.alloc([128, 64]); t4 = p.alloc([128, 64])
    cmul(p, "dve", kin[:, 0, :], kin[:, 1, :], S.cth[:], S.sth[:], own[:, 0, :], own[:, 1, :], t3[:], t4[:],
         [S.cth, S.sth, own, t3, t4, kin], [t3, t4, kin])
    p.release(m)


def build_s5_test():
    p = Prog(); p.init_pool()
    io = {}
    for name, shape in [("xl", [NL, D]), ("xc", [NCX, D]), ("cT", [128, 8, 2]), ("modw", [D, 6144]), ("modb", [1, 6144]),
                        ("n1g", [1, D]), ("ident", [128, 128]), ("lamP", [128, 2, 64]), ("ldtP", [128, 64]),
                        ("bP", [128, 2, 32, 16]), ("cP", [128, 2, 32, 16]), ("dP", [128, 8]), ("onehot", [128, 4])]:
        io[name] = p.dram(name, shape)
    dbg = p.dram("dbg", [128, 8, NT], kind="ExternalOutput")
    dbgF = p.dram("dbgF", [128, 2, 2, 64], kind="ExternalOutput")
    dbgK = p.dram("dbgK", [128, 2, 64], kind="ExternalOutput")
    modsc = p.dram("modsc", [2, 6144], kind="Internal")
    tabsc = p.dram("tabsc", [64, 2, 128, 2048], kind="Internal")
    fsrc = p.dram("fsrc", [128, 128], kind="Internal")
    fdst = p.dram("fdst", [512, 128], kind="Internal")
    ident = p.alloc([128, 128]); p.dma(ident[:], io["ident"][:], writes=[ident])
    onehot = p.alloc([128, 4]); p.dma(onehot[:], io["onehot"][:], writes=[onehot])
    dP = p.alloc([128, 8]); p.dma(dP[:], io["dP"][:], writes=[dP])
    phase_mod(p, io["cT"], io["modw"], io["modb"], modsc, ident)
    uT = p.alloc([128, 8, NT], BF16, name="uT")
    phase_hn(p, io["xl"], io["xc"], modsc, io["n1g"], ident, uT, 0, 1)
    S = s5_params(p, io, ident)
    Fsum = p.alloc([128, 2, 2, 64]); kin = p.alloc([128, 2, 64])
    s5_pass(p, S, uT, tabsc, False, Fsum, None, ident)
    p.dma(fsrc[:].rearrange("q (r c) -> q r c", r=2), Fsum[:, :, 1, :], reads=[Fsum], writes=[fsrc], stream="st")
    p.cc(lambda h: h.collective_compute("AllGather", ALU.bypass, replica_groups=[[0, 1, 2, 3], [4, 5, 6, 7]],
                                        ins=[fsrc.t.opt()], outs=[fdst.t.opt()]), reads=[fsrc], writes=[fdst])
    gath = p.alloc([128, 4, 2, 64])
    p.dma(gath[:], fdst[:].rearrange("(q x) (r c) -> x q r c", x=128, r=2), reads=[fdst], writes=[gath])
    s5_incoming(p, S, Fsum, gath, onehot, kin)
    p.dma(dbgF[:], Fsum[:], reads=[Fsum], stream="st"); p.dma(dbgK[:], kin[:], reads=[kin], stream="st")
    vbuf = [p.alloc([128, 512]) for _ in range(2)]

    def evac(ct):
        for bi_ in range(5):
            n = NCX if bi_ == 0 else 512
            c0 = 0 if bi_ == 0 else NCX + (bi_ - 1) * 512
            v = vbuf[bi_ % 2]
            e_stt(p, v[:, 0:n], uT[:, ct, c0:c0 + n], dP[:, ct:ct + 1], p.pb[bi_][:, 0:n], ALU.mult, ALU.add,
                  [uT, dP, p.pb[bi_]], [v])
            p.dma(dbg[:, ct, c0:c0 + n], v[:, 0:n], reads=[v], stream="st")

    s5_pass(p, S, uT, tabsc, True, None, kin, ident, evac)
    p.wait_all()
    return p


def host_inputs(z, layer=0):
    x = z["x"]; c = z["c"]; ctx = z["ctx"]; c_ctx = z["c_ctx"]
    lam = np.stack([z["s5_lam_re"][0], z["s5_lam_im"][0]], 0)
    lamP = lam.reshape(2, 2, 32, 2, 64).transpose(3, 4, 0, 1, 2).reshape(128, 2, 64)
    ldt = z["s5_log_dt"][0]
    ldtP = np.broadcast_to(ldt.reshape(2, 32, 2)[:, :, :, None], (2, 32, 2, 64)).transpose(2, 3, 0, 1).reshape(128, 64)
    b = np.stack([z["s5_b_re"][0], z["s5_b_im"][0]], 0)
    bP = b.reshape(2, 32, 2, 64, 16).transpose(2, 3, 0, 1, 4).reshape(128, 2, 32, 16)
    cc = np.stack([z["s5_c_re"][0], z["s5_c_im"][0]], 0)
    cP = cc.reshape(2, 32, 2, 16, 64).transpose(2, 4, 0, 1, 3).reshape(128, 2, 32, 16)
    dP = z["s5_d"][0].reshape(8, 128).T
    maps = []
    for k in range(8):
        bb = k // 4; q = k % 4
        cT = np.stack([c[bb], c_ctx], axis=-1).reshape(8, 128, 2).transpose(1, 0, 2)
        oh = np.zeros((128, 4), np.float32); oh[:, q] = 1
        maps.append(dict(xl=x[bb, q * NL:(q + 1) * NL], xc=ctx[bb], cT=np.ascontiguousarray(cT),
                         modw=z["mod_w"][layer], modb=z["mod_b"][layer][None], n1g=z["norm1_g"][layer][None],
                         ident=np.eye(128, dtype=np.float32), lamP=np.ascontiguousarray(lamP),
                         ldtP=np.ascontiguousarray(ldtP), bP=np.ascontiguousarray(bP), cP=np.ascontiguousarray(cP),
                         dP=np.ascontiguousarray(dP), onehot=oh))
    return maps


def ref_s5(z, bb, groups):
    f8 = np.float64
    x = z["x"][bb].astype(f8); ctx = z["ctx"][bb].astype(f8); c = z["c"][bb].astype(f8); c_ctx = z["c_ctx"].astype(f8)
    silu = lambda v: v / (1 + np.exp(-v))
    rms = lambda v, g: v / np.sqrt((v * v).mean(-1, keepdims=True) + 1e-6) * g
    mw = z["mod_w"][0].astype(f8); mb = z["mod_b"][0].astype(f8); g1 = z["norm1_g"][0].astype(f8)
    ml = silu(c) @ mw + mb; mc = silu(c_ctx) @ mw + mb
    hn = rms(x, g1) * (1 + ml[D:2 * D]) + ml[:D]
    cn = rms(ctx, g1) * (1 + mc[D:2 * D]) + mc[:D]
    out = {}
    for g in groups:
        ch = slice(g * 16, (g + 1) * 16)
        tot = np.zeros((NCX + 8192, 16))
        for d in range(2):
            lr = z["s5_lam_re"][0, d, g].astype(f8); li = z["s5_lam_im"][0, d, g].astype(f8)
            step = np.exp(z["s5_log_dt"][0, d, g].astype(f8))
            lamc = lr + 1j * li
            abar = np.exp(lamc * step)
            Bc = z["s5_b_re"][0, g].astype(f8) + 1j * z["s5_b_im"][0, g].astype(f8)
            Cc = z["s5_c_re"][0, g].astype(f8) + 1j * z["s5_c_im"][0, g].astype(f8)
            Bbar = ((abar - 1) / lamc)[:, None] * Bc
            seq = np.concatenate([cn[:, ch], hn[:, ch]], 0) if d == 0 else np.concatenate([cn[::-1, ch], hn[::-1, ch]], 0)
            bu = seq @ Bbar.T
            h = np.zeros(64, complex); ys = np.zeros((len(seq), 16))
            for t in range(len(seq)):
                h = abar * h + bu[t]
                ys[t] = (Cc @ h).real
            if d == 1:
                ys = np.concatenate([ys[:NCX][::-1], ys[NCX:][::-1]], 0)
            tot += ys
        u = np.concatenate([cn[:, ch], hn[:, ch]], 0)
        out[g] = tot + z["s5_d"][0, ch].astype(f8) * u
    return out


def gelu_evac(p, uT, dP, gT):
    vb = [p.alloc([128, 512]) for _ in range(2)]
    wb = [p.alloc([128, 512]) for _ in range(1)]
    cnt = [0]

    def evac(ct):
        for bi_ in range(5):
            n = NCX if bi_ == 0 else 512
            c0 = 0 if bi_ == 0 else NCX + (bi_ - 1) * 512
            v = vb[cnt[0] % 2]; w = wb[0]
            cnt[0] += 1
            e_stt(p, v[:, 0:n], uT[:, ct, c0:c0 + n], dP[:, ct:ct + 1], p.pb[bi_][:, 0:n], ALU.mult, ALU.add,
                  [uT, dP, p.pb[bi_]], [v])
            e_act(p, w[:, 0:n], v[:, 0:n], AF.Square, [v], [w])
            e_ts(p, "dve", w[:, 0:n], w[:, 0:n], 0.044715, 1.0, ALU.mult, ALU.add, [w], [w])
            e_tt(p, "dve", w[:, 0:n], w[:, 0:n], v[:, 0:n], ALU.mult, [w, v], [w])
            e_act(p, w[:, 0:n], w[:, 0:n], AF.Sigmoid, [w], [w], scale=1.5957691216057308)
            e_tt(p, "pool", gT[:, ct, c0:c0 + n], v[:, 0:n], w[:, 0:n], ALU.mult, [v, w], [gT])
    return evac


def load_w_bf16(p, dst, src_ap, w):
    p.dma(dst, src_ap, writes=w, eng="pool")


def phase_c(p, io, modsc, ident, gT, h2T, gates, x1sc, layer_glu=True):
    m = p.mark()
    W = p.alloc([128, 4, 8, 512], BF16)
    for nb in range(4):
        load_w_bf16(p, W[:, nb],
                    io["gluw"][:, nb * 512:(nb + 1) * 512].rearrange("(k q) n -> q k n", q=128), [W])
    gb = p.alloc([128, 2048]); load_bcast(p, gb[:], io["glub"][0:1, :], [], [gb])
    n2 = p.alloc([128, D]); load_bcast(p, n2[:], io["n2g"][0:1, :], [], [n2])
    G1 = []; A2 = []; SH2 = []
    for which in range(2):
        g1 = p.alloc([128, D]); a2 = p.alloc([128, D]); s2 = p.alloc([128, D])
        load_bcast(p, g1[:], modsc[which:which + 1, 2 * D:3 * D], [modsc], [g1])
        load_bcast(p, s2[:], modsc[which:which + 1, 3 * D:4 * D], [modsc], [s2])
        load_bcast(p, a2[:], modsc[which:which + 1, 4 * D:5 * D], [modsc], [a2])
        e_stt(p, a2[:], a2[:], 1.0, n2[:], ALU.add, ALU.mult, [a2, n2], [a2])
        G1.append(g1); A2.append(a2); SH2.append(s2)
    rw = p.alloc([128, 8, 16]); p.dma(rw[:], io["rw"][:].rearrange("(k q) n -> q k n", q=128), writes=[rw])
    rb = p.alloc([128, 16]); load_bcast(p, rb[:], io["rb"][0:1, :], [], [rb])
    xs = [p.alloc([128, D]) for _ in range(2)]
    val = p.alloc([128, D]); gat = p.alloc([128, D]); x1 = [p.alloc([128, D]) for _ in range(2)]
    hn2 = p.alloc([128, D]); junk = p.alloc([128, D]); hT32 = p.alloc([128, 8, 128])
    sm = p.alloc([128, 128])
    ss = p.alloc([128, 1]); rs = p.alloc([128, 1])
    for t in range(18):
        which = 1 if t < 2 else 0
        src = io["xc"][t * 128:(t + 1) * 128, :] if t < 2 else io["xl"][(t - 2) * 128:(t - 1) * 128, :]
        x = xs[t % 2]; xo = x1[t % 2]
        p.dma(x[:], src, writes=[x])
        tok = slice(t * 128, (t + 1) * 128)
        for nb in range(4):
            ps = p.pb[nb]
            for k in range(8):
                e_mm(p, ps[:], gT[:, k, tok], W[:, nb, k, :], k == 0, k == 7, [gT, W], [ps])
        for nb in range(2):
            e_tt(p, "dve", val[:, nb * 512:(nb + 1) * 512], p.pb[nb][:], gb[:, nb * 512:(nb + 1) * 512], ALU.add,
                 [p.pb[nb], gb], [val])
            e_tt(p, "dve", gat[:, nb * 512:(nb + 1) * 512], p.pb[2 + nb][:], gb[:, D + nb * 512:D + (nb + 1) * 512],
                 ALU.add, [p.pb[2 + nb], gb], [gat])
        e_act(p, gat[:], gat[:], AF.Sigmoid, [gat], [gat])
        e_tt(p, "pool", val[:], val[:], gat[:], ALU.mult, [val, gat], [val])
        e_tt(p, "pool", val[:], val[:], G1[which][:], ALU.mult, [val, G1[which]], [val])
        e_tt(p, "dve", xo[:], val[:], x[:], ALU.add, [val, x], [xo])
        p.dma(x1sc[tok, :], xo[:], reads=[xo], writes=[x1sc])
        e_act(p, junk[:], xo[:], AF.Square, [xo], [junk, ss], accum_out=ss[:])
        e_ts(p, "dve", rs[:], ss[:], 1.0 / D, 1e-6, ALU.mult, ALU.add, [ss], [rs])
        e_act(p, rs[:], rs[:], AF.Sqrt, [rs], [rs])
        p.op("dve", lambda h: h.reciprocal(out=rs[:], in_=rs[:]), reads=[rs], writes=[rs])
        e_stt(p, hn2[:], xo[:], rs[:], A2[which][:], ALU.mult, ALU.mult, [xo, rs, A2[which]], [hn2])
        e_tt(p, "pool", hn2[:], hn2[:], SH2[which][:], ALU.add, [hn2, SH2[which]], [hn2])
        for half in range(2):
            ps = p.pb[4 + half]
            for c4 in range(4):
                ct = half * 4 + c4
                e_tr(p, ps[:, c4 * 128:(c4 + 1) * 128], hn2[:, ct * 128:(ct + 1) * 128], ident[:], [hn2, ident], [ps])
            pv = ps[:].rearrange("q (c n) -> q c n", c=4)
            e_copy(p, "act", h2T[:, half * 4:(half + 1) * 4, tok], pv, [ps], [h2T])
            e_copy(p, "dve", hT32[:, half * 4:(half + 1) * 4, :], pv, [ps], [hT32])
        pl = p.pb[6]
        for k in range(8):
            e_mm(p, pl[:, 0:16], hT32[:, k, :], rw[:, k, :], k == 0, k == 7, [hT32, rw], [pl])
        routing(p, pl, rb, sm, gates[:, t, :], gates)
    p.release(m)


def routing(p, pl, rb, sm, gout, gates_buf):
    R = [sm]
    s = sm[:, 0:16]; sel2 = sm[:, 16:48]; ps_ = sm[:, 48:72]; gs = sm[:, 72:76]; t2 = sm[:, 76:78]
    gmax = sm[:, 78:79]; Gm = sm[:, 80:84]; g1 = sm[:, 84:100]; cnt = sm[:, 100:116]; wsum = sm[:, 116:117]
    e_act(p, s, pl[:, 0:16], AF.Sigmoid, [pl], R)
    sel2v = sel2.rearrange("q (g e) -> q g e", g=4)
    sv = s.rearrange("q (g e) -> q g e", g=4)
    rbv = rb[:].rearrange("q (g e) -> q g e", g=4)
    e_tt(p, "dve", sel2v[:, :, 0:4], sv, rbv, ALU.add, R + [rb], R)
    e_copy(p, "dve", sel2v[:, :, 4:8], sel2v[:, :, 0:4], R, R)
    pv = ps_.rearrange("q (g e) -> q g e", g=4)
    pairs = [(0, 1), (0, 2), (0, 3), (1, 2), (1, 3), (2, 3)]
    for i, (a, b) in enumerate(pairs):
        e_tt(p, "dve", pv[:, :, i:i + 1], sel2v[:, :, a:a + 1], sel2v[:, :, b:b + 1], ALU.add, R, R)
    e_tt(p, "dve", pv[:, :, 0:3], pv[:, :, 0:3], pv[:, :, 3:6], ALU.max, R, R)
    e_tt(p, "dve", pv[:, :, 0:1], pv[:, :, 0:1], pv[:, :, 1:2], ALU.max, R, R)
    e_tt(p, "dve", gs.unsqueeze(2), pv[:, :, 0:1], pv[:, :, 2:3], ALU.max, R, R)
    e_tt(p, "dve", t2, gs[:, 0:2], gs[:, 2:4], ALU.max, R, R)
    e_tt(p, "dve", gmax, t2[:, 0:1], t2[:, 1:2], ALU.max, R, R)
    e_ts(p, "dve", Gm, gs, gmax, 1.0, ALU.is_ge, ALU.mult, R, R)
    g1v = g1.rearrange("q (g e) -> q g e", g=4); cv = cnt.rearrange("q (g e) -> q g e", g=4)
    e_tt(p, "dve", cv, sel2v[:, :, 1:5], sel2v[:, :, 0:4], ALU.is_gt, R, R)
    for r in (2, 3):
        e_tt(p, "dve", g1v, sel2v[:, :, r:r + 4], sel2v[:, :, 0:4], ALU.is_gt, R, R)
        e_tt(p, "dve", cv, cv, g1v, ALU.add, R, R)
    e_ts(p, "dve", cv, cv, 1.5, 1.0, ALU.is_lt, ALU.mult, R, R)
    e_tt(p, "dve", cv, cv, Gm.unsqueeze(2).to_broadcast([128, 4, 4]), ALU.mult, R, R)
    e_tt(p, "dve", cnt, cnt, s, ALU.mult, R, R)
    p.op("dve", lambda h: h.reduce_sum(out=wsum, in_=cnt, axis=AX.X), reads=R, writes=R)
    p.op("dve", lambda h: h.reciprocal(out=wsum, in_=wsum), reads=R, writes=R)
    e_ts(p, "dve", gout, cnt, wsum, 1.0, ALU.mult, ALU.mult, R, [gates_buf])


def phase_moe(p, io, layer_w, modsc, h2T, gates, x1sc, ntiles, out_cb, mod_which_of_tile):
    m = p.mark()
    w1d, w3d, w2d = layer_w
    ntok = ntiles * 128
    yacc = p.alloc([128, ntiles, D], name="yacc")
    for t0 in range(0, ntiles, 4):
        t1 = min(ntiles, t0 + 4)
        p.op("pool", lambda h, t0=t0, t1=t1: h.memset(yacc[:, t0:t1, :], 0.0), writes=[yacc])
    w1 = [p.alloc([128, 8, 512], BF16) for _ in range(2)]; w3 = [p.alloc([128, 8, 512], BF16) for _ in range(2)]
    w2 = [p.alloc([128, 4, D], BF16) for _ in range(2)]
    hT = [p.alloc([128, 4, 512], BF16) for _ in range(2)]
    s1 = [p.alloc([128, 512]) for _ in range(2)]
    blocks = [(b0, min(512, ntok - b0)) for b0 in range(0, ntok, 512)]
    ib = 0; iy = 0; ih = 0
    for e in range(16):
        a1 = w1[e % 2]; a3 = w3[e % 2]; a2 = w2[e % 2]
        load_w_bf16(p, a1[:], w1d[e].rearrange("(k q) n -> q k n", q=128), [a1])
        load_w_bf16(p, a3[:], w3d[e].rearrange("(k q) n -> q k n", q=128), [a3])
        load_w_bf16(p, a2[:], w2d[e].rearrange("(k q) n -> q k n", q=128), [a2])
        for (b0, n) in blocks:
            h = hT[ib % 2]; ib += 1
            for hc in range(4):
                p1 = p.pb[ih % 2]; p3 = p.pb[2 + ih % 2]; sb1 = s1[ih % 2]; ih += 1
                for k in range(8):
                    e_mm(p, p1[:, 0:n], a1[:, k, hc * 128:(hc + 1) * 128], h2T[:, k, b0:b0 + n], k == 0, k == 7, [a1, h2T], [p1])
                for k in range(8):
                    e_mm(p, p3[:, 0:n], a3[:, k, hc * 128:(hc + 1) * 128], h2T[:, k, b0:b0 + n], k == 0, k == 7, [a3, h2T], [p3])
                e_act(p, sb1[:, 0:n], p1[:, 0:n], AF.Silu, [p1], [sb1])
                e_tt(p, "dve", h[:, hc, 0:n], sb1[:, 0:n], p3[:, 0:n], ALU.mult, [sb1, p3], [h])
            for tt in range(n // 128):
                t = b0 // 128 + tt
                for dh in range(2):
                    py = p.pb[4 + iy % 4]; iy += 1
                    for hc in range(4):
                        e_mm(p, py[:], h[:, hc, tt * 128:(tt + 1) * 128], a2[:, hc, dh * 512:(dh + 1) * 512],
                             hc == 0, hc == 3, [h, a2], [py])
                    ya = yacc[:, t, dh * 512:(dh + 1) * 512]
                    e_stt(p, ya, py[:], gates[:, t, e:e + 1], ya, ALU.mult, ALU.add, [py, gates, yacc], [yacc])
    G2 = []
    for which in range(2):
        g2 = p.alloc([128, D]); load_bcast(p, g2[:], modsc[which:which + 1, 5 * D:6 * D], [modsc], [g2]); G2.append(g2)
    xb = [p.alloc([128, D]) for _ in range(2)]
    for t in range(ntiles):
        which = mod_which_of_tile(t)
        x = xb[t % 2]
        p.dma(x[:], x1sc[t * 128:(t + 1) * 128, :], reads=[x1sc], writes=[x])
        e_tt(p, "pool", yacc[:, t, :], yacc[:, t, :], G2[which][:], ALU.mult, [yacc, G2[which]], [yacc])
        e_tt(p, "dve", x[:], x[:], yacc[:, t, :], ALU.add, [x, yacc], [x])
        out_cb(t, x)
    p.release(m)


L0_INPUTS = [("xl", [NL, D]), ("xc", [NCX, D]), ("cT", [128, 8, 2]), ("modw", [D, 6144]), ("modb", [1, 6144]),
             ("n1g", [1, D]), ("ident", [128, 128]), ("lamP", [128, 2, 64]), ("ldtP", [128, 64]),
             ("bP", [128, 2, 32, 16]), ("cP", [128, 2, 32, 16]), ("dP", [128, 8]), ("onehot", [128, 4]),
             ("gluw", [D, 2048]), ("glub", [1, 2048]), ("n2g", [1, D]), ("rw", [D, 16]), ("rb", [1, 16]),
             ("w1", [16, D, 512]), ("w3", [16, D, 512]), ("w2", [16, 512, D])]


def layer0(p, io, sc, ident, onehot, out_cb, skip_s5=False, stop_after_c=False, dbg=None):
    dP = p.alloc([128, 8]); p.dma(dP[:], io["dP"][:], writes=[dP])
    phase_mod(p, io["cT"], io["modw"], io["modb"], sc["modsc"], ident)
    gates = p.alloc([128, 18, 16])
    hi0 = p.hi
    gT = p.alloc([128, 8, NT], BF16, name="gT", hi=True)
    m_g = p.mark()
    uT = p.alloc([128, 8, NT], BF16, name="uT")
    phase_hn(p, io["xl"], io["xc"], sc["modsc"], io["n1g"], ident, uT, 0, 1)
    if skip_s5:
        for ct in range(8):
            e_copy(p, "dve", gT[:, ct, :], uT[:, ct, :], [uT], [gT])
    S = None if skip_s5 else s5_params(p, io, ident)
    Fsum = p.alloc([128, 2, 2, 64]); kin = p.alloc([128, 2, 64])
    if not skip_s5:
      s5_pass(p, S, uT, sc["tabsc"], False, Fsum, None, ident)
    p.dma(sc["fsrc"][:].rearrange("q (r c) -> q r c", r=2), Fsum[:, :, 1, :], reads=[Fsum], writes=[sc["fsrc"]])
    p.cc(lambda h: h.collective_compute("AllGather", ALU.bypass, replica_groups=[[0, 1, 2, 3], [4, 5, 6, 7]],
                                        ins=[sc["fsrc"].t.opt()], outs=[sc["fdst"].t.opt()]),
         reads=[sc["fsrc"]], writes=[sc["fdst"]])
    gath = p.alloc([128, 4, 2, 64])
    p.dma(gath[:], sc["fdst"][:].rearrange("(q x) (r c) -> x q r c", x=128, r=2), reads=[sc["fdst"]], writes=[gath])
    if not skip_s5:
        s5_incoming(p, S, Fsum, gath, onehot, kin)
        evac = gelu_evac(p, uT, dP, gT)
        s5_pass(p, S, uT, sc["tabsc"], True, None, kin, ident, evac)
    p.release(m_g)
    h2T = p.alloc([128, 8, NT], BF16, name="h2T")
    phase_c(p, io, sc["modsc"], ident, gT, h2T, gates, sc["x1sc"])
    p.hi = hi0
    if dbg is not None:
        p.dma(dbg["gates"][:], gates[:], reads=[gates])
    if stop_after_c:
        return
    phase_moe(p, io, (io["w1"], io["w3"], io["w2"]), sc["modsc"], h2T, gates, sc["x1sc"], 18,
              out_cb, lambda t: 1 if t < 2 else 0)


def build_l0_test():
    p = Prog(); p.init_pool()
    io = {name: p.dram(name, shape) for name, shape in L0_INPUTS}
    xo = p.dram("xo", [NT, D], kind="ExternalOutput")
    sc = dict(modsc=p.dram("modsc", [2, 6144], kind="Internal"),
              tabsc=p.dram("tabsc", [64, 2, 128, 2048], kind="Internal"),
              fsrc=p.dram("fsrc", [128, 128], kind="Internal"), fdst=p.dram("fdst", [512, 128], kind="Internal"),
              x1sc=p.dram("x1sc", [NT, D], kind="Internal"))
    ident = p.alloc([128, 128]); p.dma(ident[:], io["ident"][:], writes=[ident])
    onehot = p.alloc([128, 4]); p.dma(onehot[:], io["onehot"][:], writes=[onehot])

    def out_cb(t, x):
        p.dma(xo[t * 128:(t + 1) * 128, :], x[:], reads=[x])

    layer0(p, io, sc, ident, onehot, out_cb)
    p.wait_all()
    return p


def host_inputs_l0(z):
    maps = host_inputs(z, 0)
    for k in range(8):
        maps[k].update(gluw=z["s5_glu_w"][0], glub=z["s5_glu_b"][0][None], n2g=z["norm2_g"][0][None],
                       rw=z["router_w"], rb=z["router_b"][None], w1=z["moe_w1"][0], w3=z["moe_w3"][0], w2=z["moe_w2"][0])
    return maps


NCH = 66
PADW = 8456
CTX0 = 2
LAT0 = 262
RSW = 1028

L1_INPUTS = [("cT", [128, 8, 2]), ("modw1", [D, 6144]), ("modb1", [1, 6144]), ("n1g1", [1, D]), ("n2g1", [1, D]),
             ("fing", [1, D]), ("wz", [D, 512]), ("wxbc", [D, 1024]), ("wdt", [D, 16]), ("convw", [128, 8, 5]),
             ("convb", [128, 8]), ("dtb", [1, 16]), ("alog", [1, 16]), ("dsk", [1, 8]), ("sng", [1, 512]),
             ("outw", [512, D]), ("triU", [128, 128]), ("triL", [128, 128]), ("ones", [128, 128]),
             ("rw", [D, 16]), ("rb", [1, 16]), ("w1b", [16, D, 512]), ("w3b", [16, D, 512]), ("w2b", [16, 512, D])]


def l1_scratch(p):
    sc = {}
    sc["modsc1"] = p.dram("modsc1", [2, 6144], kind="Internal")
    sc["pre"] = p.dram("pre", [8, 128, PADW], kind="Internal")
    sc["X"] = p.dram("Xs", [NCH, 128, 512], BF16, kind="Internal")
    sc["Bt"] = p.dram("Bts", [NCH, 128, 256], BF16, kind="Internal")
    sc["BT"] = p.dram("BTs", [NCH, 128, 256], BF16, kind="Internal")
    sc["CT"] = p.dram("CTs", [NCH, 128, 256], BF16, kind="Internal")
    sc["dts"] = p.dram("dts", [NCH, 128, 16], kind="Internal")
    sc["Z"] = p.dram("Zs", [NCH, 128, 512], BF16, kind="Internal")
    sc["yf"] = p.dram("yfs", [NCH, 128, 512], kind="Internal")
    sc["yb"] = p.dram("ybs", [NCH, 128, 512], kind="Internal")
    sc["rsrc"] = p.dram("rsrc", [8192, RSW], kind="Internal")
    sc["rdst"] = p.dram("rdst", [2048, RSW], kind="Internal")
    sc["x1b"] = p.dram("x1b", [2048, D], kind="Internal")
    return sc


def chunk_rows(xg, ctx1, c):
    if c < 2:
        return ctx1[c * 128:(c + 1) * 128, :]
    return xg[:].rearrange("(r w) d -> w r d", w=64)[c - 2]


def l1_proj(p, io, sc, xg, ctx1, ident):
    m = p.mark()
    modsc = sc["modsc1"]
    g_sb = p.alloc([128, D]); load_bcast(p, g_sb[:], io["n1g1"][0:1, :], [], [g_sb])
    A = []; SH = []
    for which in range(2):
        a = p.alloc([128, D]); s = p.alloc([128, D])
        load_bcast(p, a[:], modsc[which:which + 1, D:2 * D], [modsc], [a])
        load_bcast(p, s[:], modsc[which:which + 1, 0:D], [modsc], [s])
        e_stt(p, a[:], a[:], 1.0, g_sb[:], ALU.add, ALU.mult, [a, g_sb], [a])
        A.append(a); SH.append(s)
    Wx = p.alloc([128, 8, 1024], BF16); Wz = p.alloc([128, 8, 512], BF16); Wd = p.alloc([128, 8, 16])
    for half in range(2):
        load_w_bf16(p, Wx[:, :, half * 512:(half + 1) * 512] if False else Wx[:, half * 4:(half + 1) * 4, :],
                    io["wxbc"][half * 512:(half + 1) * 512, :].rearrange("(k q) n -> q k n", q=128), [Wx])
    load_w_bf16(p, Wz[:], io["wz"][:].rearrange("(k q) n -> q k n", q=128), [Wz])
    p.dma(Wd[:], io["wdt"][:].rearrange("(k q) n -> q k n", q=128), writes=[Wd])
    dtb = p.alloc([128, 16]); load_bcast(p, dtb[:], io["dtb"][0:1, :], [], [dtb])
    zero = p.alloc([128, 8]); p.op("pool", lambda h: h.memset(zero[:], 0.0), writes=[zero])
    for ft in range(8):
        for c0 in (0, CTX0 + 256, LAT0 - 2, LAT0 + 8192):
            p.dma(sc["pre"][ft, :, c0:c0 + 2], zero[:, 0:2], reads=[zero], writes=[sc["pre"]])
    xs = [p.alloc([128, D]) for _ in range(2)]; ys = [p.alloc([128, D]) for _ in range(2)]
    junk = p.alloc([128, D]); ss = p.alloc([128, 1]); rs = p.alloc([128, 1])
    hT = [p.alloc([128, 8, 512], BF16) for _ in range(2)]
    hT32 = p.alloc([128, 8, 128])
    zt = [p.alloc([128, 512], BF16) for _ in range(2)]
    dtt = [p.alloc([128, 16]) for _ in range(2)]
    pre_sb = [p.alloc([128, 512]) for _ in range(2)]
    blocks = [(0, [0, 1])] + [(1, [2 + 4 * b + i for i in range(4)]) for b in range(16)]
    ib = 0; it = 0; ip = 0
    for (islat, chunks) in blocks:
        which = 0 if islat else 1
        h = hT[ib % 2]; ib += 1
        n = 128 * len(chunks)
        for ti, c in enumerate(chunks):
            x = xs[it % 2]; y = ys[it % 2]; it += 1
            p.dma(x[:], chunk_rows(xg, ctx1, c), reads=[xg, ctx1], writes=[x])
            e_act(p, junk[:], x[:], AF.Square, [x], [junk, ss], accum_out=ss[:])
            e_ts(p, "dve", rs[:], ss[:], 1.0 / D, 1e-6, ALU.mult, ALU.add, [ss], [rs])
            e_act(p, rs[:], rs[:], AF.Sqrt, [rs], [rs])
            p.op("dve", lambda hh: hh.reciprocal(out=rs[:], in_=rs[:]), reads=[rs], writes=[rs])
            e_stt(p, y[:], x[:], rs[:], A[which][:], ALU.mult, ALU.mult, [x, rs, A[which]], [y])
            e_tt(p, "pool", y[:], y[:], SH[which][:], ALU.add, [y, SH[which]], [y])
            for half in range(2):
                ps = p.pb[half]
                for c4 in range(4):
                    ct = half * 4 + c4
                    e_tr(p, ps[:, c4 * 128:(c4 + 1) * 128], y[:, ct * 128:(ct + 1) * 128], ident[:], [y, ident], [ps])
                pv = ps[:].rearrange("q (c n) -> q c n", c=4)
                e_copy(p, "act", h[:, half * 4:(half + 1) * 4, ti * 128:(ti + 1) * 128], pv, [ps], [h])
                e_copy(p, "dve", hT32[:, half * 4:(half + 1) * 4, :], pv, [ps], [hT32])
            tokc = slice(ti * 128, (ti + 1) * 128)
            if islat:
                pz = p.pb[2]
                for k in range(8):
                    e_mm(p, pz[:], h[:, k, tokc], Wz[:, k, :], k == 0, k == 7, [h, Wz], [pz])
                z = zt[it % 2]
                e_act(p, z[:], pz[:], AF.Silu, [pz], [z])
                p.dma(sc["Z"][c], z[:], reads=[z], writes=[sc["Z"]])
            pd = p.pb[3]
            for k in range(8):
                e_mm(p, pd[:, 0:16], hT32[:, k, :], Wd[:, k, :], k == 0, k == 7, [hT32, Wd], [pd])
            dt_ = dtt[it % 2]
            e_tt(p, "dve", dt_[:], pd[:, 0:16], dtb[:], ALU.add, [pd, dtb], [dt_])
            e_act(p, dt_[:], dt_[:], AF.Exp, [dt_], [dt_])
            e_ts(p, "dve", dt_[:], dt_[:], 1.0, 1.0, ALU.add, ALU.mult, [dt_], [dt_])
            e_act(p, dt_[:], dt_[:], AF.Ln, [dt_], [dt_])
            p.dma(sc["dts"][c], dt_[:], reads=[dt_], writes=[sc["dts"]])
        col0 = (LAT0 + (chunks[0] - 2) * 128) if islat else CTX0
        for ft in range(8):
            px = p.pb[4 + ft % 4]
            for k in range(8):
                e_mm(p, px[:, 0:n], Wx[:, k, ft * 128:(ft + 1) * 128], h[:, k, 0:n], k == 0, k == 7, [Wx, h], [px])
            o = pre_sb[ip % 2]; ip += 1
            e_copy(p, "act", o[:, 0:n], px[:, 0:n], [px], [o])
            p.dma(sc["pre"][ft, :, col0:col0 + n], o[:, 0:n], reads=[o], writes=[sc["pre"]])
    p.release(m)


def l1_conv(p, io, sc, identb):
    m = p.mark()
    cw = p.alloc([128, 8, 5]); cb = p.alloc([128, 8])
    p.dma(cw[:], io["convw"][:], writes=[cw]); p.dma(cb[:], io["convb"][:], writes=[cb])
    inb = [p.alloc([128, 516]) for _ in range(3)]
    acc = [p.alloc([128, 512]) for _ in range(2)]
    act = [p.alloc([128, 8, 512], BF16) for _ in range(2)]
    xo = [p.alloc([128, 512], BF16) for _ in range(2)]; bo = [p.alloc([128, 256], BF16) for _ in range(2)]
    blocks = [(0, [0, 1])] + [(1, [2 + 4 * b + i for i in range(4)]) for b in range(16)]
    ii = 0; ia = 0; ib = 0; ix = 0
    for (islat, chunks) in blocks:
        n = 128 * len(chunks)
        col0 = (LAT0 + (chunks[0] - 2) * 128) if islat else CTX0
        a8 = act[ib % 2]; ib += 1
        for ft in range(8):
            xin = inb[ii % 3]; ii += 1
            p.dma(xin[:, 0:n + 4], sc["pre"][ft, :, col0 - 2:col0 + n + 2], reads=[sc["pre"]], writes=[xin])
            a = acc[ia % 2]; ia += 1
            e_ts(p, "dve", a[:, 0:n], xin[:, 0:n], cw[:, ft, 0:1], cb[:, ft:ft + 1], ALU.mult, ALU.add, [xin, cw, cb], [a])
            for k in range(1, 5):
                e_stt(p, a[:, 0:n], xin[:, k:k + n], cw[:, ft, k:k + 1], a[:, 0:n], ALU.mult, ALU.add, [xin, cw, a], [a])
            e_act(p, a8[:, ft, 0:n], a[:, 0:n], AF.Silu, [a], [a8])
        for ti, c in enumerate(chunks):
            tok = slice(ti * 128, (ti + 1) * 128)
            pt = p.pb[ix % 2]; ptb = pt.t.bitcast(BF16)
            x_o = xo[ix % 2]; b_o = bo[ix % 2]; ix += 1
            for ft in range(6):
                e_tr(p, ptb[:, ft * 128:(ft + 1) * 128], a8[:, ft, tok], identb[:], [a8, identb], [pt])
            e_copy(p, "act", x_o[:], ptb[:, 0:512], [pt], [x_o])
            e_copy(p, "dve", b_o[:], ptb[:, 512:768], [pt], [b_o])
            p.dma(sc["X"][c], x_o[:], reads=[x_o], writes=[sc["X"]])
            p.dma(sc["Bt"][c], b_o[:], reads=[b_o], writes=[sc["Bt"]])
            p.dma(sc["BT"][c].rearrange("q (g s) -> q g s", g=2), a8[:, 4:6, tok], reads=[a8], writes=[sc["BT"]])
            p.dma(sc["CT"][c].rearrange("q (g s) -> q g s", g=2), a8[:, 6:8, tok], reads=[a8], writes=[sc["CT"]])
    p.release(m)


def l1_ssd(p, io, sc):
    m = p.mark()
    triU = p.alloc([128, 128]); triL = p.alloc([128, 128]); ones = p.alloc([128, 128])
    p.dma(triU[:], io["triU"][:], writes=[triU]); p.dma(triL[:], io["triL"][:], writes=[triL])
    p.dma(ones[:], io["ones"][:], writes=[ones])
    Aneg = p.alloc([128, 16]); load_bcast(p, Aneg[:], io["alog"][0:1, :], [], [Aneg])
    e_act(p, Aneg[:], Aneg[:], AF.Exp, [Aneg], [Aneg])
    e_ts(p, "dve", Aneg[:], Aneg[:], -1.0, 0.0, ALU.mult, ALU.add, [Aneg], [Aneg])
    dsk = p.alloc([128, 8]); load_bcast(p, dsk[:], io["dsk"][0:1, :], [], [dsk])
    sng = p.alloc([128, 512]); load_bcast(p, sng[:], io["sng"][0:1, :], [], [sng])
    Wo = p.alloc([128, 4, D], BF16); load_w_bf16(p, Wo[:], io["outw"][:].rearrange("(k q) n -> q k n", q=128), [Wo])
    identb = p.identb
    nb = 2

    def mk():
        T = {}
        T["S"] = p.alloc([128, 512]); T["Sb"] = p.alloc([128, 512], BF16)
        T["X"] = [p.alloc([128, 512], BF16) for _ in range(nb)]; T["Bt"] = [p.alloc([128, 256], BF16) for _ in range(nb)]
        T["BT"] = [p.alloc([128, 2, 128], BF16) for _ in range(nb)]; T["CT"] = [p.alloc([128, 2, 128], BF16) for _ in range(nb)]
        T["dt"] = [p.alloc([128, 16]) for _ in range(nb)]
        T["da"] = p.alloc([128, 8]); T["dabc"] = p.alloc([128, 8, 128]); T["acs"] = p.alloc([128, 8]); T["ea"] = p.alloc([128, 8])
        T["wdec"] = p.alloc([128, 8]); T["dch"] = p.alloc([128, 8]); T["BCm"] = p.alloc([128, 2, 128], BF16)
        T["tmpH"] = [p.alloc([128, 4, 128]) for _ in range(2)]; T["exH"] = [p.alloc([128, 4, 128], BF16) for _ in range(2)]
        T["MH"] = [p.alloc([128, 4, 128], BF16) for _ in range(2)]
        T["xdt"] = p.alloc([128, 512], BF16); T["xw"] = p.alloc([128, 512], BF16); T["yt"] = [p.alloc([128, 512]) for _ in range(2)]
        return T

    TT = [mk(), mk()]
    orders = [list(range(NCH)), [1, 0] + list(range(NCH - 1, 1, -1))]
    ydst = [sc["yf"], sc["yb"]]

    def chunk(d, pos, c):
        T = TT[d]
        b_ = pos % nb
        islat = c >= 2
        tri = triU if d == 0 else triL
        S = T["S"]; Sb = T["Sb"]
        X = T["X"][b_]; Bt = T["Bt"][b_]; BT = T["BT"][b_]; CT = T["CT"][b_]; dt = T["dt"][b_]
        da = T["da"]; dabc = T["dabc"]; acs = T["acs"]; ea = T["ea"]; wdec = T["wdec"]; dch = T["dch"]; BCm = T["BCm"]
        xdt = T["xdt"]; xw = T["xw"]; yt = T["yt"][b_]
        p.dma(X[:], sc["X"][c], reads=[sc["X"]], writes=[X])
        p.dma(Bt[:], sc["Bt"][c], reads=[sc["Bt"]], writes=[Bt])
        p.dma(BT[:], sc["BT"][c].rearrange("q (g s) -> q g s", g=2), reads=[sc["BT"]], writes=[BT])
        p.dma(CT[:], sc["CT"][c].rearrange("q (g s) -> q g s", g=2), reads=[sc["CT"]], writes=[CT])
        p.dma(dt[:], sc["dts"][c], reads=[sc["dts"]], writes=[dt])
        dtd = dt[:, d * 8:(d + 1) * 8]
        if pos == 0:
            p.op("pool", lambda h: h.memset(S[:], 0.0), writes=[S])
            p.op("pool", lambda h: h.memset(Sb[:], 0.0), writes=[Sb])
        e_tt(p, "dve", da[:], dtd, Aneg[:, d * 8:(d + 1) * 8], ALU.mult, [dt, Aneg], [da])
        e_copy(p, "dve", dabc[:], da[:].unsqueeze(2).to_broadcast([128, 8, 128]), [da], [dabc])
        p0 = p.pb[0] if d == 0 else p.pb[7]
        e_mm(p, p0[:, 0:8], tri[:], da[:], True, True, [tri, da], [p0])
        e_mm(p, p0[:, 8:16], ones[:], da[:], True, True, [ones, da], [p0])
        e_copy(p, "dve", acs[:], p0[:, 0:8], [p0], [acs])
        e_act(p, ea[:], p0[:, 0:8], AF.Exp, [p0], [ea])
        e_tt(p, "dve", wdec[:], p0[:, 8:16], acs[:], ALU.subtract, [p0, acs], [wdec])
        e_act(p, wdec[:], wdec[:], AF.Exp, [wdec], [wdec])
        e_act(p, dch[:], p0[:, 8:16], AF.Exp, [p0], [dch])
        p1 = p.pb[1]
        for g in range(2):
            e_mm(p, p1[:, g * 128:(g + 1) * 128], BT[:, g, :], CT[:, g, :], True, True, [BT, CT], [p1])
        e_tt(p, "dve", BCm[:], p1[:, 0:256].rearrange("q (g s) -> q g s", g=2),
             tri[:].unsqueeze(1).to_broadcast([128, 2, 128]), ALU.mult, [p1, tri], [BCm])
        e_tt(p, "dve", xdt[:].rearrange("q (h e) -> q h e", h=8), X[:].rearrange("q (h e) -> q h e", h=8),
             dtd.unsqueeze(2).to_broadcast([128, 8, 64]), ALU.mult, [X, dt], [xdt])
        e_tt(p, "dve", xw[:].rearrange("q (h e) -> q h e", h=8), xdt[:].rearrange("q (h e) -> q h e", h=8),
             wdec[:].unsqueeze(2).to_broadcast([128, 8, 64]), ALU.mult, [xdt, wdec], [xw])
        if islat:
            pyd = p.pb[4]
            for hh in range(8):
                e_mm(p, p.pb[2 + hh // 4][:, (hh % 4) * 128:(hh % 4 + 1) * 128], dabc[:, hh, :], tri[:], True, True,
                     [dabc, tri], [p.pb[2 + hh // 4]])
            for half in range(2):
                pr = p.pb[2 + half]; t_ = T["tmpH"][half]; e_ = T["exH"][half]; M = T["MH"][half]
                e_tt(p, "dve", t_[:], pr[:].rearrange("q (h s) -> q h s", h=4),
                     acs[:, 4 * half:4 * half + 4].unsqueeze(2).to_broadcast([128, 4, 128]), ALU.subtract, [pr, acs], [t_])
                e_ts(p, "dve", t_[:], t_[:], 0.0, 0.0, ALU.min, ALU.add, [t_], [t_])
                e_act(p, e_[:], t_[:], AF.Exp, [t_], [e_])
                e_tt(p, "dve", M[:], e_[:], BCm[:, half, :].unsqueeze(1).to_broadcast([128, 4, 128]), ALU.mult, [e_, BCm], [M])
                for h4 in range(4):
                    hh = 4 * half + h4
                    e_mm(p, pyd[:, hh * 64:(hh + 1) * 64], M[:, h4, :], xdt[:, hh * 64:(hh + 1) * 64], True, True, [M, xdt], [pyd])
            pyo = p.pb[5]
            for g in range(2):
                e_mm(p, pyo[:, g * 256:(g + 1) * 256], CT[:, g, :], Sb[:, g * 256:(g + 1) * 256], True, True, [CT, Sb], [pyo])
            e_tt(p, "dve", yt[:].rearrange("q (h e) -> q h e", h=8), pyo[:].rearrange("q (h e) -> q h e", h=8),
                 ea[:].unsqueeze(2).to_broadcast([128, 8, 64]), ALU.mult, [pyo, ea], [yt])
            e_tt(p, "dve", yt[:], yt[:], pyd[:], ALU.add, [yt, pyd], [yt])
            p.dma(ydst[d][c], yt[:], reads=[yt], writes=[ydst[d]])
        pst = p.pb[6]
        for g in range(2):
            e_mm(p, pst[:, g * 256:(g + 1) * 256], Bt[:, g * 128:(g + 1) * 128], xw[:, g * 256:(g + 1) * 256],
                 True, True, [Bt, xw], [pst])
        e_tt(p, "dve", S[:].rearrange("q (h e) -> q h e", h=8), S[:].rearrange("q (h e) -> q h e", h=8),
             dch[:].unsqueeze(2).to_broadcast([128, 8, 64]), ALU.mult, [S, dch], [S])
        e_tt(p, "dve", S[:], S[:], pst[:], ALU.add, [S, pst], [S])
        e_copy(p, "act", Sb[:], S[:], [S], [Sb])

    for pos in range(NCH):
        chunk(0, pos, orders[0][pos])
        chunk(1, pos, orders[1][pos])

    Xc = [p.alloc([128, 512], BF16) for _ in range(nb)]; Zc = [p.alloc([128, 512], BF16) for _ in range(nb)]
    yfc = [p.alloc([128, 512]) for _ in range(nb)]; ybc = [p.alloc([128, 512]) for _ in range(nb)]
    v = [p.alloc([128, 512]) for _ in range(nb)]; vb = p.alloc([128, 512], BF16); vT = p.alloc([128, 4, 128], BF16)
    junk = p.alloc([128, 512]); orow = [p.alloc([128, RSW]) for _ in range(2)]
    for c in range(2, NCH):
        b_ = c % nb
        X = Xc[b_]; Z = Zc[b_]; yf = yfc[b_]; yb = ybc[b_]; vv = v[b_]; o = orow[b_]
        p.dma(X[:], sc["X"][c], reads=[sc["X"]], writes=[X])
        p.dma(Z[:], sc["Z"][c], reads=[sc["Z"]], writes=[Z])
        p.dma(yf[:], sc["yf"][c], reads=[sc["yf"]], writes=[yf])
        p.dma(yb[:], sc["yb"][c], reads=[sc["yb"]], writes=[yb])
        e_tt(p, "dve", vv[:].rearrange("q (h e) -> q h e", h=8), X[:].rearrange("q (h e) -> q h e", h=8),
             dsk[:].unsqueeze(2).to_broadcast([128, 8, 64]), ALU.mult, [X, dsk], [vv])
        e_tt(p, "dve", yf[:], yf[:], yb[:], ALU.add, [yf, yb], [yf])
        e_tt(p, "dve", vv[:], vv[:], yf[:], ALU.add, [vv, yf], [vv])
        e_tt(p, "dve", vv[:], vv[:], Z[:], ALU.mult, [vv, Z], [vv])
        e_act(p, junk[:], vv[:], AF.Square, [vv], [junk, o], accum_out=o[:, 1024:1025])
        e_tt(p, "dve", vb[:], vv[:], sng[:], ALU.mult, [vv, sng], [vb])
        pt = p.pb[c % 2]; ptb = pt.t.bitcast(BF16)
        for k_ in range(4):
            e_tr(p, ptb[:, k_ * 128:(k_ + 1) * 128], vb[:, k_ * 128:(k_ + 1) * 128], identb[:], [vb, identb], [pt])
        e_copy(p, "act", vT[:], ptb[:, 0:512].rearrange("q (k s) -> q k s", k=4), [pt], [vT])
        for dh in range(2):
            pp = p.pb[2 + (2 * c + dh) % 4]
            for k_ in range(4):
                e_mm(p, pp[:], vT[:, k_, :], Wo[:, k_, dh * 512:(dh + 1) * 512], k_ == 0, k_ == 3, [vT, Wo], [pp])
            e_copy(p, "act", o[:, dh * 512:(dh + 1) * 512], pp[:], [pp], [o])
        p.op("pool", lambda h, o=o: h.memset(o[:, 1025:RSW], 0.0), writes=[o])
        w = c - 2
        p.dma(sc["rsrc"][w * 128:(w + 1) * 128, :], o[:], reads=[o], writes=[sc["rsrc"]])
    p.release(m)


def l1_tail(p, io, sc, xg, onehot, ident, out_dram):
    modsc = sc["modsc1"]
    gates = p.alloc([128, 16, 16]); h2T = p.alloc([128, 8, NL], BF16)
    m = p.mark()
    n2 = p.alloc([128, D]); load_bcast(p, n2[:], io["n2g1"][0:1, :], [], [n2])
    g1 = p.alloc([128, D]); a2 = p.alloc([128, D]); s2 = p.alloc([128, D])
    load_bcast(p, g1[:], modsc[0:1, 2 * D:3 * D], [modsc], [g1])
    load_bcast(p, s2[:], modsc[0:1, 3 * D:4 * D], [modsc], [s2])
    load_bcast(p, a2[:], modsc[0:1, 4 * D:5 * D], [modsc], [a2])
    e_stt(p, a2[:], a2[:], 1.0, n2[:], ALU.add, ALU.mult, [a2, n2], [a2])
    rw = p.alloc([128, 8, 16]); p.dma(rw[:], io["rw"][:].rearrange("(k q) n -> q k n", q=128), writes=[rw])
    rb = p.alloc([128, 16]); load_bcast(p, rb[:], io["rb"][0:1, :], [], [rb])
    rt = [p.alloc([128, RSW]) for _ in range(2)]
    xq = [p.alloc([128, D]) for _ in range(2)]; xa = p.alloc([128, D]); x1 = [p.alloc([128, D]) for _ in range(2)]
    hn2 = p.alloc([128, D]); junk = p.alloc([128, D]); hT32 = p.alloc([128, 8, 128]); sm = p.alloc([128, 128])
    ss = p.alloc([128, 1]); rs = p.alloc([128, 1]); rsd = p.alloc([128, 1])
    xgv = xg[:].rearrange("(r w) d -> w r d", w=64)
    iq = 0
    for t in range(16):
        r_ = rt[t % 2]; xo = x1[t % 2]
        p.dma(r_[:], sc["rdst"][t * 128:(t + 1) * 128, :], reads=[sc["rdst"]], writes=[r_])
        for j in range(4):
            xj = xq[iq % 2]; iq += 1
            p.dma(xj[:], xgv[16 * j + t], reads=[xg], writes=[xj])
            if j == 0:
                e_ts(p, "dve", xa[:], xj[:], onehot[:, 0:1], 0.0, ALU.mult, ALU.add, [xj, onehot], [xa])
            else:
                e_stt(p, xa[:], xj[:], onehot[:, j:j + 1], xa[:], ALU.mult, ALU.add, [xj, onehot, xa], [xa])
        e_ts(p, "dve", rsd[:], r_[:, 1024:1025], 1.0 / 2048, 1e-6, ALU.mult, ALU.add, [r_], [rsd])
        e_act(p, rsd[:], rsd[:], AF.Sqrt, [rsd], [rsd])
        p.op("dve", lambda h: h.reciprocal(out=rsd[:], in_=rsd[:]), reads=[rsd], writes=[rsd])
        e_stt(p, xo[:], r_[:, 0:D], rsd[:], g1[:], ALU.mult, ALU.mult, [r_, rsd, g1], [xo])
        e_tt(p, "pool", xo[:], xo[:], xa[:], ALU.add, [xo, xa], [xo])
        tok = slice(t * 128, (t + 1) * 128)
        p.dma(sc["x1b"][tok, :], xo[:], reads=[xo], writes=[sc["x1b"]])
        e_act(p, junk[:], xo[:], AF.Square, [xo], [junk, ss], accum_out=ss[:])
        e_ts(p, "dve", rs[:], ss[:], 1.0 / D, 1e-6, ALU.mult, ALU.add, [ss], [rs])
        e_act(p, rs[:], rs[:], AF.Sqrt, [rs], [rs])
        p.op("dve", lambda h: h.reciprocal(out=rs[:], in_=rs[:]), reads=[rs], writes=[rs])
        e_stt(p, hn2[:], xo[:], rs[:], a2[:], ALU.mult, ALU.mult, [xo, rs, a2], [hn2])
        e_tt(p, "pool", hn2[:], hn2[:], s2[:], ALU.add, [hn2, s2], [hn2])
        for half in range(2):
            ps = p.pb[4 + half]
            for c4 in range(4):
                ct = half * 4 + c4
                e_tr(p, ps[:, c4 * 128:(c4 + 1) * 128], hn2[:, ct * 128:(ct + 1) * 128], ident[:], [hn2, ident], [ps])
            pv = ps[:].rearrange("q (c n) -> q c n", c=4)
            e_copy(p, "act", h2T[:, half * 4:(half + 1) * 4, tok], pv, [ps], [h2T])
            e_copy(p, "dve", hT32[:, half * 4:(half + 1) * 4, :], pv, [ps], [hT32])
        pl = p.pb[6]
        for k in range(8):
            e_mm(p, pl[:, 0:16], hT32[:, k, :], rw[:, k, :], k == 0, k == 7, [hT32, rw], [pl])
        routing(p, pl, rb, sm, gates[:, t, :], gates)
    p.release(m)
    fg = p.alloc([128, D]); load_bcast(p, fg[:], io["fing"][0:1, :], [], [fg])
    junk2 = p.alloc([128, D]); ss2 = p.alloc([128, 1]); rs2 = p.alloc([128, 1])

    def out_cb(t, x):
        e_act(p, junk2[:], x[:], AF.Square, [x], [junk2, ss2], accum_out=ss2[:])
        e_ts(p, "dve", rs2[:], ss2[:], 1.0 / D, 1e-6, ALU.mult, ALU.add, [ss2], [rs2])
        e_act(p, rs2[:], rs2[:], AF.Sqrt, [rs2], [rs2])
        p.op("dve", lambda h: h.reciprocal(out=rs2[:], in_=rs2[:]), reads=[rs2], writes=[rs2])
        e_stt(p, x[:], x[:], rs2[:], fg[:], ALU.mult, ALU.mult, [x, rs2, fg], [x])
        p.dma(out_dram[t * 128:(t + 1) * 128, :], x[:], reads=[x], writes=[out_dram])

    phase_moe(p, io, (io["w1b"], io["w3b"], io["w2b"]), modsc, h2T, gates, sc["x1b"], 16, out_cb, lambda t: 0)


def layer1(p, io, sc, xg, ctx1, ident, onehot, out_dram, dbg=None):
    p.identb = p.alloc([128, 128], BF16)
    e_copy(p, "dve", p.identb[:], ident[:], [ident], [p.identb])
    phase_mod(p, io["cT"], io["modw1"], io["modb1"], sc["modsc1"], ident)
    l1_proj(p, io, sc, xg, ctx1, ident)
    l1_conv(p, io, sc, p.identb)
    l1_ssd(p, io, sc)
    p.cc(lambda h: h.collective_compute("ReduceScatter", ALU.add, replica_groups=[[0, 1, 2, 3], [4, 5, 6, 7]],
                                        ins=[sc["rsrc"].t.opt()], outs=[sc["rdst"].t.opt()]),
         reads=[sc["rsrc"]], writes=[sc["rdst"]])
    l1_tail(p, io, sc, xg, onehot, ident, out_dram)


def host_inputs_l1(z):
    i = 1
    maps = []
    inw = z["ssd_in_w"][0]
    for k in range(8):
        bb = k // 4; j = k % 4
        cT = np.stack([z["c"][bb], z["c_ctx"]], axis=-1).reshape(8, 128, 2).transpose(1, 0, 2)
        wz = inw[:, 512 * j:512 * (j + 1)]
        wx = inw[:, 2048 + 512 * j:2048 + 512 * (j + 1)]
        wB = inw[:, 4096 + 256 * j:4096 + 256 * (j + 1)]
        wC = inw[:, 5120 + 256 * j:5120 + 256 * (j + 1)]
        wdt = np.concatenate([inw[:, 6144 + 8 * j:6144 + 8 * (j + 1)], inw[:, 6176 + 8 * j:6176 + 8 * (j + 1)]], 1)
        chs = np.concatenate([np.arange(512 * j, 512 * (j + 1)), 2048 + np.arange(256 * j, 256 * (j + 1)),
                              3072 + np.arange(256 * j, 256 * (j + 1))])
        cw = z["ssd_conv_w"][0][:, chs]
        convw = cw.T.reshape(8, 128, 5).transpose(1, 0, 2)
        convb = z["ssd_conv_b"][0][chs].reshape(8, 128).T
        hs = slice(8 * j, 8 * (j + 1))
        dtb = np.concatenate([z["ssd_dt_bias"][0, 0, hs], z["ssd_dt_bias"][0, 1, hs]])[None]
        alog = np.concatenate([z["ssd_a_log"][0, 0, hs], z["ssd_a_log"][0, 1, hs]])[None]
        oh = np.zeros((128, 4), np.float32); oh[:, j] = 1
        maps.append(dict(
            cT=np.ascontiguousarray(cT), modw1=z["mod_w"][i], modb1=z["mod_b"][i][None], n1g1=z["norm1_g"][i][None],
            n2g1=z["norm2_g"][i][None], fing=z["final_g"][None], wz=np.ascontiguousarray(wz),
            wxbc=np.ascontiguousarray(np.concatenate([wx, wB, wC], 1)), wdt=np.ascontiguousarray(wdt),
            convw=np.ascontiguousarray(convw), convb=np.ascontiguousarray(convb), dtb=np.ascontiguousarray(dtb),
            alog=np.ascontiguousarray(alog), dsk=z["ssd_d"][0][hs][None].copy(),
            sng=z["ssd_norm_g"][0][512 * j:512 * (j + 1)][None].copy(),
            outw=np.ascontiguousarray(z["ssd_out_w"][0][512 * j:512 * (j + 1), :]),
            triU=np.triu(np.ones((128, 128), np.float32)), triL=np.tril(np.ones((128, 128), np.float32)),
            ones=np.ones((128, 128), np.float32), rw=z["router_w"], rb=z["router_b"][None],
            w1b=z["moe_w1"][i], w3b=z["moe_w3"][i], w2b=z["moe_w2"][i],
            ident=np.eye(128, dtype=np.float32), onehot=oh))
    return maps


def colmajor(xb):
    return xb.reshape(128, 64, -1).transpose(1, 0, 2).reshape(8192, -1)


def build_full():
    p = Prog(); p.init_pool()
    io = {}
    for name, shape in L0_INPUTS + L1_INPUTS:
        if name not in io:
            io[name] = p.dram(name, shape)
    out = p.dram("out", [2048, D], kind="ExternalOutput")
    sc0 = dict(modsc=p.dram("modsc", [2, 6144], kind="Internal"),
               tabsc=p.dram("tabsc", [64, 2, 128, 2048], kind="Internal"),
               fsrc=p.dram("fsrc", [128, 128], kind="Internal"), fdst=p.dram("fdst", [512, 128], kind="Internal"),
               x1sc=p.dram("x1sc", [NT, D], kind="Internal"))
    xsrcs = [p.dram(f"xsrc{a}", [128, 2048], kind="Internal") for a in range(8)]
    xdsts = [p.dram(f"xdst{a}", [512, 2048], kind="Internal") for a in range(8)]
    xg = p.dram("xg", [8192, D], kind="Internal")
    ctx1 = p.dram("ctx1", [NCX, D], kind="Internal")
    sc1 = l1_scratch(p)
    ident = p.alloc([128, 128]); p.dma(ident[:], io["ident"][:], writes=[ident])
    onehot = p.alloc([128, 4]); p.dma(onehot[:], io["onehot"][:], writes=[onehot])
    m0 = p.mark()

    def out_cb(t, x):
        if t < 2:
            p.dma(ctx1[t * 128:(t + 1) * 128, :], x[:], reads=[x], writes=[ctx1])
        else:
            a = (t - 2) // 2; half = (t - 2) % 2
            dst = xsrcs[a][:].rearrange("i (b d) -> (i b) d", d=D)[half * 128:(half + 1) * 128, :]
            p.dma(dst, x[:], reads=[x], writes=[xsrcs[a]])

    layer0(p, io, sc0, ident, onehot, out_cb)
    p.release(m0)
    xgv = xg[:].rearrange("(r a t) d -> a r t d", r=4, a=8)
    for a in range(8):
        p.cc(lambda h, a=a: h.collective_compute("AllGather", ALU.bypass, replica_groups=[[0, 1, 2, 3], [4, 5, 6, 7]],
                                                 ins=[xsrcs[a].t.opt()], outs=[xdsts[a].t.opt()]),
             reads=[xsrcs[a]], writes=[xdsts[a]])
        p.dma(xgv[a], xdsts[a][:].rearrange("(r i) (b d) -> r (i b) d", r=4, d=D), reads=[xdsts[a]], writes=[xg])
    layer1(p, io, sc1, xg, ctx1, ident, onehot, out)
    p.wait_all()
    return p


def kernel(**inputs):
    z = {k: np.asarray(v) for k, v in inputs.items()}
    m0 = host_inputs_l0(z)
    m1 = host_inputs_l1(z)
    maps = []
    for k in range(8):
        d = dict(m0[k]); d.update(m1[k])
        maps.append({kk: np.ascontiguousarray(vv, dtype=np.float32) for kk, vv in d.items()})
    p = build_full()
    nc = p.finish()
    res = run_bass_kernel_spmd(nc, maps, core_ids=list(range(8))).results
    outp = np.zeros((2, 8192, D), np.float32)
    for bb in range(2):
        cm = np.concatenate([res[4 * bb + j]["out"] for j in range(4)], 0)
        outp[bb] = cm.reshape(64, 128, D).transpose(1, 0, 2).reshape(8192, D)
    return outp
```

```python
import math, time, sys
import numpy as np
import contextlib
import concourse.bass as bass
import concourse.mybir as mybir
from concourse.bass_utils import run_bass_kernel_spmd

F32 = mybir.dt.float32
BF16 = mybir.dt.bfloat16
AF = mybir.ActivationFunctionType
ALU = mybir.AluOpType
AX = mybir.AxisListType

ENGS = ("pe", "dve", "act", "pool", "sp")


class Buf:
    def __init__(self, t, name, excl=False):
        self.t = t
        self.name = name
        self.excl = excl
        self.w = None
        self.r = []

    def __getitem__(self, k):
        return self.t[k]


class Prog:
    def __init__(self, name="k"):
        self.nc = bass.Bass("TRN2", target_bir_lowering=False)
        self.es = contextlib.ExitStack()
        self.q = {e: [] for e in ENGS}
        self.cnt = {e: 0 for e in ENGS}
        self.known = {e: {} for e in ENGS}
        self.sems = {}
        self.dcnt = {}
        self.mult = {}
        self.nbuf = 0
        self.sb_bytes = 0
        for e in ENGS:
            self.sems[e] = self.es.enter_context(self.nc.semaphore("s_" + e))
        self.NDS = 48
        self.rr = 0
        for i in range(self.NDS):
            nm = f"dq{i}"
            self.sems[nm] = self.es.enter_context(self.nc.semaphore(nm))
            self.dcnt[nm] = 0
            self.mult[nm] = 16

    def dram(self, name, shape, dtype=F32, kind="ExternalInput"):
        t = self.nc.dram_tensor(name, list(shape), dtype, kind=kind)
        b = Buf(t.ap(), name)
        b.is_dram = True
        return b

    def sb(self, shape, dtype=F32, name=None):
        self.nbuf += 1
        name = name or f"sb{self.nbuf}"
        t = self.es.enter_context(self.nc.sbuf_tensor(name, list(shape), dtype))
        sz = int(np.prod(shape[1:])) * (4 if dtype == F32 else 2)
        self.sb_bytes += sz
        return Buf(t, name)

    def init_pool(self, words=48 * 1024):
        self.big = self.es.enter_context(self.nc.sbuf_tensor("big", [128, words], F32))
        self.words = words
        self.top = 0
        self.hi = words
        self.peak = 0
        self.banks = [self.es.enter_context(self.nc.psum_tensor(f"bank{i}", [128, 512], F32)) for i in range(8)]
        self.pb = [Buf(self.banks[i][:], f"bank{i}", excl=True) for i in range(8)]

    def alloc(self, shape, dtype=F32, name=None, hi=False):
        n = int(np.prod(shape[1:]))
        w = n if dtype == F32 else (n + 1) // 2
        assert self.top + w <= self.hi, f"SBUF overflow {self.top}+{w} > {self.hi} ({name})"
        if hi:
            self.hi -= w
            ap = self.big[:, self.hi:self.hi + w]
        else:
            ap = self.big[:, self.top:self.top + w]
            self.top += w
        self.peak = max(self.peak, self.top + (self.words - self.hi))
        if dtype != F32:
            ap = ap.bitcast(dtype)[:, 0:n]
        if len(shape) > 2:
            names = " ".join(f"d{i}" for i in range(1, len(shape)))
            kw = {f"d{i}": shape[i] for i in range(1, len(shape))}
            ap = ap.rearrange(f"p ({names}) -> p {names}", **kw)
        if shape[0] < 128:
            ap = ap[0:shape[0]]
        self.nbuf += 1
        return Buf(ap, name or f"a{self.nbuf}")


    def barrier(self):
        snap_c = dict(self.cnt)
        snap_d = dict(self.dcnt)
        for e in ENGS:
            for src, n in snap_c.items():
                if src != e and n > self.known[e].get(src, 0):
                    self.q[e].append(("wait", src, n))
                    self.known[e][src] = n
            for s_, n in snap_d.items():
                if n > self.known[e].get(s_, 0):
                    self.q[e].append(("wait", s_, n * self.mult[s_]))
                    self.known[e][s_] = n

    def mark(self):
        return self.top

    def release(self, m):
        self.barrier()
        self.top = m

    def ps(self, shape, dtype=F32, name=None):
        self.nbuf += 1
        name = name or f"ps{self.nbuf}"
        t = self.es.enter_context(self.nc.psum_tensor(name, list(shape), dtype))
        return Buf(t, name)

    def stream(self, s):
        if s not in self.sems:
            self.sems[s] = self.es.enter_context(self.nc.semaphore("d_" + s))
            self.dcnt[s] = 0
            self.mult[s] = 16
        return s

    def _deps(self, eng, reads, writes):
        deps = {}

        def add(x):
            if x is None:
                return
            src, n = x
            if deps.get(src, 0) < n:
                deps[src] = n

        for b in reads:
            add(b.w)
            if b.excl:
                for x in b.r:
                    if x[0] != eng:
                        add(x)
        for b in writes:
            add(b.w)
            for x in b.r:
                add(x)
        for src, n in deps.items():
            if src == eng and eng == "pe":
                continue
            if self.known[eng].get(src, 0) >= n:
                continue
            self.known[eng][src] = n
            mult = self.mult[src] if src in self.dcnt else 1
            self.q[eng].append(("wait", src, n * mult))

    def _mark(self, tag, reads, writes):
        for b in reads:
            b.r.append(tag)
        for b in writes:
            b.w = tag
            b.r = []

    def op(self, eng, fn, reads=(), writes=()):
        self._deps(eng, reads, writes)
        self.cnt[eng] += 1
        self.q[eng].append(("op", fn))
        self._mark((eng, self.cnt[eng]), reads, writes)

    def dma(self, out, in_, reads=(), writes=(), eng="sp", stream=None, **kw):
        s_ = f"dq{self.rr % self.NDS}"
        self.rr += 1
        if eng == "sp" and any(not getattr(b, "is_dram", False) for b in reads):
            eng = "pool"
        self._deps(eng, reads, writes)
        prev = self.dcnt[s_]
        if prev and self.known[eng].get(s_, 0) < prev:
            self.q[eng].append(("wait", s_, prev * 16))
            self.known[eng][s_] = prev
        self.dcnt[s_] += 1
        self.q[eng].append(("dma", s_, out, in_, kw))
        self._mark((s_, self.dcnt[s_]), reads, writes)

    def cc(self, fn, reads=(), writes=(), eng="pool", stream="cc"):
        self.stream(stream)
        self.mult[stream] = 1
        self._deps(eng, reads, writes)
        prev = self.dcnt[stream]
        if prev and self.known[eng].get(stream, 0) < prev:
            self.q[eng].append(("wait", stream, prev))
            self.known[eng][stream] = prev
        self.dcnt[stream] += 1
        self.q[eng].append(("cc", stream, fn))
        self._mark((stream, self.dcnt[stream]), reads, writes)

    def wait_all(self, eng="sp"):
        for s, n in self.dcnt.items():
            if n:
                self.q[eng].append(("wait", s, self.mult[s] * n))
        for e in ENGS:
            if e != eng and self.cnt[e]:
                self.q[eng].append(("wait", e, self.cnt[e]))

    def finish(self):
        nc = self.nc
        sems = self.sems

        def replay(e):
            def body(h):
                for item in self.q[e]:
                    if item[0] == "wait":
                        h.wait_ge(sems[item[1]], item[2])
                    elif item[0] == "op":
                        item[1](h).then_inc(sems[e], 1)
                    elif item[0] == "cc":
                        item[2](h).then_inc(sems[item[1]], 1)
                    else:
                        _, s, out, in_, kw = item
                        h.dma_start(out=out, in_=in_, **kw).then_inc(sems[s], 16)
            return body

        with nc.Block() as block:
            block.tensor(replay("pe"))
            block.vector(replay("dve"))
            block.scalar(replay("act"))
            block.gpsimd(replay("pool"))
            block.sync(replay("sp"))
        self.es.close()
        return nc


def run(prog, in_maps):
    nc = prog.finish()
    res = run_bass_kernel_spmd(nc, in_maps, core_ids=list(range(len(in_maps))))
    return res.results


def _bufs(xs):
    return [x for x in xs if isinstance(x, Buf)]


def e_act(p, out, in_, func, r, w, eng="act", **kw):
    p.op(eng, lambda h: h.activation(out=out, in_=in_, func=func, **kw), reads=r, writes=w)


def e_tt(p, eng, out, in0, in1, op, r, w):
    p.op(eng, lambda h: h.tensor_tensor(out=out, in0=in0, in1=in1, op=op), reads=r, writes=w)


def e_ts(p, eng, out, in0, s1, s2, op0, op1, r, w):
    if op1 is None:
        if op0 == ALU.add:
            op1, s2 = ALU.mult, 1.0
        else:
            op1, s2 = ALU.add, 0.0
    if True:
        p.op(eng, lambda h: h.tensor_scalar(out=out, in0=in0, scalar1=s1, scalar2=s2, op0=op0, op1=op1), reads=r, writes=w)


def e_stt(p, out, in0, scalar, in1, op0, op1, r, w):
    p.op("dve", lambda h: h.scalar_tensor_tensor(out=out, in0=in0, scalar=scalar, in1=in1, op0=op0, op1=op1),
         reads=r, writes=w)


def e_copy(p, eng, out, in_, r, w):
    if eng == "act":
        p.op(eng, lambda h: h.activation(out=out, in_=in_, func=AF.Copy), reads=r, writes=w)
    else:
        p.op(eng, lambda h: h.tensor_copy(out=out, in_=in_), reads=r, writes=w)


def e_mm(p, out, lhsT, rhs, start, stop, r, w):
    p.op("pe", lambda h: h.matmul(out, lhsT=lhsT, rhs=rhs, start=start, stop=stop), reads=r, writes=w)


def e_tr(p, out, in_, ident, r, w):
    p.op("pe", lambda h: h.transpose(out, in_, ident), reads=r, writes=w)


def rev_ap(ap):
    (pst, pn), (st, n) = ap.ap
    return bass.AP(ap.tensor, ap.offset + (n - 1) * st, [[pst, pn], [-st, n]])


D = 1024
NL = 2048
NCX = 256
NT = NL + NCX
PI = math.pi


def phase_mod(p, cT, modw, modb, modsc, ident):
    m = p.mark()
    c_sb = p.alloc([128, 8, 2]); s_sb = p.alloc([128, 8, 2])
    p.dma(c_sb[:], cT[:], writes=[c_sb])
    e_act(p, s_sb[:], c_sb[:], AF.Silu, [c_sb], [s_sb])
    b_sb = p.alloc([1, 6144]); ones = p.alloc([1, 2])
    p.dma(b_sb[:], modb[:], writes=[b_sb])
    p.op("pool", lambda h: h.memset(ones[:], 1.0), writes=[ones])
    wb = [p.alloc([128, 8, 512]) for _ in range(2)]
    ob = [p.alloc([2, 512]) for _ in range(2)]
    for nb in range(12):
        w = wb[nb % 2]; o = ob[nb % 2]; ps = p.pb[nb % 2]
        p.dma(w[:], modw[:, nb * 512:(nb + 1) * 512].rearrange("(k q) n -> q k n", q=128), writes=[w])
        for k in range(8):
            e_mm(p, ps[0:2, :], s_sb[:, k, :], w[:, k, :], k == 0, False, [s_sb, w], [ps])
        e_mm(p, ps[0:2, :], ones[:], b_sb[:, nb * 512:(nb + 1) * 512], False, True, [ones, b_sb], [ps])
        e_copy(p, "act", o[:], ps[0:2, :], [ps], [o])
        p.dma(modsc[:, nb * 512:(nb + 1) * 512], o[:], reads=[o], writes=[modsc], stream="st")
    p.release(m)


def load_bcast(p, dst, src_row_ap, r, w):
    p.dma(dst, src_row_ap.to_broadcast([128, src_row_ap.shape[-1]]), reads=r, writes=w)


def phase_hn(p, xl, xc, modsc, gvec, ident, uT, part_sh, part_sc):
    m = p.mark()
    g_sb = p.alloc([128, D]); A = [p.alloc([128, D]) for _ in range(2)]; SH = [p.alloc([128, D]) for _ in range(2)]
    load_bcast(p, g_sb[:], gvec[0:1, :], [], [g_sb])
    for which in range(2):
        load_bcast(p, A[which][:], modsc[which:which + 1, part_sc * D:(part_sc + 1) * D], [modsc], [A[which]])
        load_bcast(p, SH[which][:], modsc[which:which + 1, part_sh * D:(part_sh + 1) * D], [modsc], [SH[which]])
        e_stt(p, A[which][:], A[which][:], 1.0, g_sb[:], ALU.add, ALU.mult, [A[which], g_sb], [A[which]])
    nbuf = 2
    xs = [p.alloc([128, D]) for _ in range(nbuf)]; ys = [p.alloc([128, D]) for _ in range(nbuf)]
    junk = p.alloc([128, D]); ss = [p.alloc([128, 1]) for _ in range(nbuf)]; rs = [p.alloc([128, 1]) for _ in range(nbuf)]
    for t in range(18):
        which = 1 if t < 2 else 0
        src = xc[t * 128:(t + 1) * 128, :] if t < 2 else xl[(t - 2) * 128:(t - 1) * 128, :]
        x = xs[t % nbuf]; y = ys[t % nbuf]; s = ss[t % nbuf]; r = rs[t % nbuf]
        p.dma(x[:], src, writes=[x])
        e_act(p, junk[:], x[:], AF.Square, [x], [junk, s], accum_out=s[:])
        e_ts(p, "dve", r[:], s[:], 1.0 / D, 1e-6, ALU.mult, ALU.add, [s], [r])
        e_act(p, r[:], r[:], AF.Sqrt, [r], [r])
        p.op("dve", lambda h, r=r: h.reciprocal(out=r[:], in_=r[:]), reads=[r], writes=[r])
        e_stt(p, y[:], x[:], r[:], A[which][:], ALU.mult, ALU.mult, [x, r, A[which]], [y])
        e_tt(p, "dve", y[:], y[:], SH[which][:], ALU.add, [y, SH[which]], [y])
        for half in range(2):
            ps = p.pb[(2 * t + half) % 4]
            for c4 in range(4):
                ct = half * 4 + c4
                e_tr(p, ps[:, c4 * 128:(c4 + 1) * 128], y[:, ct * 128:(ct + 1) * 128], ident[:], [y, ident], [ps])
            e_copy(p, "act", uT[:, half * 4:(half + 1) * 4, t * 128:(t + 1) * 128],
                   ps[:].rearrange("q (c n) -> q c n", c=4), [ps], [uT])
    p.release(m)


def cmul(p, eng, outr, outi, ar, ai, br, bi, t1, t2, bufs_r, bufs_w):
    e_tt(p, eng, t1, ar, br, ALU.mult, bufs_r, bufs_w)
    e_tt(p, eng, t2, ai, bi, ALU.mult, bufs_r, bufs_w)
    e_tt(p, eng, outr, t1, t2, ALU.subtract, bufs_r, bufs_w)
    e_tt(p, eng, t1, ar, bi, ALU.mult, bufs_r, bufs_w)
    e_tt(p, eng, t2, ai, br, ALU.mult, bufs_r, bufs_w)
    e_tt(p, eng, outi, t1, t2, ALU.add, bufs_r, bufs_w)


class S5:
    pass


def s5_params(p, io, ident):
    S = S5()
    lam = p.alloc([128, 2, 64]); ldt = p.alloc([128, 64])
    p.dma(lam[:], io["lamP"][:], writes=[lam]); p.dma(ldt[:], io["ldtP"][:], writes=[ldt])
    lr = lam[:, 0, :]; li = lam[:, 1, :]
    W = p.alloc([128, 10, 64], name="s5w")
    sl = lambda i: W[:, i, :]
    R = [W]
    step, th, mag, t1, t2, cs, sn, den, am1 = (sl(i) for i in range(9))
    S.ar = p.alloc([128, 64]); S.ai = p.alloc([128, 64]); S.cth = p.alloc([128, 64]); S.sth = p.alloc([128, 64])
    S.mag = p.alloc([128, 64]); S.qr = p.alloc([128, 64]); S.qi = p.alloc([128, 64])
    e_act(p, step, ldt[:], AF.Exp, [ldt], R)
    e_tt(p, "dve", th, li, step, ALU.mult, [lam, W], R)
    e_tt(p, "dve", t1, lr, step, ALU.mult, [lam, W], R)
    e_act(p, S.mag[:], t1, AF.Exp, R, [S.mag])
    e_act(p, sn, th, AF.Sin, R, R, scale=1.0 / 16)
    e_ts(p, "dve", t1, th, 1.0 / 16, PI / 2, ALU.mult, ALU.add, R, R)
    e_act(p, cs, t1, AF.Sin, R, R)
    for _ in range(4):
        e_tt(p, "dve", t1, cs, cs, ALU.mult, R, R)
        e_tt(p, "dve", t2, sn, sn, ALU.mult, R, R)
        e_tt(p, "dve", sn, sn, cs, ALU.mult, R, R)
        e_ts(p, "dve", sn, sn, 2.0, None, ALU.mult, None, R, R)
        e_tt(p, "dve", cs, t1, t2, ALU.subtract, R, R)
    e_copy(p, "dve", S.cth[:], cs, R, [S.cth]); e_copy(p, "dve", S.sth[:], sn, R, [S.sth])
    e_tt(p, "dve", S.ar[:], S.mag[:], cs, ALU.mult, R + [S.mag], [S.ar])
    e_tt(p, "dve", S.ai[:], S.mag[:], sn, ALU.mult, R + [S.mag], [S.ai])
    e_tt(p, "dve", t1, lr, lr, ALU.mult, [lam], R)
    e_tt(p, "dve", t2, li, li, ALU.mult, [lam], R)
    e_tt(p, "dve", den, t1, t2, ALU.add, R, R)
    p.op("dve", lambda h: h.reciprocal(out=den, in_=den), reads=R, writes=R)
    e_ts(p, "dve", am1, S.ar[:], -1.0, None, ALU.add, None, [S.ar], R)
    e_tt(p, "dve", t1, am1, lr, ALU.mult, R + [lam], R)
    e_tt(p, "dve", t2, S.ai[:], li, ALU.mult, [S.ai, lam], R)
    e_tt(p, "dve", t1, t1, t2, ALU.add, R, R)
    e_tt(p, "dve", S.qr[:], t1, den, ALU.mult, R, [S.qr])
    e_tt(p, "dve", t1, S.ai[:], lr, ALU.mult, [S.ai, lam], R)
    e_tt(p, "dve", t2, am1, li, ALU.mult, R + [lam], R)
    e_tt(p, "dve", t1, t1, t2, ALU.subtract, R, R)
    e_tt(p, "dve", S.qi[:], t1, den, ALU.mult, R, [S.qi])
    S.wc = p.alloc([128, 11, 64]); S.ws = p.alloc([128, 11, 64])
    e_copy(p, "dve", S.wc[:, 0, :], S.cth[:], [S.cth], [S.wc]); e_copy(p, "dve", S.ws[:, 0, :], S.sth[:], [S.sth], [S.ws])
    for k in range(10):
        e_tt(p, "dve", t1, S.wc[:, k, :], S.wc[:, k, :], ALU.mult, [S.wc], R)
        e_tt(p, "dve", t2, S.ws[:, k, :], S.ws[:, k, :], ALU.mult, [S.ws], R)
        e_tt(p, "dve", S.wc[:, k + 1, :], t1, t2, ALU.subtract, R, [S.wc])
        e_tt(p, "dve", t1, S.wc[:, k, :], S.ws[:, k, :], ALU.mult, [S.wc, S.ws], R)
        e_ts(p, "dve", S.ws[:, k + 1, :], t1, 2.0, None, ALU.mult, None, R, [S.ws])
    S.nws = p.alloc([128, 11, 64])
    e_ts(p, "dve", S.nws[:], S.ws[:], -1.0, None, ALU.mult, None, [S.ws], [S.nws])
    S.Ar = p.alloc([128, 64]); S.Ai = p.alloc([128, 64])
    e_copy(p, "dve", S.Ar[:], S.ar[:], [S.ar], [S.Ar]); e_copy(p, "dve", S.Ai[:], S.ai[:], [S.ai], [S.Ai])
    for k in range(11):
        e_tt(p, "dve", t1, S.Ar[:], S.Ar[:], ALU.mult, [S.Ar], R)
        e_tt(p, "dve", t2, S.Ai[:], S.Ai[:], ALU.mult, [S.Ai], R)
        e_tt(p, "dve", den, S.Ar[:], S.Ai[:], ALU.mult, [S.Ar, S.Ai], R)
        e_tt(p, "dve", S.Ar[:], t1, t2, ALU.subtract, R, [S.Ar])
        e_ts(p, "dve", S.Ai[:], den, 2.0, None, ALU.mult, None, R, [S.Ai])
    S.cP = p.alloc([128, 2, 32, 16]); p.dma(S.cP[:], io["cP"][:], writes=[S.cP])
    S.Bb = p.alloc([128, 2, 2, 32, 16])
    m = p.mark()
    bP = p.alloc([128, 2, 32, 16]); p.dma(bP[:], io["bP"][:], writes=[bP])
    Bb = S.Bb
    tA = p.alloc([128, 32, 16]); tB = p.alloc([128, 32, 16])
    for d in range(2):
        qr_b = S.qr[:, d * 32:(d + 1) * 32].unsqueeze(2).to_broadcast([128, 32, 16])
        qi_b = S.qi[:, d * 32:(d + 1) * 32].unsqueeze(2).to_broadcast([128, 32, 16])
        e_tt(p, "dve", tA[:], bP[:, 0], qr_b, ALU.mult, [bP, S.qr], [tA])
        e_tt(p, "dve", tB[:], bP[:, 1], qi_b, ALU.mult, [bP, S.qi], [tB])
        e_tt(p, "dve", Bb[:, d, 0], tA[:], tB[:], ALU.subtract, [tA, tB], [Bb])
        e_tt(p, "dve", tA[:], bP[:, 1], qr_b, ALU.mult, [bP, S.qr], [tA])
        e_tt(p, "dve", tB[:], bP[:, 0], qi_b, ALU.mult, [bP, S.qi], [tB])
        e_tt(p, "dve", Bb[:, d, 1], tA[:], tB[:], ALU.add, [tA, tB], [Bb])
    p.release(m)
    return S


def s5_build_ct(p, S, ct, ident, BbT, Cp, src):
    i = 0
    for gpl in range(4):
        gp = ct * 4 + gpl
        for d in range(2):
            for ri in range(2):
                s_ = src[i % 2]; ps = p.pb[7]
                for g2 in range(2):
                    e_copy(p, "dve", s_[g2 * 64:(g2 + 1) * 64, gpl, g2, :], S.Bb[g2 * 64:(g2 + 1) * 64, d, ri, gp, :], [S.Bb], [s_])
                e_tr(p, ps[:, (i % 4) * 128:(i % 4 + 1) * 128], s_[:].rearrange("q a b c -> q (a b c)"), ident[:], [s_, ident], [ps])
                e_copy(p, "act", BbT[:, gpl, d, ri, :], ps[:, (i % 4) * 128:(i % 4 + 1) * 128], [ps], [BbT])
                for g2 in range(2):
                    p.op("pool", lambda h, s_=s_, g2=g2, gpl=gpl: h.memset(s_[g2 * 64:(g2 + 1) * 64, gpl, g2, :], 0.0),
                         reads=[], writes=[s_])
                i += 1
    p.op("pool", lambda h: h.memset(Cp[:], 0.0), writes=[Cp])
    Cv = Cp[:].rearrange("q g r (a b k) -> q g r a b k", a=4, b=2)
    for gpl in range(4):
        gp = ct * 4 + gpl
        for g2 in range(2):
            rows = slice(g2 * 64, (g2 + 1) * 64)
            e_copy(p, "dve", Cv[rows, gpl, 0, gpl, g2, :], S.cP[rows, 0, gp, :], [S.cP], [Cp])
            e_ts(p, "dve", Cv[rows, gpl, 1, gpl, g2, :], S.cP[rows, 1, gp, :], -1.0, None, ALU.mult, None, [S.cP], [Cp])


def s5_tables(p, S, col, cosT, sinT):
    p.op("pool", lambda h: h.memset(cosT[:, 0:1], 1.0), writes=[cosT])
    p.op("pool", lambda h: h.memset(sinT[:, 0:1], 0.0), writes=[sinT])
    R = [cosT, sinT, S.wc, S.ws, S.nws]
    for k in range(11):
        n = 1 << k
        wc = S.wc[:, k, col:col + 1]; ws = S.ws[:, k, col:col + 1]; nws = S.nws[:, k, col:col + 1]
        lo = slice(0, n); hi = slice(n, 2 * n)
        e_ts(p, "dve", cosT[:, hi], cosT[:, lo], wc, None, ALU.mult, None, R, [cosT])
        e_stt(p, cosT[:, hi], sinT[:, lo], nws, cosT[:, hi], ALU.mult, ALU.add, R, [cosT])
        e_ts(p, "dve", sinT[:, hi], sinT[:, lo], wc, None, ALU.mult, None, R, [sinT])
        e_stt(p, sinT[:, hi], cosT[:, lo], ws, sinT[:, hi], ALU.mult, ALU.add, R, [sinT])


def s5_pass(p, S, uT, tabsc, full, Fsum, kin, ident, yacc_evac=None):
    m = p.mark()
    cos2 = [p.alloc([128, 2048]) for _ in range(2)]; sin2 = [p.alloc([128, 2048]) for _ in range(2)]
    TL = 2048
    CH = 512
    nb = 2
    bur = [p.alloc([128, CH]) for _ in range(nb)]; bui = [p.alloc([128, CH]) for _ in range(nb)]
    m1 = [p.alloc([128, CH]) for _ in range(nb)]; m2 = [p.alloc([128, CH]) for _ in range(nb)]
    cr = [p.alloc([128, CH]) for _ in range(nb)]; ci = [p.alloc([128, CH]) for _ in range(nb)]
    kr = [p.alloc([128, CH]) for _ in range(nb)]; ki = [p.alloc([128, CH]) for _ in range(nb)]
    hr = [p.alloc([128, CH], BF16) for _ in range(nb)]; hi = [p.alloc([128, CH], BF16) for _ in range(nb)]
    tiny = p.alloc([128, 4])
    BbT2 = [p.alloc([128, 4, 2, 2, 128], BF16) for _ in range(2)]
    Cp2 = [p.alloc([128, 4, 2, 128], BF16) for _ in range(2)]
    srcs = [p.alloc([128, 4, 2, 16]) for _ in range(2)]
    for s_ in srcs:
        p.op("pool", lambda h, s_=s_: h.memset(s_[:], 0.0), writes=[s_])
    subs = [(0, 0, NCX), (1, NCX, NL)]
    it = 0
    ci_ = 0
    for ct in range(8):
        BbT = BbT2[ct % 2]; Cp = Cp2[ct % 2]
        s5_build_ct(p, S, ct, ident, BbT, Cp, srcs)
        for gpl in range(4):
            gp = ct * 4 + gpl
            for d in range(2):
                col = d * 32 + gp
                if not full:
                    s5_tables(p, S, col, cos2[0], sin2[0])
                    if d == 0:
                        cosT = cos2[0]; sinT = sin2[0]
                    else:
                        cosT = cos2[1]; sinT = sin2[1]
                        e_copy(p, "dve", rev_ap(cosT[:]), cos2[0][:], [cos2[0]], [cosT])
                        e_copy(p, "dve", rev_ap(sinT[:]), sin2[0][:], [sin2[0]], [sinT])
                    p.dma(tabsc[col, 0], cosT[:], reads=[cosT], writes=[tabsc], stream="tb")
                    p.dma(tabsc[col, 1], sinT[:], reads=[sinT], writes=[tabsc], stream="tb")
                else:
                    cosT = cos2[it % 2]; sinT = sin2[it % 2]
                    it += 1
                    p.dma(cosT[:], tabsc[col, 0], reads=[tabsc], writes=[cosT], stream="tbl")
                    p.dma(sinT[:], tabsc[col, 1], reads=[tabsc], writes=[sinT], stream="tbl")
                mcol = S.mag[:, col:col + 1]
                for (which, c0, L) in subs:
                    nch = (L + CH - 1) // CH
                    carry_r = None
                    for cj in range(nch):
                        n = min(CH, L)
                        a = cj * n if d == 0 else L - (cj + 1) * n
                        tau0 = cj * n
                        b_ = ci_ % nb
                        ci_ += 1
                        tok = slice(c0 + a, c0 + a + n)
                        pr = p.pb[5]; pi_ = p.pb[6]
                        e_mm(p, pr[:, 0:n], BbT[:, gpl, d, 0, :], uT[:, ct, tok], True, True, [BbT, uT], [pr])
                        e_mm(p, pi_[:, 0:n], BbT[:, gpl, d, 1, :], uT[:, ct, tok], True, True, [BbT, uT], [pi_])
                        e_copy(p, "act", bur[b_][:, 0:n], pr[:, 0:n], [pr], [bur[b_]])
                        e_copy(p, "act", bui[b_][:, 0:n], pi_[:, 0:n], [pi_], [bui[b_]])
                        br = bur[b_][:, 0:n]; bi = bui[b_][:, 0:n]
                        ts0 = tau0 if d == 0 else (TL - L) + a
                        cs = cosT[:, ts0:ts0 + n]; sn = sinT[:, ts0:ts0 + n]
                        R = [bur[b_], bui[b_], cosT, sinT]
                        e_tt(p, "dve", m1[b_][:, 0:n], cs, br, ALU.mult, R, [m1[b_]])
                        e_tt(p, "dve", m2[b_][:, 0:n], sn, bi, ALU.mult, R, [m2[b_]])
                        e_tt(p, "dve", m1[b_][:, 0:n], m1[b_][:, 0:n], m2[b_][:, 0:n], ALU.add, [m1[b_], m2[b_]], [m1[b_]])
                        e_tt(p, "dve", cr[b_][:, 0:n], cs, bi, ALU.mult, R, [cr[b_]])
                        e_tt(p, "dve", ci[b_][:, 0:n], sn, br, ALU.mult, R, [ci[b_]])
                        e_tt(p, "dve", cr[b_][:, 0:n], cr[b_][:, 0:n], ci[b_][:, 0:n], ALU.subtract, [cr[b_], ci[b_]], [cr[b_]])
                        if cj == 0:
                            if full and which == 1:
                                ini_r = kin[:, 0, col:col + 1]; ini_i = kin[:, 1, col:col + 1]; rd = [kin]
                            else:
                                ini_r = 0.0; ini_i = 0.0; rd = []
                        else:
                            ini_r = carry_r; ini_i = carry_i; rd = [carry_br, carry_bi]
                        mb = mcol.to_broadcast([128, n])
                        ko_r = kr[b_][:, 0:n]; ko_i = ki[b_][:, 0:n]; xi_r = m1[b_][:, 0:n]; xi_i = cr[b_][:, 0:n]
                        if d == 1:
                            ko_r = rev_ap(ko_r); ko_i = rev_ap(ko_i); xi_r = rev_ap(xi_r); xi_i = rev_ap(xi_i)
                        p.op("dve", lambda h, o=ko_r, x=xi_r, ini=ini_r, mb=mb: h.tensor_tensor_scan(
                            out=o, data0=mb, data1=x, initial=ini, op0=ALU.mult, op1=ALU.add),
                            reads=[m1[b_], S.mag] + rd, writes=[kr[b_]])
                        p.op("dve", lambda h, o=ko_i, x=xi_i, ini=ini_i, mb=mb: h.tensor_tensor_scan(
                            out=o, data0=mb, data1=x, initial=ini, op0=ALU.mult, op1=ALU.add),
                            reads=[cr[b_], S.mag] + rd, writes=[ki[b_]])
                        lastc = n - 1 if d == 0 else 0
                        carry_r = kr[b_][:, lastc:lastc + 1]; carry_i = ki[b_][:, lastc:lastc + 1]
                        carry_br = kr[b_]; carry_bi = ki[b_]
                        if full:
                            o_r = hr[b_][:, 0:n]; o_i = hi[b_][:, 0:n]
                            K = [kr[b_], ki[b_], cosT, sinT]
                            e_tt(p, "dve", m1[b_][:, 0:n], cs, kr[b_][:, 0:n], ALU.mult, K, [m1[b_]])
                            e_tt(p, "dve", m2[b_][:, 0:n], sn, ki[b_][:, 0:n], ALU.mult, K, [m2[b_]])
                            e_tt(p, "dve", o_r, m1[b_][:, 0:n], m2[b_][:, 0:n], ALU.subtract, [m1[b_], m2[b_]], [hr[b_]])
                            e_tt(p, "dve", cr[b_][:, 0:n], sn, kr[b_][:, 0:n], ALU.mult, K, [cr[b_]])
                            e_tt(p, "dve", ci[b_][:, 0:n], cs, ki[b_][:, 0:n], ALU.mult, K, [ci[b_]])
                            e_tt(p, "dve", o_i, cr[b_][:, 0:n], ci[b_][:, 0:n], ALU.add, [cr[b_], ci[b_]], [hi[b_]])
                            if which == 0:
                                bank = p.pb[0]; bsl = slice(0, n)
                            else:
                                bank = p.pb[1 + a // CH]; bsl = slice(0, n)
                            first = (gpl == 0 and d == 0)
                            last = (gpl == 3 and d == 1)
                            e_mm(p, bank[:, bsl], Cp[:, gpl, 0, :], hr[b_][:, 0:n], first, False, [Cp, hr[b_]], [bank])
                            e_mm(p, bank[:, bsl], Cp[:, gpl, 1, :], hi[b_][:, 0:n], False, last, [Cp, hi[b_]], [bank])
                    if not full:
                        fl = L - 1 if d == 0 else TL - L
                        csl = cosT[:, fl:fl + 1]; snl = sinT[:, fl:fl + 1]
                        Kt = [carry_br, carry_bi, cosT, sinT, tiny]
                        e_tt(p, "dve", tiny[:, 0:1], csl, carry_r, ALU.mult, Kt, [tiny])
                        e_tt(p, "dve", tiny[:, 1:2], snl, carry_i, ALU.mult, Kt, [tiny])
                        e_tt(p, "dve", Fsum[:, 0, which, col:col + 1], tiny[:, 0:1], tiny[:, 1:2], ALU.subtract, [tiny], [Fsum])
                        e_tt(p, "dve", tiny[:, 2:3], snl, carry_r, ALU.mult, Kt, [tiny])
                        e_tt(p, "dve", tiny[:, 3:4], csl, carry_i, ALU.mult, Kt, [tiny])
                        e_tt(p, "dve", Fsum[:, 1, which, col:col + 1], tiny[:, 2:3], tiny[:, 3:4], ALU.add, [tiny], [Fsum])
        if full:
            yacc_evac(ct)
    p.release(m)


def s5_incoming(p, S, Fsum, gath, onehot, kin):
    m = p.mark()
    I = p.alloc([128, 4, 2, 64])
    t1 = p.alloc([128, 32]); t2 = p.alloc([128, 32]); nr = p.alloc([128, 32]); ni = p.alloc([128, 32])
    f = slice(0, 32); b = slice(32, 64)
    R = [I, gath, Fsum, S.Ar, S.Ai, t1, t2, nr, ni]
    e_copy(p, "dve", I[:, 0, 0, f], Fsum[:, 0, 0, f], R, [I]); e_copy(p, "dve", I[:, 0, 1, f], Fsum[:, 1, 0, f], R, [I])
    for q in range(3):
        cmul(p, "dve", nr[:], ni[:], S.Ar[:, f], S.Ai[:, f], I[:, q, 0, f], I[:, q, 1, f], t1[:], t2[:], R, [t1, t2, nr, ni])
        e_tt(p, "dve", I[:, q + 1, 0, f], nr[:], gath[:, q, 0, f], ALU.add, R, [I])
        e_tt(p, "dve", I[:, q + 1, 1, f], ni[:], gath[:, q, 1, f], ALU.add, R, [I])
    e_copy(p, "dve", I[:, 3, 0, b], Fsum[:, 0, 0, b], R, [I]); e_copy(p, "dve", I[:, 3, 1, b], Fsum[:, 1, 0, b], R, [I])
    for q in (3, 2, 1):
        cmul(p, "dve", nr[:], ni[:], S.Ar[:, b], S.Ai[:, b], I[:, q, 0, b], I[:, q, 1, b], t1[:], t2[:], R, [t1, t2, nr, ni])
        e_tt(p, "dve", I[:, q - 1, 0, b], nr[:], gath[:, q, 0, b], ALU.add, R, [I])
        e_tt(p, "dve", I[:, q - 1, 1, b], ni[:], gath[:, q, 1, b], ALU.add, R, [I])
    own = p.alloc([128, 2, 64])
    e_ts(p, "dve", own[:], I[:, 0], onehot[:, 0:1], None, ALU.mult, None, [I, onehot], [own])
    for q in range(1, 4):
        e_stt(p, own[:], I[:, q], onehot[:, q:q + 1], own[:], ALU.mult, ALU.add, [I, onehot, own], [own])
    t3 = p.alloc([128, 64]); t4 = p.alloc([128, 64])
    cmul(p, "dve", kin[:, 0, :], kin[:, 1, :], S.cth[:], S.sth[:], own[:, 0, :], own[:, 1, :], t3[:], t4[:],
         [S.cth, S.sth, own, t3, t4, kin], [t3, t4, kin])
    p.release(m)


def build_s5_test():
    p = Prog(); p.init_pool()
    io = {}
    for name, shape in [("xl", [NL, D]), ("xc", [NCX, D]), ("cT", [128, 8, 2]), ("modw", [D, 6144]), ("modb", [1, 6144]),
                        ("n1g", [1, D]), ("ident", [128, 128]), ("lamP", [128, 2, 64]), ("ldtP", [128, 64]),
                        ("bP", [128, 2, 32, 16]), ("cP", [128, 2, 32, 16]), ("dP", [128, 8]), ("onehot", [128, 4])]:
        io[name] = p.dram(name, shape)
    dbg = p.dram("dbg", [128, 8, NT], kind="ExternalOutput")
    dbgF = p.dram("dbgF", [128, 2, 2, 64], kind="ExternalOutput")
    dbgK = p.dram("dbgK", [128, 2, 64], kind="ExternalOutput")
    modsc = p.dram("modsc", [2, 6144], kind="Internal")
    tabsc = p.dram("tabsc", [64, 2, 128, 2048], kind="Internal")
    fsrc = p.dram("fsrc", [128, 128], kind="Internal")
    fdst = p.dram("fdst", [512, 128], kind="Internal")
    ident = p.alloc([128, 128]); p.dma(ident[:], io["ident"][:], writes=[ident])
    onehot = p.alloc([128, 4]); p.dma(onehot[:], io["onehot"][:], writes=[onehot])
    dP = p.alloc([128, 8]); p.dma(dP[:], io["dP"][:], writes=[dP])
    phase_mod(p, io["cT"], io["modw"], io["modb"], modsc, ident)
    uT = p.alloc([128, 8, NT], BF16, name="uT")
    phase_hn(p, io["xl"], io["xc"], modsc, io["n1g"], ident, uT, 0, 1)
    S = s5_params(p, io, ident)
    Fsum = p.alloc([128, 2, 2, 64]); kin = p.alloc([128, 2, 64])
    s5_pass(p, S, uT, tabsc, False, Fsum, None, ident)
    p.dma(fsrc[:].rearrange("q (r c) -> q r c", r=2), Fsum[:, :, 1, :], reads=[Fsum], writes=[fsrc], stream="st")
    p.cc(lambda h: h.collective_compute("AllGather", ALU.bypass, replica_groups=[[0, 1, 2, 3], [4, 5, 6, 7]],
                                        ins=[fsrc.t.opt()], outs=[fdst.t.opt()]), reads=[fsrc], writes=[fdst])
    gath = p.alloc([128, 4, 2, 64])
    p.dma(gath[:], fdst[:].rearrange("(q x) (r c) -> x q r c", x=128, r=2), reads=[fdst], writes=[gath])
    s5_incoming(p, S, Fsum, gath, onehot, kin)
    p.dma(dbgF[:], Fsum[:], reads=[Fsum], stream="st"); p.dma(dbgK[:], kin[:], reads=[kin], stream="st")
    vbuf = [p.alloc([128, 512]) for _ in range(2)]

    def evac(ct):
        for bi_ in range(5):
            n = NCX if bi_ == 0 else 512
            c0 = 0 if bi_ == 0 else NCX + (bi_ - 1) * 512
            v = vbuf[bi_ % 2]
            e_stt(p, v[:, 0:n], uT[:, ct, c0:c0 + n], dP[:, ct:ct + 1], p.pb[bi_][:, 0:n], ALU.mult, ALU.add,
                  [uT, dP, p.pb[bi_]], [v])
            p.dma(dbg[:, ct, c0:c0 + n], v[:, 0:n], reads=[v], stream="st")

    s5_pass(p, S, uT, tabsc, True, None, kin, ident, evac)
    p.wait_all()
    return p


def host_inputs(z, layer=0):
    x = z["x"]; c = z["c"]; ctx = z["ctx"]; c_ctx = z["c_ctx"]
    lam = np.stack([z["s5_lam_re"][0], z["s5_lam_im"][0]], 0)
    lamP = lam.reshape(2, 2, 32, 2, 64).transpose(3, 4, 0, 1, 2).reshape(128, 2, 64)
    ldt = z["s5_log_dt"][0]
    ldtP = np.broadcast_to(ldt.reshape(2, 32, 2)[:, :, :, None], (2, 32, 2, 64)).transpose(2, 3, 0, 1).reshape(128, 64)
    b = np.stack([z["s5_b_re"][0], z["s5_b_im"][0]], 0)
    bP = b.reshape(2, 32, 2, 64, 16).transpose(2, 3, 0, 1, 4).reshape(128, 2, 32, 16)
    cc = np.stack([z["s5_c_re"][0], z["s5_c_im"][0]], 0)
    cP = cc.reshape(2, 32, 2, 16, 64).transpose(2, 4, 0, 1, 3).reshape(128, 2, 32, 16)
    dP = z["s5_d"][0].reshape(8, 128).T
    maps = []
    for k in range(8):
        bb = k // 4; q = k % 4
        cT = np.stack([c[bb], c_ctx], axis=-1).reshape(8, 128, 2).transpose(1, 0, 2)
        oh = np.zeros((128, 4), np.float32); oh[:, q] = 1
        maps.append(dict(xl=x[bb, q * NL:(q + 1) * NL], xc=ctx[bb], cT=np.ascontiguousarray(cT),
                         modw=z["mod_w"][layer], modb=z["mod_b"][layer][None], n1g=z["norm1_g"][layer][None],
                         ident=np.eye(128, dtype=np.float32), lamP=np.ascontiguousarray(lamP),
                         ldtP=np.ascontiguousarray(ldtP), bP=np.ascontiguousarray(bP), cP=np.ascontiguousarray(cP),
                         dP=np.ascontiguousarray(dP), onehot=oh))
    return maps


def ref_s5(z, bb, groups):
    f8 = np.float64
    x = z["x"][bb].astype(f8); ctx = z["ctx"][bb].astype(f8); c = z["c"][bb].astype(f8); c_ctx = z["c_ctx"].astype(f8)
    silu = lambda v: v / (1 + np.exp(-v))
    rms = lambda v, g: v / np.sqrt((v * v).mean(-1, keepdims=True) + 1e-6) * g
    mw = z["mod_w"][0].astype(f8); mb = z["mod_b"][0].astype(f8); g1 = z["norm1_g"][0].astype(f8)
    ml = silu(c) @ mw + mb; mc = silu(c_ctx) @ mw + mb
    hn = rms(x, g1) * (1 + ml[D:2 * D]) + ml[:D]
    cn = rms(ctx, g1) * (1 + mc[D:2 * D]) + mc[:D]
    out = {}
    for g in groups:
        ch = slice(g * 16, (g + 1) * 16)
        tot = np.zeros((NCX + 8192, 16))
        for d in range(2):
            lr = z["s5_lam_re"][0, d, g].astype(f8); li = z["s5_lam_im"][0, d, g].astype(f8)
            step = np.exp(z["s5_log_dt"][0, d, g].astype(f8))
            lamc = lr + 1j * li
            abar = np.exp(lamc * step)
            Bc = z["s5_b_re"][0, g].astype(f8) + 1j * z["s5_b_im"][0, g].astype(f8)
            Cc = z["s5_c_re"][0, g].astype(f8) + 1j * z["s5_c_im"][0, g].astype(f8)
            Bbar = ((abar - 1) / lamc)[:, None] * Bc
            seq = np.concatenate([cn[:, ch], hn[:, ch]], 0) if d == 0 else np.concatenate([cn[::-1, ch], hn[::-1, ch]], 0)
            bu = seq @ Bbar.T
            h = np.zeros(64, complex); ys = np.zeros((len(seq), 16))
            for t in range(len(seq)):
                h = abar * h + bu[t]
                ys[t] = (Cc @ h).real
            if d == 1:
                ys = np.concatenate([ys[:NCX][::-1], ys[NCX:][::-1]], 0)
            tot += ys
        u = np.concatenate([cn[:, ch], hn[:, ch]], 0)
        out[g] = tot + z["s5_d"][0, ch].astype(f8) * u
    return out


def gelu_evac(p, uT, dP, gT):
    vb = [p.alloc([128, 512]) for _ in range(2)]
    wb = [p.alloc([128, 512]) for _ in range(1)]
    cnt = [0]

    def evac(ct):
        for bi_ in range(5):
            n = NCX if bi_ == 0 else 512
            c0 = 0 if bi_ == 0 else NCX + (bi_ - 1) * 512
            v = vb[cnt[0] % 2]; w = wb[0]
            cnt[0] += 1
            e_stt(p, v[:, 0:n], uT[:, ct, c0:c0 + n], dP[:, ct:ct + 1], p.pb[bi_][:, 0:n], ALU.mult, ALU.add,
                  [uT, dP, p.pb[bi_]], [v])
            e_act(p, w[:, 0:n], v[:, 0:n], AF.Square, [v], [w])
            e_ts(p, "dve", w[:, 0:n], w[:, 0:n], 0.044715, 1.0, ALU.mult, ALU.add, [w], [w])
            e_tt(p, "dve", w[:, 0:n], w[:, 0:n], v[:, 0:n], ALU.mult, [w, v], [w])
            e_act(p, w[:, 0:n], w[:, 0:n], AF.Sigmoid, [w], [w], scale=1.5957691216057308)
            e_tt(p, "pool", gT[:, ct, c0:c0 + n], v[:, 0:n], w[:, 0:n], ALU.mult, [v, w], [gT])
    return evac


def load_w_bf16(p, dst, src_ap, w):
    p.dma(dst, src_ap, writes=w, eng="pool")


def phase_c(p, io, modsc, ident, gT, h2T, gates, x1sc, layer_glu=True):
    m = p.mark()
    W = p.alloc([128, 4, 8, 512], BF16)
    for nb in range(4):
        load_w_bf16(p, W[:, nb],
                    io["gluw"][:, nb * 512:(nb + 1) * 512].rearrange("(k q) n -> q k n", q=128), [W])
    gb = p.alloc([128, 2048]); load_bcast(p, gb[:], io["glub"][0:1, :], [], [gb])
    n2 = p.alloc([128, D]); load_bcast(p, n2[:], io["n2g"][0:1, :], [], [n2])
    G1 = []; A2 = []; SH2 = []
    for which in range(2):
        g1 = p.alloc([128, D]); a2 = p.alloc([128, D]); s2 = p.alloc([128, D])
        load_bcast(p, g1[:], modsc[which:which + 1, 2 * D:3 * D], [modsc], [g1])
        load_bcast(p, s2[:], modsc[which:which + 1, 3 * D:4 * D], [modsc], [s2])
        load_bcast(p, a2[:], modsc[which:which + 1, 4 * D:5 * D], [modsc], [a2])
        e_stt(p, a2[:], a2[:], 1.0, n2[:], ALU.add, ALU.mult, [a2, n2], [a2])
        G1.append(g1); A2.append(a2); SH2.append(s2)
    rw = p.alloc([128, 8, 16]); p.dma(rw[:], io["rw"][:].rearrange("(k q) n -> q k n", q=128), writes=[rw])
    rb = p.alloc([128, 16]); load_bcast(p, rb[:], io["rb"][0:1, :], [], [rb])
    xs = [p.alloc([128, D]) for _ in range(2)]
    val = p.alloc([128, D]); gat = p.alloc([128, D]); x1 = [p.alloc([128, D]) for _ in range(2)]
    hn2 = p.alloc([128, D]); junk = p.alloc([128, D]); hT32 = p.alloc([128, 8, 128])
    sm = p.alloc([128, 128])
    ss = p.alloc([128, 1]); rs = p.alloc([128, 1])
    for t in range(18):
        which = 1 if t < 2 else 0
        src = io["xc"][t * 128:(t + 1) * 128, :] if t < 2 else io["xl"][(t - 2) * 128:(t - 1) * 128, :]
        x = xs[t % 2]; xo = x1[t % 2]
        p.dma(x[:], src, writes=[x])
        tok = slice(t * 128, (t + 1) * 128)
        for nb in range(4):
            ps = p.pb[nb]
            for k in range(8):
                e_mm(p, ps[:], gT[:, k, tok], W[:, nb, k, :], k == 0, k == 7, [gT, W], [ps])
        for nb in range(2):
            e_tt(p, "dve", val[:, nb * 512:(nb + 1) * 512], p.pb[nb][:], gb[:, nb * 512:(nb + 1) * 512], ALU.add,
                 [p.pb[nb], gb], [val])
            e_tt(p, "dve", gat[:, nb * 512:(nb + 1) * 512], p.pb[2 + nb][:], gb[:, D + nb * 512:D + (nb + 1) * 512],
                 ALU.add, [p.pb[2 + nb], gb], [gat])
        e_act(p, gat[:], gat[:], AF.Sigmoid, [gat], [gat])
        e_tt(p, "dve", val[:], val[:], gat[:], ALU.mult, [val, gat], [val])
        e_tt(p, "dve", val[:], val[:], G1[which][:], ALU.mult, [val, G1[which]], [val])
        e_tt(p, "dve", xo[:], val[:], x[:], ALU.add, [val, x], [xo])
        p.dma(x1sc[tok, :], xo[:], reads=[xo], writes=[x1sc])
        e_act(p, junk[:], xo[:], AF.Square, [xo], [junk, ss], accum_out=ss[:])
        e_ts(p, "dve", rs[:], ss[:], 1.0 / D, 1e-6, ALU.mult, ALU.add, [ss], [rs])
        e_act(p, rs[:], rs[:], AF.Sqrt, [rs], [rs])
        p.op("dve", lambda h: h.reciprocal(out=rs[:], in_=rs[:]), reads=[rs], writes=[rs])
        e_stt(p, hn2[:], xo[:], rs[:], A2[which][:], ALU.mult, ALU.mult, [xo, rs, A2[which]], [hn2])
        e_tt(p, "dve", hn2[:], hn2[:], SH2[which][:], ALU.add, [hn2, SH2[which]], [hn2])
        for half in range(2):
            ps = p.pb[4 + half]
            for c4 in range(4):
                ct = half * 4 + c4
                e_tr(p, ps[:, c4 * 128:(c4 + 1) * 128], hn2[:, ct * 128:(ct + 1) * 128], ident[:], [hn2, ident], [ps])
            pv = ps[:].rearrange("q (c n) -> q c n", c=4)
            e_copy(p, "act", h2T[:, half * 4:(half + 1) * 4, tok], pv, [ps], [h2T])
            e_copy(p, "dve", hT32[:, half * 4:(half + 1) * 4, :], pv, [ps], [hT32])
        pl = p.pb[6]
        for k in range(8):
            e_mm(p, pl[:, 0:16], hT32[:, k, :], rw[:, k, :], k == 0, k == 7, [hT32, rw], [pl])
        routing(p, pl, rb, sm, gates[:, t, :], gates)
    p.release(m)


def routing(p, pl, rb, sm, gout, gates_buf):
    R = [sm]
    s = sm[:, 0:16]; sel2 = sm[:, 16:48]; ps_ = sm[:, 48:72]; gs = sm[:, 72:76]; t2 = sm[:, 76:78]
    gmax = sm[:, 78:79]; Gm = sm[:, 80:84]; g1 = sm[:, 84:100]; cnt = sm[:, 100:116]; wsum = sm[:, 116:117]
    e_act(p, s, pl[:, 0:16], AF.Sigmoid, [pl], R)
    sel2v = sel2.rearrange("q (g e) -> q g e", g=4)
    sv = s.rearrange("q (g e) -> q g e", g=4)
    rbv = rb[:].rearrange("q (g e) -> q g e", g=4)
    e_tt(p, "dve", sel2v[:, :, 0:4], sv, rbv, ALU.add, R + [rb], R)
    e_copy(p, "dve", sel2v[:, :, 4:8], sel2v[:, :, 0:4], R, R)
    pv = ps_.rearrange("q (g e) -> q g e", g=4)
    pairs = [(0, 1), (0, 2), (0, 3), (1, 2), (1, 3), (2, 3)]
    for i, (a, b) in enumerate(pairs):
        e_tt(p, "dve", pv[:, :, i:i + 1], sel2v[:, :, a:a + 1], sel2v[:, :, b:b + 1], ALU.add, R, R)
    e_tt(p, "dve", pv[:, :, 0:3], pv[:, :, 0:3], pv[:, :, 3:6], ALU.max, R, R)
    e_tt(p, "dve", pv[:, :, 0:1], pv[:, :, 0:1], pv[:, :, 1:2], ALU.max, R, R)
    e_tt(p, "dve", gs.unsqueeze(2), pv[:, :, 0:1], pv[:, :, 2:3], ALU.max, R, R)
    e_tt(p, "dve", t2, gs[:, 0:2], gs[:, 2:4], ALU.max, R, R)
    e_tt(p, "dve", gmax, t2[:, 0:1], t2[:, 1:2], ALU.max, R, R)
    e_ts(p, "dve", Gm, gs, gmax, 1.0, ALU.is_ge, ALU.mult, R, R)
    g1v = g1.rearrange("q (g e) -> q g e", g=4); cv = cnt.rearrange("q (g e) -> q g e", g=4)
    e_tt(p, "dve", cv, sel2v[:, :, 1:5], sel2v[:, :, 0:4], ALU.is_gt, R, R)
    for r in (2, 3):
        e_tt(p, "dve", g1v, sel2v[:, :, r:r + 4], sel2v[:, :, 0:4], ALU.is_gt, R, R)
        e_tt(p, "dve", cv, cv, g1v, ALU.add, R, R)
    e_ts(p, "dve", cv, cv, 1.5, 1.0, ALU.is_lt, ALU.mult, R, R)
    e_tt(p, "dve", cv, cv, Gm.unsqueeze(2).to_broadcast([128, 4, 4]), ALU.mult, R, R)
    e_tt(p, "dve", cnt, cnt, s, ALU.mult, R, R)
    p.op("dve", lambda h: h.reduce_sum(out=wsum, in_=cnt, axis=AX.X), reads=R, writes=R)
    p.op("dve", lambda h: h.reciprocal(out=wsum, in_=wsum), reads=R, writes=R)
    e_ts(p, "dve", gout, cnt, wsum, 1.0, ALU.mult, ALU.mult, R, [gates_buf])


def phase_moe(p, io, layer_w, modsc, h2T, gates, x1sc, ntiles, out_cb, mod_which_of_tile):
    m = p.mark()
    w1d, w3d, w2d = layer_w
    ntok = ntiles * 128
    yacc = p.alloc([128, ntiles, D], name="yacc")
    for t0 in range(0, ntiles, 4):
        t1 = min(ntiles, t0 + 4)
        p.op("pool", lambda h, t0=t0, t1=t1: h.memset(yacc[:, t0:t1, :], 0.0), writes=[yacc])
    w1 = [p.alloc([128, 8, 512], BF16) for _ in range(2)]; w3 = [p.alloc([128, 8, 512], BF16) for _ in range(2)]
    w2 = [p.alloc([128, 4, D], BF16) for _ in range(2)]
    hT = [p.alloc([128, 4, 512], BF16) for _ in range(2)]
    s1 = [p.alloc([128, 512]) for _ in range(2)]
    blocks = [(b0, min(512, ntok - b0)) for b0 in range(0, ntok, 512)]
    ib = 0; iy = 0; ih = 0
    for e in range(16):
        a1 = w1[e % 2]; a3 = w3[e % 2]; a2 = w2[e % 2]
        load_w_bf16(p, a1[:], w1d[e].rearrange("(k q) n -> q k n", q=128), [a1])
        load_w_bf16(p, a3[:], w3d[e].rearrange("(k q) n -> q k n", q=128), [a3])
        load_w_bf16(p, a2[:], w2d[e].rearrange("(k q) n -> q k n", q=128), [a2])
        for (b0, n) in blocks:
            h = hT[ib % 2]; ib += 1
            for hc in range(4):
                p1 = p.pb[ih % 2]; p3 = p.pb[2 + ih % 2]; sb1 = s1[ih % 2]; ih += 1
                for k in range(8):
                    e_mm(p, p1[:, 0:n], a1[:, k, hc * 128:(hc + 1) * 128], h2T[:, k, b0:b0 + n], k == 0, k == 7, [a1, h2T], [p1])
                for k in range(8):
                    e_mm(p, p3[:, 0:n], a3[:, k, hc * 128:(hc + 1) * 128], h2T[:, k, b0:b0 + n], k == 0, k == 7, [a3, h2T], [p3])
                e_act(p, sb1[:, 0:n], p1[:, 0:n], AF.Silu, [p1], [sb1])
                e_tt(p, "dve", h[:, hc, 0:n], sb1[:, 0:n], p3[:, 0:n], ALU.mult, [sb1, p3], [h])
            for tt in range(n // 128):
                t = b0 // 128 + tt
                for dh in range(2):
                    py = p.pb[4 + iy % 4]; iy += 1
                    for hc in range(4):
                        e_mm(p, py[:], h[:, hc, tt * 128:(tt + 1) * 128], a2[:, hc, dh * 512:(dh + 1) * 512],
                             hc == 0, hc == 3, [h, a2], [py])
                    ya = yacc[:, t, dh * 512:(dh + 1) * 512]
                    e_stt(p, ya, py[:], gates[:, t, e:e + 1], ya, ALU.mult, ALU.add, [py, gates, yacc], [yacc])
    G2 = []
    for which in range(2):
        g2 = p.alloc([128, D]); load_bcast(p, g2[:], modsc[which:which + 1, 5 * D:6 * D], [modsc], [g2]); G2.append(g2)
    xb = [p.alloc([128, D]) for _ in range(2)]
    for t in range(ntiles):
        which = mod_which_of_tile(t)
        x = xb[t % 2]
        p.dma(x[:], x1sc[t * 128:(t + 1) * 128, :], reads=[x1sc], writes=[x])
        e_tt(p, "dve", yacc[:, t, :], yacc[:, t, :], G2[which][:], ALU.mult, [yacc, G2[which]], [yacc])
        e_tt(p, "dve", x[:], x[:], yacc[:, t, :], ALU.add, [x, yacc], [x])
        out_cb(t, x)
    p.release(m)


L0_INPUTS = [("xl", [NL, D]), ("xc", [NCX, D]), ("cT", [128, 8, 2]), ("modw", [D, 6144]), ("modb", [1, 6144]),
             ("n1g", [1, D]), ("ident", [128, 128]), ("lamP", [128, 2, 64]), ("ldtP", [128, 64]),
             ("bP", [128, 2, 32, 16]), ("cP", [128, 2, 32, 16]), ("dP", [128, 8]), ("onehot", [128, 4]),
             ("gluw", [D, 2048]), ("glub", [1, 2048]), ("n2g", [1, D]), ("rw", [D, 16]), ("rb", [1, 16]),
             ("w1", [16, D, 512]), ("w3", [16, D, 512]), ("w2", [16, 512, D])]


def layer0(p, io, sc, ident, onehot, out_cb, skip_s5=False, stop_after_c=False, dbg=None):
    dP = p.alloc([128, 8]); p.dma(dP[:], io["dP"][:], writes=[dP])
    phase_mod(p, io["cT"], io["modw"], io["modb"], sc["modsc"], ident)
    gates = p.alloc([128, 18, 16])
    hi0 = p.hi
    gT = p.alloc([128, 8, NT], BF16, name="gT", hi=True)
    m_g = p.mark()
    uT = p.alloc([128, 8, NT], BF16, name="uT")
    phase_hn(p, io["xl"], io["xc"], sc["modsc"], io["n1g"], ident, uT, 0, 1)
    if skip_s5:
        for ct in range(8):
            e_copy(p, "dve", gT[:, ct, :], uT[:, ct, :], [uT], [gT])
    S = None if skip_s5 else s5_params(p, io, ident)
    Fsum = p.alloc([128, 2, 2, 64]); kin = p.alloc([128, 2, 64])
    if not skip_s5:
      s5_pass(p, S, uT, sc["tabsc"], False, Fsum, None, ident)
    p.dma(sc["fsrc"][:].rearrange("q (r c) -> q r c", r=2), Fsum[:, :, 1, :], reads=[Fsum], writes=[sc["fsrc"]])
    p.cc(lambda h: h.collective_compute("AllGather", ALU.bypass, replica_groups=[[0, 1, 2, 3], [4, 5, 6, 7]],
                                        ins=[sc["fsrc"].t.opt()], outs=[sc["fdst"].t.opt()]),
         reads=[sc["fsrc"]], writes=[sc["fdst"]])
    gath = p.alloc([128, 4, 2, 64])
    p.dma(gath[:], sc["fdst"][:].rearrange("(q x) (r c) -> x q r c", x=128, r=2), reads=[sc["fdst"]], writes=[gath])
    if not skip_s5:
        s5_incoming(p, S, Fsum, gath, onehot, kin)
        evac = gelu_evac(p, uT, dP, gT)
        s5_pass(p, S, uT, sc["tabsc"], True, None, kin, ident, evac)
    p.release(m_g)
    h2T = p.alloc([128, 8, NT], BF16, name="h2T")
    phase_c(p, io, sc["modsc"], ident, gT, h2T, gates, sc["x1sc"])
    p.hi = hi0
    if dbg is not None:
        p.dma(dbg["gates"][:], gates[:], reads=[gates])
    if stop_after_c:
        return
    phase_moe(p, io, (io["w1"], io["w3"], io["w2"]), sc["modsc"], h2T, gates, sc["x1sc"], 18,
              out_cb, lambda t: 1 if t < 2 else 0)


def build_l0_test():
    p = Prog(); p.init_pool()
    io = {name: p.dram(name, shape) for name, shape in L0_INPUTS}
    xo = p.dram("xo", [NT, D], kind="ExternalOutput")
    sc = dict(modsc=p.dram("modsc", [2, 6144], kind="Internal"),
              tabsc=p.dram("tabsc", [64, 2, 128, 2048], kind="Internal"),
              fsrc=p.dram("fsrc", [128, 128], kind="Internal"), fdst=p.dram("fdst", [512, 128], kind="Internal"),
              x1sc=p.dram("x1sc", [NT, D], kind="Internal"))
    ident = p.alloc([128, 128]); p.dma(ident[:], io["ident"][:], writes=[ident])
    onehot = p.alloc([128, 4]); p.dma(onehot[:], io["onehot"][:], writes=[onehot])

    def out_cb(t, x):
        p.dma(xo[t * 128:(t + 1) * 128, :], x[:], reads=[x])

    layer0(p, io, sc, ident, onehot, out_cb)
    p.wait_all()
    return p


def host_inputs_l0(z):
    maps = host_inputs(z, 0)
    for k in range(8):
        maps[k].update(gluw=z["s5_glu_w"][0], glub=z["s5_glu_b"][0][None], n2g=z["norm2_g"][0][None],
                       rw=z["router_w"], rb=z["router_b"][None], w1=z["moe_w1"][0], w3=z["moe_w3"][0], w2=z["moe_w2"][0])
    return maps


NCH = 66
PADW = 8456
CTX0 = 2
LAT0 = 262
RSW = 1028

L1_INPUTS = [("cT", [128, 8, 2]), ("modw1", [D, 6144]), ("modb1", [1, 6144]), ("n1g1", [1, D]), ("n2g1", [1, D]),
             ("fing", [1, D]), ("wz", [D, 512]), ("wxbc", [D, 1024]), ("wdt", [D, 16]), ("convw", [128, 8, 5]),
             ("convb", [128, 8]), ("dtb", [1, 16]), ("alog", [1, 16]), ("dsk", [1, 8]), ("sng", [1, 512]),
             ("outw", [512, D]), ("triU", [128, 128]), ("triL", [128, 128]), ("ones", [128, 128]),
             ("rw", [D, 16]), ("rb", [1, 16]), ("w1b", [16, D, 512]), ("w3b", [16, D, 512]), ("w2b", [16, 512, D])]


def l1_scratch(p):
    sc = {}
    sc["modsc1"] = p.dram("modsc1", [2, 6144], kind="Internal")
    sc["pre"] = p.dram("pre", [8, 128, PADW], kind="Internal")
    sc["X"] = p.dram("Xs", [NCH, 128, 512], BF16, kind="Internal")
    sc["Bt"] = p.dram("Bts", [NCH, 128, 256], BF16, kind="Internal")
    sc["BT"] = p.dram("BTs", [NCH, 128, 256], BF16, kind="Internal")
    sc["CT"] = p.dram("CTs", [NCH, 128, 256], BF16, kind="Internal")
    sc["dts"] = p.dram("dts", [NCH, 128, 16], kind="Internal")
    sc["Z"] = p.dram("Zs", [NCH, 128, 512], BF16, kind="Internal")
    sc["yf"] = p.dram("yfs", [NCH, 128, 512], kind="Internal")
    sc["yb"] = p.dram("ybs", [NCH, 128, 512], kind="Internal")
    sc["rsrc"] = p.dram("rsrc", [8192, RSW], kind="Internal")
    sc["rdst"] = p.dram("rdst", [2048, RSW], kind="Internal")
    sc["x1b"] = p.dram("x1b", [2048, D], kind="Internal")
    return sc


def chunk_rows(xg, ctx1, c):
    if c < 2:
        return ctx1[c * 128:(c + 1) * 128, :]
    return xg[:].rearrange("(r w) d -> w r d", w=64)[c - 2]


def l1_proj(p, io, sc, xg, ctx1, ident):
    m = p.mark()
    modsc = sc["modsc1"]
    g_sb = p.alloc([128, D]); load_bcast(p, g_sb[:], io["n1g1"][0:1, :], [], [g_sb])
    A = []; SH = []
    for which in range(2):
        a = p.alloc([128, D]); s = p.alloc([128, D])
        load_bcast(p, a[:], modsc[which:which + 1, D:2 * D], [modsc], [a])
        load_bcast(p, s[:], modsc[which:which + 1, 0:D], [modsc], [s])
        e_stt(p, a[:], a[:], 1.0, g_sb[:], ALU.add, ALU.mult, [a, g_sb], [a])
        A.append(a); SH.append(s)
    Wx = p.alloc([128, 8, 1024], BF16); Wz = p.alloc([128, 8, 512], BF16); Wd = p.alloc([128, 8, 16])
    for half in range(2):
        load_w_bf16(p, Wx[:, :, half * 512:(half + 1) * 512] if False else Wx[:, half * 4:(half + 1) * 4, :],
                    io["wxbc"][half * 512:(half + 1) * 512, :].rearrange("(k q) n -> q k n", q=128), [Wx])
    load_w_bf16(p, Wz[:], io["wz"][:].rearrange("(k q) n -> q k n", q=128), [Wz])
    p.dma(Wd[:], io["wdt"][:].rearrange("(k q) n -> q k n", q=128), writes=[Wd])
    dtb = p.alloc([128, 16]); load_bcast(p, dtb[:], io["dtb"][0:1, :], [], [dtb])
    zero = p.alloc([128, 8]); p.op("pool", lambda h: h.memset(zero[:], 0.0), writes=[zero])
    for ft in range(8):
        for c0 in (0, CTX0 + 256, LAT0 - 2, LAT0 + 8192):
            p.dma(sc["pre"][ft, :, c0:c0 + 2], zero[:, 0:2], reads=[zero], writes=[sc["pre"]])
    xs = [p.alloc([128, D]) for _ in range(2)]; ys = [p.alloc([128, D]) for _ in range(2)]
    junk = p.alloc([128, D]); ss = p.alloc([128, 1]); rs = p.alloc([128, 1])
    hT = [p.alloc([128, 8, 512], BF16) for _ in range(2)]
    hT32 = p.alloc([128, 8, 128])
    zt = [p.alloc([128, 512], BF16) for _ in range(2)]
    dtt = [p.alloc([128, 16]) for _ in range(2)]
    pre_sb = [p.alloc([128, 512]) for _ in range(2)]
    blocks = [(0, [0, 1])] + [(1, [2 + 4 * b + i for i in range(4)]) for b in range(16)]
    ib = 0; it = 0; ip = 0
    for (islat, chunks) in blocks:
        which = 0 if islat else 1
        h = hT[ib % 2]; ib += 1
        n = 128 * len(chunks)
        for ti, c in enumerate(chunks):
            x = xs[it % 2]; y = ys[it % 2]; it += 1
            p.dma(x[:], chunk_rows(xg, ctx1, c), reads=[xg, ctx1], writes=[x])
            e_act(p, junk[:], x[:], AF.Square, [x], [junk, ss], accum_out=ss[:])
            e_ts(p, "dve", rs[:], ss[:], 1.0 / D, 1e-6, ALU.mult, ALU.add, [ss], [rs])
            e_act(p, rs[:], rs[:], AF.Sqrt, [rs], [rs])
            p.op("dve", lambda hh: hh.reciprocal(out=rs[:], in_=rs[:]), reads=[rs], writes=[rs])
            e_stt(p, y[:], x[:], rs[:], A[which][:], ALU.mult, ALU.mult, [x, rs, A[which]], [y])
            e_tt(p, "dve", y[:], y[:], SH[which][:], ALU.add, [y, SH[which]], [y])
            for half in range(2):
                ps = p.pb[half]
                for c4 in range(4):
                    ct = half * 4 + c4
                    e_tr(p, ps[:, c4 * 128:(c4 + 1) * 128], y[:, ct * 128:(ct + 1) * 128], ident[:], [y, ident], [ps])
                pv = ps[:].rearrange("q (c n) -> q c n", c=4)
                e_copy(p, "act", h[:, half * 4:(half + 1) * 4, ti * 128:(ti + 1) * 128], pv, [ps], [h])
                e_copy(p, "dve", hT32[:, half * 4:(half + 1) * 4, :], pv, [ps], [hT32])
            tokc = slice(ti * 128, (ti + 1) * 128)
            if islat:
                pz = p.pb[2]
                for k in range(8):
                    e_mm(p, pz[:], h[:, k, tokc], Wz[:, k, :], k == 0, k == 7, [h, Wz], [pz])
                z = zt[it % 2]
                e_act(p, z[:], pz[:], AF.Silu, [pz], [z])
                p.dma(sc["Z"][c], z[:], reads=[z], writes=[sc["Z"]])
            pd = p.pb[3]
            for k in range(8):
                e_mm(p, pd[:, 0:16], hT32[:, k, :], Wd[:, k, :], k == 0, k == 7, [hT32, Wd], [pd])
            dt_ = dtt[it % 2]
            e_tt(p, "dve", dt_[:], pd[:, 0:16], dtb[:], ALU.add, [pd, dtb], [dt_])
            e_act(p, dt_[:], dt_[:], AF.Exp, [dt_], [dt_])
            e_ts(p, "dve", dt_[:], dt_[:], 1.0, 1.0, ALU.add, ALU.mult, [dt_], [dt_])
            e_act(p, dt_[:], dt_[:], AF.Ln, [dt_], [dt_])
            p.dma(sc["dts"][c], dt_[:], reads=[dt_], writes=[sc["dts"]])
        col0 = (LAT0 + (chunks[0] - 2) * 128) if islat else CTX0
        for ft in range(8):
            px = p.pb[4 + ft % 4]
            for k in range(8):
                e_mm(p, px[:, 0:n], Wx[:, k, ft * 128:(ft + 1) * 128], h[:, k, 0:n], k == 0, k == 7, [Wx, h], [px])
            o = pre_sb[ip % 2]; ip += 1
            e_copy(p, "act", o[:, 0:n], px[:, 0:n], [px], [o])
            p.dma(sc["pre"][ft, :, col0:col0 + n], o[:, 0:n], reads=[o], writes=[sc["pre"]])
    p.release(m)


def l1_conv(p, io, sc, identb):
    m = p.mark()
    cw = p.alloc([128, 8, 5]); cb = p.alloc([128, 8])
    p.dma(cw[:], io["convw"][:], writes=[cw]); p.dma(cb[:], io["convb"][:], writes=[cb])
    inb = [p.alloc([128, 516]) for _ in range(3)]
    acc = [p.alloc([128, 512]) for _ in range(2)]
    act = [p.alloc([128, 8, 512], BF16) for _ in range(2)]
    xo = [p.alloc([128, 512], BF16) for _ in range(2)]; bo = [p.alloc([128, 256], BF16) for _ in range(2)]
    blocks = [(0, [0, 1])] + [(1, [2 + 4 * b + i for i in range(4)]) for b in range(16)]
    ii = 0; ia = 0; ib = 0; ix = 0
    for (islat, chunks) in blocks:
        n = 128 * len(chunks)
        col0 = (LAT0 + (chunks[0] - 2) * 128) if islat else CTX0
        a8 = act[ib % 2]; ib += 1
        for ft in range(8):
            xin = inb[ii % 3]; ii += 1
            p.dma(xin[:, 0:n + 4], sc["pre"][ft, :, col0 - 2:col0 + n + 2], reads=[sc["pre"]], writes=[xin])
            a = acc[ia % 2]; ia += 1
            e_ts(p, "dve", a[:, 0:n], xin[:, 0:n], cw[:, ft, 0:1], cb[:, ft:ft + 1], ALU.mult, ALU.add, [xin, cw, cb], [a])
            for k in range(1, 5):
                e_stt(p, a[:, 0:n], xin[:, k:k + n], cw[:, ft, k:k + 1], a[:, 0:n], ALU.mult, ALU.add, [xin, cw, a], [a])
            e_act(p, a8[:, ft, 0:n], a[:, 0:n], AF.Silu, [a], [a8])
        for ti, c in enumerate(chunks):
            tok = slice(ti * 128, (ti + 1) * 128)
            pt = p.pb[ix % 2]; ptb = pt.t.bitcast(BF16)
            x_o = xo[ix % 2]; b_o = bo[ix % 2]; ix += 1
            for ft in range(6):
                e_tr(p, ptb[:, ft * 128:(ft + 1) * 128], a8[:, ft, tok], identb[:], [a8, identb], [pt])
            e_copy(p, "act", x_o[:], ptb[:, 0:512], [pt], [x_o])
            e_copy(p, "dve", b_o[:], ptb[:, 512:768], [pt], [b_o])
            p.dma(sc["X"][c], x_o[:], reads=[x_o], writes=[sc["X"]])
            p.dma(sc["Bt"][c], b_o[:], reads=[b_o], writes=[sc["Bt"]])
            p.dma(sc["BT"][c].rearrange("q (g s) -> q g s", g=2), a8[:, 4:6, tok], reads=[a8], writes=[sc["BT"]])
            p.dma(sc["CT"][c].rearrange("q (g s) -> q g s", g=2), a8[:, 6:8, tok], reads=[a8], writes=[sc["CT"]])
    p.release(m)


def l1_ssd(p, io, sc):
    m = p.mark()
    triU = p.alloc([128, 128]); triL = p.alloc([128, 128]); ones = p.alloc([128, 128])
    p.dma(triU[:], io["triU"][:], writes=[triU]); p.dma(triL[:], io["triL"][:], writes=[triL])
    p.dma(ones[:], io["ones"][:], writes=[ones])
    Aneg = p.alloc([128, 16]); load_bcast(p, Aneg[:], io["alog"][0:1, :], [], [Aneg])
    e_act(p, Aneg[:], Aneg[:], AF.Exp, [Aneg], [Aneg])
    e_ts(p, "dve", Aneg[:], Aneg[:], -1.0, 0.0, ALU.mult, ALU.add, [Aneg], [Aneg])
    dsk = p.alloc([128, 8]); load_bcast(p, dsk[:], io["dsk"][0:1, :], [], [dsk])
    sng = p.alloc([128, 512]); load_bcast(p, sng[:], io["sng"][0:1, :], [], [sng])
    Wo = p.alloc([128, 4, D], BF16); load_w_bf16(p, Wo[:], io["outw"][:].rearrange("(k q) n -> q k n", q=128), [Wo])
    identb = p.identb
    nb = 2

    def mk():
        T = {}
        T["S"] = p.alloc([128, 512]); T["Sb"] = p.alloc([128, 512], BF16)
        T["X"] = [p.alloc([128, 512], BF16) for _ in range(nb)]; T["Bt"] = [p.alloc([128, 256], BF16) for _ in range(nb)]
        T["BT"] = [p.alloc([128, 2, 128], BF16) for _ in range(nb)]; T["CT"] = [p.alloc([128, 2, 128], BF16) for _ in range(nb)]
        T["dt"] = [p.alloc([128, 16]) for _ in range(nb)]
        T["da"] = p.alloc([128, 8]); T["dabc"] = p.alloc([128, 8, 128]); T["acs"] = p.alloc([128, 8]); T["ea"] = p.alloc([128, 8])
        T["wdec"] = p.alloc([128, 8]); T["dch"] = p.alloc([128, 8]); T["BCm"] = p.alloc([128, 2, 128], BF16)
        T["tmpH"] = [p.alloc([128, 4, 128]) for _ in range(2)]; T["exH"] = [p.alloc([128, 4, 128], BF16) for _ in range(2)]
        T["MH"] = [p.alloc([128, 4, 128], BF16) for _ in range(2)]
        T["xdt"] = p.alloc([128, 512], BF16); T["xw"] = p.alloc([128, 512], BF16); T["yt"] = [p.alloc([128, 512]) for _ in range(2)]
        return T

    TT = [mk(), mk()]
    orders = [list(range(NCH)), [1, 0] + list(range(NCH - 1, 1, -1))]
    ydst = [sc["yf"], sc["yb"]]

    def chunk(d, pos, c):
        T = TT[d]
        b_ = pos % nb
        islat = c >= 2
        tri = triU if d == 0 else triL
        S = T["S"]; Sb = T["Sb"]
        X = T["X"][b_]; Bt = T["Bt"][b_]; BT = T["BT"][b_]; CT = T["CT"][b_]; dt = T["dt"][b_]
        da = T["da"]; dabc = T["dabc"]; acs = T["acs"]; ea = T["ea"]; wdec = T["wdec"]; dch = T["dch"]; BCm = T["BCm"]
        xdt = T["xdt"]; xw = T["xw"]; yt = T["yt"][b_]
        p.dma(X[:], sc["X"][c], reads=[sc["X"]], writes=[X])
        p.dma(Bt[:], sc["Bt"][c], reads=[sc["Bt"]], writes=[Bt])
        p.dma(BT[:], sc["BT"][c].rearrange("q (g s) -> q g s", g=2), reads=[sc["BT"]], writes=[BT])
        p.dma(CT[:], sc["CT"][c].rearrange("q (g s) -> q g s", g=2), reads=[sc["CT"]], writes=[CT])
        p.dma(dt[:], sc["dts"][c], reads=[sc["dts"]], writes=[dt])
        dtd = dt[:, d * 8:(d + 1) * 8]
        if pos == 0:
            p.op("pool", lambda h: h.memset(S[:], 0.0), writes=[S])
            p.op("pool", lambda h: h.memset(Sb[:], 0.0), writes=[Sb])
        e_tt(p, "dve", da[:], dtd, Aneg[:, d * 8:(d + 1) * 8], ALU.mult, [dt, Aneg], [da])
        e_copy(p, "dve", dabc[:], da[:].unsqueeze(2).to_broadcast([128, 8, 128]), [da], [dabc])
        p0 = p.pb[0] if d == 0 else p.pb[7]
        e_mm(p, p0[:, 0:8], tri[:], da[:], True, True, [tri, da], [p0])
        e_mm(p, p0[:, 8:16], ones[:], da[:], True, True, [ones, da], [p0])
        e_copy(p, "dve", acs[:], p0[:, 0:8], [p0], [acs])
        e_act(p, ea[:], p0[:, 0:8], AF.Exp, [p0], [ea])
        e_tt(p, "dve", wdec[:], p0[:, 8:16], acs[:], ALU.subtract, [p0, acs], [wdec])
        e_act(p, wdec[:], wdec[:], AF.Exp, [wdec], [wdec])
        e_act(p, dch[:], p0[:, 8:16], AF.Exp, [p0], [dch])
        p1 = p.pb[1]
        for g in range(2):
            e_mm(p, p1[:, g * 128:(g + 1) * 128], BT[:, g, :], CT[:, g, :], True, True, [BT, CT], [p1])
        e_tt(p, "dve", BCm[:], p1[:, 0:256].rearrange("q (g s) -> q g s", g=2),
             tri[:].unsqueeze(1).to_broadcast([128, 2, 128]), ALU.mult, [p1, tri], [BCm])
        e_tt(p, "dve", xdt[:].rearrange("q (h e) -> q h e", h=8), X[:].rearrange("q (h e) -> q h e", h=8),
             dtd.unsqueeze(2).to_broadcast([128, 8, 64]), ALU.mult, [X, dt], [xdt])
        e_tt(p, "dve", xw[:].rearrange("q (h e) -> q h e", h=8), xdt[:].rearrange("q (h e) -> q h e", h=8),
             wdec[:].unsqueeze(2).to_broadcast([128, 8, 64]), ALU.mult, [xdt, wdec], [xw])
        if islat:
            pyd = p.pb[4]
            for hh in range(8):
                e_mm(p, p.pb[2 + hh // 4][:, (hh % 4) * 128:(hh % 4 + 1) * 128], dabc[:, hh, :], tri[:], True, True,
                     [dabc, tri], [p.pb[2 + hh // 4]])
            for half in range(2):
                pr = p.pb[2 + half]; t_ = T["tmpH"][half]; e_ = T["exH"][half]; M = T["MH"][half]
                e_tt(p, "dve", t_[:], pr[:].rearrange("q (h s) -> q h s", h=4),
                     acs[:, 4 * half:4 * half + 4].unsqueeze(2).to_broadcast([128, 4, 128]), ALU.subtract, [pr, acs], [t_])
                e_ts(p, "dve", t_[:], t_[:], 0.0, 0.0, ALU.min, ALU.add, [t_], [t_])
                e_act(p, e_[:], t_[:], AF.Exp, [t_], [e_])
                e_tt(p, "dve", M[:], e_[:], BCm[:, half, :].unsqueeze(1).to_broadcast([128, 4, 128]), ALU.mult, [e_, BCm], [M])
                for h4 in range(4):
                    hh = 4 * half + h4
                    e_mm(p, pyd[:, hh * 64:(hh + 1) * 64], M[:, h4, :], xdt[:, hh * 64:(hh + 1) * 64], True, True, [M, xdt], [pyd])
            pyo = p.pb[5]
            for g in range(2):
                e_mm(p, pyo[:, g * 256:(g + 1) * 256], CT[:, g, :], Sb[:, g * 256:(g + 1) * 256], True, True, [CT, Sb], [pyo])
            e_tt(p, "dve", yt[:].rearrange("q (h e) -> q h e", h=8), pyo[:].rearrange("q (h e) -> q h e", h=8),
                 ea[:].unsqueeze(2).to_broadcast([128, 8, 64]), ALU.mult, [pyo, ea], [yt])
            e_tt(p, "dve", yt[:], yt[:], pyd[:], ALU.add, [yt, pyd], [yt])
            p.dma(ydst[d][c], yt[:], reads=[yt], writes=[ydst[d]])
        pst = p.pb[6]
        for g in range(2):
            e_mm(p, pst[:, g * 256:(g + 1) * 256], Bt[:, g * 128:(g + 1) * 128], xw[:, g * 256:(g + 1) * 256],
                 True, True, [Bt, xw], [pst])
        e_tt(p, "dve", S[:].rearrange("q (h e) -> q h e", h=8), S[:].rearrange("q (h e) -> q h e", h=8),
             dch[:].unsqueeze(2).to_broadcast([128, 8, 64]), ALU.mult, [S, dch], [S])
        e_tt(p, "dve", S[:], S[:], pst[:], ALU.add, [S, pst], [S])
        e_copy(p, "act", Sb[:], S[:], [S], [Sb])

    for pos in range(NCH):
        chunk(0, pos, orders[0][pos])
        chunk(1, pos, orders[1][pos])

    Xc = [p.alloc([128, 512], BF16) for _ in range(nb)]; Zc = [p.alloc([128, 512], BF16) for _ in range(nb)]
    yfc = [p.alloc([128, 512]) for _ in range(nb)]; ybc = [p.alloc([128, 512]) for _ in range(nb)]
    v = [p.alloc([128, 512]) for _ in range(nb)]; vb = p.alloc([128, 512], BF16); vT = p.alloc([128, 4, 128], BF16)
    junk = p.alloc([128, 512]); orow = [p.alloc([128, RSW]) for _ in range(2)]
    for c in range(2, NCH):
        b_ = c % nb
        X = Xc[b_]; Z = Zc[b_]; yf = yfc[b_]; yb = ybc[b_]; vv = v[b_]; o = orow[b_]
        p.dma(X[:], sc["X"][c], reads=[sc["X"]], writes=[X])
        p.dma(Z[:], sc["Z"][c], reads=[sc["Z"]], writes=[Z])
        p.dma(yf[:], sc["yf"][c], reads=[sc["yf"]], writes=[yf])
        p.dma(yb[:], sc["yb"][c], reads=[sc["yb"]], writes=[yb])
        e_tt(p, "dve", vv[:].rearrange("q (h e) -> q h e", h=8), X[:].rearrange("q (h e) -> q h e", h=8),
             dsk[:].unsqueeze(2).to_broadcast([128, 8, 64]), ALU.mult, [X, dsk], [vv])
        e_tt(p, "dve", yf[:], yf[:], yb[:], ALU.add, [yf, yb], [yf])
        e_tt(p, "dve", vv[:], vv[:], yf[:], ALU.add, [vv, yf], [vv])
        e_tt(p, "dve", vv[:], vv[:], Z[:], ALU.mult, [vv, Z], [vv])
        e_act(p, junk[:], vv[:], AF.Square, [vv], [junk, o], accum_out=o[:, 1024:1025])
        e_tt(p, "dve", vb[:], vv[:], sng[:], ALU.mult, [vv, sng], [vb])
        pt = p.pb[c % 2]; ptb = pt.t.bitcast(BF16)
        for k_ in range(4):
            e_tr(p, ptb[:, k_ * 128:(k_ + 1) * 128], vb[:, k_ * 128:(k_ + 1) * 128], identb[:], [vb, identb], [pt])
        e_copy(p, "act", vT[:], ptb[:, 0:512].rearrange("q (k s) -> q k s", k=4), [pt], [vT])
        for dh in range(2):
            pp = p.pb[2 + (2 * c + dh) % 4]
            for k_ in range(4):
                e_mm(p, pp[:], vT[:, k_, :], Wo[:, k_, dh * 512:(dh + 1) * 512], k_ == 0, k_ == 3, [vT, Wo], [pp])
            e_copy(p, "act", o[:, dh * 512:(dh + 1) * 512], pp[:], [pp], [o])
        p.op("pool", lambda h, o=o: h.memset(o[:, 1025:RSW], 0.0), writes=[o])
        w = c - 2
        p.dma(sc["rsrc"][w * 128:(w + 1) * 128, :], o[:], reads=[o], writes=[sc["rsrc"]])
    p.release(m)


def l1_tail(p, io, sc, xg, onehot, ident, out_dram):
    modsc = sc["modsc1"]
    gates = p.alloc([128, 16, 16]); h2T = p.alloc([128, 8, NL], BF16)
    m = p.mark()
    n2 = p.alloc([128, D]); load_bcast(p, n2[:], io["n2g1"][0:1, :], [], [n2])
    g1 = p.alloc([128, D]); a2 = p.alloc([128, D]); s2 = p.alloc([128, D])
    load_bcast(p, g1[:], modsc[0:1, 2 * D:3 * D], [modsc], [g1])
    load_bcast(p, s2[:], modsc[0:1, 3 * D:4 * D], [modsc], [s2])
    load_bcast(p, a2[:], modsc[0:1, 4 * D:5 * D], [modsc], [a2])
    e_stt(p, a2[:], a2[:], 1.0, n2[:], ALU.add, ALU.mult, [a2, n2], [a2])
    rw = p.alloc([128, 8, 16]); p.dma(rw[:], io["rw"][:].rearrange("(k q) n -> q k n", q=128), writes=[rw])
    rb = p.alloc([128, 16]); load_bcast(p, rb[:], io["rb"][0:1, :], [], [rb])
    rt = [p.alloc([128, RSW]) for _ in range(2)]
    xq = [p.alloc([128, D]) for _ in range(2)]; xa = p.alloc([128, D]); x1 = [p.alloc([128, D]) for _ in range(2)]
    hn2 = p.alloc([128, D]); junk = p.alloc([128, D]); hT32 = p.alloc([128, 8, 128]); sm = p.alloc([128, 128])
    ss = p.alloc([128, 1]); rs = p.alloc([128, 1]); rsd = p.alloc([128, 1])
    xgv = xg[:].rearrange("(r w) d -> w r d", w=64)
    iq = 0
    for t in range(16):
        r_ = rt[t % 2]; xo = x1[t % 2]
        p.dma(r_[:], sc["rdst"][t * 128:(t + 1) * 128, :], reads=[sc["rdst"]], writes=[r_])
        for j in range(4):
            xj = xq[iq % 2]; iq += 1
            p.dma(xj[:], xgv[16 * j + t], reads=[xg], writes=[xj])
            if j == 0:
                e_ts(p, "dve", xa[:], xj[:], onehot[:, 0:1], 0.0, ALU.mult, ALU.add, [xj, onehot], [xa])
            else:
                e_stt(p, xa[:], xj[:], onehot[:, j:j + 1], xa[:], ALU.mult, ALU.add, [xj, onehot, xa], [xa])
        e_ts(p, "dve", rsd[:], r_[:, 1024:1025], 1.0 / 2048, 1e-6, ALU.mult, ALU.add, [r_], [rsd])
        e_act(p, rsd[:], rsd[:], AF.Sqrt, [rsd], [rsd])
        p.op("dve", lambda h: h.reciprocal(out=rsd[:], in_=rsd[:]), reads=[rsd], writes=[rsd])
        e_stt(p, xo[:], r_[:, 0:D], rsd[:], g1[:], ALU.mult, ALU.mult, [r_, rsd, g1], [xo])
        e_tt(p, "dve", xo[:], xo[:], xa[:], ALU.add, [xo, xa], [xo])
        tok = slice(t * 128, (t + 1) * 128)
        p.dma(sc["x1b"][tok, :], xo[:], reads=[xo], writes=[sc["x1b"]])
        e_act(p, junk[:], xo[:], AF.Square, [xo], [junk, ss], accum_out=ss[:])
        e_ts(p, "dve", rs[:], ss[:], 1.0 / D, 1e-6, ALU.mult, ALU.add, [ss], [rs])
        e_act(p, rs[:], rs[:], AF.Sqrt, [rs], [rs])
        p.op("dve", lambda h: h.reciprocal(out=rs[:], in_=rs[:]), reads=[rs], writes=[rs])
        e_stt(p, hn2[:], xo[:], rs[:], a2[:], ALU.mult, ALU.mult, [xo, rs, a2], [hn2])
        e_tt(p, "dve", hn2[:], hn2[:], s2[:], ALU.add, [hn2, s2], [hn2])
        for half in range(2):
            ps = p.pb[4 + half]
            for c4 in range(4):
                ct = half * 4 + c4
                e_tr(p, ps[:, c4 * 128:(c4 + 1) * 128], hn2[:, ct * 128:(ct + 1) * 128], ident[:], [hn2, ident], [ps])
            pv = ps[:].rearrange("q (c n) -> q c n", c=4)
            e_copy(p, "act", h2T[:, half * 4:(half + 1) * 4, tok], pv, [ps], [h2T])
            e_copy(p, "dve", hT32[:, half * 4:(half + 1) * 4, :], pv, [ps], [hT32])
        pl = p.pb[6]
        for k in range(8):
            e_mm(p, pl[:, 0:16], hT32[:, k, :], rw[:, k, :], k == 0, k == 7, [hT32, rw], [pl])
        routing(p, pl, rb, sm, gates[:, t, :], gates)
    p.release(m)
    fg = p.alloc([128, D]); load_bcast(p, fg[:], io["fing"][0:1, :], [], [fg])
    junk2 = p.alloc([128, D]); ss2 = p.alloc([128, 1]); rs2 = p.alloc([128, 1])

    def out_cb(t, x):
        e_act(p, junk2[:], x[:], AF.Square, [x], [junk2, ss2], accum_out=ss2[:])
        e_ts(p, "dve", rs2[:], ss2[:], 1.0 / D, 1e-6, ALU.mult, ALU.add, [ss2], [rs2])
        e_act(p, rs2[:], rs2[:], AF.Sqrt, [rs2], [rs2])
        p.op("dve", lambda h: h.reciprocal(out=rs2[:], in_=rs2[:]), reads=[rs2], writes=[rs2])
        e_stt(p, x[:], x[:], rs2[:], fg[:], ALU.mult, ALU.mult, [x, rs2, fg], [x])
        p.dma(out_dram[t * 128:(t + 1) * 128, :], x[:], reads=[x], writes=[out_dram])

    phase_moe(p, io, (io["w1b"], io["w3b"], io["w2b"]), modsc, h2T, gates, sc["x1b"], 16, out_cb, lambda t: 0)


def layer1(p, io, sc, xg, ctx1, ident, onehot, out_dram, dbg=None):
    p.identb = p.alloc([128, 128], BF16)
    e_copy(p, "dve", p.identb[:], ident[:], [ident], [p.identb])
    phase_mod(p, io["cT"], io["modw1"], io["modb1"], sc["modsc1"], ident)
    l1_proj(p, io, sc, xg, ctx1, ident)
    l1_conv(p, io, sc, p.identb)
    l1_ssd(p, io, sc)
    p.cc(lambda h: h.collective_compute("ReduceScatter", ALU.add, replica_groups=[[0, 1, 2, 3], [4, 5, 6, 7]],
                                        ins=[sc["rsrc"].t.opt()], outs=[sc["rdst"].t.opt()]),
         reads=[sc["rsrc"]], writes=[sc["rdst"]])
    l1_tail(p, io, sc, xg, onehot, ident, out_dram)


def host_inputs_l1(z):
    i = 1
    maps = []
    inw = z["ssd_in_w"][0]
    for k in range(8):
        bb = k // 4; j = k % 4
        cT = np.stack([z["c"][bb], z["c_ctx"]], axis=-1).reshape(8, 128, 2).transpose(1, 0, 2)
        wz = inw[:, 512 * j:512 * (j + 1)]
        wx = inw[:, 2048 + 512 * j:2048 + 512 * (j + 1)]
        wB = inw[:, 4096 + 256 * j:4096 + 256 * (j + 1)]
        wC = inw[:, 5120 + 256 * j:5120 + 256 * (j + 1)]
        wdt = np.concatenate([inw[:, 6144 + 8 * j:6144 + 8 * (j + 1)], inw[:, 6176 + 8 * j:6176 + 8 * (j + 1)]], 1)
        chs = np.concatenate([np.arange(512 * j, 512 * (j + 1)), 2048 + np.arange(256 * j, 256 * (j + 1)),
                              3072 + np.arange(256 * j, 256 * (j + 1))])
        cw = z["ssd_conv_w"][0][:, chs]
        convw = cw.T.reshape(8, 128, 5).transpose(1, 0, 2)
        convb = z["ssd_conv_b"][0][chs].reshape(8, 128).T
        hs = slice(8 * j, 8 * (j + 1))
        dtb = np.concatenate([z["ssd_dt_bias"][0, 0, hs], z["ssd_dt_bias"][0, 1, hs]])[None]
        alog = np.concatenate([z["ssd_a_log"][0, 0, hs], z["ssd_a_log"][0, 1, hs]])[None]
        oh = np.zeros((128, 4), np.float32); oh[:, j] = 1
        maps.append(dict(
            cT=np.ascontiguousarray(cT), modw1=z["mod_w"][i], modb1=z["mod_b"][i][None], n1g1=z["norm1_g"][i][None],
            n2g1=z["norm2_g"][i][None], fing=z["final_g"][None], wz=np.ascontiguousarray(wz),
            wxbc=np.ascontiguousarray(np.concatenate([wx, wB, wC], 1)), wdt=np.ascontiguousarray(wdt),
            convw=np.ascontiguousarray(convw), convb=np.ascontiguousarray(convb), dtb=np.ascontiguousarray(dtb),
            alog=np.ascontiguousarray(alog), dsk=z["ssd_d"][0][hs][None].copy(),
            sng=z["ssd_norm_g"][0][512 * j:512 * (j + 1)][None].copy(),
            outw=np.ascontiguousarray(z["ssd_out_w"][0][512 * j:512 * (j + 1), :]),
            triU=np.triu(np.ones((128, 128), np.float32)), triL=np.tril(np.ones((128, 128), np.float32)),
            ones=np.ones((128, 128), np.float32), rw=z["router_w"], rb=z["router_b"][None],
            w1b=z["moe_w1"][i], w3b=z["moe_w3"][i], w2b=z["moe_w2"][i],
            ident=np.eye(128, dtype=np.float32), onehot=oh))
    return maps


def colmajor(xb):
    return xb.reshape(128, 64, -1).transpose(1, 0, 2).reshape(8192, -1)


def build_full():
    p = Prog(); p.init_pool()
    io = {}
    for name, shape in L0_INPUTS + L1_INPUTS:
        if name not in io:
            io[name] = p.dram(name, shape)
    out = p.dram("out", [2048, D], kind="ExternalOutput")
    sc0 = dict(modsc=p.dram("modsc", [2, 6144], kind="Internal"),
               tabsc=p.dram("tabsc", [64, 2, 128, 2048], kind="Internal"),
               fsrc=p.dram("fsrc", [128, 128], kind="Internal"), fdst=p.dram("fdst", [512, 128], kind="Internal"),
               x1sc=p.dram("x1sc", [NT, D], kind="Internal"))
    xsrcs = [p.dram(f"xsrc{a}", [128, 2048], kind="Internal") for a in range(8)]
    xdsts = [p.dram(f"xdst{a}", [512, 2048], kind="Internal") for a in range(8)]
    xg = p.dram("xg", [8192, D], kind="Internal")
    ctx1 = p.dram("ctx1", [NCX, D], kind="Internal")
    sc1 = l1_scratch(p)
    ident = p.alloc([128, 128]); p.dma(ident[:], io["ident"][:], writes=[ident])
    onehot = p.alloc([128, 4]); p.dma(onehot[:], io["onehot"][:], writes=[onehot])
    m0 = p.mark()

    def out_cb(t, x):
        if t < 2:
            p.dma(ctx1[t * 128:(t + 1) * 128, :], x[:], reads=[x], writes=[ctx1])
        else:
            a = (t - 2) // 2; half = (t - 2) % 2
            dst = xsrcs[a][:].rearrange("i (b d) -> (i b) d", d=D)[half * 128:(half + 1) * 128, :]
            p.dma(dst, x[:], reads=[x], writes=[xsrcs[a]])

    layer0(p, io, sc0, ident, onehot, out_cb)
    p.release(m0)
    xgv = xg[:].rearrange("(r a t) d -> a r t d", r=4, a=8)
    for a in range(8):
        p.cc(lambda h, a=a: h.collective_compute("AllGather", ALU.bypass, replica_groups=[[0, 1, 2, 3], [4, 5, 6, 7]],
                                                 ins=[xsrcs[a].t.opt()], outs=[xdsts[a].t.opt()]),
             reads=[xsrcs[a]], writes=[xdsts[a]])
        p.dma(xgv[a], xdsts[a][:].rearrange("(r i) (b d) -> r (i b) d", r=4, d=D), reads=[xdsts[a]], writes=[xg])
    layer1(p, io, sc1, xg, ctx1, ident, onehot, out)
    p.wait_all()
    return p


def kernel(**inputs):
    z = {k: np.asarray(v) for k, v in inputs.items()}
    m0 = host_inputs_l0(z)
    m1 = host_inputs_l1(z)
    maps = []
    for k in range(8):
        d = dict(m0[k]); d.update(m1[k])
        maps.append({kk: np.ascontiguousarray(vv, dtype=np.float32) for kk, vv in d.items()})
    p = build_full()
    nc = p.finish()
    res = run_bass_kernel_spmd(nc, maps, core_ids=list(range(8))).results
    outp = np.zeros((2, 8192, D), np.float32)
    for bb in range(2):
        cm = np.concatenate([res[4 * bb + j]["out"] for j in range(4)], 0)
        outp[bb] = cm.reshape(64, 128, D).transpose(1, 0, 2).reshape(8192, D)
    return outp
```

```python
import math, time, sys
import numpy as np
import contextlib
import concourse.bass as bass
import concourse.mybir as mybir
from concourse.bass_utils import run_bass_kernel_spmd

F32 = mybir.dt.float32
BF16 = mybir.dt.bfloat16
AF = mybir.ActivationFunctionType
ALU = mybir.AluOpType
AX = mybir.AxisListType

ENGS = ("pe", "dve", "act", "pool", "sp")


class Buf:
    def __init__(self, t, name, excl=False):
        self.t = t
        self.name = name
        self.excl = excl
        self.w = None
        self.r = []

    def __getitem__(self, k):
        return self.t[k]


class Prog:
    def __init__(self, name="k"):
        self.nc = bass.Bass("TRN2", target_bir_lowering=False)
        self.es = contextlib.ExitStack()
        self.q = {e: [] for e in ENGS}
        self.cnt = {e: 0 for e in ENGS}
        self.known = {e: {} for e in ENGS}
        self.sems = {}
        self.dcnt = {}
        self.mult = {}
        self.nbuf = 0
        self.sb_bytes = 0
        for e in ENGS:
            self.sems[e] = self.es.enter_context(self.nc.semaphore("s_" + e))
        self.NDS = 48
        self.rr = 0
        for i in range(self.NDS):
            nm = f"dq{i}"
            self.sems[nm] = self.es.enter_context(self.nc.semaphore(nm))
            self.dcnt[nm] = 0
            self.mult[nm] = 16

    def dram(self, name, shape, dtype=F32, kind="ExternalInput"):
        t = self.nc.dram_tensor(name, list(shape), dtype, kind=kind)
        b = Buf(t.ap(), name)
        b.is_dram = True
        return b

    def sb(self, shape, dtype=F32, name=None):
        self.nbuf += 1
        name = name or f"sb{self.nbuf}"
        t = self.es.enter_context(self.nc.sbuf_tensor(name, list(shape), dtype))
        sz = int(np.prod(shape[1:])) * (4 if dtype == F32 else 2)
        self.sb_bytes += sz
        return Buf(t, name)

    def init_pool(self, words=48 * 1024):
        self.big = self.es.enter_context(self.nc.sbuf_tensor("big", [128, words], F32))
        self.words = words
        self.top = 0
        self.hi = words
        self.peak = 0
        self.banks = [self.es.enter_context(self.nc.psum_tensor(f"bank{i}", [128, 512], F32)) for i in range(8)]
        self.pb = [Buf(self.banks[i][:], f"bank{i}", excl=True) for i in range(8)]

    def alloc(self, shape, dtype=F32, name=None, hi=False):
        n = int(np.prod(shape[1:]))
        w = n if dtype == F32 else (n + 1) // 2
        assert self.top + w <= self.hi, f"SBUF overflow {self.top}+{w} > {self.hi} ({name})"
        if hi:
            self.hi -= w
            ap = self.big[:, self.hi:self.hi + w]
        else:
            ap = self.big[:, self.top:self.top + w]
            self.top += w
        self.peak = max(self.peak, self.top + (self.words - self.hi))
        if dtype != F32:
            ap = ap.bitcast(dtype)[:, 0:n]
        if len(shape) > 2:
            names = " ".join(f"d{i}" for i in range(1, len(shape)))
            kw = {f"d{i}": shape[i] for i in range(1, len(shape))}
            ap = ap.rearrange(f"p ({names}) -> p {names}", **kw)
        if shape[0] < 128:
            ap = ap[0:shape[0]]
        self.nbuf += 1
        return Buf(ap, name or f"a{self.nbuf}")


    def barrier(self):
        snap_c = dict(self.cnt)
        snap_d = dict(self.dcnt)
        for e in ENGS:
            for src, n in snap_c.items():
                if src != e and n > self.known[e].get(src, 0):
                    self.q[e].append(("wait", src, n))
                    self.known[e][src] = n
            for s_, n in snap_d.items():
                if n > self.known[e].get(s_, 0):
                    self.q[e].append(("wait", s_, n * self.mult[s_]))
                    self.known[e][s_] = n

    def mark(self):
        return self.top

    def release(self, m):
        self.barrier()
        self.top = m

    def ps(self, shape, dtype=F32, name=None):
        self.nbuf += 1
        name = name or f"ps{self.nbuf}"
        t = self.es.enter_context(self.nc.psum_tensor(name, list(shape), dtype))
        return Buf(t, name)

    def stream(self, s):
        if s not in self.sems:
            self.sems[s] = self.es.enter_context(self.nc.semaphore("d_" + s))
            self.dcnt[s] = 0
            self.mult[s] = 16
        return s

    def _deps(self, eng, reads, writes):
        deps = {}

        def add(x):
            if x is None:
                return
            src, n = x
            if deps.get(src, 0) < n:
                deps[src] = n

        for b in reads:
            add(b.w)
            if b.excl:
                for x in b.r:
                    if x[0] != eng:
                        add(x)
        for b in writes:
            add(b.w)
            for x in b.r:
                add(x)
        for src, n in deps.items():
            if src == eng and eng == "pe":
                continue
            if self.known[eng].get(src, 0) >= n:
                continue
            self.known[eng][src] = n
            mult = self.mult[src] if src in self.dcnt else 1
            self.q[eng].append(("wait", src, n * mult))

    def _mark(self, tag, reads, writes):
        for b in reads:
            b.r.append(tag)
        for b in writes:
            b.w = tag
            b.r = []

    def op(self, eng, fn, reads=(), writes=()):
        self._deps(eng, reads, writes)
        self.cnt[eng] += 1
        self.q[eng].append(("op", fn))
        self._mark((eng, self.cnt[eng]), reads, writes)

    def dma(self, out, in_, reads=(), writes=(), eng="sp", stream=None, **kw):
        s_ = f"dq{self.rr % self.NDS}"
        self.rr += 1
        if eng == "sp" and any(not getattr(b, "is_dram", False) for b in reads):
            eng = "pool"
        self._deps(eng, reads, writes)
        prev = self.dcnt[s_]
        if prev and self.known[eng].get(s_, 0) < prev:
            self.q[eng].append(("wait", s_, prev * 16))
            self.known[eng][s_] = prev
        self.dcnt[s_] += 1
        self.q[eng].append(("dma", s_, out, in_, kw))
        self._mark((s_, self.dcnt[s_]), reads, writes)

    def cc(self, fn, reads=(), writes=(), eng="pool", stream="cc"):
        self.stream(stream)
        self.mult[stream] = 1
        self._deps(eng, reads, writes)
        prev = self.dcnt[stream]
        if prev and self.known[eng].get(stream, 0) < prev:
            self.q[eng].append(("wait", stream, prev))
            self.known[eng][stream] = prev
        self.dcnt[stream] += 1
        self.q[eng].append(("cc", stream, fn))
        self._mark((stream, self.dcnt[stream]), reads, writes)

    def wait_all(self, eng="sp"):
        for s, n in self.dcnt.items():
            if n:
                self.q[eng].append(("wait", s, self.mult[s] * n))
        for e in ENGS:
            if e != eng and self.cnt[e]:
                self.q[eng].append(("wait", e, self.cnt[e]))

    def finish(self):
        nc = self.nc
        sems = self.sems

        def replay(e):
            def body(h):
                for item in self.q[e]:
                    if item[0] == "wait":
                        h.wait_ge(sems[item[1]], item[2])
                    elif item[0] == "op":
                        item[1](h).then_inc(sems[e], 1)
                    elif item[0] == "cc":
                        item[2](h).then_inc(sems[item[1]], 1)
                    else:
                        _, s, out, in_, kw = item
                        h.dma_start(out=out, in_=in_, **kw).then_inc(sems[s], 16)
            return body

        with nc.Block() as block:
            block.tensor(replay("pe"))
            block.vector(replay("dve"))
            block.scalar(replay("act"))
            block.gpsimd(replay("pool"))
            block.sync(replay("sp"))
        self.es.close()
        return nc


def run(prog, in_maps):
    nc = prog.finish()
    res = run_bass_kernel_spmd(nc, in_maps, core_ids=list(range(len(in_maps))))
    return res.results


def _bufs(xs):
    return [x for x in xs if isinstance(x, Buf)]


def e_act(p, out, in_, func, r, w, eng="act", **kw):
    p.op(eng, lambda h: h.activation(out=out, in_=in_, func=func, **kw), reads=r, writes=w)


def e_tt(p, eng, out, in0, in1, op, r, w):
    p.op(eng, lambda h: h.tensor_tensor(out=out, in0=in0, in1=in1, op=op), reads=r, writes=w)


def e_ts(p, eng, out, in0, s1, s2, op0, op1, r, w):
    if op1 is None:
        if op0 == ALU.add:
            op1, s2 = ALU.mult, 1.0
        else:
            op1, s2 = ALU.add, 0.0
    if True:
        p.op(eng, lambda h: h.tensor_scalar(out=out, in0=in0, scalar1=s1, scalar2=s2, op0=op0, op1=op1), reads=r, writes=w)


def e_stt(p, out, in0, scalar, in1, op0, op1, r, w):
    p.op("dve", lambda h: h.scalar_tensor_tensor(out=out, in0=in0, scalar=scalar, in1=in1, op0=op0, op1=op1),
         reads=r, writes=w)


def e_copy(p, eng, out, in_, r, w):
    if eng == "act":
        p.op(eng, lambda h: h.activation(out=out, in_=in_, func=AF.Copy), reads=r, writes=w)
    else:
        p.op(eng, lambda h: h.tensor_copy(out=out, in_=in_), reads=r, writes=w)


def e_mm(p, out, lhsT, rhs, start, stop, r, w):
    p.op("pe", lambda h: h.matmul(out, lhsT=lhsT, rhs=rhs, start=start, stop=stop), reads=r, writes=w)


def e_tr(p, out, in_, ident, r, w):
    p.op("pe", lambda h: h.transpose(out, in_, ident), reads=r, writes=w)


def rev_ap(ap):
    (pst, pn), (st, n) = ap.ap
    return bass.AP(ap.tensor, ap.offset + (n - 1) * st, [[pst, pn], [-st, n]])


D = 1024
NL = 2048
NCX = 256
NT = NL + NCX
PI = math.pi


def phase_mod(p, cT, modw, modb, modsc, ident):
    m = p.mark()
    c_sb = p.alloc([128, 8, 2]); s_sb = p.alloc([128, 8, 2])
    p.dma(c_sb[:], cT[:], writes=[c_sb])
    e_act(p, s_sb[:], c_sb[:], AF.Silu, [c_sb], [s_sb])
    b_sb = p.alloc([1, 6144]); ones = p.alloc([1, 2])
    p.dma(b_sb[:], modb[:], writes=[b_sb])
    p.op("pool", lambda h: h.memset(ones[:], 1.0), writes=[ones])
    wb = [p.alloc([128, 8, 512]) for _ in range(2)]
    ob = [p.alloc([2, 512]) for _ in range(2)]
    for nb in range(12):
        w = wb[nb % 2]; o = ob[nb % 2]; ps = p.pb[nb % 2]
        p.dma(w[:], modw[:, nb * 512:(nb + 1) * 512].rearrange("(k q) n -> q k n", q=128), writes=[w])
        for k in range(8):
            e_mm(p, ps[0:2, :], s_sb[:, k, :], w[:, k, :], k == 0, False, [s_sb, w], [ps])
        e_mm(p, ps[0:2, :], ones[:], b_sb[:, nb * 512:(nb + 1) * 512], False, True, [ones, b_sb], [ps])
        e_copy(p, "act", o[:], ps[0:2, :], [ps], [o])
        p.dma(modsc[:, nb * 512:(nb + 1) * 512], o[:], reads=[o], writes=[modsc], stream="st")
    p.release(m)


def load_bcast(p, dst, src_row_ap, r, w):
    p.dma(dst, src_row_ap.to_broadcast([128, src_row_ap.shape[-1]]), reads=r, writes=w)


def phase_hn(p, xl, xc, modsc, gvec, ident, uT, part_sh, part_sc):
    m = p.mark()
    g_sb = p.alloc([128, D]); A = [p.alloc([128, D]) for _ in range(2)]; SH = [p.alloc([128, D]) for _ in range(2)]
    load_bcast(p, g_sb[:], gvec[0:1, :], [], [g_sb])
    for which in range(2):
        load_bcast(p, A[which][:], modsc[which:which + 1, part_sc * D:(part_sc + 1) * D], [modsc], [A[which]])
        load_bcast(p, SH[which][:], modsc[which:which + 1, part_sh * D:(part_sh + 1) * D], [modsc], [SH[which]])
        e_stt(p, A[which][:], A[which][:], 1.0, g_sb[:], ALU.add, ALU.mult, [A[which], g_sb], [A[which]])
    nbuf = 2
    xs = [p.alloc([128, D]) for _ in range(nbuf)]; ys = [p.alloc([128, D]) for _ in range(nbuf)]
    junk = p.alloc([128, D]); ss = [p.alloc([128, 1]) for _ in range(nbuf)]; rs = [p.alloc([128, 1]) for _ in range(nbuf)]
    for t in range(18):
        which = 1 if t < 2 else 0
        src = xc[t * 128:(t + 1) * 128, :] if t < 2 else xl[(t - 2) * 128:(t - 1) * 128, :]
        x = xs[t % nbuf]; y = ys[t % nbuf]; s = ss[t % nbuf]; r = rs[t % nbuf]
        p.dma(x[:], src, writes=[x])
        e_act(p, junk[:], x[:], AF.Square, [x], [junk, s], accum_out=s[:])
        e_ts(p, "dve", r[:], s[:], 1.0 / D, 1e-6, ALU.mult, ALU.add, [s], [r])
        e_act(p, r[:], r[:], AF.Ln, [r], [r])
        e_act(p, r[:], r[:], AF.Exp, [r], [r], scale=-0.5)
        e_stt(p, y[:], x[:], r[:], A[which][:], ALU.mult, ALU.mult, [x, r, A[which]], [y])
        e_tt(p, "dve", y[:], y[:], SH[which][:], ALU.add, [y, SH[which]], [y])
        for half in range(2):
            ps = p.pb[(2 * t + half) % 4]
            for c4 in range(4):
                ct = half * 4 + c4
                e_tr(p, ps[:, c4 * 128:(c4 + 1) * 128], y[:, ct * 128:(ct + 1) * 128], ident[:], [y, ident], [ps])
            e_copy(p, "act", uT[:, half * 4:(half + 1) * 4, t * 128:(t + 1) * 128],
                   ps[:].rearrange("q (c n) -> q c n", c=4), [ps], [uT])
    p.release(m)


def cmul(p, eng, outr, outi, ar, ai, br, bi, t1, t2, bufs_r, bufs_w):
    e_tt(p, eng, t1, ar, br, ALU.mult, bufs_r, bufs_w)
    e_tt(p, eng, t2, ai, bi, ALU.mult, bufs_r, bufs_w)
    e_tt(p, eng, outr, t1, t2, ALU.subtract, bufs_r, bufs_w)
    e_tt(p, eng, t1, ar, bi, ALU.mult, bufs_r, bufs_w)
    e_tt(p, eng, t2, ai, br, ALU.mult, bufs_r, bufs_w)
    e_tt(p, eng, outi, t1, t2, ALU.add, bufs_r, bufs_w)


class S5:
    pass


def s5_params(p, io, ident):
    S = S5()
    lam = p.alloc([128, 2, 64]); ldt = p.alloc([128, 64])
    p.dma(lam[:], io["lamP"][:], writes=[lam]); p.dma(ldt[:], io["ldtP"][:], writes=[ldt])
    lr = lam[:, 0, :]; li = lam[:, 1, :]
    W = p.alloc([128, 10, 64], name="s5w")
    sl = lambda i: W[:, i, :]
    R = [W]
    step, th, mag, t1, t2, cs, sn, den, am1 = (sl(i) for i in range(9))
    S.ar = p.alloc([128, 64]); S.ai = p.alloc([128, 64]); S.cth = p.alloc([128, 64]); S.sth = p.alloc([128, 64])
    S.mag = p.alloc([128, 64]); S.qr = p.alloc([128, 64]); S.qi = p.alloc([128, 64])
    e_act(p, step, ldt[:], AF.Exp, [ldt], R)
    e_tt(p, "dve", th, li, step, ALU.mult, [lam, W], R)
    e_tt(p, "dve", t1, lr, step, ALU.mult, [lam, W], R)
    e_act(p, S.mag[:], t1, AF.Exp, R, [S.mag])
    e_act(p, sn, th, AF.Sin, R, R, scale=1.0 / 16)
    e_ts(p, "dve", t1, th, 1.0 / 16, PI / 2, ALU.mult, ALU.add, R, R)
    e_act(p, cs, t1, AF.Sin, R, R)
    for _ in range(4):
        e_tt(p, "dve", t1, cs, cs, ALU.mult, R, R)
        e_tt(p, "dve", t2, sn, sn, ALU.mult, R, R)
        e_tt(p, "dve", sn, sn, cs, ALU.mult, R, R)
        e_ts(p, "dve", sn, sn, 2.0, None, ALU.mult, None, R, R)
        e_tt(p, "dve", cs, t1, t2, ALU.subtract, R, R)
    e_copy(p, "dve", S.cth[:], cs, R, [S.cth]); e_copy(p, "dve", S.sth[:], sn, R, [S.sth])
    e_tt(p, "dve", S.ar[:], S.mag[:], cs, ALU.mult, R + [S.mag], [S.ar])
    e_tt(p, "dve", S.ai[:], S.mag[:], sn, ALU.mult, R + [S.mag], [S.ai])
    e_tt(p, "dve", t1, lr, lr, ALU.mult, [lam], R)
    e_tt(p, "dve", t2, li, li, ALU.mult, [lam], R)
    e_tt(p, "dve", den, t1, t2, ALU.add, R, R)
    p.op("dve", lambda h: h.reciprocal(out=den, in_=den), reads=R, writes=R)
    e_ts(p, "dve", am1, S.ar[:], -1.0, None, ALU.add, None, [S.ar], R)
    e_tt(p, "dve", t1, am1, lr, ALU.mult, R + [lam], R)
    e_tt(p, "dve", t2, S.ai[:], li, ALU.mult, [S.ai, lam], R)
    e_tt(p, "dve", t1, t1, t2, ALU.add, R, R)
    e_tt(p, "dve", S.qr[:], t1, den, ALU.mult, R, [S.qr])
    e_tt(p, "dve", t1, S.ai[:], lr, ALU.mult, [S.ai, lam], R)
    e_tt(p, "dve", t2, am1, li, ALU.mult, R + [lam], R)
    e_tt(p, "dve", t1, t1, t2, ALU.subtract, R, R)
    e_tt(p, "dve", S.qi[:], t1, den, ALU.mult, R, [S.qi])
    S.wc = p.alloc([128, 11, 64]); S.ws = p.alloc([128, 11, 64])
    e_copy(p, "dve", S.wc[:, 0, :], S.cth[:], [S.cth], [S.wc]); e_copy(p, "dve", S.ws[:, 0, :], S.sth[:], [S.sth], [S.ws])
    for k in range(10):
        e_tt(p, "dve", t1, S.wc[:, k, :], S.wc[:, k, :], ALU.mult, [S.wc], R)
        e_tt(p, "dve", t2, S.ws[:, k, :], S.ws[:, k, :], ALU.mult, [S.ws], R)
        e_tt(p, "dve", S.wc[:, k + 1, :], t1, t2, ALU.subtract, R, [S.wc])
        e_tt(p, "dve", t1, S.wc[:, k, :], S.ws[:, k, :], ALU.mult, [S.wc, S.ws], R)
        e_ts(p, "dve", S.ws[:, k + 1, :], t1, 2.0, None, ALU.mult, None, R, [S.ws])
    S.nws = p.alloc([128, 11, 64])
    e_ts(p, "dve", S.nws[:], S.ws[:], -1.0, None, ALU.mult, None, [S.ws], [S.nws])
    S.Ar = p.alloc([128, 64]); S.Ai = p.alloc([128, 64])
    e_copy(p, "dve", S.Ar[:], S.ar[:], [S.ar], [S.Ar]); e_copy(p, "dve", S.Ai[:], S.ai[:], [S.ai], [S.Ai])
    for k in range(11):
        e_tt(p, "dve", t1, S.Ar[:], S.Ar[:], ALU.mult, [S.Ar], R)
        e_tt(p, "dve", t2, S.Ai[:], S.Ai[:], ALU.mult, [S.Ai], R)
        e_tt(p, "dve", den, S.Ar[:], S.Ai[:], ALU.mult, [S.Ar, S.Ai], R)
        e_tt(p, "dve", S.Ar[:], t1, t2, ALU.subtract, R, [S.Ar])
        e_ts(p, "dve", S.Ai[:], den, 2.0, None, ALU.mult, None, R, [S.Ai])
    S.cP = p.alloc([128, 2, 32, 16]); p.dma(S.cP[:], io["cP"][:], writes=[S.cP])
    S.Bb = p.alloc([128, 2, 2, 32, 16])
    m = p.mark()
    bP = p.alloc([128, 2, 32, 16]); p.dma(bP[:], io["bP"][:], writes=[bP])
    Bb = S.Bb
    tA = p.alloc([128, 32, 16]); tB = p.alloc([128, 32, 16])
    for d in range(2):
        qr_b = S.qr[:, d * 32:(d + 1) * 32].unsqueeze(2).to_broadcast([128, 32, 16])
        qi_b = S.qi[:, d * 32:(d + 1) * 32].unsqueeze(2).to_broadcast([128, 32, 16])
        e_tt(p, "dve", tA[:], bP[:, 0], qr_b, ALU.mult, [bP, S.qr], [tA])
        e_tt(p, "dve", tB[:], bP[:, 1], qi_b, ALU.mult, [bP, S.qi], [tB])
        e_tt(p, "dve", Bb[:, d, 0], tA[:], tB[:], ALU.subtract, [tA, tB], [Bb])
        e_tt(p, "dve", tA[:], bP[:, 1], qr_b, ALU.mult, [bP, S.qr], [tA])
        e_tt(p, "dve", tB[:], bP[:, 0], qi_b, ALU.mult, [bP, S.qi], [tB])
        e_tt(p, "dve", Bb[:, d, 1], tA[:], tB[:], ALU.add, [tA, tB], [Bb])
    p.release(m)
    return S


def s5_build_ct(p, S, ct, ident, BbT, Cp, src):
    i = 0
    for gpl in range(4):
        gp = ct * 4 + gpl
        for d in range(2):
            for ri in range(2):
                s_ = src[i % 2]; ps = p.pb[7]
                for g2 in range(2):
                    e_copy(p, "dve", s_[g2 * 64:(g2 + 1) * 64, gpl, g2, :], S.Bb[g2 * 64:(g2 + 1) * 64, d, ri, gp, :], [S.Bb], [s_])
                e_tr(p, ps[:, (i % 4) * 128:(i % 4 + 1) * 128], s_[:].rearrange("q a b c -> q (a b c)"), ident[:], [s_, ident], [ps])
                e_copy(p, "act", BbT[:, gpl, d, ri, :], ps[:, (i % 4) * 128:(i % 4 + 1) * 128], [ps], [BbT])
                for g2 in range(2):
                    p.op("pool", lambda h, s_=s_, g2=g2, gpl=gpl: h.memset(s_[g2 * 64:(g2 + 1) * 64, gpl, g2, :], 0.0),
                         reads=[], writes=[s_])
                i += 1
    p.op("pool", lambda h: h.memset(Cp[:], 0.0), writes=[Cp])
    Cv = Cp[:].rearrange("q g r (a b k) -> q g r a b k", a=4, b=2)
    for gpl in range(4):
        gp = ct * 4 + gpl
        for g2 in range(2):
            rows = slice(g2 * 64, (g2 + 1) * 64)
            e_copy(p, "dve", Cv[rows, gpl, 0, gpl, g2, :], S.cP[rows, 0, gp, :], [S.cP], [Cp])
            e_ts(p, "dve", Cv[rows, gpl, 1, gpl, g2, :], S.cP[rows, 1, gp, :], -1.0, None, ALU.mult, None, [S.cP], [Cp])


def s5_tables(p, S, col, cosT, sinT):
    p.op("pool", lambda h: h.memset(cosT[:, 0:1], 1.0), writes=[cosT])
    p.op("pool", lambda h: h.memset(sinT[:, 0:1], 0.0), writes=[sinT])
    R = [cosT, sinT, S.wc, S.ws, S.nws]
    for k in range(11):
        n = 1 << k
        wc = S.wc[:, k, col:col + 1]; ws = S.ws[:, k, col:col + 1]; nws = S.nws[:, k, col:col + 1]
        lo = slice(0, n); hi = slice(n, 2 * n)
        e_ts(p, "dve", cosT[:, hi], cosT[:, lo], wc, None, ALU.mult, None, R, [cosT])
        e_stt(p, cosT[:, hi], sinT[:, lo], nws, cosT[:, hi], ALU.mult, ALU.add, R, [cosT])
        e_ts(p, "dve", sinT[:, hi], sinT[:, lo], wc, None, ALU.mult, None, R, [sinT])
        e_stt(p, sinT[:, hi], cosT[:, lo], ws, sinT[:, hi], ALU.mult, ALU.add, R, [sinT])


def s5_pass(p, S, uT, tabsc, full, Fsum, kin, ident, yacc_evac=None):
    m = p.mark()
    cos2 = [p.alloc([128, 2048]) for _ in range(2)]; sin2 = [p.alloc([128, 2048]) for _ in range(2)]
    TL = 2048
    CH = 512
    nb = 2
    bur = [p.alloc([128, CH]) for _ in range(nb)]; bui = [p.alloc([128, CH]) for _ in range(nb)]
    m1 = [p.alloc([128, CH]) for _ in range(nb)]; m2 = [p.alloc([128, CH]) for _ in range(nb)]
    cr = [p.alloc([128, CH]) for _ in range(nb)]; ci = [p.alloc([128, CH]) for _ in range(nb)]
    kr = [p.alloc([128, CH]) for _ in range(nb)]; ki = [p.alloc([128, CH]) for _ in range(nb)]
    hr = [p.alloc([128, CH], BF16) for _ in range(nb)]; hi = [p.alloc([128, CH], BF16) for _ in range(nb)]
    tiny = p.alloc([128, 4])
    BbT2 = [p.alloc([128, 4, 2, 2, 128], BF16) for _ in range(2)]
    Cp2 = [p.alloc([128, 4, 2, 128], BF16) for _ in range(2)]
    srcs = [p.alloc([128, 4, 2, 16]) for _ in range(2)]
    for s_ in srcs:
        p.op("pool", lambda h, s_=s_: h.memset(s_[:], 0.0), writes=[s_])
    subs = [(0, 0, NCX), (1, NCX, NL)]
    it = 0
    ci_ = 0
    for ct in range(8):
        BbT = BbT2[ct % 2]; Cp = Cp2[ct % 2]
        s5_build_ct(p, S, ct, ident, BbT, Cp, srcs)
        for gpl in range(4):
            gp = ct * 4 + gpl
            for d in range(2):
                col = d * 32 + gp
                if not full:
                    s5_tables(p, S, col, cos2[0], sin2[0])
                    if d == 0:
                        cosT = cos2[0]; sinT = sin2[0]
                    else:
                        cosT = cos2[1]; sinT = sin2[1]
                        e_copy(p, "dve", rev_ap(cosT[:]), cos2[0][:], [cos2[0]], [cosT])
                        e_copy(p, "dve", rev_ap(sinT[:]), sin2[0][:], [sin2[0]], [sinT])
                    p.dma(tabsc[col, 0], cosT[:], reads=[cosT], writes=[tabsc], stream="tb")
                    p.dma(tabsc[col, 1], sinT[:], reads=[sinT], writes=[tabsc], stream="tb")
                else:
                    cosT = cos2[it % 2]; sinT = sin2[it % 2]
                    it += 1
                    p.dma(cosT[:], tabsc[col, 0], reads=[tabsc], writes=[cosT], stream="tbl")
                    p.dma(sinT[:], tabsc[col, 1], reads=[tabsc], writes=[sinT], stream="tbl")
                mcol = S.mag[:, col:col + 1]
                for (which, c0, L) in subs:
                    nch = (L + CH - 1) // CH
                    carry_r = None
                    for cj in range(nch):
                        n = min(CH, L)
                        a = cj * n if d == 0 else L - (cj + 1) * n
                        tau0 = cj * n
                        b_ = ci_ % nb
                        ci_ += 1
                        tok = slice(c0 + a, c0 + a + n)
                        pr = p.pb[5]; pi_ = p.pb[6]
                        e_mm(p, pr[:, 0:n], BbT[:, gpl, d, 0, :], uT[:, ct, tok], True, True, [BbT, uT], [pr])
                        e_mm(p, pi_[:, 0:n], BbT[:, gpl, d, 1, :], uT[:, ct, tok], True, True, [BbT, uT], [pi_])
                        e_copy(p, "act", bur[b_][:, 0:n], pr[:, 0:n], [pr], [bur[b_]])
                        e_copy(p, "act", bui[b_][:, 0:n], pi_[:, 0:n], [pi_], [bui[b_]])
                        br = bur[b_][:, 0:n]; bi = bui[b_][:, 0:n]
                        ts0 = tau0 if d == 0 else (TL - L) + a
                        cs = cosT[:, ts0:ts0 + n]; sn = sinT[:, ts0:ts0 + n]
                        R = [bur[b_], bui[b_], cosT, sinT]
                        e_tt(p, "dve", m1[b_][:, 0:n], cs, br, ALU.mult, R, [m1[b_]])
                        e_tt(p, "dve", m2[b_][:, 0:n], sn, bi, ALU.mult, R, [m2[b_]])
                        e_tt(p, "dve", m1[b_][:, 0:n], m1[b_][:, 0:n], m2[b_][:, 0:n], ALU.add, [m1[b_], m2[b_]], [m1[b_]])
                        e_tt(p, "dve", cr[b_][:, 0:n], cs, bi, ALU.mult, R, [cr[b_]])
                        e_tt(p, "dve", ci[b_][:, 0:n], sn, br, ALU.mult, R, [ci[b_]])
                        e_tt(p, "dve", cr[b_][:, 0:n], cr[b_][:, 0:n], ci[b_][:, 0:n], ALU.subtract, [cr[b_], ci[b_]], [cr[b_]])
                        if cj == 0:
                            if full and which == 1:
                                ini_r = kin[:, 0, col:col + 1]; ini_i = kin[:, 1, col:col + 1]; rd = [kin]
                            else:
                                ini_r = 0.0; ini_i = 0.0; rd = []
                        else:
                            ini_r = carry_r; ini_i = carry_i; rd = [carry_br, carry_bi]
                        mb = mcol.to_broadcast([128, n])
                        ko_r = kr[b_][:, 0:n]; ko_i = ki[b_][:, 0:n]; xi_r = m1[b_][:, 0:n]; xi_i = cr[b_][:, 0:n]
                        if d == 1:
                            ko_r = rev_ap(ko_r); ko_i = rev_ap(ko_i); xi_r = rev_ap(xi_r); xi_i = rev_ap(xi_i)
                        p.op("dve", lambda h, o=ko_r, x=xi_r, ini=ini_r, mb=mb: h.tensor_tensor_scan(
                            out=o, data0=mb, data1=x, initial=ini, op0=ALU.mult, op1=ALU.add),
                            reads=[m1[b_], S.mag] + rd, writes=[kr[b_]])
                        p.op("dve", lambda h, o=ko_i, x=xi_i, ini=ini_i, mb=mb: h.tensor_tensor_scan(
                            out=o, data0=mb, data1=x, initial=ini, op0=ALU.mult, op1=ALU.add),
                            reads=[cr[b_], S.mag] + rd, writes=[ki[b_]])
                        lastc = n - 1 if d == 0 else 0
                        carry_r = kr[b_][:, lastc:lastc + 1]; carry_i = ki[b_][:, lastc:lastc + 1]
                        carry_br = kr[b_]; carry_bi = ki[b_]
                        if full:
                            o_r = hr[b_][:, 0:n]; o_i = hi[b_][:, 0:n]
                            K = [kr[b_], ki[b_], cosT, sinT]
                            e_tt(p, "dve", m1[b_][:, 0:n], cs, kr[b_][:, 0:n], ALU.mult, K, [m1[b_]])
                            e_tt(p, "dve", m2[b_][:, 0:n], sn, ki[b_][:, 0:n], ALU.mult, K, [m2[b_]])
                            e_tt(p, "dve", o_r, m1[b_][:, 0:n], m2[b_][:, 0:n], ALU.subtract, [m1[b_], m2[b_]], [hr[b_]])
                            e_tt(p, "dve", cr[b_][:, 0:n], sn, kr[b_][:, 0:n], ALU.mult, K, [cr[b_]])
                            e_tt(p, "dve", ci[b_][:, 0:n], cs, ki[b_][:, 0:n], ALU.mult, K, [ci[b_]])
                            e_tt(p, "dve", o_i, cr[b_][:, 0:n], ci[b_][:, 0:n], ALU.add, [cr[b_], ci[b_]], [hi[b_]])
                            if which == 0:
                                bank = p.pb[0]; bsl = slice(0, n)
                            else:
                                bank = p.pb[1 + a // CH]; bsl = slice(0, n)
                            first = (gpl == 0 and d == 0)
                            last = (gpl == 3 and d == 1)
                            e_mm(p, bank[:, bsl], Cp[:, gpl, 0, :], hr[b_][:, 0:n], first, False, [Cp, hr[b_]], [bank])
                            e_mm(p, bank[:, bsl], Cp[:, gpl, 1, :], hi[b_][:, 0:n], False, last, [Cp, hi[b_]], [bank])
                    if not full:
                        fl = L - 1 if d == 0 else TL - L
                        csl = cosT[:, fl:fl + 1]; snl = sinT[:, fl:fl + 1]
                        Kt = [carry_br, carry_bi, cosT, sinT, tiny]
                        e_tt(p, "dve", tiny[:, 0:1], csl, carry_r, ALU.mult, Kt, [tiny])
                        e_tt(p, "dve", tiny[:, 1:2], snl, carry_i, ALU.mult, Kt, [tiny])
                        e_tt(p, "dve", Fsum[:, 0, which, col:col + 1], tiny[:, 0:1], tiny[:, 1:2], ALU.subtract, [tiny], [Fsum])
                        e_tt(p, "dve", tiny[:, 2:3], snl, carry_r, ALU.mult, Kt, [tiny])
                        e_tt(p, "dve", tiny[:, 3:4], csl, carry_i, ALU.mult, Kt, [tiny])
                        e_tt(p, "dve", Fsum[:, 1, which, col:col + 1], tiny[:, 2:3], tiny[:, 3:4], ALU.add, [tiny], [Fsum])
        if full:
            yacc_evac(ct)
    p.release(m)


def s5_incoming(p, S, Fsum, gath, onehot, kin):
    m = p.mark()
    I = p.alloc([128, 4, 2, 64])
    t1 = p.alloc([128, 32]); t2 = p.alloc([128, 32]); nr = p.alloc([128, 32]); ni = p.alloc([128, 32])
    f = slice(0, 32); b = slice(32, 64)
    R = [I, gath, Fsum, S.Ar, S.Ai, t1, t2, nr, ni]
    e_copy(p, "dve", I[:, 0, 0, f], Fsum[:, 0, 0, f], R, [I]); e_copy(p, "dve", I[:, 0, 1, f], Fsum[:, 1, 0, f], R, [I])
    for q in range(3):
        cmul(p, "dve", nr[:], ni[:], S.Ar[:, f], S.Ai[:, f], I[:, q, 0, f], I[:, q, 1, f], t1[:], t2[:], R, [t1, t2, nr, ni])
        e_tt(p, "dve", I[:, q + 1, 0, f], nr[:], gath[:, q, 0, f], ALU.add, R, [I])
        e_tt(p, "dve", I[:, q + 1, 1, f], ni[:], gath[:, q, 1, f], ALU.add, R, [I])
    e_copy(p, "dve", I[:, 3, 0, b], Fsum[:, 0, 0, b], R, [I]); e_copy(p, "dve", I[:, 3, 1, b], Fsum[:, 1, 0, b], R, [I])
    for q in (3, 2, 1):
        cmul(p, "dve", nr[:], ni[:], S.Ar[:, b], S.Ai[:, b], I[:, q, 0, b], I[:, q, 1, b], t1[:], t2[:], R, [t1, t2, nr, ni])
        e_tt(p, "dve", I[:, q - 1, 0, b], nr[:], gath[:, q, 0, b], ALU.add, R, [I])
        e_tt(p, "dve", I[:, q - 1, 1, b], ni[:], gath[:, q, 1, b], ALU.add, R, [I])
    own = p.alloc([128, 2, 64])
    e_ts(p, "dve", own[:], I[:, 0], onehot[:, 0:1], None, ALU.mult, None, [I, onehot], [own])
    for q in range(1, 4):
        e_stt(p, own[:], I[:, q], onehot[:, q:q + 1], own[:], ALU.mult, ALU.add, [I, onehot, own], [own])
    t3 = p.alloc([128, 64]); t4 = p.alloc([128, 64])
    cmul(p, "dve", kin[:, 0, :], kin[:, 1, :], S.cth[:], S.sth[:], own[:, 0, :], own[:, 1, :], t3[:], t4[:],
         [S.cth, S.sth, own, t3, t4, kin], [t3, t4, kin])
    p.release(m)


def build_s5_test():
    p = Prog(); p.init_pool()
    io = {}
    for name, shape in [("xl", [NL, D]), ("xc", [NCX, D]), ("cT", [128, 8, 2]), ("modw", [D, 6144]), ("modb", [1, 6144]),
                        ("n1g", [1, D]), ("ident", [128, 128]), ("lamP", [128, 2, 64]), ("ldtP", [128, 64]),
                        ("bP", [128, 2, 32, 16]), ("cP", [128, 2, 32, 16]), ("dP", [128, 8]), ("onehot", [128, 4])]:
        io[name] = p.dram(name, shape)
    dbg = p.dram("dbg", [128, 8, NT], kind="ExternalOutput")
    dbgF = p.dram("dbgF", [128, 2, 2, 64], kind="ExternalOutput")
    dbgK = p.dram("dbgK", [128, 2, 64], kind="ExternalOutput")
    modsc = p.dram("modsc", [2, 6144], kind="Internal")
    tabsc = p.dram("tabsc", [64, 2, 128, 2048], kind="Internal")
    fsrc = p.dram("fsrc", [128, 128], kind="Internal")
    fdst = p.dram("fdst", [512, 128], kind="Internal")
    ident = p.alloc([128, 128]); p.dma(ident[:], io["ident"][:], writes=[ident])
    onehot = p.alloc([128, 4]); p.dma(onehot[:], io["onehot"][:], writes=[onehot])
    dP = p.alloc([128, 8]); p.dma(dP[:], io["dP"][:], writes=[dP])
    phase_mod(p, io["cT"], io["modw"], io["modb"], modsc, ident)
    uT = p.alloc([128, 8, NT], BF16, name="uT")
    phase_hn(p, io["xl"], io["xc"], modsc, io["n1g"], ident, uT, 0, 1)
    S = s5_params(p, io, ident)
    Fsum = p.alloc([128, 2, 2, 64]); kin = p.alloc([128, 2, 64])
    s5_pass(p, S, uT, tabsc, False, Fsum, None, ident)
    p.dma(fsrc[:].rearrange("q (r c) -> q r c", r=2), Fsum[:, :, 1, :], reads=[Fsum], writes=[fsrc], stream="st")
    p.cc(lambda h: h.collective_compute("AllGather", ALU.bypass, replica_groups=[[0, 1, 2, 3], [4, 5, 6, 7]],
                                        ins=[fsrc.t.opt()], outs=[fdst.t.opt()]), reads=[fsrc], writes=[fdst])
    gath = p.alloc([128, 4, 2, 64])
    p.dma(gath[:], fdst[:].rearrange("(q x) (r c) -> x q r c", x=128, r=2), reads=[fdst], writes=[gath])
    s5_incoming(p, S, Fsum, gath, onehot, kin)
    p.dma(dbgF[:], Fsum[:], reads=[Fsum], stream="st"); p.dma(dbgK[:], kin[:], reads=[kin], stream="st")
    vbuf = [p.alloc([128, 512]) for _ in range(2)]

    def evac(ct):
        for bi_ in range(5):
            n = NCX if bi_ == 0 else 512
            c0 = 0 if bi_ == 0 else NCX + (bi_ - 1) * 512
            v = vbuf[bi_ % 2]
            e_stt(p, v[:, 0:n], uT[:, ct, c0:c0 + n], dP[:, ct:ct + 1], p.pb[bi_][:, 0:n], ALU.mult, ALU.add,
                  [uT, dP, p.pb[bi_]], [v])
            p.dma(dbg[:, ct, c0:c0 + n], v[:, 0:n], reads=[v], stream="st")

    s5_pass(p, S, uT, tabsc, True, None, kin, ident, evac)
    p.wait_all()
    return p


def host_inputs(z, layer=0):
    x = z["x"]; c = z["c"]; ctx = z["ctx"]; c_ctx = z["c_ctx"]
    lam = np.stack([z["s5_lam_re"][0], z["s5_lam_im"][0]], 0)
    lamP = lam.reshape(2, 2, 32, 2, 64).transpose(3, 4, 0, 1, 2).reshape(128, 2, 64)
    ldt = z["s5_log_dt"][0]
    ldtP = np.broadcast_to(ldt.reshape(2, 32, 2)[:, :, :, None], (2, 32, 2, 64)).transpose(2, 3, 0, 1).reshape(128, 64)
    b = np.stack([z["s5_b_re"][0], z["s5_b_im"][0]], 0)
    bP = b.reshape(2, 32, 2, 64, 16).transpose(2, 3, 0, 1, 4).reshape(128, 2, 32, 16)
    cc = np.stack([z["s5_c_re"][0], z["s5_c_im"][0]], 0)
    cP = cc.reshape(2, 32, 2, 16, 64).transpose(2, 4, 0, 1, 3).reshape(128, 2, 32, 16)
    dP = z["s5_d"][0].reshape(8, 128).T
    maps = []
    for k in range(8):
        bb = k // 4; q = k % 4
        cT = np.stack([c[bb], c_ctx], axis=-1).reshape(8, 128, 2).transpose(1, 0, 2)
        oh = np.zeros((128, 4), np.float32); oh[:, q] = 1
        maps.append(dict(xl=x[bb, q * NL:(q + 1) * NL], xc=ctx[bb], cT=np.ascontiguousarray(cT),
                         modw=z["mod_w"][layer], modb=z["mod_b"][layer][None], n1g=z["norm1_g"][layer][None],
                         ident=np.eye(128, dtype=np.float32), lamP=np.ascontiguousarray(lamP),
                         ldtP=np.ascontiguousarray(ldtP), bP=np.ascontiguousarray(bP), cP=np.ascontiguousarray(cP),
                         dP=np.ascontiguousarray(dP), onehot=oh))
    return maps


def ref_s5(z, bb, groups):
    f8 = np.float64
    x = z["x"][bb].astype(f8); ctx = z["ctx"][bb].astype(f8); c = z["c"][bb].astype(f8); c_ctx = z["c_ctx"].astype(f8)
    silu = lambda v: v / (1 + np.exp(-v))
    rms = lambda v, g: v / np.sqrt((v * v).mean(-1, keepdims=True) + 1e-6) * g
    mw = z["mod_w"][0].astype(f8); mb = z["mod_b"][0].astype(f8); g1 = z["norm1_g"][0].astype(f8)
    ml = silu(c) @ mw + mb; mc = silu(c_ctx) @ mw + mb
    hn = rms(x, g1) * (1 + ml[D:2 * D]) + ml[:D]
    cn = rms(ctx, g1) * (1 + mc[D:2 * D]) + mc[:D]
    out = {}
    for g in groups:
        ch = slice(g * 16, (g + 1) * 16)
        tot = np.zeros((NCX + 8192, 16))
        for d in range(2):
            lr = z["s5_lam_re"][0, d, g].astype(f8); li = z["s5_lam_im"][0, d, g].astype(f8)
            step = np.exp(z["s5_log_dt"][0, d, g].astype(f8))
            lamc = lr + 1j * li
            abar = np.exp(lamc * step)
            Bc = z["s5_b_re"][0, g].astype(f8) + 1j * z["s5_b_im"][0, g].astype(f8)
            Cc = z["s5_c_re"][0, g].astype(f8) + 1j * z["s5_c_im"][0, g].astype(f8)
            Bbar = ((abar - 1) / lamc)[:, None] * Bc
            seq = np.concatenate([cn[:, ch], hn[:, ch]], 0) if d == 0 else np.concatenate([cn[::-1, ch], hn[::-1, ch]], 0)
            bu = seq @ Bbar.T
            h = np.zeros(64, complex); ys = np.zeros((len(seq), 16))
            for t in range(len(seq)):
                h = abar * h + bu[t]
                ys[t] = (Cc @ h).real
            if d == 1:
                ys = np.concatenate([ys[:NCX][::-1], ys[NCX:][::-1]], 0)
            tot += ys
        u = np.concatenate([cn[:, ch], hn[:, ch]], 0)
        out[g] = tot + z["s5_d"][0, ch].astype(f8) * u
    return out


def gelu_evac(p, uT, dP, gT):
    vb = [p.alloc([128, 512]) for _ in range(2)]
    wb = [p.alloc([128, 512]) for _ in range(1)]
    cnt = [0]

    def evac(ct):
        for bi_ in range(5):
            n = NCX if bi_ == 0 else 512
            c0 = 0 if bi_ == 0 else NCX + (bi_ - 1) * 512
            v = vb[cnt[0] % 2]; w = wb[0]
            cnt[0] += 1
            e_stt(p, v[:, 0:n], uT[:, ct, c0:c0 + n], dP[:, ct:ct + 1], p.pb[bi_][:, 0:n], ALU.mult, ALU.add,
                  [uT, dP, p.pb[bi_]], [v])
            e_act(p, w[:, 0:n], v[:, 0:n], AF.Square, [v], [w])
            e_ts(p, "dve", w[:, 0:n], w[:, 0:n], 0.044715, 1.0, ALU.mult, ALU.add, [w], [w])
            e_tt(p, "dve", w[:, 0:n], w[:, 0:n], v[:, 0:n], ALU.mult, [w, v], [w])
            e_act(p, w[:, 0:n], w[:, 0:n], AF.Sigmoid, [w], [w], scale=1.5957691216057308)
            e_tt(p, "pool", gT[:, ct, c0:c0 + n], v[:, 0:n], w[:, 0:n], ALU.mult, [v, w], [gT])
    return evac


def load_w_bf16(p, dst, src_ap, w):
    p.dma(dst, src_ap, writes=w, eng="pool")


def phase_c(p, io, modsc, ident, gT, h2T, gates, x1sc, layer_glu=True):
    m = p.mark()
    W = p.alloc([128, 4, 8, 512], BF16)
    for nb in range(4):
        load_w_bf16(p, W[:, nb],
                    io["gluw"][:, nb * 512:(nb + 1) * 512].rearrange("(k q) n -> q k n", q=128), [W])
    gb = p.alloc([128, 2048]); load_bcast(p, gb[:], io["glub"][0:1, :], [], [gb])
    n2 = p.alloc([128, D]); load_bcast(p, n2[:], io["n2g"][0:1, :], [], [n2])
    G1 = []; A2 = []; SH2 = []
    for which in range(2):
        g1 = p.alloc([128, D]); a2 = p.alloc([128, D]); s2 = p.alloc([128, D])
        load_bcast(p, g1[:], modsc[which:which + 1, 2 * D:3 * D], [modsc], [g1])
        load_bcast(p, s2[:], modsc[which:which + 1, 3 * D:4 * D], [modsc], [s2])
        load_bcast(p, a2[:], modsc[which:which + 1, 4 * D:5 * D], [modsc], [a2])
        e_stt(p, a2[:], a2[:], 1.0, n2[:], ALU.add, ALU.mult, [a2, n2], [a2])
        G1.append(g1); A2.append(a2); SH2.append(s2)
    rw = p.alloc([128, 8, 16]); p.dma(rw[:], io["rw"][:].rearrange("(k q) n -> q k n", q=128), writes=[rw])
    rb = p.alloc([128, 16]); load_bcast(p, rb[:], io["rb"][0:1, :], [], [rb])
    xs = [p.alloc([128, D]) for _ in range(2)]
    val = p.alloc([128, D]); gat = p.alloc([128, D]); x1 = [p.alloc([128, D]) for _ in range(2)]
    hn2 = p.alloc([128, D]); junk = p.alloc([128, D]); hT32 = p.alloc([128, 8, 128])
    sm = p.alloc([128, 128])
    ss = p.alloc([128, 1]); rs = p.alloc([128, 1])
    for t in range(18):
        which = 1 if t < 2 else 0
        src = io["xc"][t * 128:(t + 1) * 128, :] if t < 2 else io["xl"][(t - 2) * 128:(t - 1) * 128, :]
        x = xs[t % 2]; xo = x1[t % 2]
        p.dma(x[:], src, writes=[x])
        tok = slice(t * 128, (t + 1) * 128)
        for nb in range(4):
            ps = p.pb[nb]
            for k in range(8):
                e_mm(p, ps[:], gT[:, k, tok], W[:, nb, k, :], k == 0, k == 7, [gT, W], [ps])
        for nb in range(2):
            e_tt(p, "dve", val[:, nb * 512:(nb + 1) * 512], p.pb[nb][:], gb[:, nb * 512:(nb + 1) * 512], ALU.add,
                 [p.pb[nb], gb], [val])
            e_tt(p, "dve", gat[:, nb * 512:(nb + 1) * 512], p.pb[2 + nb][:], gb[:, D + nb * 512:D + (nb + 1) * 512],
                 ALU.add, [p.pb[2 + nb], gb], [gat])
        e_act(p, gat[:], gat[:], AF.Sigmoid, [gat], [gat])
        e_tt(p, "dve", val[:], val[:], gat[:], ALU.mult, [val, gat], [val])
        e_tt(p, "dve", val[:], val[:], G1[which][:], ALU.mult, [val, G1[which]], [val])
        e_tt(p, "dve", xo[:], val[:], x[:], ALU.add, [val, x], [xo])
        p.dma(x1sc[tok, :], xo[:], reads=[xo], writes=[x1sc])
        e_act(p, junk[:], xo[:], AF.Square, [xo], [junk, ss], accum_out=ss[:])
        e_ts(p, "dve", rs[:], ss[:], 1.0 / D, 1e-6, ALU.mult, ALU.add, [ss], [rs])
        e_act(p, rs[:], rs[:], AF.Ln, [rs], [rs])
        e_act(p, rs[:], rs[:], AF.Exp, [rs], [rs], scale=-0.5)
        e_stt(p, hn2[:], xo[:], rs[:], A2[which][:], ALU.mult, ALU.mult, [xo, rs, A2[which]], [hn2])
        e_tt(p, "dve", hn2[:], hn2[:], SH2[which][:], ALU.add, [hn2, SH2[which]], [hn2])
        for half in range(2):
            ps = p.pb[4 + half]
            for c4 in range(4):
                ct = half * 4 + c4
                e_tr(p, ps[:, c4 * 128:(c4 + 1) * 128], hn2[:, ct * 128:(ct + 1) * 128], ident[:], [hn2, ident], [ps])
            pv = ps[:].rearrange("q (c n) -> q c n", c=4)
            e_copy(p, "act", h2T[:, half * 4:(half + 1) * 4, tok], pv, [ps], [h2T])
            e_copy(p, "dve", hT32[:, half * 4:(half + 1) * 4, :], pv, [ps], [hT32])
        pl = p.pb[6]
        for k in range(8):
            e_mm(p, pl[:, 0:16], hT32[:, k, :], rw[:, k, :], k == 0, k == 7, [hT32, rw], [pl])
        routing(p, pl, rb, sm, gates[:, t, :], gates)
    p.release(m)


def routing(p, pl, rb, sm, gout, gates_buf):
    R = [sm]
    s = sm[:, 0:16]; sel2 = sm[:, 16:48]; ps_ = sm[:, 48:72]; gs = sm[:, 72:76]; t2 = sm[:, 76:78]
    gmax = sm[:, 78:79]; Gm = sm[:, 80:84]; g1 = sm[:, 84:100]; cnt = sm[:, 100:116]; wsum = sm[:, 116:117]
    e_act(p, s, pl[:, 0:16], AF.Sigmoid, [pl], R)
    sel2v = sel2.rearrange("q (g e) -> q g e", g=4)
    sv = s.rearrange("q (g e) -> q g e", g=4)
    rbv = rb[:].rearrange("q (g e) -> q g e", g=4)
    e_tt(p, "dve", sel2v[:, :, 0:4], sv, rbv, ALU.add, R + [rb], R)
    e_copy(p, "dve", sel2v[:, :, 4:8], sel2v[:, :, 0:4], R, R)
    pv = ps_.rearrange("q (g e) -> q g e", g=4)
    pairs = [(0, 1), (0, 2), (0, 3), (1, 2), (1, 3), (2, 3)]
    for i, (a, b) in enumerate(pairs):
        e_tt(p, "dve", pv[:, :, i:i + 1], sel2v[:, :, a:a + 1], sel2v[:, :, b:b + 1], ALU.add, R, R)
    e_tt(p, "dve", pv[:, :, 0:3], pv[:, :, 0:3], pv[:, :, 3:6], ALU.max, R, R)
    e_tt(p, "dve", pv[:, :, 0:1], pv[:, :, 0:1], pv[:, :, 1:2], ALU.max, R, R)
    e_tt(p, "dve", gs.unsqueeze(2), pv[:, :, 0:1], pv[:, :, 2:3], ALU.max, R, R)
    e_tt(p, "dve", t2, gs[:, 0:2], gs[:, 2:4], ALU.max, R, R)
    e_tt(p, "dve", gmax, t2[:, 0:1], t2[:, 1:2], ALU.max, R, R)
    e_ts(p, "dve", Gm, gs, gmax, 1.0, ALU.is_ge, ALU.mult, R, R)
    g1v = g1.rearrange("q (g e) -> q g e", g=4); cv = cnt.rearrange("q (g e) -> q g e", g=4)
    e_tt(p, "dve", cv, sel2v[:, :, 1:5], sel2v[:, :, 0:4], ALU.is_gt, R, R)
    for r in (2, 3):
        e_tt(p, "dve", g1v, sel2v[:, :, r:r + 4], sel2v[:, :, 0:4], ALU.is_gt, R, R)
        e_tt(p, "dve", cv, cv, g1v, ALU.add, R, R)
    e_ts(p, "dve", cv, cv, 1.5, 1.0, ALU.is_lt, ALU.mult, R, R)
    e_tt(p, "dve", cv, cv, Gm.unsqueeze(2).to_broadcast([128, 4, 4]), ALU.mult, R, R)
    e_tt(p, "dve", cnt, cnt, s, ALU.mult, R, R)
    p.op("dve", lambda h: h.reduce_sum(out=wsum, in_=cnt, axis=AX.X), reads=R, writes=R)
    p.op("dve", lambda h: h.reciprocal(out=wsum, in_=wsum), reads=R, writes=R)
    e_ts(p, "dve", gout, cnt, wsum, 1.0, ALU.mult, ALU.mult, R, [gates_buf])


def phase_moe(p, io, layer_w, modsc, h2T, gates, x1sc, ntiles, out_cb, mod_which_of_tile):
    m = p.mark()
    w1d, w3d, w2d = layer_w
    ntok = ntiles * 128
    yacc = p.alloc([128, ntiles, D], name="yacc")
    for t0 in range(0, ntiles, 4):
        t1 = min(ntiles, t0 + 4)
        p.op("pool", lambda h, t0=t0, t1=t1: h.memset(yacc[:, t0:t1, :], 0.0), writes=[yacc])
    w1 = [p.alloc([128, 8, 512], BF16) for _ in range(2)]; w3 = [p.alloc([128, 8, 512], BF16) for _ in range(2)]
    w2 = [p.alloc([128, 4, D], BF16) for _ in range(2)]
    hT = [p.alloc([128, 4, 512], BF16) for _ in range(2)]
    s1 = [p.alloc([128, 512]) for _ in range(2)]
    blocks = [(b0, min(512, ntok - b0)) for b0 in range(0, ntok, 512)]
    ib = 0; iy = 0; ih = 0
    for e in range(16):
        a1 = w1[e % 2]; a3 = w3[e % 2]; a2 = w2[e % 2]
        load_w_bf16(p, a1[:], w1d[e].rearrange("(k q) n -> q k n", q=128), [a1])
        load_w_bf16(p, a3[:], w3d[e].rearrange("(k q) n -> q k n", q=128), [a3])
        load_w_bf16(p, a2[:], w2d[e].rearrange("(k q) n -> q k n", q=128), [a2])
        for (b0, n) in blocks:
            h = hT[ib % 2]; ib += 1
            for hc in range(4):
                p1 = p.pb[ih % 2]; p3 = p.pb[2 + ih % 2]; sb1 = s1[ih % 2]; ih += 1
                for k in range(8):
                    e_mm(p, p1[:, 0:n], a1[:, k, hc * 128:(hc + 1) * 128], h2T[:, k, b0:b0 + n], k == 0, k == 7, [a1, h2T], [p1])
                for k in range(8):
                    e_mm(p, p3[:, 0:n], a3[:, k, hc * 128:(hc + 1) * 128], h2T[:, k, b0:b0 + n], k == 0, k == 7, [a3, h2T], [p3])
                e_act(p, sb1[:, 0:n], p1[:, 0:n], AF.Silu, [p1], [sb1])
                e_tt(p, "dve", h[:, hc, 0:n], sb1[:, 0:n], p3[:, 0:n], ALU.mult, [sb1, p3], [h])
            for tt in range(n // 128):
                t = b0 // 128 + tt
                for dh in range(2):
                    py = p.pb[4 + iy % 4]; iy += 1
                    for hc in range(4):
                        e_mm(p, py[:], h[:, hc, tt * 128:(tt + 1) * 128], a2[:, hc, dh * 512:(dh + 1) * 512],
                             hc == 0, hc == 3, [h, a2], [py])
                    ya = yacc[:, t, dh * 512:(dh + 1) * 512]
                    e_stt(p, ya, py[:], gates[:, t, e:e + 1], ya, ALU.mult, ALU.add, [py, gates, yacc], [yacc])
    G2 = []
    for which in range(2):
        g2 = p.alloc([128, D]); load_bcast(p, g2[:], modsc[which:which + 1, 5 * D:6 * D], [modsc], [g2]); G2.append(g2)
    xb = [p.alloc([128, D]) for _ in range(2)]
    for t in range(ntiles):
        which = mod_which_of_tile(t)
        x = xb[t % 2]
        p.dma(x[:], x1sc[t * 128:(t + 1) * 128, :], reads=[x1sc], writes=[x])
        e_tt(p, "dve", yacc[:, t, :], yacc[:, t, :], G2[which][:], ALU.mult, [yacc, G2[which]], [yacc])
        e_tt(p, "dve", x[:], x[:], yacc[:, t, :], ALU.add, [x, yacc], [x])
        out_cb(t, x)
    p.release(m)


L0_INPUTS = [("xl", [NL, D]), ("xc", [NCX, D]), ("cT", [128, 8, 2]), ("modw", [D, 6144]), ("modb", [1, 6144]),
             ("n1g", [1, D]), ("ident", [128, 128]), ("lamP", [128, 2, 64]), ("ldtP", [128, 64]),
             ("bP", [128, 2, 32, 16]), ("cP", [128, 2, 32, 16]), ("dP", [128, 8]), ("onehot", [128, 4]),
             ("gluw", [D, 2048]), ("glub", [1, 2048]), ("n2g", [1, D]), ("rw", [D, 16]), ("rb", [1, 16]),
             ("w1", [16, D, 512]), ("w3", [16, D, 512]), ("w2", [16, 512, D])]


def layer0(p, io, sc, ident, onehot, out_cb, skip_s5=False, stop_after_c=False, dbg=None):
    dP = p.alloc([128, 8]); p.dma(dP[:], io["dP"][:], writes=[dP])
    phase_mod(p, io["cT"], io["modw"], io["modb"], sc["modsc"], ident)
    gates = p.alloc([128, 18, 16])
    hi0 = p.hi
    gT = p.alloc([128, 8, NT], BF16, name="gT", hi=True)
    m_g = p.mark()
    uT = p.alloc([128, 8, NT], BF16, name="uT")
    phase_hn(p, io["xl"], io["xc"], sc["modsc"], io["n1g"], ident, uT, 0, 1)
    if skip_s5:
        for ct in range(8):
            e_copy(p, "dve", gT[:, ct, :], uT[:, ct, :], [uT], [gT])
    S = None if skip_s5 else s5_params(p, io, ident)
    Fsum = p.alloc([128, 2, 2, 64]); kin = p.alloc([128, 2, 64])
    if not skip_s5:
      s5_pass(p, S, uT, sc["tabsc"], False, Fsum, None, ident)
    p.dma(sc["fsrc"][:].rearrange("q (r c) -> q r c", r=2), Fsum[:, :, 1, :], reads=[Fsum], writes=[sc["fsrc"]])
    p.cc(lambda h: h.collective_compute("AllGather", ALU.bypass, replica_groups=[[0, 1, 2, 3], [4, 5, 6, 7]],
                                        ins=[sc["fsrc"].t.opt()], outs=[sc["fdst"].t.opt()]),
         reads=[sc["fsrc"]], writes=[sc["fdst"]])
    gath = p.alloc([128, 4, 2, 64])
    p.dma(gath[:], sc["fdst"][:].rearrange("(q x) (r c) -> x q r c", x=128, r=2), reads=[sc["fdst"]], writes=[gath])
    if not skip_s5:
        s5_incoming(p, S, Fsum, gath, onehot, kin)
        evac = gelu_evac(p, uT, dP, gT)
        s5_pass(p, S, uT, sc["tabsc"], True, None, kin, ident, evac)
    p.release(m_g)
    h2T = p.alloc([128, 8, NT], BF16, name="h2T")
    phase_c(p, io, sc["modsc"], ident, gT, h2T, gates, sc["x1sc"])
    p.hi = hi0
    if dbg is not None:
        p.dma(dbg["gates"][:], gates[:], reads=[gates])
    if stop_after_c:
        return
    phase_moe(p, io, (io["w1"], io["w3"], io["w2"]), sc["modsc"], h2T, gates, sc["x1sc"], 18,
              out_cb, lambda t: 1 if t < 2 else 0)


def build_l0_test():
    p = Prog(); p.init_pool()
    io = {name: p.dram(name, shape) for name, shape in L0_INPUTS}
    xo = p.dram("xo", [NT, D], kind="ExternalOutput")
    sc = dict(modsc=p.dram("modsc", [2, 6144], kind="Internal"),
              tabsc=p.dram("tabsc", [64, 2, 128, 2048], kind="Internal"),
              fsrc=p.dram("fsrc", [128, 128], kind="Internal"), fdst=p.dram("fdst", [512, 128], kind="Internal"),
              x1sc=p.dram("x1sc", [NT, D], kind="Internal"))
    ident = p.alloc([128, 128]); p.dma(ident[:], io["ident"][:], writes=[ident])
    onehot = p.alloc([128, 4]); p.dma(onehot[:], io["onehot"][:], writes=[onehot])

    def out_cb(t, x):
        p.dma(xo[t * 128:(t + 1) * 128, :], x[:], reads=[x])

    layer0(p, io, sc, ident, onehot, out_cb)
    p.wait_all()
    return p


def host_inputs_l0(z):
    maps = host_inputs(z, 0)
    for k in range(8):
        maps[k].update(gluw=z["s5_glu_w"][0], glub=z["s5_glu_b"][0][None], n2g=z["norm2_g"][0][None],
                       rw=z["router_w"], rb=z["router_b"][None], w1=z["moe_w1"][0], w3=z["moe_w3"][0], w2=z["moe_w2"][0])
    return maps


NCH = 66
PADW = 8456
CTX0 = 2
LAT0 = 262
RSW = 1028

L1_INPUTS = [("cT", [128, 8, 2]), ("modw1", [D, 6144]), ("modb1", [1, 6144]), ("n1g1", [1, D]), ("n2g1", [1, D]),
             ("fing", [1, D]), ("wz", [D, 512]), ("wxbc", [D, 1024]), ("wdt", [D, 16]), ("convw", [128, 8, 5]),
             ("convb", [128, 8]), ("dtb", [1, 16]), ("alog", [1, 16]), ("dsk", [1, 8]), ("sng", [1, 512]),
             ("outw", [512, D]), ("triU", [128, 128]), ("triL", [128, 128]), ("ones", [128, 128]),
             ("rw", [D, 16]), ("rb", [1, 16]), ("w1b", [16, D, 512]), ("w3b", [16, D, 512]), ("w2b", [16, 512, D])]


def l1_scratch(p):
    sc = {}
    sc["modsc1"] = p.dram("modsc1", [2, 6144], kind="Internal")
    sc["pre"] = p.dram("pre", [8, 128, PADW], kind="Internal")
    sc["X"] = p.dram("Xs", [NCH, 128, 512], BF16, kind="Internal")
    sc["Bt"] = p.dram("Bts", [NCH, 128, 256], BF16, kind="Internal")
    sc["BT"] = p.dram("BTs", [NCH, 128, 256], BF16, kind="Internal")
    sc["CT"] = p.dram("CTs", [NCH, 128, 256], BF16, kind="Internal")
    sc["dts"] = p.dram("dts", [NCH, 128, 16], kind="Internal")
    sc["Z"] = p.dram("Zs", [NCH, 128, 512], BF16, kind="Internal")
    sc["yf"] = p.dram("yfs", [NCH, 128, 512], kind="Internal")
    sc["yb"] = p.dram("ybs", [NCH, 128, 512], kind="Internal")
    sc["rsrc"] = p.dram("rsrc", [8192, RSW], kind="Internal")
    sc["rdst"] = p.dram("rdst", [2048, RSW], kind="Internal")
    sc["x1b"] = p.dram("x1b", [2048, D], kind="Internal")
    return sc


def chunk_rows(xg, ctx1, c):
    if c < 2:
        return ctx1[c * 128:(c + 1) * 128, :]
    return xg[:].rearrange("(r w) d -> w r d", w=64)[c - 2]


def l1_proj(p, io, sc, xg, ctx1, ident):
    m = p.mark()
    modsc = sc["modsc1"]
    g_sb = p.alloc([128, D]); load_bcast(p, g_sb[:], io["n1g1"][0:1, :], [], [g_sb])
    A = []; SH = []
    for which in range(2):
        a = p.alloc([128, D]); s = p.alloc([128, D])
        load_bcast(p, a[:], modsc[which:which + 1, D:2 * D], [modsc], [a])
        load_bcast(p, s[:], modsc[which:which + 1, 0:D], [modsc], [s])
        e_stt(p, a[:], a[:], 1.0, g_sb[:], ALU.add, ALU.mult, [a, g_sb], [a])
        A.append(a); SH.append(s)
    Wx = p.alloc([128, 8, 1024], BF16); Wz = p.alloc([128, 8, 512], BF16); Wd = p.alloc([128, 8, 16])
    for half in range(2):
        load_w_bf16(p, Wx[:, :, half * 512:(half + 1) * 512] if False else Wx[:, half * 4:(half + 1) * 4, :],
                    io["wxbc"][half * 512:(half + 1) * 512, :].rearrange("(k q) n -> q k n", q=128), [Wx])
    load_w_bf16(p, Wz[:], io["wz"][:].rearrange("(k q) n -> q k n", q=128), [Wz])
    p.dma(Wd[:], io["wdt"][:].rearrange("(k q) n -> q k n", q=128), writes=[Wd])
    dtb = p.alloc([128, 16]); load_bcast(p, dtb[:], io["dtb"][0:1, :], [], [dtb])
    zero = p.alloc([128, 8]); p.op("pool", lambda h: h.memset(zero[:], 0.0), writes=[zero])
    for ft in range(8):
        for c0 in (0, CTX0 + 256, LAT0 - 2, LAT0 + 8192):
            p.dma(sc["pre"][ft, :, c0:c0 + 2], zero[:, 0:2], reads=[zero], writes=[sc["pre"]])
    xs = [p.alloc([128, D]) for _ in range(2)]; ys = [p.alloc([128, D]) for _ in range(2)]
    junk = p.alloc([128, D]); ss = p.alloc([128, 1]); rs = p.alloc([128, 1])
    hT = [p.alloc([128, 8, 512], BF16) for _ in range(2)]
    hT32 = p.alloc([128, 8, 128])
    zt = [p.alloc([128, 512], BF16) for _ in range(2)]
    dtt = [p.alloc([128, 16]) for _ in range(2)]
    pre_sb = [p.alloc([128, 512]) for _ in range(4)]
    blocks = [(0, [0, 1])] + [(1, [2 + 4 * b + i for i in range(4)]) for b in range(16)]
    ib = 0; it = 0; ip = 0
    for (islat, chunks) in blocks:
        which = 0 if islat else 1
        h = hT[ib % 2]; ib += 1
        n = 128 * len(chunks)
        for ti, c in enumerate(chunks):
            x = xs[it % 2]; y = ys[it % 2]; it += 1
            p.dma(x[:], chunk_rows(xg, ctx1, c), reads=[xg, ctx1], writes=[x])
            e_act(p, junk[:], x[:], AF.Square, [x], [junk, ss], accum_out=ss[:])
            e_ts(p, "dve", rs[:], ss[:], 1.0 / D, 1e-6, ALU.mult, ALU.add, [ss], [rs])
            e_act(p, rs[:], rs[:], AF.Ln, [rs], [rs])
            e_act(p, rs[:], rs[:], AF.Exp, [rs], [rs], scale=-0.5)
            e_stt(p, y[:], x[:], rs[:], A[which][:], ALU.mult, ALU.mult, [x, rs, A[which]], [y])
            e_tt(p, "dve", y[:], y[:], SH[which][:], ALU.add, [y, SH[which]], [y])
            for half in range(2):
                ps = p.pb[half]
                for c4 in range(4):
                    ct = half * 4 + c4
                    e_tr(p, ps[:, c4 * 128:(c4 + 1) * 128], y[:, ct * 128:(ct + 1) * 128], ident[:], [y, ident], [ps])
                pv = ps[:].rearrange("q (c n) -> q c n", c=4)
                e_copy(p, "act", h[:, half * 4:(half + 1) * 4, ti * 128:(ti + 1) * 128], pv, [ps], [h])
                e_copy(p, "dve", hT32[:, half * 4:(half + 1) * 4, :], pv, [ps], [hT32])
            tokc = slice(ti * 128, (ti + 1) * 128)
            if islat:
                pz = p.pb[2]
                for k in range(8):
                    e_mm(p, pz[:], h[:, k, tokc], Wz[:, k, :], k == 0, k == 7, [h, Wz], [pz])
                z = zt[it % 2]
                e_act(p, z[:], pz[:], AF.Silu, [pz], [z])
                p.dma(sc["Z"][c], z[:], reads=[z], writes=[sc["Z"]])
            pd = p.pb[3]
            for k in range(8):
                e_mm(p, pd[:, 0:16], hT32[:, k, :], Wd[:, k, :], k == 0, k == 7, [hT32, Wd], [pd])
            dt_ = dtt[it % 2]
            e_tt(p, "dve", dt_[:], pd[:, 0:16], dtb[:], ALU.add, [pd, dtb], [dt_])
            e_act(p, dt_[:], dt_[:], AF.Exp, [dt_], [dt_])
            e_ts(p, "dve", dt_[:], dt_[:], 1.0, 1.0, ALU.add, ALU.mult, [dt_], [dt_])
            e_act(p, dt_[:], dt_[:], AF.Ln, [dt_], [dt_])
            p.dma(sc["dts"][c], dt_[:], reads=[dt_], writes=[sc["dts"]])
        col0 = (LAT0 + (chunks[0] - 2) * 128) if islat else CTX0
        for ft in range(8):
            px = p.pb[4 + ft % 4]
            for k in range(8):
                e_mm(p, px[:, 0:n], Wx[:, k, ft * 128:(ft + 1) * 128], h[:, k, 0:n], k == 0, k == 7, [Wx, h], [px])
            o = pre_sb[ip % 4]; ip += 1
            e_copy(p, "act", o[:, 0:n], px[:, 0:n], [px], [o])
            p.dma(sc["pre"][ft, :, col0:col0 + n], o[:, 0:n], reads=[o], writes=[sc["pre"]])
    p.release(m)


def l1_conv(p, io, sc, identb):
    m = p.mark()
    cw = p.alloc([128, 8, 5]); cb = p.alloc([128, 8])
    p.dma(cw[:], io["convw"][:], writes=[cw]); p.dma(cb[:], io["convb"][:], writes=[cb])
    inb = [p.alloc([128, 516]) for _ in range(3)]
    acc = [p.alloc([128, 512]) for _ in range(2)]
    act = [p.alloc([128, 8, 512], BF16) for _ in range(2)]
    xo = [p.alloc([128, 512], BF16) for _ in range(2)]; bo = [p.alloc([128, 256], BF16) for _ in range(2)]
    blocks = [(0, [0, 1])] + [(1, [2 + 4 * b + i for i in range(4)]) for b in range(16)]
    ii = 0; ia = 0; ib = 0; ix = 0
    for (islat, chunks) in blocks:
        n = 128 * len(chunks)
        col0 = (LAT0 + (chunks[0] - 2) * 128) if islat else CTX0
        a8 = act[ib % 2]; ib += 1
        for ft in range(8):
            xin = inb[ii % 3]; ii += 1
            p.dma(xin[:, 0:n + 4], sc["pre"][ft, :, col0 - 2:col0 + n + 2], reads=[sc["pre"]], writes=[xin])
            a = acc[ia % 2]; ia += 1
            e_ts(p, "dve", a[:, 0:n], xin[:, 0:n], cw[:, ft, 0:1], cb[:, ft:ft + 1], ALU.mult, ALU.add, [xin, cw, cb], [a])
            for k in range(1, 5):
                e_stt(p, a[:, 0:n], xin[:, k:k + n], cw[:, ft, k:k + 1], a[:, 0:n], ALU.mult, ALU.add, [xin, cw, a], [a])
            e_act(p, a8[:, ft, 0:n], a[:, 0:n], AF.Silu, [a], [a8])
        for ti, c in enumerate(chunks):
            tok = slice(ti * 128, (ti + 1) * 128)
            pt = p.pb[ix % 2]; ptb = pt.t.bitcast(BF16)
            x_o = xo[ix % 2]; b_o = bo[ix % 2]; ix += 1
            for ft in range(6):
                e_tr(p, ptb[:, ft * 128:(ft + 1) * 128], a8[:, ft, tok], identb[:], [a8, identb], [pt])
            e_copy(p, "act", x_o[:], ptb[:, 0:512], [pt], [x_o])
            e_copy(p, "dve", b_o[:], ptb[:, 512:768], [pt], [b_o])
            p.dma(sc["X"][c], x_o[:], reads=[x_o], writes=[sc["X"]])
            p.dma(sc["Bt"][c], b_o[:], reads=[b_o], writes=[sc["Bt"]])
            p.dma(sc["BT"][c].rearrange("q (g s) -> q g s", g=2), a8[:, 4:6, tok], reads=[a8], writes=[sc["BT"]])
            p.dma(sc["CT"][c].rearrange("q (g s) -> q g s", g=2), a8[:, 6:8, tok], reads=[a8], writes=[sc["CT"]])
    p.release(m)


def l1_ssd(p, io, sc):
    m = p.mark()
    triU = p.alloc([128, 128]); triL = p.alloc([128, 128]); ones = p.alloc([128, 128])
    p.dma(triU[:], io["triU"][:], writes=[triU]); p.dma(triL[:], io["triL"][:], writes=[triL])
    p.dma(ones[:], io["ones"][:], writes=[ones])
    Aneg = p.alloc([128, 16]); load_bcast(p, Aneg[:], io["alog"][0:1, :], [], [Aneg])
    e_act(p, Aneg[:], Aneg[:], AF.Exp, [Aneg], [Aneg])
    e_ts(p, "dve", Aneg[:], Aneg[:], -1.0, 0.0, ALU.mult, ALU.add, [Aneg], [Aneg])
    dsk = p.alloc([128, 8]); load_bcast(p, dsk[:], io["dsk"][0:1, :], [], [dsk])
    sng = p.alloc([128, 512]); load_bcast(p, sng[:], io["sng"][0:1, :], [], [sng])
    Wo = p.alloc([128, 4, D], BF16); load_w_bf16(p, Wo[:], io["outw"][:].rearrange("(k q) n -> q k n", q=128), [Wo])
    identb = p.identb
    nb = 2

    def mk():
        T = {}
        T["S"] = p.alloc([128, 512]); T["Sb"] = p.alloc([128, 512], BF16)
        T["X"] = [p.alloc([128, 512], BF16) for _ in range(nb)]; T["Bt"] = [p.alloc([128, 256], BF16) for _ in range(nb)]
        T["BT"] = [p.alloc([128, 2, 128], BF16) for _ in range(nb)]; T["CT"] = [p.alloc([128, 2, 128], BF16) for _ in range(nb)]
        T["dt"] = [p.alloc([128, 16]) for _ in range(nb)]
        T["da"] = p.alloc([128, 8]); T["dabc"] = p.alloc([128, 8, 128]); T["acs"] = p.alloc([128, 8]); T["ea"] = p.alloc([128, 8])
        T["wdec"] = p.alloc([128, 8]); T["dch"] = p.alloc([128, 8]); T["BCm"] = p.alloc([128, 2, 128], BF16)
        T["tmpH"] = [p.alloc([128, 4, 128]) for _ in range(2)]; T["exH"] = [p.alloc([128, 4, 128], BF16) for _ in range(2)]
        T["MH"] = [p.alloc([128, 4, 128], BF16) for _ in range(2)]
        T["xdt"] = p.alloc([128, 512], BF16); T["xw"] = p.alloc([128, 512], BF16); T["yt"] = [p.alloc([128, 512]) for _ in range(2)]
        return T

    TT = [mk(), mk()]
    orders = [list(range(NCH)), [1, 0] + list(range(NCH - 1, 1, -1))]
    ydst = [sc["yf"], sc["yb"]]

    def chunk(d, pos, c):
        T = TT[d]
        b_ = pos % nb
        islat = c >= 2
        tri = triU if d == 0 else triL
        S = T["S"]; Sb = T["Sb"]
        X = T["X"][b_]; Bt = T["Bt"][b_]; BT = T["BT"][b_]; CT = T["CT"][b_]; dt = T["dt"][b_]
        da = T["da"]; dabc = T["dabc"]; acs = T["acs"]; ea = T["ea"]; wdec = T["wdec"]; dch = T["dch"]; BCm = T["BCm"]
        xdt = T["xdt"]; xw = T["xw"]; yt = T["yt"][b_]
        p.dma(X[:], sc["X"][c], reads=[sc["X"]], writes=[X])
        p.dma(Bt[:], sc["Bt"][c], reads=[sc["Bt"]], writes=[Bt])
        p.dma(BT[:], sc["BT"][c].rearrange("q (g s) -> q g s", g=2), reads=[sc["BT"]], writes=[BT])
        p.dma(CT[:], sc["CT"][c].rearrange("q (g s) -> q g s", g=2), reads=[sc["CT"]], writes=[CT])
        p.dma(dt[:], sc["dts"][c], reads=[sc["dts"]], writes=[dt])
        dtd = dt[:, d * 8:(d + 1) * 8]
        if pos == 0:
            p.op("pool", lambda h: h.memset(S[:], 0.0), writes=[S])
            p.op("pool", lambda h: h.memset(Sb[:], 0.0), writes=[Sb])
        e_tt(p, "dve", da[:], dtd, Aneg[:, d * 8:(d + 1) * 8], ALU.mult, [dt, Aneg], [da])
        e_copy(p, "dve", dabc[:], da[:].unsqueeze(2).to_broadcast([128, 8, 128]), [da], [dabc])
        p0 = p.pb[0] if d == 0 else p.pb[7]
        e_mm(p, p0[:, 0:8], tri[:], da[:], True, True, [tri, da], [p0])
        e_mm(p, p0[:, 8:16], ones[:], da[:], True, True, [ones, da], [p0])
        e_copy(p, "dve", acs[:], p0[:, 0:8], [p0], [acs])
        e_act(p, ea[:], p0[:, 0:8], AF.Exp, [p0], [ea])
        e_tt(p, "dve", wdec[:], p0[:, 8:16], acs[:], ALU.subtract, [p0, acs], [wdec])
        e_act(p, wdec[:], wdec[:], AF.Exp, [wdec], [wdec])
        e_act(p, dch[:], p0[:, 8:16], AF.Exp, [p0], [dch])
        p1 = p.pb[1]
        for g in range(2):
            e_mm(p, p1[:, g * 128:(g + 1) * 128], BT[:, g, :], CT[:, g, :], True, True, [BT, CT], [p1])
        e_tt(p, "dve", BCm[:], p1[:, 0:256].rearrange("q (g s) -> q g s", g=2),
             tri[:].unsqueeze(1).to_broadcast([128, 2, 128]), ALU.mult, [p1, tri], [BCm])
        e_tt(p, "dve", xdt[:].rearrange("q (h e) -> q h e", h=8), X[:].rearrange("q (h e) -> q h e", h=8),
             dtd.unsqueeze(2).to_broadcast([128, 8, 64]), ALU.mult, [X, dt], [xdt])
        e_tt(p, "dve", xw[:].rearrange("q (h e) -> q h e", h=8), xdt[:].rearrange("q (h e) -> q h e", h=8),
             wdec[:].unsqueeze(2).to_broadcast([128, 8, 64]), ALU.mult, [xdt, wdec], [xw])
        if islat:
            pyd = p.pb[4]
            for hh in range(8):
                e_mm(p, p.pb[2 + hh // 4][:, (hh % 4) * 128:(hh % 4 + 1) * 128], dabc[:, hh, :], tri[:], True, True,
                     [dabc, tri], [p.pb[2 + hh // 4]])
            for half in range(2):
                pr = p.pb[2 + half]; t_ = T["tmpH"][half]; e_ = T["exH"][half]; M = T["MH"][half]
                e_tt(p, "dve", t_[:], pr[:].rearrange("q (h s) -> q h s", h=4),
                     acs[:, 4 * half:4 * half + 4].unsqueeze(2).to_broadcast([128, 4, 128]), ALU.subtract, [pr, acs], [t_])
                e_ts(p, "dve", t_[:], t_[:], 0.0, 0.0, ALU.min, ALU.add, [t_], [t_])
                e_act(p, e_[:], t_[:], AF.Exp, [t_], [e_])
                e_tt(p, "dve", M[:], e_[:], BCm[:, half, :].unsqueeze(1).to_broadcast([128, 4, 128]), ALU.mult, [e_, BCm], [M])
                for h4 in range(4):
                    hh = 4 * half + h4
                    e_mm(p, pyd[:, hh * 64:(hh + 1) * 64], M[:, h4, :], xdt[:, hh * 64:(hh + 1) * 64], True, True, [M, xdt], [pyd])
            pyo = p.pb[5]
            for g in range(2):
                e_mm(p, pyo[:, g * 256:(g + 1) * 256], CT[:, g, :], Sb[:, g * 256:(g + 1) * 256], True, True, [CT, Sb], [pyo])
            e_tt(p, "dve", yt[:].rearrange("q (h e) -> q h e", h=8), pyo[:].rearrange("q (h e) -> q h e", h=8),
                 ea[:].unsqueeze(2).to_broadcast([128, 8, 64]), ALU.mult, [pyo, ea], [yt])
            e_tt(p, "dve", yt[:], yt[:], pyd[:], ALU.add, [yt, pyd], [yt])
            p.dma(ydst[d][c], yt[:], reads=[yt], writes=[ydst[d]])
        pst = p.pb[6]
        for g in range(2):
            e_mm(p, pst[:, g * 256:(g + 1) * 256], Bt[:, g * 128:(g + 1) * 128], xw[:, g * 256:(g + 1) * 256],
                 True, True, [Bt, xw], [pst])
        e_tt(p, "dve", S[:].rearrange("q (h e) -> q h e", h=8), S[:].rearrange("q (h e) -> q h e", h=8),
             dch[:].unsqueeze(2).to_broadcast([128, 8, 64]), ALU.mult, [S, dch], [S])
        e_tt(p, "dve", S[:], S[:], pst[:], ALU.add, [S, pst], [S])
        e_copy(p, "act", Sb[:], S[:], [S], [Sb])

    for pos in range(NCH):
        chunk(0, pos, orders[0][pos])
        chunk(1, pos, orders[1][pos])

    Xc = [p.alloc([128, 512], BF16) for _ in range(nb)]; Zc = [p.alloc([128, 512], BF16) for _ in range(nb)]
    yfc = [p.alloc([128, 512]) for _ in range(nb)]; ybc = [p.alloc([128, 512]) for _ in range(nb)]
    v = [p.alloc([128, 512]) for _ in range(nb)]; vb = p.alloc([128, 512], BF16); vT = p.alloc([128, 4, 128], BF16)
    junk = p.alloc([128, 512]); orow = [p.alloc([128, RSW]) for _ in range(2)]
    for c in range(2, NCH):
        b_ = c % nb
        X = Xc[b_]; Z = Zc[b_]; yf = yfc[b_]; yb = ybc[b_]; vv = v[b_]; o = orow[b_]
        p.dma(X[:], sc["X"][c], reads=[sc["X"]], writes=[X])
        p.dma(Z[:], sc["Z"][c], reads=[sc["Z"]], writes=[Z])
        p.dma(yf[:], sc["yf"][c], reads=[sc["yf"]], writes=[yf])
        p.dma(yb[:], sc["yb"][c], reads=[sc["yb"]], writes=[yb])
        e_tt(p, "dve", vv[:].rearrange("q (h e) -> q h e", h=8), X[:].rearrange("q (h e) -> q h e", h=8),
             dsk[:].unsqueeze(2).to_broadcast([128, 8, 64]), ALU.mult, [X, dsk], [vv])
        e_tt(p, "dve", yf[:], yf[:], yb[:], ALU.add, [yf, yb], [yf])
        e_tt(p, "dve", vv[:], vv[:], yf[:], ALU.add, [vv, yf], [vv])
        e_tt(p, "dve", vv[:], vv[:], Z[:], ALU.mult, [vv, Z], [vv])
        e_act(p, junk[:], vv[:], AF.Square, [vv], [junk, o], accum_out=o[:, 1024:1025])
        e_tt(p, "dve", vb[:], vv[:], sng[:], ALU.mult, [vv, sng], [vb])
        pt = p.pb[c % 2]; ptb = pt.t.bitcast(BF16)
        for k_ in range(4):
            e_tr(p, ptb[:, k_ * 128:(k_ + 1) * 128], vb[:, k_ * 128:(k_ + 1) * 128], identb[:], [vb, identb], [pt])
        e_copy(p, "act", vT[:], ptb[:, 0:512].rearrange("q (k s) -> q k s", k=4), [pt], [vT])
        for dh in range(2):
            pp = p.pb[2 + (2 * c + dh) % 4]
            for k_ in range(4):
                e_mm(p, pp[:], vT[:, k_, :], Wo[:, k_, dh * 512:(dh + 1) * 512], k_ == 0, k_ == 3, [vT, Wo], [pp])
            e_copy(p, "act", o[:, dh * 512:(dh + 1) * 512], pp[:], [pp], [o])
        p.op("pool", lambda h, o=o: h.memset(o[:, 1025:RSW], 0.0), writes=[o])
        w = c - 2
        p.dma(sc["rsrc"][w * 128:(w + 1) * 128, :], o[:], reads=[o], writes=[sc["rsrc"]])
    p.release(m)


def l1_tail(p, io, sc, xg, onehot, ident, out_dram):
    modsc = sc["modsc1"]
    gates = p.alloc([128, 16, 16]); h2T = p.alloc([128, 8, NL], BF16)
    m = p.mark()
    n2 = p.alloc([128, D]); load_bcast(p, n2[:], io["n2g1"][0:1, :], [], [n2])
    g1 = p.alloc([128, D]); a2 = p.alloc([128, D]); s2 = p.alloc([128, D])
    load_bcast(p, g1[:], modsc[0:1, 2 * D:3 * D], [modsc], [g1])
    load_bcast(p, s2[:], modsc[0:1, 3 * D:4 * D], [modsc], [s2])
    load_bcast(p, a2[:], modsc[0:1, 4 * D:5 * D], [modsc], [a2])
    e_stt(p, a2[:], a2[:], 1.0, n2[:], ALU.add, ALU.mult, [a2, n2], [a2])
    rw = p.alloc([128, 8, 16]); p.dma(rw[:], io["rw"][:].rearrange("(k q) n -> q k n", q=128), writes=[rw])
    rb = p.alloc([128, 16]); load_bcast(p, rb[:], io["rb"][0:1, :], [], [rb])
    rt = [p.alloc([128, RSW]) for _ in range(2)]
    xq = [p.alloc([128, D]) for _ in range(2)]; xa = p.alloc([128, D]); x1 = [p.alloc([128, D]) for _ in range(2)]
    hn2 = p.alloc([128, D]); junk = p.alloc([128, D]); hT32 = p.alloc([128, 8, 128]); sm = p.alloc([128, 128])
    ss = p.alloc([128, 1]); rs = p.alloc([128, 1]); rsd = p.alloc([128, 1])
    xgv = xg[:].rearrange("(r w) d -> w r d", w=64)
    iq = 0
    for t in range(16):
        r_ = rt[t % 2]; xo = x1[t % 2]
        p.dma(r_[:], sc["rdst"][t * 128:(t + 1) * 128, :], reads=[sc["rdst"]], writes=[r_])
        for j in range(4):
            xj = xq[iq % 2]; iq += 1
            p.dma(xj[:], xgv[16 * j + t], reads=[xg], writes=[xj])
            if j == 0:
                e_ts(p, "dve", xa[:], xj[:], onehot[:, 0:1], 0.0, ALU.mult, ALU.add, [xj, onehot], [xa])
            else:
                e_stt(p, xa[:], xj[:], onehot[:, j:j + 1], xa[:], ALU.mult, ALU.add, [xj, onehot, xa], [xa])
        e_ts(p, "dve", rsd[:], r_[:, 1024:1025], 1.0 / 2048, 1e-6, ALU.mult, ALU.add, [r_], [rsd])
        e_act(p, rsd[:], rsd[:], AF.Ln, [rsd], [rsd])
        e_act(p, rsd[:], rsd[:], AF.Exp, [rsd], [rsd], scale=-0.5)
        e_stt(p, xo[:], r_[:, 0:D], rsd[:], g1[:], ALU.mult, ALU.mult, [r_, rsd, g1], [xo])
        e_tt(p, "dve", xo[:], xo[:], xa[:], ALU.add, [xo, xa], [xo])
        tok = slice(t * 128, (t + 1) * 128)
        p.dma(sc["x1b"][tok, :], xo[:], reads=[xo], writes=[sc["x1b"]])
        e_act(p, junk[:], xo[:], AF.Square, [xo], [junk, ss], accum_out=ss[:])
        e_ts(p, "dve", rs[:], ss[:], 1.0 / D, 1e-6, ALU.mult, ALU.add, [ss], [rs])
        e_act(p, rs[:], rs[:], AF.Ln, [rs], [rs])
        e_act(p, rs[:], rs[:], AF.Exp, [rs], [rs], scale=-0.5)
        e_stt(p, hn2[:], xo[:], rs[:], a2[:], ALU.mult, ALU.mult, [xo, rs, a2], [hn2])
        e_tt(p, "dve", hn2[:], hn2[:], s2[:], ALU.add, [hn2, s2], [hn2])
        for half in range(2):
            ps = p.pb[4 + half]
            for c4 in range(4):
                ct = half * 4 + c4
                e_tr(p, ps[:, c4 * 128:(c4 + 1) * 128], hn2[:, ct * 128:(ct + 1) * 128], ident[:], [hn2, ident], [ps])
            pv = ps[:].rearrange("q (c n) -> q c n", c=4)
            e_copy(p, "act", h2T[:, half * 4:(half + 1) * 4, tok], pv, [ps], [h2T])
            e_copy(p, "dve", hT32[:, half * 4:(half + 1) * 4, :], pv, [ps], [hT32])
        pl = p.pb[6]
        for k in range(8):
            e_mm(p, pl[:, 0:16], hT32[:, k, :], rw[:, k, :], k == 0, k == 7, [hT32, rw], [pl])
        routing(p, pl, rb, sm, gates[:, t, :], gates)
    p.release(m)
    fg = p.alloc([128, D]); load_bcast(p, fg[:], io["fing"][0:1, :], [], [fg])
    junk2 = p.alloc([128, D]); ss2 = p.alloc([128, 1]); rs2 = p.alloc([128, 1])

    def out_cb(t, x):
        e_act(p, junk2[:], x[:], AF.Square, [x], [junk2, ss2], accum_out=ss2[:])
        e_ts(p, "dve", rs2[:], ss2[:], 1.0 / D, 1e-6, ALU.mult, ALU.add, [ss2], [rs2])
        e_act(p, rs2[:], rs2[:], AF.Ln, [rs2], [rs2])
        e_act(p, rs2[:], rs2[:], AF.Exp, [rs2], [rs2], scale=-0.5)
        e_stt(p, x[:], x[:], rs2[:], fg[:], ALU.mult, ALU.mult, [x, rs2, fg], [x])
        p.dma(out_dram[t * 128:(t + 1) * 128, :], x[:], reads=[x], writes=[out_dram])

    phase_moe(p, io, (io["w1b"], io["w3b"], io["w2b"]), modsc, h2T, gates, sc["x1b"], 16, out_cb, lambda t: 0)


def layer1(p, io, sc, xg, ctx1, ident, onehot, out_dram, dbg=None):
    p.identb = p.alloc([128, 128], BF16)
    e_copy(p, "dve", p.identb[:], ident[:], [ident], [p.identb])
    phase_mod(p, io["cT"], io["modw1"], io["modb1"], sc["modsc1"], ident)
    l1_proj(p, io, sc, xg, ctx1, ident)
    l1_conv(p, io, sc, p.identb)
    l1_ssd(p, io, sc)
    p.cc(lambda h: h.collective_compute("ReduceScatter", ALU.add, replica_groups=[[0, 1, 2, 3], [4, 5, 6, 7]],
                                        ins=[sc["rsrc"].t.opt()], outs=[sc["rdst"].t.opt()]),
         reads=[sc["rsrc"]], writes=[sc["rdst"]])
    l1_tail(p, io, sc, xg, onehot, ident, out_dram)


def host_inputs_l1(z):
    i = 1
    maps = []
    inw = z["ssd_in_w"][0]
    for k in range(8):
        bb = k // 4; j = k % 4
        cT = np.stack([z["c"][bb], z["c_ctx"]], axis=-1).reshape(8, 128, 2).transpose(1, 0, 2)
        wz = inw[:, 512 * j:512 * (j + 1)]
        wx = inw[:, 2048 + 512 * j:2048 + 512 * (j + 1)]
        wB = inw[:, 4096 + 256 * j:4096 + 256 * (j + 1)]
        wC = inw[:, 5120 + 256 * j:5120 + 256 * (j + 1)]
        wdt = np.concatenate([inw[:, 6144 + 8 * j:6144 + 8 * (j + 1)], inw[:, 6176 + 8 * j:6176 + 8 * (j + 1)]], 1)
        chs = np.concatenate([np.arange(512 * j, 512 * (j + 1)), 2048 + np.arange(256 * j, 256 * (j + 1)),
                              3072 + np.arange(256 * j, 256 * (j + 1))])
        cw = z["ssd_conv_w"][0][:, chs]
        convw = cw.T.reshape(8, 128, 5).transpose(1, 0, 2)
        convb = z["ssd_conv_b"][0][chs].reshape(8, 128).T
        hs = slice(8 * j, 8 * (j + 1))
        dtb = np.concatenate([z["ssd_dt_bias"][0, 0, hs], z["ssd_dt_bias"][0, 1, hs]])[None]
        alog = np.concatenate([z["ssd_a_log"][0, 0, hs], z["ssd_a_log"][0, 1, hs]])[None]
        oh = np.zeros((128, 4), np.float32); oh[:, j] = 1
        maps.append(dict(
            cT=np.ascontiguousarray(cT), modw1=z["mod_w"][i], modb1=z["mod_b"][i][None], n1g1=z["norm1_g"][i][None],
            n2g1=z["norm2_g"][i][None], fing=z["final_g"][None], wz=np.ascontiguousarray(wz),
            wxbc=np.ascontiguousarray(np.concatenate([wx, wB, wC], 1)), wdt=np.ascontiguousarray(wdt),
            convw=np.ascontiguousarray(convw), convb=np.ascontiguousarray(convb), dtb=np.ascontiguousarray(dtb),
            alog=np.ascontiguousarray(alog), dsk=z["ssd_d"][0][hs][None].copy(),
            sng=z["ssd_norm_g"][0][512 * j:512 * (j + 1)][None].copy(),
            outw=np.ascontiguousarray(z["ssd_out_w"][0][512 * j:512 * (j + 1), :]),
            triU=np.triu(np.ones((128, 128), np.float32)), triL=np.tril(np.ones((128, 128), np.float32)),
            ones=np.ones((128, 128), np.float32), rw=z["router_w"], rb=z["router_b"][None],
            w1b=z["moe_w1"][i], w3b=z["moe_w3"][i], w2b=z["moe_w2"][i],
            ident=np.eye(128, dtype=np.float32), onehot=oh))
    return maps


def colmajor(xb):
    return xb.reshape(128, 64, -1).transpose(1, 0, 2).reshape(8192, -1)


def build_full():
    p = Prog(); p.init_pool()
    io = {}
    for name, shape in L0_INPUTS + L1_INPUTS:
        if name not in io:
            io[name] = p.dram(name, shape)
    out = p.dram("out", [2048, D], kind="ExternalOutput")
    sc0 = dict(modsc=p.dram("modsc", [2, 6144], kind="Internal"),
               tabsc=p.dram("tabsc", [64, 2, 128, 2048], kind="Internal"),
               fsrc=p.dram("fsrc", [128, 128], kind="Internal"), fdst=p.dram("fdst", [512, 128], kind="Internal"),
               x1sc=p.dram("x1sc", [NT, D], kind="Internal"))
    xsrcs = [p.dram(f"xsrc{a}", [128, 2048], kind="Internal") for a in range(8)]
    xdsts = [p.dram(f"xdst{a}", [512, 2048], kind="Internal") for a in range(8)]
    xg = p.dram("xg", [8192, D], kind="Internal")
    ctx1 = p.dram("ctx1", [NCX, D], kind="Internal")
    sc1 = l1_scratch(p)
    ident = p.alloc([128, 128]); p.dma(ident[:], io["ident"][:], writes=[ident])
    onehot = p.alloc([128, 4]); p.dma(onehot[:], io["onehot"][:], writes=[onehot])
    m0 = p.mark()

    def out_cb(t, x):
        if t < 2:
            p.dma(ctx1[t * 128:(t + 1) * 128, :], x[:], reads=[x], writes=[ctx1])
        else:
            a = (t - 2) // 2; half = (t - 2) % 2
            dst = xsrcs[a][:].rearrange("i (b d) -> (i b) d", d=D)[half * 128:(half + 1) * 128, :]
            p.dma(dst, x[:], reads=[x], writes=[xsrcs[a]])

    layer0(p, io, sc0, ident, onehot, out_cb)
    p.release(m0)
    xgv = xg[:].rearrange("(r a t) d -> a r t d", r=4, a=8)
    for a in range(8):
        p.cc(lambda h, a=a: h.collective_compute("AllGather", ALU.bypass, replica_groups=[[0, 1, 2, 3], [4, 5, 6, 7]],
                                                 ins=[xsrcs[a].t.opt()], outs=[xdsts[a].t.opt()]),
             reads=[xsrcs[a]], writes=[xdsts[a]])
        p.dma(xgv[a], xdsts[a][:].rearrange("(r i) (b d) -> r (i b) d", r=4, d=D), reads=[xdsts[a]], writes=[xg])
    layer1(p, io, sc1, xg, ctx1, ident, onehot, out)
    p.wait_all()
    return p


def kernel(**inputs):
    z = {k: np.asarray(v) for k, v in inputs.items()}
    m0 = host_inputs_l0(z)
    m1 = host_inputs_l1(z)
    maps = []
    for k in range(8):
        d = dict(m0[k]); d.update(m1[k])
        maps.append({kk: np.ascontiguousarray(vv, dtype=np.float32) for kk, vv in d.items()})
    p = build_full()
    nc = p.finish()
    res = run_bass_kernel_spmd(nc, maps, core_ids=list(range(8))).results
    outp = np.zeros((2, 8192, D), np.float32)
    for bb in range(2):
        cm = np.concatenate([res[4 * bb + j]["out"] for j in range(4)], 0)
        outp[bb] = cm.reshape(64, 128, D).transpose(1, 0, 2).reshape(8192, D)
    return outp
```

```python
import math, time, sys
import numpy as np
import contextlib
import concourse.bass as bass
import concourse.mybir as mybir
from concourse.bass_utils import run_bass_kernel_spmd

F32 = mybir.dt.float32
BF16 = mybir.dt.bfloat16
AF = mybir.ActivationFunctionType
ALU = mybir.AluOpType
AX = mybir.AxisListType

ENGS = ("pe", "dve", "act", "pool", "sp")


class Buf:
    def __init__(self, t, name, excl=False):
        self.t = t
        self.name = name
        self.excl = excl
        self.w = None
        self.r = []

    def __getitem__(self, k):
        return self.t[k]


class Prog:
    def __init__(self, name="k"):
        self.nc = bass.Bass("TRN2", target_bir_lowering=False)
        self.es = contextlib.ExitStack()
        self.q = {e: [] for e in ENGS}
        self.cnt = {e: 0 for e in ENGS}
        self.known = {e: {} for e in ENGS}
        self.sems = {}
        self.dcnt = {}
        self.mult = {}
        self.nbuf = 0
        self.sb_bytes = 0
        for e in ENGS:
            self.sems[e] = self.es.enter_context(self.nc.semaphore("s_" + e))
        self.NDS = 48
        self.rr = 0
        self.rr_sw = 0
        for i in range(self.NDS):
            nm = f"dq{i}"
            self.sems[nm] = self.es.enter_context(self.nc.semaphore(nm))
            self.dcnt[nm] = 0
            self.mult[nm] = 16

    def dram(self, name, shape, dtype=F32, kind="ExternalInput"):
        t = self.nc.dram_tensor(name, list(shape), dtype, kind=kind)
        b = Buf(t.ap(), name)
        b.is_dram = True
        return b

    def sb(self, shape, dtype=F32, name=None):
        self.nbuf += 1
        name = name or f"sb{self.nbuf}"
        t = self.es.enter_context(self.nc.sbuf_tensor(name, list(shape), dtype))
        sz = int(np.prod(shape[1:])) * (4 if dtype == F32 else 2)
        self.sb_bytes += sz
        return Buf(t, name)

    def init_pool(self, words=48 * 1024):
        self.big = self.es.enter_context(self.nc.sbuf_tensor("big", [128, words], F32))
        self.words = words
        self.top = 0
        self.hi = words
        self.peak = 0
        self.banks = [self.es.enter_context(self.nc.psum_tensor(f"bank{i}", [128, 512], F32)) for i in range(8)]
        self.pb = [Buf(self.banks[i][:], f"bank{i}", excl=True) for i in range(8)]

    def alloc(self, shape, dtype=F32, name=None, hi=False):
        n = int(np.prod(shape[1:]))
        w = n if dtype == F32 else (n + 1) // 2
        assert self.top + w <= self.hi, f"SBUF overflow {self.top}+{w} > {self.hi} ({name})"
        if hi:
            self.hi -= w
            ap = self.big[:, self.hi:self.hi + w]
        else:
            ap = self.big[:, self.top:self.top + w]
            self.top += w
        self.peak = max(self.peak, self.top + (self.words - self.hi))
        if dtype != F32:
            ap = ap.bitcast(dtype)[:, 0:n]
        if len(shape) > 2:
            names = " ".join(f"d{i}" for i in range(1, len(shape)))
            kw = {f"d{i}": shape[i] for i in range(1, len(shape))}
            ap = ap.rearrange(f"p ({names}) -> p {names}", **kw)
        if shape[0] < 128:
            ap = ap[0:shape[0]]
        self.nbuf += 1
        return Buf(ap, name or f"a{self.nbuf}")


    def barrier(self):
        snap_c = dict(self.cnt)
        snap_d = dict(self.dcnt)
        for e in ENGS:
            for src, n in snap_c.items():
                if src != e and n > self.known[e].get(src, 0):
                    self.q[e].append(("wait", src, n))
                    self.known[e][src] = n
            for s_, n in snap_d.items():
                if n > self.known[e].get(s_, 0):
                    self.q[e].append(("wait", s_, n * self.mult[s_]))
                    self.known[e][s_] = n

    def mark(self):
        return self.top

    def release(self, m):
        self.barrier()
        self.top = m

    def ps(self, shape, dtype=F32, name=None):
        self.nbuf += 1
        name = name or f"ps{self.nbuf}"
        t = self.es.enter_context(self.nc.psum_tensor(name, list(shape), dtype))
        return Buf(t, name)

    def stream(self, s):
        if s not in self.sems:
            self.sems[s] = self.es.enter_context(self.nc.semaphore("d_" + s))
            self.dcnt[s] = 0
            self.mult[s] = 16
        return s

    def _deps(self, eng, reads, writes):
        deps = {}

        def add(x):
            if x is None:
                return
            src, n = x
            if deps.get(src, 0) < n:
                deps[src] = n

        for b in reads:
            add(b.w)
            if b.excl:
                for x in b.r:
                    if x[0] != eng:
                        add(x)
        for b in writes:
            add(b.w)
            for x in b.r:
                add(x)
        for src, n in deps.items():
            if src == eng and eng == "pe":
                continue
            if self.known[eng].get(src, 0) >= n:
                continue
            self.known[eng][src] = n
            mult = self.mult[src] if src in self.dcnt else 1
            self.q[eng].append(("wait", src, n * mult))

    def _mark(self, tag, reads, writes):
        for b in reads:
            b.r.append(tag)
        for b in writes:
            b.w = tag
            b.r = []

    def op(self, eng, fn, reads=(), writes=()):
        self._deps(eng, reads, writes)
        self.cnt[eng] += 1
        self.q[eng].append(("op", fn))
        self._mark((eng, self.cnt[eng]), reads, writes)

    def dma(self, out, in_, reads=(), writes=(), eng="sp", stream=None, **kw):
        if eng == "sp" and any(not getattr(b, "is_dram", False) for b in reads):
            eng = "pool"
        half = self.NDS // 2
        if eng == "pool":
            s_ = f"dq{half + self.rr_sw % half}"
            self.rr_sw += 1
        else:
            s_ = f"dq{self.rr % half}"
            self.rr += 1
        self._deps(eng, reads, writes)
        prev = self.dcnt[s_]
        if prev and self.known[eng].get(s_, 0) < prev:
            self.q[eng].append(("wait", s_, prev * 16))
            self.known[eng][s_] = prev
        self.dcnt[s_] += 1
        self.q[eng].append(("dma", s_, out, in_, kw))
        self._mark((s_, self.dcnt[s_]), reads, writes)

    def cc(self, fn, reads=(), writes=(), eng="pool", stream="cc"):
        self.stream(stream)
        self.mult[stream] = 1
        self._deps(eng, reads, writes)
        prev = self.dcnt[stream]
        if prev and self.known[eng].get(stream, 0) < prev:
            self.q[eng].append(("wait", stream, prev))
            self.known[eng][stream] = prev
        self.dcnt[stream] += 1
        self.q[eng].append(("cc", stream, fn))
        self._mark((stream, self.dcnt[stream]), reads, writes)

    def wait_all(self, eng="sp"):
        for s, n in self.dcnt.items():
            if n:
                self.q[eng].append(("wait", s, self.mult[s] * n))
        for e in ENGS:
            if e != eng and self.cnt[e]:
                self.q[eng].append(("wait", e, self.cnt[e]))

    def finish(self):
        nc = self.nc
        sems = self.sems

        def replay(e):
            def body(h):
                for item in self.q[e]:
                    if item[0] == "wait":
                        h.wait_ge(sems[item[1]], item[2])
                    elif item[0] == "op":
                        item[1](h).then_inc(sems[e], 1)
                    elif item[0] == "cc":
                        item[2](h).then_inc(sems[item[1]], 1)
                    else:
                        _, s, out, in_, kw = item
                        h.dma_start(out=out, in_=in_, **kw).then_inc(sems[s], 16)
            return body

        with nc.Block() as block:
            block.tensor(replay("pe"))
            block.vector(replay("dve"))
            block.scalar(replay("act"))
            block.gpsimd(replay("pool"))
            block.sync(replay("sp"))
        self.es.close()
        return nc


def run(prog, in_maps):
    nc = prog.finish()
    res = run_bass_kernel_spmd(nc, in_maps, core_ids=list(range(len(in_maps))))
    return res.results


def _bufs(xs):
    return [x for x in xs if isinstance(x, Buf)]


def e_act(p, out, in_, func, r, w, eng="act", **kw):
    p.op(eng, lambda h: h.activation(out=out, in_=in_, func=func, **kw), reads=r, writes=w)


def e_tt(p, eng, out, in0, in1, op, r, w):
    p.op(eng, lambda h: h.tensor_tensor(out=out, in0=in0, in1=in1, op=op), reads=r, writes=w)


def e_ts(p, eng, out, in0, s1, s2, op0, op1, r, w):
    if op1 is None:
        if op0 == ALU.add:
            op1, s2 = ALU.mult, 1.0
        else:
            op1, s2 = ALU.add, 0.0
    if True:
        p.op(eng, lambda h: h.tensor_scalar(out=out, in0=in0, scalar1=s1, scalar2=s2, op0=op0, op1=op1), reads=r, writes=w)


def e_stt(p, out, in0, scalar, in1, op0, op1, r, w):
    p.op("dve", lambda h: h.scalar_tensor_tensor(out=out, in0=in0, scalar=scalar, in1=in1, op0=op0, op1=op1),
         reads=r, writes=w)


def e_copy(p, eng, out, in_, r, w):
    if eng == "act":
        p.op(eng, lambda h: h.activation(out=out, in_=in_, func=AF.Copy), reads=r, writes=w)
    else:
        p.op(eng, lambda h: h.tensor_copy(out=out, in_=in_), reads=r, writes=w)


def e_mm(p, out, lhsT, rhs, start, stop, r, w):
    p.op("pe", lambda h: h.matmul(out, lhsT=lhsT, rhs=rhs, start=start, stop=stop), reads=r, writes=w)


def e_tr(p, out, in_, ident, r, w):
    p.op("pe", lambda h: h.transpose(out, in_, ident), reads=r, writes=w)


def rev_ap(ap):
    (pst, pn), (st, n) = ap.ap
    return bass.AP(ap.tensor, ap.offset + (n - 1) * st, [[pst, pn], [-st, n]])


D = 1024
NL = 2048
NCX = 256
NT = NL + NCX
PI = math.pi


def phase_mod(p, cT, modw, modb, modsc, ident):
    m = p.mark()
    c_sb = p.alloc([128, 8, 2]); s_sb = p.alloc([128, 8, 2])
    p.dma(c_sb[:], cT[:], writes=[c_sb])
    e_act(p, s_sb[:], c_sb[:], AF.Silu, [c_sb], [s_sb])
    b_sb = p.alloc([1, 6144]); ones = p.alloc([1, 2])
    p.dma(b_sb[:], modb[:], writes=[b_sb])
    p.op("pool", lambda h: h.memset(ones[:], 1.0), writes=[ones])
    wb = [p.alloc([128, 8, 512]) for _ in range(2)]
    ob = [p.alloc([2, 512]) for _ in range(2)]
    for nb in range(12):
        w = wb[nb % 2]; o = ob[nb % 2]; ps = p.pb[nb % 2]
        p.dma(w[:], modw[:, nb * 512:(nb + 1) * 512].rearrange("(k q) n -> q k n", q=128), writes=[w])
        for k in range(8):
            e_mm(p, ps[0:2, :], s_sb[:, k, :], w[:, k, :], k == 0, False, [s_sb, w], [ps])
        e_mm(p, ps[0:2, :], ones[:], b_sb[:, nb * 512:(nb + 1) * 512], False, True, [ones, b_sb], [ps])
        e_copy(p, "act", o[:], ps[0:2, :], [ps], [o])
        p.dma(modsc[:, nb * 512:(nb + 1) * 512], o[:], reads=[o], writes=[modsc], stream="st")
    p.release(m)


def load_bcast(p, dst, src_row_ap, r, w):
    p.dma(dst, src_row_ap.to_broadcast([128, src_row_ap.shape[-1]]), reads=r, writes=w)


def phase_hn(p, xl, xc, modsc, gvec, ident, uT, part_sh, part_sc):
    m = p.mark()
    g_sb = p.alloc([128, D]); A = [p.alloc([128, D]) for _ in range(2)]; SH = [p.alloc([128, D]) for _ in range(2)]
    load_bcast(p, g_sb[:], gvec[0:1, :], [], [g_sb])
    for which in range(2):
        load_bcast(p, A[which][:], modsc[which:which + 1, part_sc * D:(part_sc + 1) * D], [modsc], [A[which]])
        load_bcast(p, SH[which][:], modsc[which:which + 1, part_sh * D:(part_sh + 1) * D], [modsc], [SH[which]])
        e_stt(p, A[which][:], A[which][:], 1.0, g_sb[:], ALU.add, ALU.mult, [A[which], g_sb], [A[which]])
    nbuf = 2
    xs = [p.alloc([128, D]) for _ in range(nbuf)]; ys = [p.alloc([128, D]) for _ in range(nbuf)]
    junk = p.alloc([128, D]); ss = [p.alloc([128, 1]) for _ in range(nbuf)]; rs = [p.alloc([128, 1]) for _ in range(nbuf)]
    for t in range(18):
        which = 1 if t < 2 else 0
        src = xc[t * 128:(t + 1) * 128, :] if t < 2 else xl[(t - 2) * 128:(t - 1) * 128, :]
        x = xs[t % nbuf]; y = ys[t % nbuf]; s = ss[t % nbuf]; r = rs[t % nbuf]
        p.dma(x[:], src, writes=[x])
        e_act(p, junk[:], x[:], AF.Square, [x], [junk, s], accum_out=s[:])
        e_ts(p, "dve", r[:], s[:], 1.0 / D, 1e-6, ALU.mult, ALU.add, [s], [r])
        e_act(p, r[:], r[:], AF.Ln, [r], [r])
        e_act(p, r[:], r[:], AF.Exp, [r], [r], scale=-0.5)
        e_stt(p, y[:], x[:], r[:], A[which][:], ALU.mult, ALU.mult, [x, r, A[which]], [y])
        e_tt(p, "dve", y[:], y[:], SH[which][:], ALU.add, [y, SH[which]], [y])
        for half in range(2):
            ps = p.pb[(2 * t + half) % 4]
            for c4 in range(4):
                ct = half * 4 + c4
                e_tr(p, ps[:, c4 * 128:(c4 + 1) * 128], y[:, ct * 128:(ct + 1) * 128], ident[:], [y, ident], [ps])
            e_copy(p, "act", uT[:, half * 4:(half + 1) * 4, t * 128:(t + 1) * 128],
                   ps[:].rearrange("q (c n) -> q c n", c=4), [ps], [uT])
    p.release(m)


def cmul(p, eng, outr, outi, ar, ai, br, bi, t1, t2, bufs_r, bufs_w):
    e_tt(p, eng, t1, ar, br, ALU.mult, bufs_r, bufs_w)
    e_tt(p, eng, t2, ai, bi, ALU.mult, bufs_r, bufs_w)
    e_tt(p, eng, outr, t1, t2, ALU.subtract, bufs_r, bufs_w)
    e_tt(p, eng, t1, ar, bi, ALU.mult, bufs_r, bufs_w)
    e_tt(p, eng, t2, ai, br, ALU.mult, bufs_r, bufs_w)
    e_tt(p, eng, outi, t1, t2, ALU.add, bufs_r, bufs_w)


class S5:
    pass


def s5_params(p, io, ident):
    S = S5()
    lam = p.alloc([128, 2, 64]); ldt = p.alloc([128, 64])
    p.dma(lam[:], io["lamP"][:], writes=[lam]); p.dma(ldt[:], io["ldtP"][:], writes=[ldt])
    lr = lam[:, 0, :]; li = lam[:, 1, :]
    W = p.alloc([128, 10, 64], name="s5w")
    sl = lambda i: W[:, i, :]
    R = [W]
    step, th, mag, t1, t2, cs, sn, den, am1 = (sl(i) for i in range(9))
    S.ar = p.alloc([128, 64]); S.ai = p.alloc([128, 64]); S.cth = p.alloc([128, 64]); S.sth = p.alloc([128, 64])
    S.mag = p.alloc([128, 64]); S.qr = p.alloc([128, 64]); S.qi = p.alloc([128, 64])
    e_act(p, step, ldt[:], AF.Exp, [ldt], R)
    e_tt(p, "dve", th, li, step, ALU.mult, [lam, W], R)
    e_tt(p, "dve", t1, lr, step, ALU.mult, [lam, W], R)
    e_act(p, S.mag[:], t1, AF.Exp, R, [S.mag])
    e_act(p, sn, th, AF.Sin, R, R, scale=1.0 / 16)
    e_ts(p, "dve", t1, th, 1.0 / 16, PI / 2, ALU.mult, ALU.add, R, R)
    e_act(p, cs, t1, AF.Sin, R, R)
    for _ in range(4):
        e_tt(p, "dve", t1, cs, cs, ALU.mult, R, R)
        e_tt(p, "dve", t2, sn, sn, ALU.mult, R, R)
        e_tt(p, "dve", sn, sn, cs, ALU.mult, R, R)
        e_ts(p, "dve", sn, sn, 2.0, None, ALU.mult, None, R, R)
        e_tt(p, "dve", cs, t1, t2, ALU.subtract, R, R)
    e_copy(p, "dve", S.cth[:], cs, R, [S.cth]); e_copy(p, "dve", S.sth[:], sn, R, [S.sth])
    e_tt(p, "dve", S.ar[:], S.mag[:], cs, ALU.mult, R + [S.mag], [S.ar])
    e_tt(p, "dve", S.ai[:], S.mag[:], sn, ALU.mult, R + [S.mag], [S.ai])
    e_tt(p, "dve", t1, lr, lr, ALU.mult, [lam], R)
    e_tt(p, "dve", t2, li, li, ALU.mult, [lam], R)
    e_tt(p, "dve", den, t1, t2, ALU.add, R, R)
    p.op("dve", lambda h: h.reciprocal(out=den, in_=den), reads=R, writes=R)
    e_ts(p, "dve", am1, S.ar[:], -1.0, None, ALU.add, None, [S.ar], R)
    e_tt(p, "dve", t1, am1, lr, ALU.mult, R + [lam], R)
    e_tt(p, "dve", t2, S.ai[:], li, ALU.mult, [S.ai, lam], R)
    e_tt(p, "dve", t1, t1, t2, ALU.add, R, R)
    e_tt(p, "dve", S.qr[:], t1, den, ALU.mult, R, [S.qr])
    e_tt(p, "dve", t1, S.ai[:], lr, ALU.mult, [S.ai, lam], R)
    e_tt(p, "dve", t2, am1, li, ALU.mult, R + [lam], R)
    e_tt(p, "dve", t1, t1, t2, ALU.subtract, R, R)
    e_tt(p, "dve", S.qi[:], t1, den, ALU.mult, R, [S.qi])
    S.wc = p.alloc([128, 11, 64]); S.ws = p.alloc([128, 11, 64])
    e_copy(p, "dve", S.wc[:, 0, :], S.cth[:], [S.cth], [S.wc]); e_copy(p, "dve", S.ws[:, 0, :], S.sth[:], [S.sth], [S.ws])
    for k in range(10):
        e_tt(p, "dve", t1, S.wc[:, k, :], S.wc[:, k, :], ALU.mult, [S.wc], R)
        e_tt(p, "dve", t2, S.ws[:, k, :], S.ws[:, k, :], ALU.mult, [S.ws], R)
        e_tt(p, "dve", S.wc[:, k + 1, :], t1, t2, ALU.subtract, R, [S.wc])
        e_tt(p, "dve", t1, S.wc[:, k, :], S.ws[:, k, :], ALU.mult, [S.wc, S.ws], R)
        e_ts(p, "dve", S.ws[:, k + 1, :], t1, 2.0, None, ALU.mult, None, R, [S.ws])
    S.nws = p.alloc([128, 11, 64])
    e_ts(p, "dve", S.nws[:], S.ws[:], -1.0, None, ALU.mult, None, [S.ws], [S.nws])
    S.Ar = p.alloc([128, 64]); S.Ai = p.alloc([128, 64])
    e_copy(p, "dve", S.Ar[:], S.ar[:], [S.ar], [S.Ar]); e_copy(p, "dve", S.Ai[:], S.ai[:], [S.ai], [S.Ai])
    for k in range(11):
        e_tt(p, "dve", t1, S.Ar[:], S.Ar[:], ALU.mult, [S.Ar], R)
        e_tt(p, "dve", t2, S.Ai[:], S.Ai[:], ALU.mult, [S.Ai], R)
        e_tt(p, "dve", den, S.Ar[:], S.Ai[:], ALU.mult, [S.Ar, S.Ai], R)
        e_tt(p, "dve", S.Ar[:], t1, t2, ALU.subtract, R, [S.Ar])
        e_ts(p, "dve", S.Ai[:], den, 2.0, None, ALU.mult, None, R, [S.Ai])
    S.cP = p.alloc([128, 2, 32, 16]); p.dma(S.cP[:], io["cP"][:], writes=[S.cP])
    S.Bb = p.alloc([128, 2, 2, 32, 16])
    m = p.mark()
    bP = p.alloc([128, 2, 32, 16]); p.dma(bP[:], io["bP"][:], writes=[bP])
    Bb = S.Bb
    tA = p.alloc([128, 32, 16]); tB = p.alloc([128, 32, 16])
    for d in range(2):
        qr_b = S.qr[:, d * 32:(d + 1) * 32].unsqueeze(2).to_broadcast([128, 32, 16])
        qi_b = S.qi[:, d * 32:(d + 1) * 32].unsqueeze(2).to_broadcast([128, 32, 16])
        e_tt(p, "dve", tA[:], bP[:, 0], qr_b, ALU.mult, [bP, S.qr], [tA])
        e_tt(p, "dve", tB[:], bP[:, 1], qi_b, ALU.mult, [bP, S.qi], [tB])
        e_tt(p, "dve", Bb[:, d, 0], tA[:], tB[:], ALU.subtract, [tA, tB], [Bb])
        e_tt(p, "dve", tA[:], bP[:, 1], qr_b, ALU.mult, [bP, S.qr], [tA])
        e_tt(p, "dve", tB[:], bP[:, 0], qi_b, ALU.mult, [bP, S.qi], [tB])
        e_tt(p, "dve", Bb[:, d, 1], tA[:], tB[:], ALU.add, [tA, tB], [Bb])
    p.release(m)
    return S


def s5_build_ct(p, S, ct, ident, BbT, Cp, src):
    i = 0
    for gpl in range(4):
        gp = ct * 4 + gpl
        for d in range(2):
            for ri in range(2):
                s_ = src[i % 2]; ps = p.pb[7]
                for g2 in range(2):
                    e_copy(p, "dve", s_[g2 * 64:(g2 + 1) * 64, gpl, g2, :], S.Bb[g2 * 64:(g2 + 1) * 64, d, ri, gp, :], [S.Bb], [s_])
                e_tr(p, ps[:, (i % 4) * 128:(i % 4 + 1) * 128], s_[:].rearrange("q a b c -> q (a b c)"), ident[:], [s_, ident], [ps])
                e_copy(p, "act", BbT[:, gpl, d, ri, :], ps[:, (i % 4) * 128:(i % 4 + 1) * 128], [ps], [BbT])
                for g2 in range(2):
                    p.op("pool", lambda h, s_=s_, g2=g2, gpl=gpl: h.memset(s_[g2 * 64:(g2 + 1) * 64, gpl, g2, :], 0.0),
                         reads=[], writes=[s_])
                i += 1
    p.op("pool", lambda h: h.memset(Cp[:], 0.0), writes=[Cp])
    Cv = Cp[:].rearrange("q g r (a b k) -> q g r a b k", a=4, b=2)
    for gpl in range(4):
        gp = ct * 4 + gpl
        for g2 in range(2):
            rows = slice(g2 * 64, (g2 + 1) * 64)
            e_copy(p, "dve", Cv[rows, gpl, 0, gpl, g2, :], S.cP[rows, 0, gp, :], [S.cP], [Cp])
            e_ts(p, "dve", Cv[rows, gpl, 1, gpl, g2, :], S.cP[rows, 1, gp, :], -1.0, None, ALU.mult, None, [S.cP], [Cp])


def s5_tables(p, S, col, cosT, sinT):
    p.op("pool", lambda h: h.memset(cosT[:, 0:1], 1.0), writes=[cosT])
    p.op("pool", lambda h: h.memset(sinT[:, 0:1], 0.0), writes=[sinT])
    R = [cosT, sinT, S.wc, S.ws, S.nws]
    for k in range(11):
        n = 1 << k
        wc = S.wc[:, k, col:col + 1]; ws = S.ws[:, k, col:col + 1]; nws = S.nws[:, k, col:col + 1]
        lo = slice(0, n); hi = slice(n, 2 * n)
        e_ts(p, "dve", cosT[:, hi], cosT[:, lo], wc, None, ALU.mult, None, R, [cosT])
        e_stt(p, cosT[:, hi], sinT[:, lo], nws, cosT[:, hi], ALU.mult, ALU.add, R, [cosT])
        e_ts(p, "dve", sinT[:, hi], sinT[:, lo], wc, None, ALU.mult, None, R, [sinT])
        e_stt(p, sinT[:, hi], cosT[:, lo], ws, sinT[:, hi], ALU.mult, ALU.add, R, [sinT])


def s5_pass(p, S, uT, tabsc, full, Fsum, kin, ident, yacc_evac=None):
    m = p.mark()
    cos2 = [p.alloc([128, 2048]) for _ in range(2)]; sin2 = [p.alloc([128, 2048]) for _ in range(2)]
    TL = 2048
    CH = 512
    nb = 2
    bur = [p.alloc([128, CH]) for _ in range(nb)]; bui = [p.alloc([128, CH]) for _ in range(nb)]
    m1 = [p.alloc([128, CH]) for _ in range(nb)]; m2 = [p.alloc([128, CH]) for _ in range(nb)]
    cr = [p.alloc([128, CH]) for _ in range(nb)]; ci = [p.alloc([128, CH]) for _ in range(nb)]
    kr = [p.alloc([128, CH]) for _ in range(nb)]; ki = [p.alloc([128, CH]) for _ in range(nb)]
    hr = [p.alloc([128, CH], BF16) for _ in range(nb)]; hi = [p.alloc([128, CH], BF16) for _ in range(nb)]
    tiny = p.alloc([128, 4])
    BbT2 = [p.alloc([128, 4, 2, 2, 128], BF16) for _ in range(2)]
    Cp2 = [p.alloc([128, 4, 2, 128], BF16) for _ in range(2)]
    srcs = [p.alloc([128, 4, 2, 16]) for _ in range(2)]
    for s_ in srcs:
        p.op("pool", lambda h, s_=s_: h.memset(s_[:], 0.0), writes=[s_])
    subs = [(0, 0, NCX), (1, NCX, NL)]
    it = 0
    ci_ = 0
    for ct in range(8):
        BbT = BbT2[ct % 2]; Cp = Cp2[ct % 2]
        s5_build_ct(p, S, ct, ident, BbT, Cp, srcs)
        for gpl in range(4):
            gp = ct * 4 + gpl
            for d in range(2):
                col = d * 32 + gp
                if not full:
                    s5_tables(p, S, col, cos2[0], sin2[0])
                    if d == 0:
                        cosT = cos2[0]; sinT = sin2[0]
                    else:
                        cosT = cos2[1]; sinT = sin2[1]
                        e_copy(p, "dve", rev_ap(cosT[:]), cos2[0][:], [cos2[0]], [cosT])
                        e_copy(p, "dve", rev_ap(sinT[:]), sin2[0][:], [sin2[0]], [sinT])
                    p.dma(tabsc[col, 0], cosT[:], reads=[cosT], writes=[tabsc], stream="tb")
                    p.dma(tabsc[col, 1], sinT[:], reads=[sinT], writes=[tabsc], stream="tb")
                else:
                    cosT = cos2[it % 2]; sinT = sin2[it % 2]
                    it += 1
                    p.dma(cosT[:], tabsc[col, 0], reads=[tabsc], writes=[cosT], stream="tbl")
                    p.dma(sinT[:], tabsc[col, 1], reads=[tabsc], writes=[sinT], stream="tbl")
                mcol = S.mag[:, col:col + 1]
                for (which, c0, L) in subs:
                    nch = (L + CH - 1) // CH
                    carry_r = None
                    for cj in range(nch):
                        n = min(CH, L)
                        a = cj * n if d == 0 else L - (cj + 1) * n
                        tau0 = cj * n
                        b_ = ci_ % nb
                        ci_ += 1
                        tok = slice(c0 + a, c0 + a + n)
                        pr = p.pb[5]; pi_ = p.pb[6]
                        e_mm(p, pr[:, 0:n], BbT[:, gpl, d, 0, :], uT[:, ct, tok], True, True, [BbT, uT], [pr])
                        e_mm(p, pi_[:, 0:n], BbT[:, gpl, d, 1, :], uT[:, ct, tok], True, True, [BbT, uT], [pi_])
                        e_copy(p, "act", bur[b_][:, 0:n], pr[:, 0:n], [pr], [bur[b_]])
                        e_copy(p, "act", bui[b_][:, 0:n], pi_[:, 0:n], [pi_], [bui[b_]])
                        br = bur[b_][:, 0:n]; bi = bui[b_][:, 0:n]
                        ts0 = tau0 if d == 0 else (TL - L) + a
                        cs = cosT[:, ts0:ts0 + n]; sn = sinT[:, ts0:ts0 + n]
                        R = [bur[b_], bui[b_], cosT, sinT]
                        e_tt(p, "dve", m1[b_][:, 0:n], cs, br, ALU.mult, R, [m1[b_]])
                        e_tt(p, "dve", m2[b_][:, 0:n], sn, bi, ALU.mult, R, [m2[b_]])
                        e_tt(p, "dve", m1[b_][:, 0:n], m1[b_][:, 0:n], m2[b_][:, 0:n], ALU.add, [m1[b_], m2[b_]], [m1[b_]])
                        e_tt(p, "dve", cr[b_][:, 0:n], cs, bi, ALU.mult, R, [cr[b_]])
                        e_tt(p, "dve", ci[b_][:, 0:n], sn, br, ALU.mult, R, [ci[b_]])
                        e_tt(p, "dve", cr[b_][:, 0:n], cr[b_][:, 0:n], ci[b_][:, 0:n], ALU.subtract, [cr[b_], ci[b_]], [cr[b_]])
                        if cj == 0:
                            if full and which == 1:
                                ini_r = kin[:, 0, col:col + 1]; ini_i = kin[:, 1, col:col + 1]; rd = [kin]
                            else:
                                ini_r = 0.0; ini_i = 0.0; rd = []
                        else:
                            ini_r = carry_r; ini_i = carry_i; rd = [carry_br, carry_bi]
                        mb = mcol.to_broadcast([128, n])
                        ko_r = kr[b_][:, 0:n]; ko_i = ki[b_][:, 0:n]; xi_r = m1[b_][:, 0:n]; xi_i = cr[b_][:, 0:n]
                        if d == 1:
                            ko_r = rev_ap(ko_r); ko_i = rev_ap(ko_i); xi_r = rev_ap(xi_r); xi_i = rev_ap(xi_i)
                        p.op("dve", lambda h, o=ko_r, x=xi_r, ini=ini_r, mb=mb: h.tensor_tensor_scan(
                            out=o, data0=mb, data1=x, initial=ini, op0=ALU.mult, op1=ALU.add),
                            reads=[m1[b_], S.mag] + rd, writes=[kr[b_]])
                        p.op("dve", lambda h, o=ko_i, x=xi_i, ini=ini_i, mb=mb: h.tensor_tensor_scan(
                            out=o, data0=mb, data1=x, initial=ini, op0=ALU.mult, op1=ALU.add),
                            reads=[cr[b_], S.mag] + rd, writes=[ki[b_]])
                        lastc = n - 1 if d == 0 else 0
                        carry_r = kr[b_][:, lastc:lastc + 1]; carry_i = ki[b_][:, lastc:lastc + 1]
                        carry_br = kr[b_]; carry_bi = ki[b_]
                        if full:
                            o_r = hr[b_][:, 0:n]; o_i = hi[b_][:, 0:n]
                            K = [kr[b_], ki[b_], cosT, sinT]
                            e_tt(p, "dve", m1[b_][:, 0:n], cs, kr[b_][:, 0:n], ALU.mult, K, [m1[b_]])
                            e_tt(p, "dve", m2[b_][:, 0:n], sn, ki[b_][:, 0:n], ALU.mult, K, [m2[b_]])
                            e_tt(p, "dve", o_r, m1[b_][:, 0:n], m2[b_][:, 0:n], ALU.subtract, [m1[b_], m2[b_]], [hr[b_]])
                            e_tt(p, "dve", cr[b_][:, 0:n], sn, kr[b_][:, 0:n], ALU.mult, K, [cr[b_]])
                            e_tt(p, "dve", ci[b_][:, 0:n], cs, ki[b_][:, 0:n], ALU.mult, K, [ci[b_]])
                            e_tt(p, "dve", o_i, cr[b_][:, 0:n], ci[b_][:, 0:n], ALU.add, [cr[b_], ci[b_]], [hi[b_]])
                            if which == 0:
                                bank = p.pb[0]; bsl = slice(0, n)
                            else:
                                bank = p.pb[1 + a // CH]; bsl = slice(0, n)
                            first = (gpl == 0 and d == 0)
                            last = (gpl == 3 and d == 1)
                            e_mm(p, bank[:, bsl], Cp[:, gpl, 0, :], hr[b_][:, 0:n], first, False, [Cp, hr[b_]], [bank])
                            e_mm(p, bank[:, bsl], Cp[:, gpl, 1, :], hi[b_][:, 0:n], False, last, [Cp, hi[b_]], [bank])
                    if not full:
                        fl = L - 1 if d == 0 else TL - L
                        csl = cosT[:, fl:fl + 1]; snl = sinT[:, fl:fl + 1]
                        Kt = [carry_br, carry_bi, cosT, sinT, tiny]
                        e_tt(p, "dve", tiny[:, 0:1], csl, carry_r, ALU.mult, Kt, [tiny])
                        e_tt(p, "dve", tiny[:, 1:2], snl, carry_i, ALU.mult, Kt, [tiny])
                        e_tt(p, "dve", Fsum[:, 0, which, col:col + 1], tiny[:, 0:1], tiny[:, 1:2], ALU.subtract, [tiny], [Fsum])
                        e_tt(p, "dve", tiny[:, 2:3], snl, carry_r, ALU.mult, Kt, [tiny])
                        e_tt(p, "dve", tiny[:, 3:4], csl, carry_i, ALU.mult, Kt, [tiny])
                        e_tt(p, "dve", Fsum[:, 1, which, col:col + 1], tiny[:, 2:3], tiny[:, 3:4], ALU.add, [tiny], [Fsum])
        if full:
            yacc_evac(ct)
    p.release(m)


def s5_incoming(p, S, Fsum, gath, onehot, kin):
    m = p.mark()
    I = p.alloc([128, 4, 2, 64])
    t1 = p.alloc([128, 32]); t2 = p.alloc([128, 32]); nr = p.alloc([128, 32]); ni = p.alloc([128, 32])
    f = slice(0, 32); b = slice(32, 64)
    R = [I, gath, Fsum, S.Ar, S.Ai, t1, t2, nr, ni]
    e_copy(p, "dve", I[:, 0, 0, f], Fsum[:, 0, 0, f], R, [I]); e_copy(p, "dve", I[:, 0, 1, f], Fsum[:, 1, 0, f], R, [I])
    for q in range(3):
        cmul(p, "dve", nr[:], ni[:], S.Ar[:, f], S.Ai[:, f], I[:, q, 0, f], I[:, q, 1, f], t1[:], t2[:], R, [t1, t2, nr, ni])
        e_tt(p, "dve", I[:, q + 1, 0, f], nr[:], gath[:, q, 0, f], ALU.add, R, [I])
        e_tt(p, "dve", I[:, q + 1, 1, f], ni[:], gath[:, q, 1, f], ALU.add, R, [I])
    e_copy(p, "dve", I[:, 3, 0, b], Fsum[:, 0, 0, b], R, [I]); e_copy(p, "dve", I[:, 3, 1, b], Fsum[:, 1, 0, b], R, [I])
    for q in (3, 2, 1):
        cmul(p, "dve", nr[:], ni[:], S.Ar[:, b], S.Ai[:, b], I[:, q, 0, b], I[:, q, 1, b], t1[:], t2[:], R, [t1, t2, nr, ni])
        e_tt(p, "dve", I[:, q - 1, 0, b], nr[:], gath[:, q, 0, b], ALU.add, R, [I])
        e_tt(p, "dve", I[:, q - 1, 1, b], ni[:], gath[:, q, 1, b], ALU.add, R, [I])
    own = p.alloc([128, 2, 64])
    e_ts(p, "dve", own[:], I[:, 0], onehot[:, 0:1], None, ALU.mult, None, [I, onehot], [own])
    for q in range(1, 4):
        e_stt(p, own[:], I[:, q], onehot[:, q:q + 1], own[:], ALU.mult, ALU.add, [I, onehot, own], [own])
    t3 = p.alloc([128, 64]); t4 = p.alloc([128, 64])
    cmul(p, "dve", kin[:, 0, :], kin[:, 1, :], S.cth[:], S.sth[:], own[:, 0, :], own[:, 1, :], t3[:], t4[:],
         [S.cth, S.sth, own, t3, t4, kin], [t3, t4, kin])
    p.release(m)


def build_s5_test():
    p = Prog(); p.init_pool()
    io = {}
    for name, shape in [("xl", [NL, D]), ("xc", [NCX, D]), ("cT", [128, 8, 2]), ("modw", [D, 6144]), ("modb", [1, 6144]),
                        ("n1g", [1, D]), ("ident", [128, 128]), ("lamP", [128, 2, 64]), ("ldtP", [128, 64]),
                        ("bP", [128, 2, 32, 16]), ("cP", [128, 2, 32, 16]), ("dP", [128, 8]), ("onehot", [128, 4])]:
        io[name] = p.dram(name, shape)
    dbg = p.dram("dbg", [128, 8, NT], kind="ExternalOutput")
    dbgF = p.dram("dbgF", [128, 2, 2, 64], kind="ExternalOutput")
    dbgK = p.dram("dbgK", [128, 2, 64], kind="ExternalOutput")
    modsc = p.dram("modsc", [2, 6144], kind="Internal")
    tabsc = p.dram("tabsc", [64, 2, 128, 2048], kind="Internal")
    fsrc = p.dram("fsrc", [128, 128], kind="Internal")
    fdst = p.dram("fdst", [512, 128], kind="Internal")
    ident = p.alloc([128, 128]); p.dma(ident[:], io["ident"][:], writes=[ident])
    onehot = p.alloc([128, 4]); p.dma(onehot[:], io["onehot"][:], writes=[onehot])
    dP = p.alloc([128, 8]); p.dma(dP[:], io["dP"][:], writes=[dP])
    phase_mod(p, io["cT"], io["modw"], io["modb"], modsc, ident)
    uT = p.alloc([128, 8, NT], BF16, name="uT")
    phase_hn(p, io["xl"], io["xc"], modsc, io["n1g"], ident, uT, 0, 1)
    S = s5_params(p, io, ident)
    Fsum = p.alloc([128, 2, 2, 64]); kin = p.alloc([128, 2, 64])
    s5_pass(p, S, uT, tabsc, False, Fsum, None, ident)
    p.dma(fsrc[:].rearrange("q (r c) -> q r c", r=2), Fsum[:, :, 1, :], reads=[Fsum], writes=[fsrc], stream="st")
    p.cc(lambda h: h.collective_compute("AllGather", ALU.bypass, replica_groups=[[0, 1, 2, 3], [4, 5, 6, 7]],
                                        ins=[fsrc.t.opt()], outs=[fdst.t.opt()]), reads=[fsrc], writes=[fdst])
    gath = p.alloc([128, 4, 2, 64])
    p.dma(gath[:], fdst[:].rearrange("(q x) (r c) -> x q r c", x=128, r=2), reads=[fdst], writes=[gath])
    s5_incoming(p, S, Fsum, gath, onehot, kin)
    p.dma(dbgF[:], Fsum[:], reads=[Fsum], stream="st"); p.dma(dbgK[:], kin[:], reads=[kin], stream="st")
    vbuf = [p.alloc([128, 512]) for _ in range(2)]

    def evac(ct):
        for bi_ in range(5):
            n = NCX if bi_ == 0 else 512
            c0 = 0 if bi_ == 0 else NCX + (bi_ - 1) * 512
            v = vbuf[bi_ % 2]
            e_stt(p, v[:, 0:n], uT[:, ct, c0:c0 + n], dP[:, ct:ct + 1], p.pb[bi_][:, 0:n], ALU.mult, ALU.add,
                  [uT, dP, p.pb[bi_]], [v])
            p.dma(dbg[:, ct, c0:c0 + n], v[:, 0:n], reads=[v], stream="st")

    s5_pass(p, S, uT, tabsc, True, None, kin, ident, evac)
    p.wait_all()
    return p


def host_inputs(z, layer=0):
    x = z["x"]; c = z["c"]; ctx = z["ctx"]; c_ctx = z["c_ctx"]
    lam = np.stack([z["s5_lam_re"][0], z["s5_lam_im"][0]], 0)
    lamP = lam.reshape(2, 2, 32, 2, 64).transpose(3, 4, 0, 1, 2).reshape(128, 2, 64)
    ldt = z["s5_log_dt"][0]
    ldtP = np.broadcast_to(ldt.reshape(2, 32, 2)[:, :, :, None], (2, 32, 2, 64)).transpose(2, 3, 0, 1).reshape(128, 64)
    b = np.stack([z["s5_b_re"][0], z["s5_b_im"][0]], 0)
    bP = b.reshape(2, 32, 2, 64, 16).transpose(2, 3, 0, 1, 4).reshape(128, 2, 32, 16)
    cc = np.stack([z["s5_c_re"][0], z["s5_c_im"][0]], 0)
    cP = cc.reshape(2, 32, 2, 16, 64).transpose(2, 4, 0, 1, 3).reshape(128, 2, 32, 16)
    dP = z["s5_d"][0].reshape(8, 128).T
    maps = []
    for k in range(8):
        bb = k // 4; q = k % 4
        cT = np.stack([c[bb], c_ctx], axis=-1).reshape(8, 128, 2).transpose(1, 0, 2)
        oh = np.zeros((128, 4), np.float32); oh[:, q] = 1
        maps.append(dict(xl=x[bb, q * NL:(q + 1) * NL], xc=ctx[bb], cT=np.ascontiguousarray(cT),
                         modw=z["mod_w"][layer], modb=z["mod_b"][layer][None], n1g=z["norm1_g"][layer][None],
                         ident=np.eye(128, dtype=np.float32), lamP=np.ascontiguousarray(lamP),
                         ldtP=np.ascontiguousarray(ldtP), bP=np.ascontiguousarray(bP), cP=np.ascontiguousarray(cP),
                         dP=np.ascontiguousarray(dP), onehot=oh))
    return maps


def ref_s5(z, bb, groups):
    f8 = np.float64
    x = z["x"][bb].astype(f8); ctx = z["ctx"][bb].astype(f8); c = z["c"][bb].astype(f8); c_ctx = z["c_ctx"].astype(f8)
    silu = lambda v: v / (1 + np.exp(-v))
    rms = lambda v, g: v / np.sqrt((v * v).mean(-1, keepdims=True) + 1e-6) * g
    mw = z["mod_w"][0].astype(f8); mb = z["mod_b"][0].astype(f8); g1 = z["norm1_g"][0].astype(f8)
    ml = silu(c) @ mw + mb; mc = silu(c_ctx) @ mw + mb
    hn = rms(x, g1) * (1 + ml[D:2 * D]) + ml[:D]
    cn = rms(ctx, g1) * (1 + mc[D:2 * D]) + mc[:D]
    out = {}
    for g in groups:
        ch = slice(g * 16, (g + 1) * 16)
        tot = np.zeros((NCX + 8192, 16))
        for d in range(2):
            lr = z["s5_lam_re"][0, d, g].astype(f8); li = z["s5_lam_im"][0, d, g].astype(f8)
            step = np.exp(z["s5_log_dt"][0, d, g].astype(f8))
            lamc = lr + 1j * li
            abar = np.exp(lamc * step)
            Bc = z["s5_b_re"][0, g].astype(f8) + 1j * z["s5_b_im"][0, g].astype(f8)
            Cc = z["s5_c_re"][0, g].astype(f8) + 1j * z["s5_c_im"][0, g].astype(f8)
            Bbar = ((abar - 1) / lamc)[:, None] * Bc
            seq = np.concatenate([cn[:, ch], hn[:, ch]], 0) if d == 0 else np.concatenate([cn[::-1, ch], hn[::-1, ch]], 0)
            bu = seq @ Bbar.T
            h = np.zeros(64, complex); ys = np.zeros((len(seq), 16))
            for t in range(len(seq)):
                h = abar * h + bu[t]
                ys[t] = (Cc @ h).real
            if d == 1:
                ys = np.concatenate([ys[:NCX][::-1], ys[NCX:][::-1]], 0)
            tot += ys
        u = np.concatenate([cn[:, ch], hn[:, ch]], 0)
        out[g] = tot + z["s5_d"][0, ch].astype(f8) * u
    return out


def gelu_evac(p, uT, dP, gT):
    vb = [p.alloc([128, 512]) for _ in range(2)]
    wb = [p.alloc([128, 512]) for _ in range(1)]
    cnt = [0]

    def evac(ct):
        for bi_ in range(5):
            n = NCX if bi_ == 0 else 512
            c0 = 0 if bi_ == 0 else NCX + (bi_ - 1) * 512
            v = vb[cnt[0] % 2]; w = wb[0]
            cnt[0] += 1
            e_stt(p, v[:, 0:n], uT[:, ct, c0:c0 + n], dP[:, ct:ct + 1], p.pb[bi_][:, 0:n], ALU.mult, ALU.add,
                  [uT, dP, p.pb[bi_]], [v])
            e_act(p, w[:, 0:n], v[:, 0:n], AF.Square, [v], [w])
            e_ts(p, "dve", w[:, 0:n], w[:, 0:n], 0.044715, 1.0, ALU.mult, ALU.add, [w], [w])
            e_tt(p, "dve", w[:, 0:n], w[:, 0:n], v[:, 0:n], ALU.mult, [w, v], [w])
            e_act(p, w[:, 0:n], w[:, 0:n], AF.Sigmoid, [w], [w], scale=1.5957691216057308)
            e_tt(p, "pool", gT[:, ct, c0:c0 + n], v[:, 0:n], w[:, 0:n], ALU.mult, [v, w], [gT])
    return evac


def load_w_bf16(p, dst, src_ap, w):
    p.dma(dst, src_ap, writes=w, eng="pool")


def phase_c(p, io, modsc, ident, gT, h2T, gates, x1sc, layer_glu=True):
    m = p.mark()
    W = p.alloc([128, 4, 8, 512], BF16)
    for nb in range(4):
        load_w_bf16(p, W[:, nb],
                    io["gluw"][:, nb * 512:(nb + 1) * 512].rearrange("(k q) n -> q k n", q=128), [W])
    gb = p.alloc([128, 2048]); load_bcast(p, gb[:], io["glub"][0:1, :], [], [gb])
    n2 = p.alloc([128, D]); load_bcast(p, n2[:], io["n2g"][0:1, :], [], [n2])
    G1 = []; A2 = []; SH2 = []
    for which in range(2):
        g1 = p.alloc([128, D]); a2 = p.alloc([128, D]); s2 = p.alloc([128, D])
        load_bcast(p, g1[:], modsc[which:which + 1, 2 * D:3 * D], [modsc], [g1])
        load_bcast(p, s2[:], modsc[which:which + 1, 3 * D:4 * D], [modsc], [s2])
        load_bcast(p, a2[:], modsc[which:which + 1, 4 * D:5 * D], [modsc], [a2])
        e_stt(p, a2[:], a2[:], 1.0, n2[:], ALU.add, ALU.mult, [a2, n2], [a2])
        G1.append(g1); A2.append(a2); SH2.append(s2)
    rw = p.alloc([128, 8, 16]); p.dma(rw[:], io["rw"][:].rearrange("(k q) n -> q k n", q=128), writes=[rw])
    rb = p.alloc([128, 16]); load_bcast(p, rb[:], io["rb"][0:1, :], [], [rb])
    xs = [p.alloc([128, D]) for _ in range(2)]
    val = p.alloc([128, D]); gat = p.alloc([128, D]); x1 = [p.alloc([128, D]) for _ in range(2)]
    hn2 = p.alloc([128, D]); junk = p.alloc([128, D]); hT32 = p.alloc([128, 8, 128])
    sm = p.alloc([128, 128])
    ss = p.alloc([128, 1]); rs = p.alloc([128, 1])
    for t in range(18):
        which = 1 if t < 2 else 0
        src = io["xc"][t * 128:(t + 1) * 128, :] if t < 2 else io["xl"][(t - 2) * 128:(t - 1) * 128, :]
        x = xs[t % 2]; xo = x1[t % 2]
        p.dma(x[:], src, writes=[x])
        tok = slice(t * 128, (t + 1) * 128)
        for nb in range(4):
            ps = p.pb[nb]
            for k in range(8):
                e_mm(p, ps[:], gT[:, k, tok], W[:, nb, k, :], k == 0, k == 7, [gT, W], [ps])
        for nb in range(2):
            e_tt(p, "dve", val[:, nb * 512:(nb + 1) * 512], p.pb[nb][:], gb[:, nb * 512:(nb + 1) * 512], ALU.add,
                 [p.pb[nb], gb], [val])
            e_tt(p, "dve", gat[:, nb * 512:(nb + 1) * 512], p.pb[2 + nb][:], gb[:, D + nb * 512:D + (nb + 1) * 512],
                 ALU.add, [p.pb[2 + nb], gb], [gat])
        e_act(p, gat[:], gat[:], AF.Sigmoid, [gat], [gat])
        e_tt(p, "dve", val[:], val[:], gat[:], ALU.mult, [val, gat], [val])
        e_tt(p, "dve", val[:], val[:], G1[which][:], ALU.mult, [val, G1[which]], [val])
        e_tt(p, "dve", xo[:], val[:], x[:], ALU.add, [val, x], [xo])
        p.dma(x1sc[tok, :], xo[:], reads=[xo], writes=[x1sc])
        e_act(p, junk[:], xo[:], AF.Square, [xo], [junk, ss], accum_out=ss[:])
        e_ts(p, "dve", rs[:], ss[:], 1.0 / D, 1e-6, ALU.mult, ALU.add, [ss], [rs])
        e_act(p, rs[:], rs[:], AF.Ln, [rs], [rs])
        e_act(p, rs[:], rs[:], AF.Exp, [rs], [rs], scale=-0.5)
        e_stt(p, hn2[:], xo[:], rs[:], A2[which][:], ALU.mult, ALU.mult, [xo, rs, A2[which]], [hn2])
        e_tt(p, "dve", hn2[:], hn2[:], SH2[which][:], ALU.add, [hn2, SH2[which]], [hn2])
        for half in range(2):
            ps = p.pb[4 + half]
            for c4 in range(4):
                ct = half * 4 + c4
                e_tr(p, ps[:, c4 * 128:(c4 + 1) * 128], hn2[:, ct * 128:(ct + 1) * 128], ident[:], [hn2, ident], [ps])
            pv = ps[:].rearrange("q (c n) -> q c n", c=4)
            e_copy(p, "act", h2T[:, half * 4:(half + 1) * 4, tok], pv, [ps], [h2T])
            e_copy(p, "dve", hT32[:, half * 4:(half + 1) * 4, :], pv, [ps], [hT32])
        pl = p.pb[6]
        for k in range(8):
            e_mm(p, pl[:, 0:16], hT32[:, k, :], rw[:, k, :], k == 0, k == 7, [hT32, rw], [pl])
        routing(p, pl, rb, sm, gates[:, t, :], gates)
    p.release(m)


def routing(p, pl, rb, sm, gout, gates_buf):
    R = [sm]
    s = sm[:, 0:16]; sel2 = sm[:, 16:48]; ps_ = sm[:, 48:72]; gs = sm[:, 72:76]; t2 = sm[:, 76:78]
    gmax = sm[:, 78:79]; Gm = sm[:, 80:84]; g1 = sm[:, 84:100]; cnt = sm[:, 100:116]; wsum = sm[:, 116:117]
    e_act(p, s, pl[:, 0:16], AF.Sigmoid, [pl], R)
    sel2v = sel2.rearrange("q (g e) -> q g e", g=4)
    sv = s.rearrange("q (g e) -> q g e", g=4)
    rbv = rb[:].rearrange("q (g e) -> q g e", g=4)
    e_tt(p, "dve", sel2v[:, :, 0:4], sv, rbv, ALU.add, R + [rb], R)
    e_copy(p, "dve", sel2v[:, :, 4:8], sel2v[:, :, 0:4], R, R)
    pv = ps_.rearrange("q (g e) -> q g e", g=4)
    pairs = [(0, 1), (0, 2), (0, 3), (1, 2), (1, 3), (2, 3)]
    for i, (a, b) in enumerate(pairs):
        e_tt(p, "dve", pv[:, :, i:i + 1], sel2v[:, :, a:a + 1], sel2v[:, :, b:b + 1], ALU.add, R, R)
    e_tt(p, "dve", pv[:, :, 0:3], pv[:, :, 0:3], pv[:, :, 3:6], ALU.max, R, R)
    e_tt(p, "dve", pv[:, :, 0:1], pv[:, :, 0:1], pv[:, :, 1:2], ALU.max, R, R)
    e_tt(p, "dve", gs.unsqueeze(2), pv[:, :, 0:1], pv[:, :, 2:3], ALU.max, R, R)
    e_tt(p, "dve", t2, gs[:, 0:2], gs[:, 2:4], ALU.max, R, R)
    e_tt(p, "dve", gmax, t2[:, 0:1], t2[:, 1:2], ALU.max, R, R)
    e_ts(p, "dve", Gm, gs, gmax, 1.0, ALU.is_ge, ALU.mult, R, R)
    g1v = g1.rearrange("q (g e) -> q g e", g=4); cv = cnt.rearrange("q (g e) -> q g e", g=4)
    e_tt(p, "dve", cv, sel2v[:, :, 1:5], sel2v[:, :, 0:4], ALU.is_gt, R, R)
    for r in (2, 3):
        e_tt(p, "dve", g1v, sel2v[:, :, r:r + 4], sel2v[:, :, 0:4], ALU.is_gt, R, R)
        e_tt(p, "dve", cv, cv, g1v, ALU.add, R, R)
    e_ts(p, "dve", cv, cv, 1.5, 1.0, ALU.is_lt, ALU.mult, R, R)
    e_tt(p, "dve", cv, cv, Gm.unsqueeze(2).to_broadcast([128, 4, 4]), ALU.mult, R, R)
    e_tt(p, "dve", cnt, cnt, s, ALU.mult, R, R)
    p.op("dve", lambda h: h.reduce_sum(out=wsum, in_=cnt, axis=AX.X), reads=R, writes=R)
    p.op("dve", lambda h: h.reciprocal(out=wsum, in_=wsum), reads=R, writes=R)
    e_ts(p, "dve", gout, cnt, wsum, 1.0, ALU.mult, ALU.mult, R, [gates_buf])


def phase_moe(p, io, layer_w, modsc, h2T, gates, x1sc, ntiles, out_cb, mod_which_of_tile):
    m = p.mark()
    w1d, w3d, w2d = layer_w
    ntok = ntiles * 128
    yacc = p.alloc([128, ntiles, D], name="yacc")
    for t0 in range(0, ntiles, 4):
        t1 = min(ntiles, t0 + 4)
        p.op("pool", lambda h, t0=t0, t1=t1: h.memset(yacc[:, t0:t1, :], 0.0), writes=[yacc])
    w1 = [p.alloc([128, 8, 512], BF16) for _ in range(2)]; w3 = [p.alloc([128, 8, 512], BF16) for _ in range(2)]
    w2 = [p.alloc([128, 4, D], BF16) for _ in range(2)]
    hT = [p.alloc([128, 4, 512], BF16) for _ in range(2)]
    s1 = [p.alloc([128, 512]) for _ in range(2)]
    blocks = [(b0, min(512, ntok - b0)) for b0 in range(0, ntok, 512)]
    ib = 0; iy = 0; ih = 0
    for e in range(16):
        a1 = w1[e % 2]; a3 = w3[e % 2]; a2 = w2[e % 2]
        load_w_bf16(p, a1[:], w1d[e].rearrange("(k q) n -> q k n", q=128), [a1])
        load_w_bf16(p, a3[:], w3d[e].rearrange("(k q) n -> q k n", q=128), [a3])
        load_w_bf16(p, a2[:], w2d[e].rearrange("(k q) n -> q k n", q=128), [a2])
        for (b0, n) in blocks:
            h = hT[ib % 2]; ib += 1
            for hc in range(4):
                p1 = p.pb[ih % 2]; p3 = p.pb[2 + ih % 2]; sb1 = s1[ih % 2]; ih += 1
                for k in range(8):
                    e_mm(p, p1[:, 0:n], a1[:, k, hc * 128:(hc + 1) * 128], h2T[:, k, b0:b0 + n], k == 0, k == 7, [a1, h2T], [p1])
                for k in range(8):
                    e_mm(p, p3[:, 0:n], a3[:, k, hc * 128:(hc + 1) * 128], h2T[:, k, b0:b0 + n], k == 0, k == 7, [a3, h2T], [p3])
                e_act(p, sb1[:, 0:n], p1[:, 0:n], AF.Silu, [p1], [sb1])
                e_tt(p, "dve", h[:, hc, 0:n], sb1[:, 0:n], p3[:, 0:n], ALU.mult, [sb1, p3], [h])
            for tt in range(n // 128):
                t = b0 // 128 + tt
                for dh in range(2):
                    py = p.pb[4 + iy % 4]; iy += 1
                    for hc in range(4):
                        e_mm(p, py[:], h[:, hc, tt * 128:(tt + 1) * 128], a2[:, hc, dh * 512:(dh + 1) * 512],
                             hc == 0, hc == 3, [h, a2], [py])
                    ya = yacc[:, t, dh * 512:(dh + 1) * 512]
                    e_stt(p, ya, py[:], gates[:, t, e:e + 1], ya, ALU.mult, ALU.add, [py, gates, yacc], [yacc])
    G2 = []
    for which in range(2):
        g2 = p.alloc([128, D]); load_bcast(p, g2[:], modsc[which:which + 1, 5 * D:6 * D], [modsc], [g2]); G2.append(g2)
    xb = [p.alloc([128, D]) for _ in range(2)]
    for t in range(ntiles):
        which = mod_which_of_tile(t)
        x = xb[t % 2]
        p.dma(x[:], x1sc[t * 128:(t + 1) * 128, :], reads=[x1sc], writes=[x])
        e_tt(p, "dve", yacc[:, t, :], yacc[:, t, :], G2[which][:], ALU.mult, [yacc, G2[which]], [yacc])
        e_tt(p, "dve", x[:], x[:], yacc[:, t, :], ALU.add, [x, yacc], [x])
        out_cb(t, x)
    p.release(m)


L0_INPUTS = [("xl", [NL, D]), ("xc", [NCX, D]), ("cT", [128, 8, 2]), ("modw", [D, 6144]), ("modb", [1, 6144]),
             ("n1g", [1, D]), ("ident", [128, 128]), ("lamP", [128, 2, 64]), ("ldtP", [128, 64]),
             ("bP", [128, 2, 32, 16]), ("cP", [128, 2, 32, 16]), ("dP", [128, 8]), ("onehot", [128, 4]),
             ("gluw", [D, 2048]), ("glub", [1, 2048]), ("n2g", [1, D]), ("rw", [D, 16]), ("rb", [1, 16]),
             ("w1", [16, D, 512]), ("w3", [16, D, 512]), ("w2", [16, 512, D])]


def layer0(p, io, sc, ident, onehot, out_cb, skip_s5=False, stop_after_c=False, dbg=None):
    dP = p.alloc([128, 8]); p.dma(dP[:], io["dP"][:], writes=[dP])
    phase_mod(p, io["cT"], io["modw"], io["modb"], sc["modsc"], ident)
    gates = p.alloc([128, 18, 16])
    hi0 = p.hi
    gT = p.alloc([128, 8, NT], BF16, name="gT", hi=True)
    m_g = p.mark()
    uT = p.alloc([128, 8, NT], BF16, name="uT")
    phase_hn(p, io["xl"], io["xc"], sc["modsc"], io["n1g"], ident, uT, 0, 1)
    if skip_s5:
        for ct in range(8):
            e_copy(p, "dve", gT[:, ct, :], uT[:, ct, :], [uT], [gT])
    S = None if skip_s5 else s5_params(p, io, ident)
    Fsum = p.alloc([128, 2, 2, 64]); kin = p.alloc([128, 2, 64])
    if not skip_s5:
      s5_pass(p, S, uT, sc["tabsc"], False, Fsum, None, ident)
    p.dma(sc["fsrc"][:].rearrange("q (r c) -> q r c", r=2), Fsum[:, :, 1, :], reads=[Fsum], writes=[sc["fsrc"]])
    p.cc(lambda h: h.collective_compute("AllGather", ALU.bypass, replica_groups=[[0, 1, 2, 3], [4, 5, 6, 7]],
                                        ins=[sc["fsrc"].t.opt()], outs=[sc["fdst"].t.opt()]),
         reads=[sc["fsrc"]], writes=[sc["fdst"]])
    gath = p.alloc([128, 4, 2, 64])
    p.dma(gath[:], sc["fdst"][:].rearrange("(q x) (r c) -> x q r c", x=128, r=2), reads=[sc["fdst"]], writes=[gath])
    if not skip_s5:
        s5_incoming(p, S, Fsum, gath, onehot, kin)
        evac = gelu_evac(p, uT, dP, gT)
        s5_pass(p, S, uT, sc["tabsc"], True, None, kin, ident, evac)
    p.release(m_g)
    h2T = p.alloc([128, 8, NT], BF16, name="h2T")
    phase_c(p, io, sc["modsc"], ident, gT, h2T, gates, sc["x1sc"])
    p.hi = hi0
    if dbg is not None:
        p.dma(dbg["gates"][:], gates[:], reads=[gates])
    if stop_after_c:
        return
    phase_moe(p, io, (io["w1"], io["w3"], io["w2"]), sc["modsc"], h2T, gates, sc["x1sc"], 18,
              out_cb, lambda t: 1 if t < 2 else 0)


def build_l0_test():
    p = Prog(); p.init_pool()
    io = {name: p.dram(name, shape) for name, shape in L0_INPUTS}
    xo = p.dram("xo", [NT, D], kind="ExternalOutput")
    sc = dict(modsc=p.dram("modsc", [2, 6144], kind="Internal"),
              tabsc=p.dram("tabsc", [64, 2, 128, 2048], kind="Internal"),
              fsrc=p.dram("fsrc", [128, 128], kind="Internal"), fdst=p.dram("fdst", [512, 128], kind="Internal"),
              x1sc=p.dram("x1sc", [NT, D], kind="Internal"))
    ident = p.alloc([128, 128]); p.dma(ident[:], io["ident"][:], writes=[ident])
    onehot = p.alloc([128, 4]); p.dma(onehot[:], io["onehot"][:], writes=[onehot])

    def out_cb(t, x):
        p.dma(xo[t * 128:(t + 1) * 128, :], x[:], reads=[x])

    layer0(p, io, sc, ident, onehot, out_cb)
    p.wait_all()
    return p


def host_inputs_l0(z):
    maps = host_inputs(z, 0)
    for k in range(8):
        maps[k].update(gluw=z["s5_glu_w"][0], glub=z["s5_glu_b"][0][None], n2g=z["norm2_g"][0][None],
                       rw=z["router_w"], rb=z["router_b"][None], w1=z["moe_w1"][0], w3=z["moe_w3"][0], w2=z["moe_w2"][0])
    return maps


NCH = 66
PADW = 8456
CTX0 = 2
LAT0 = 262
RSW = 1028

L1_INPUTS = [("cT", [128, 8, 2]), ("modw1", [D, 6144]), ("modb1", [1, 6144]), ("n1g1", [1, D]), ("n2g1", [1, D]),
             ("fing", [1, D]), ("wz", [D, 512]), ("wxbc", [D, 1024]), ("wdt", [D, 16]), ("convw", [128, 8, 5]),
             ("convb", [128, 8]), ("dtb", [1, 16]), ("alog", [1, 16]), ("dsk", [1, 8]), ("sng", [1, 512]),
             ("outw", [512, D]), ("triU", [128, 128]), ("triL", [128, 128]), ("ones", [128, 128]),
             ("rw", [D, 16]), ("rb", [1, 16]), ("w1b", [16, D, 512]), ("w3b", [16, D, 512]), ("w2b", [16, 512, D])]


def l1_scratch(p):
    sc = {}
    sc["modsc1"] = p.dram("modsc1", [2, 6144], kind="Internal")
    sc["pre"] = p.dram("pre", [8, 128, PADW], kind="Internal")
    sc["X"] = p.dram("Xs", [NCH, 128, 512], BF16, kind="Internal")
    sc["Bt"] = p.dram("Bts", [NCH, 128, 256], BF16, kind="Internal")
    sc["BT"] = p.dram("BTs", [NCH, 128, 256], BF16, kind="Internal")
    sc["CT"] = p.dram("CTs", [NCH, 128, 256], BF16, kind="Internal")
    sc["dts"] = p.dram("dts", [NCH, 128, 16], kind="Internal")
    sc["Z"] = p.dram("Zs", [NCH, 128, 512], BF16, kind="Internal")
    sc["yf"] = p.dram("yfs", [NCH, 128, 512], kind="Internal")
    sc["yb"] = p.dram("ybs", [NCH, 128, 512], kind="Internal")
    sc["rsrc"] = p.dram("rsrc", [8192, RSW], kind="Internal")
    sc["rdst"] = p.dram("rdst", [2048, RSW], kind="Internal")
    sc["x1b"] = p.dram("x1b", [2048, D], kind="Internal")
    return sc


def chunk_rows(xg, ctx1, c):
    if c < 2:
        return ctx1[c * 128:(c + 1) * 128, :]
    return xg[:].rearrange("(r w) d -> w r d", w=64)[c - 2]


def l1_proj(p, io, sc, xg, ctx1, ident):
    m = p.mark()
    modsc = sc["modsc1"]
    g_sb = p.alloc([128, D]); load_bcast(p, g_sb[:], io["n1g1"][0:1, :], [], [g_sb])
    A = []; SH = []
    for which in range(2):
        a = p.alloc([128, D]); s = p.alloc([128, D])
        load_bcast(p, a[:], modsc[which:which + 1, D:2 * D], [modsc], [a])
        load_bcast(p, s[:], modsc[which:which + 1, 0:D], [modsc], [s])
        e_stt(p, a[:], a[:], 1.0, g_sb[:], ALU.add, ALU.mult, [a, g_sb], [a])
        A.append(a); SH.append(s)
    Wx = p.alloc([128, 8, 1024], BF16); Wz = p.alloc([128, 8, 512], BF16); Wd = p.alloc([128, 8, 16])
    for half in range(2):
        load_w_bf16(p, Wx[:, :, half * 512:(half + 1) * 512] if False else Wx[:, half * 4:(half + 1) * 4, :],
                    io["wxbc"][half * 512:(half + 1) * 512, :].rearrange("(k q) n -> q k n", q=128), [Wx])
    load_w_bf16(p, Wz[:], io["wz"][:].rearrange("(k q) n -> q k n", q=128), [Wz])
    p.dma(Wd[:], io["wdt"][:].rearrange("(k q) n -> q k n", q=128), writes=[Wd])
    dtb = p.alloc([128, 16]); load_bcast(p, dtb[:], io["dtb"][0:1, :], [], [dtb])
    zero = p.alloc([128, 8]); p.op("pool", lambda h: h.memset(zero[:], 0.0), writes=[zero])
    for ft in range(8):
        for c0 in (0, CTX0 + 256, LAT0 - 2, LAT0 + 8192):
            p.dma(sc["pre"][ft, :, c0:c0 + 2], zero[:, 0:2], reads=[zero], writes=[sc["pre"]])
    xs = [p.alloc([128, D]) for _ in range(2)]; ys = [p.alloc([128, D]) for _ in range(2)]
    junk = p.alloc([128, D]); ss = p.alloc([128, 1]); rs = p.alloc([128, 1])
    hT = [p.alloc([128, 8, 512], BF16) for _ in range(2)]
    hT32 = p.alloc([128, 8, 128])
    zt = [p.alloc([128, 512], BF16) for _ in range(2)]
    dtt = [p.alloc([128, 16]) for _ in range(2)]
    pre_sb = [p.alloc([128, 512]) for _ in range(4)]
    blocks = [(0, [0, 1])] + [(1, [2 + 4 * b + i for i in range(4)]) for b in range(16)]
    ib = 0; it = 0; ip = 0
    for (islat, chunks) in blocks:
        which = 0 if islat else 1
        h = hT[ib % 2]; ib += 1
        n = 128 * len(chunks)
        for ti, c in enumerate(chunks):
            x = xs[it % 2]; y = ys[it % 2]; it += 1
            p.dma(x[:], chunk_rows(xg, ctx1, c), reads=[xg, ctx1], writes=[x])
            e_act(p, junk[:], x[:], AF.Square, [x], [junk, ss], accum_out=ss[:])
            e_ts(p, "dve", rs[:], ss[:], 1.0 / D, 1e-6, ALU.mult, ALU.add, [ss], [rs])
            e_act(p, rs[:], rs[:], AF.Ln, [rs], [rs])
            e_act(p, rs[:], rs[:], AF.Exp, [rs], [rs], scale=-0.5)
            e_stt(p, y[:], x[:], rs[:], A[which][:], ALU.mult, ALU.mult, [x, rs, A[which]], [y])
            e_tt(p, "dve", y[:], y[:], SH[which][:], ALU.add, [y, SH[which]], [y])
            for half in range(2):
                ps = p.pb[half]
                for c4 in range(4):
                    ct = half * 4 + c4
                    e_tr(p, ps[:, c4 * 128:(c4 + 1) * 128], y[:, ct * 128:(ct + 1) * 128], ident[:], [y, ident], [ps])
                pv = ps[:].rearrange("q (c n) -> q c n", c=4)
                e_copy(p, "act", h[:, half * 4:(half + 1) * 4, ti * 128:(ti + 1) * 128], pv, [ps], [h])
                e_copy(p, "dve", hT32[:, half * 4:(half + 1) * 4, :], pv, [ps], [hT32])
            tokc = slice(ti * 128, (ti + 1) * 128)
            if islat:
                pz = p.pb[2]
                for k in range(8):
                    e_mm(p, pz[:], h[:, k, tokc], Wz[:, k, :], k == 0, k == 7, [h, Wz], [pz])
                z = zt[it % 2]
                e_act(p, z[:], pz[:], AF.Silu, [pz], [z])
                p.dma(sc["Z"][c], z[:], reads=[z], writes=[sc["Z"]])
            pd = p.pb[3]
            for k in range(8):
                e_mm(p, pd[:, 0:16], hT32[:, k, :], Wd[:, k, :], k == 0, k == 7, [hT32, Wd], [pd])
            dt_ = dtt[it % 2]
            e_tt(p, "dve", dt_[:], pd[:, 0:16], dtb[:], ALU.add, [pd, dtb], [dt_])
            e_act(p, dt_[:], dt_[:], AF.Exp, [dt_], [dt_])
            e_ts(p, "dve", dt_[:], dt_[:], 1.0, 1.0, ALU.add, ALU.mult, [dt_], [dt_])
            e_act(p, dt_[:], dt_[:], AF.Ln, [dt_], [dt_])
            p.dma(sc["dts"][c], dt_[:], reads=[dt_], writes=[sc["dts"]])
        col0 = (LAT0 + (chunks[0] - 2) * 128) if islat else CTX0
        for ft in range(8):
            px = p.pb[4 + ft % 4]
            for k in range(8):
                e_mm(p, px[:, 0:n], Wx[:, k, ft * 128:(ft + 1) * 128], h[:, k, 0:n], k == 0, k == 7, [Wx, h], [px])
            o = pre_sb[ip % 4]; ip += 1
            e_copy(p, "act", o[:, 0:n], px[:, 0:n], [px], [o])
            p.dma(sc["pre"][ft, :, col0:col0 + n], o[:, 0:n], reads=[o], writes=[sc["pre"]])
    p.release(m)


def l1_conv(p, io, sc, identb):
    m = p.mark()
    cw = p.alloc([128, 8, 5]); cb = p.alloc([128, 8])
    p.dma(cw[:], io["convw"][:], writes=[cw]); p.dma(cb[:], io["convb"][:], writes=[cb])
    inb = [p.alloc([128, 516]) for _ in range(3)]
    acc = [p.alloc([128, 512]) for _ in range(2)]
    act = [p.alloc([128, 8, 512], BF16) for _ in range(2)]
    xo = [p.alloc([128, 512], BF16) for _ in range(4)]; bo = [p.alloc([128, 256], BF16) for _ in range(4)]
    blocks = [(0, [0, 1])] + [(1, [2 + 4 * b + i for i in range(4)]) for b in range(16)]
    ii = 0; ia = 0; ib = 0; ix = 0
    for (islat, chunks) in blocks:
        n = 128 * len(chunks)
        col0 = (LAT0 + (chunks[0] - 2) * 128) if islat else CTX0
        a8 = act[ib % 2]; ib += 1
        for ft in range(8):
            xin = inb[ii % 3]; ii += 1
            p.dma(xin[:, 0:n + 4], sc["pre"][ft, :, col0 - 2:col0 + n + 2], reads=[sc["pre"]], writes=[xin])
            a = acc[ia % 2]; ia += 1
            e_ts(p, "dve", a[:, 0:n], xin[:, 0:n], cw[:, ft, 0:1], cb[:, ft:ft + 1], ALU.mult, ALU.add, [xin, cw, cb], [a])
            for k in range(1, 5):
                e_stt(p, a[:, 0:n], xin[:, k:k + n], cw[:, ft, k:k + 1], a[:, 0:n], ALU.mult, ALU.add, [xin, cw, a], [a])
            e_act(p, a8[:, ft, 0:n], a[:, 0:n], AF.Silu, [a], [a8])
        for ti, c in enumerate(chunks):
            tok = slice(ti * 128, (ti + 1) * 128)
            pt = p.pb[ix % 2]; ptb = pt.t.bitcast(BF16)
            x_o = xo[ix % 4]; b_o = bo[ix % 4]; ix += 1
            for ft in range(6):
                e_tr(p, ptb[:, ft * 128:(ft + 1) * 128], a8[:, ft, tok], identb[:], [a8, identb], [pt])
            e_copy(p, "act", x_o[:], ptb[:, 0:512], [pt], [x_o])
            e_copy(p, "dve", b_o[:], ptb[:, 512:768], [pt], [b_o])
            p.dma(sc["X"][c], x_o[:], reads=[x_o], writes=[sc["X"]])
            p.dma(sc["Bt"][c], b_o[:], reads=[b_o], writes=[sc["Bt"]])
            p.dma(sc["BT"][c].rearrange("q (g s) -> q g s", g=2), a8[:, 4:6, tok], reads=[a8], writes=[sc["BT"]])
            p.dma(sc["CT"][c].rearrange("q (g s) -> q g s", g=2), a8[:, 6:8, tok], reads=[a8], writes=[sc["CT"]])
    p.release(m)


def l1_ssd(p, io, sc):
    m = p.mark()
    triU = p.alloc([128, 128]); triL = p.alloc([128, 128]); ones = p.alloc([128, 128])
    p.dma(triU[:], io["triU"][:], writes=[triU]); p.dma(triL[:], io["triL"][:], writes=[triL])
    p.dma(ones[:], io["ones"][:], writes=[ones])
    Aneg = p.alloc([128, 16]); load_bcast(p, Aneg[:], io["alog"][0:1, :], [], [Aneg])
    e_act(p, Aneg[:], Aneg[:], AF.Exp, [Aneg], [Aneg])
    e_ts(p, "dve", Aneg[:], Aneg[:], -1.0, 0.0, ALU.mult, ALU.add, [Aneg], [Aneg])
    dsk = p.alloc([128, 8]); load_bcast(p, dsk[:], io["dsk"][0:1, :], [], [dsk])
    sng = p.alloc([128, 512]); load_bcast(p, sng[:], io["sng"][0:1, :], [], [sng])
    Wo = p.alloc([128, 4, D], BF16); load_w_bf16(p, Wo[:], io["outw"][:].rearrange("(k q) n -> q k n", q=128), [Wo])
    identb = p.identb
    nb = 2

    def mk():
        T = {}
        T["S"] = p.alloc([128, 512]); T["Sb"] = p.alloc([128, 512], BF16)
        T["X"] = [p.alloc([128, 512], BF16) for _ in range(nb)]; T["Bt"] = [p.alloc([128, 256], BF16) for _ in range(nb)]
        T["BT"] = [p.alloc([128, 2, 128], BF16) for _ in range(nb)]; T["CT"] = [p.alloc([128, 2, 128], BF16) for _ in range(nb)]
        T["dt"] = [p.alloc([128, 16]) for _ in range(nb)]
        T["da"] = p.alloc([128, 8]); T["dabc"] = p.alloc([128, 8, 128]); T["acs"] = p.alloc([128, 8]); T["ea"] = p.alloc([128, 8])
        T["wdec"] = p.alloc([128, 8]); T["dch"] = p.alloc([128, 8]); T["BCm"] = p.alloc([128, 2, 128], BF16)
        T["tmpH"] = [p.alloc([128, 4, 128]) for _ in range(2)]; T["exH"] = [p.alloc([128, 4, 128], BF16) for _ in range(2)]
        T["MH"] = [p.alloc([128, 4, 128], BF16) for _ in range(2)]
        T["xdt"] = p.alloc([128, 512], BF16); T["xw"] = p.alloc([128, 512], BF16); T["yt"] = [p.alloc([128, 512]) for _ in range(2)]
        return T

    TT = [mk(), mk()]
    orders = [list(range(NCH)), [1, 0] + list(range(NCH - 1, 1, -1))]
    ydst = [sc["yf"], sc["yb"]]

    def chunk(d, pos, c):
        T = TT[d]
        b_ = pos % nb
        islat = c >= 2
        tri = triU if d == 0 else triL
        S = T["S"]; Sb = T["Sb"]
        X = T["X"][b_]; Bt = T["Bt"][b_]; BT = T["BT"][b_]; CT = T["CT"][b_]; dt = T["dt"][b_]
        da = T["da"]; dabc = T["dabc"]; acs = T["acs"]; ea = T["ea"]; wdec = T["wdec"]; dch = T["dch"]; BCm = T["BCm"]
        xdt = T["xdt"]; xw = T["xw"]; yt = T["yt"][b_]
        p.dma(X[:], sc["X"][c], reads=[sc["X"]], writes=[X])
        p.dma(Bt[:], sc["Bt"][c], reads=[sc["Bt"]], writes=[Bt])
        p.dma(BT[:], sc["BT"][c].rearrange("q (g s) -> q g s", g=2), reads=[sc["BT"]], writes=[BT])
        p.dma(CT[:], sc["CT"][c].rearrange("q (g s) -> q g s", g=2), reads=[sc["CT"]], writes=[CT])
        p.dma(dt[:], sc["dts"][c], reads=[sc["dts"]], writes=[dt])
        dtd = dt[:, d * 8:(d + 1) * 8]
        if pos == 0:
            p.op("pool", lambda h: h.memset(S[:], 0.0), writes=[S])
            p.op("pool", lambda h: h.memset(Sb[:], 0.0), writes=[Sb])
        e_tt(p, "dve", da[:], dtd, Aneg[:, d * 8:(d + 1) * 8], ALU.mult, [dt, Aneg], [da])
        e_copy(p, "dve", dabc[:], da[:].unsqueeze(2).to_broadcast([128, 8, 128]), [da], [dabc])
        p0 = p.pb[0] if d == 0 else p.pb[7]
        e_mm(p, p0[:, 0:8], tri[:], da[:], True, True, [tri, da], [p0])
        e_mm(p, p0[:, 8:16], ones[:], da[:], True, True, [ones, da], [p0])
        e_copy(p, "dve", acs[:], p0[:, 0:8], [p0], [acs])
        e_act(p, ea[:], p0[:, 0:8], AF.Exp, [p0], [ea])
        e_tt(p, "dve", wdec[:], p0[:, 8:16], acs[:], ALU.subtract, [p0, acs], [wdec])
        e_act(p, wdec[:], wdec[:], AF.Exp, [wdec], [wdec])
        e_act(p, dch[:], p0[:, 8:16], AF.Exp, [p0], [dch])
        p1 = p.pb[1]
        for g in range(2):
            e_mm(p, p1[:, g * 128:(g + 1) * 128], BT[:, g, :], CT[:, g, :], True, True, [BT, CT], [p1])
        e_tt(p, "dve", BCm[:], p1[:, 0:256].rearrange("q (g s) -> q g s", g=2),
             tri[:].unsqueeze(1).to_broadcast([128, 2, 128]), ALU.mult, [p1, tri], [BCm])
        e_tt(p, "dve", xdt[:].rearrange("q (h e) -> q h e", h=8), X[:].rearrange("q (h e) -> q h e", h=8),
             dtd.unsqueeze(2).to_broadcast([128, 8, 64]), ALU.mult, [X, dt], [xdt])
        e_tt(p, "dve", xw[:].rearrange("q (h e) -> q h e", h=8), xdt[:].rearrange("q (h e) -> q h e", h=8),
             wdec[:].unsqueeze(2).to_broadcast([128, 8, 64]), ALU.mult, [xdt, wdec], [xw])
        if islat:
            pyd = p.pb[4]
            for hh in range(8):
                e_mm(p, p.pb[2 + hh // 4][:, (hh % 4) * 128:(hh % 4 + 1) * 128], dabc[:, hh, :], tri[:], True, True,
                     [dabc, tri], [p.pb[2 + hh // 4]])
            for half in range(2):
                pr = p.pb[2 + half]; t_ = T["tmpH"][half]; e_ = T["exH"][half]; M = T["MH"][half]
                e_tt(p, "dve", t_[:], pr[:].rearrange("q (h s) -> q h s", h=4),
                     acs[:, 4 * half:4 * half + 4].unsqueeze(2).to_broadcast([128, 4, 128]), ALU.subtract, [pr, acs], [t_])
                e_ts(p, "dve", t_[:], t_[:], 0.0, 0.0, ALU.min, ALU.add, [t_], [t_])
                e_act(p, e_[:], t_[:], AF.Exp, [t_], [e_])
                e_tt(p, "dve", M[:], e_[:], BCm[:, half, :].unsqueeze(1).to_broadcast([128, 4, 128]), ALU.mult, [e_, BCm], [M])
                for h4 in range(4):
                    hh = 4 * half + h4
                    e_mm(p, pyd[:, hh * 64:(hh + 1) * 64], M[:, h4, :], xdt[:, hh * 64:(hh + 1) * 64], True, True, [M, xdt], [pyd])
            pyo = p.pb[5]
            for g in range(2):
                e_mm(p, pyo[:, g * 256:(g + 1) * 256], CT[:, g, :], Sb[:, g * 256:(g + 1) * 256], True, True, [CT, Sb], [pyo])
            e_tt(p, "dve", yt[:].rearrange("q (h e) -> q h e", h=8), pyo[:].rearrange("q (h e) -> q h e", h=8),
                 ea[:].unsqueeze(2).to_broadcast([128, 8, 64]), ALU.mult, [pyo, ea], [yt])
            e_tt(p, "dve", yt[:], yt[:], pyd[:], ALU.add, [yt, pyd], [yt])
            p.dma(ydst[d][c], yt[:], reads=[yt], writes=[ydst[d]])
        pst = p.pb[6]
        for g in range(2):
            e_mm(p, pst[:, g * 256:(g + 1) * 256], Bt[:, g * 128:(g + 1) * 128], xw[:, g * 256:(g + 1) * 256],
                 True, True, [Bt, xw], [pst])
        e_tt(p, "dve", S[:].rearrange("q (h e) -> q h e", h=8), S[:].rearrange("q (h e) -> q h e", h=8),
             dch[:].unsqueeze(2).to_broadcast([128, 8, 64]), ALU.mult, [S, dch], [S])
        e_tt(p, "dve", S[:], S[:], pst[:], ALU.add, [S, pst], [S])
        e_copy(p, "act", Sb[:], S[:], [S], [Sb])

    for pos in range(NCH):
        chunk(0, pos, orders[0][pos])
        chunk(1, pos, orders[1][pos])

    Xc = [p.alloc([128, 512], BF16) for _ in range(nb)]; Zc = [p.alloc([128, 512], BF16) for _ in range(nb)]
    yfc = [p.alloc([128, 512]) for _ in range(nb)]; ybc = [p.alloc([128, 512]) for _ in range(nb)]
    v = [p.alloc([128, 512]) for _ in range(nb)]; vb = p.alloc([128, 512], BF16); vT = p.alloc([128, 4, 128], BF16)
    junk = p.alloc([128, 512]); orow = [p.alloc([128, RSW]) for _ in range(2)]
    for c in range(2, NCH):
        b_ = c % nb
        X = Xc[b_]; Z = Zc[b_]; yf = yfc[b_]; yb = ybc[b_]; vv = v[b_]; o = orow[b_]
        p.dma(X[:], sc["X"][c], reads=[sc["X"]], writes=[X])
        p.dma(Z[:], sc["Z"][c], reads=[sc["Z"]], writes=[Z])
        p.dma(yf[:], sc["yf"][c], reads=[sc["yf"]], writes=[yf])
        p.dma(yb[:], sc["yb"][c], reads=[sc["yb"]], writes=[yb])
        e_tt(p, "dve", vv[:].rearrange("q (h e) -> q h e", h=8), X[:].rearrange("q (h e) -> q h e", h=8),
             dsk[:].unsqueeze(2).to_broadcast([128, 8, 64]), ALU.mult, [X, dsk], [vv])
        e_tt(p, "dve", yf[:], yf[:], yb[:], ALU.add, [yf, yb], [yf])
        e_tt(p, "dve", vv[:], vv[:], yf[:], ALU.add, [vv, yf], [vv])
        e_tt(p, "dve", vv[:], vv[:], Z[:], ALU.mult, [vv, Z], [vv])
        e_act(p, junk[:], vv[:], AF.Square, [vv], [junk, o], accum_out=o[:, 1024:1025])
        e_tt(p, "dve", vb[:], vv[:], sng[:], ALU.mult, [vv, sng], [vb])
        pt = p.pb[c % 2]; ptb = pt.t.bitcast(BF16)
        for k_ in range(4):
            e_tr(p, ptb[:, k_ * 128:(k_ + 1) * 128], vb[:, k_ * 128:(k_ + 1) * 128], identb[:], [vb, identb], [pt])
        e_copy(p, "act", vT[:], ptb[:, 0:512].rearrange("q (k s) -> q k s", k=4), [pt], [vT])
        for dh in range(2):
            pp = p.pb[2 + (2 * c + dh) % 4]
            for k_ in range(4):
                e_mm(p, pp[:], vT[:, k_, :], Wo[:, k_, dh * 512:(dh + 1) * 512], k_ == 0, k_ == 3, [vT, Wo], [pp])
            e_copy(p, "act", o[:, dh * 512:(dh + 1) * 512], pp[:], [pp], [o])
        p.op("pool", lambda h, o=o: h.memset(o[:, 1025:RSW], 0.0), writes=[o])
        w = c - 2
        p.dma(sc["rsrc"][w * 128:(w + 1) * 128, :], o[:], reads=[o], writes=[sc["rsrc"]])
    p.release(m)


def l1_tail(p, io, sc, xg, onehot, ident, out_dram):
    modsc = sc["modsc1"]
    gates = p.alloc([128, 16, 16]); h2T = p.alloc([128, 8, NL], BF16)
    m = p.mark()
    n2 = p.alloc([128, D]); load_bcast(p, n2[:], io["n2g1"][0:1, :], [], [n2])
    g1 = p.alloc([128, D]); a2 = p.alloc([128, D]); s2 = p.alloc([128, D])
    load_bcast(p, g1[:], modsc[0:1, 2 * D:3 * D], [modsc], [g1])
    load_bcast(p, s2[:], modsc[0:1, 3 * D:4 * D], [modsc], [s2])
    load_bcast(p, a2[:], modsc[0:1, 4 * D:5 * D], [modsc], [a2])
    e_stt(p, a2[:], a2[:], 1.0, n2[:], ALU.add, ALU.mult, [a2, n2], [a2])
    rw = p.alloc([128, 8, 16]); p.dma(rw[:], io["rw"][:].rearrange("(k q) n -> q k n", q=128), writes=[rw])
    rb = p.alloc([128, 16]); load_bcast(p, rb[:], io["rb"][0:1, :], [], [rb])
    rt = [p.alloc([128, RSW]) for _ in range(2)]
    xq = [p.alloc([128, D]) for _ in range(2)]; xa = p.alloc([128, D]); x1 = [p.alloc([128, D]) for _ in range(2)]
    hn2 = p.alloc([128, D]); junk = p.alloc([128, D]); hT32 = p.alloc([128, 8, 128]); sm = p.alloc([128, 128])
    ss = p.alloc([128, 1]); rs = p.alloc([128, 1]); rsd = p.alloc([128, 1])
    xgv = xg[:].rearrange("(r w) d -> w r d", w=64)
    iq = 0
    for t in range(16):
        r_ = rt[t % 2]; xo = x1[t % 2]
        p.dma(r_[:], sc["rdst"][t * 128:(t + 1) * 128, :], reads=[sc["rdst"]], writes=[r_])
        for j in range(4):
            xj = xq[iq % 2]; iq += 1
            p.dma(xj[:], xgv[16 * j + t], reads=[xg], writes=[xj])
            if j == 0:
                e_ts(p, "dve", xa[:], xj[:], onehot[:, 0:1], 0.0, ALU.mult, ALU.add, [xj, onehot], [xa])
            else:
                e_stt(p, xa[:], xj[:], onehot[:, j:j + 1], xa[:], ALU.mult, ALU.add, [xj, onehot, xa], [xa])
        e_ts(p, "dve", rsd[:], r_[:, 1024:1025], 1.0 / 2048, 1e-6, ALU.mult, ALU.add, [r_], [rsd])
        e_act(p, rsd[:], rsd[:], AF.Ln, [rsd], [rsd])
        e_act(p, rsd[:], rsd[:], AF.Exp, [rsd], [rsd], scale=-0.5)
        e_stt(p, xo[:], r_[:, 0:D], rsd[:], g1[:], ALU.mult, ALU.mult, [r_, rsd, g1], [xo])
        e_tt(p, "dve", xo[:], xo[:], xa[:], ALU.add, [xo, xa], [xo])
        tok = slice(t * 128, (t + 1) * 128)
        p.dma(sc["x1b"][tok, :], xo[:], reads=[xo], writes=[sc["x1b"]])
        e_act(p, junk[:], xo[:], AF.Square, [xo], [junk, ss], accum_out=ss[:])
        e_ts(p, "dve", rs[:], ss[:], 1.0 / D, 1e-6, ALU.mult, ALU.add, [ss], [rs])
        e_act(p, rs[:], rs[:], AF.Ln, [rs], [rs])
        e_act(p, rs[:], rs[:], AF.Exp, [rs], [rs], scale=-0.5)
        e_stt(p, hn2[:], xo[:], rs[:], a2[:], ALU.mult, ALU.mult, [xo, rs, a2], [hn2])
        e_tt(p, "dve", hn2[:], hn2[:], s2[:], ALU.add, [hn2, s2], [hn2])
        for half in range(2):
            ps = p.pb[4 + half]
            for c4 in range(4):
                ct = half * 4 + c4
                e_tr(p, ps[:, c4 * 128:(c4 + 1) * 128], hn2[:, ct * 128:(ct + 1) * 128], ident[:], [hn2, ident], [ps])
            pv = ps[:].rearrange("q (c n) -> q c n", c=4)
            e_copy(p, "act", h2T[:, half * 4:(half + 1) * 4, tok], pv, [ps], [h2T])
            e_copy(p, "dve", hT32[:, half * 4:(half + 1) * 4, :], pv, [ps], [hT32])
        pl = p.pb[6]
        for k in range(8):
            e_mm(p, pl[:, 0:16], hT32[:, k, :], rw[:, k, :], k == 0, k == 7, [hT32, rw], [pl])
        routing(p, pl, rb, sm, gates[:, t, :], gates)
    p.release(m)
    fg = p.alloc([128, D]); load_bcast(p, fg[:], io["fing"][0:1, :], [], [fg])
    junk2 = p.alloc([128, D]); ss2 = p.alloc([128, 1]); rs2 = p.alloc([128, 1])

    def out_cb(t, x):
        e_act(p, junk2[:], x[:], AF.Square, [x], [junk2, ss2], accum_out=ss2[:])
        e_ts(p, "dve", rs2[:], ss2[:], 1.0 / D, 1e-6, ALU.mult, ALU.add, [ss2], [rs2])
        e_act(p, rs2[:], rs2[:], AF.Ln, [rs2], [rs2])
        e_act(p, rs2[:], rs2[:], AF.Exp, [rs2], [rs2], scale=-0.5)
        e_stt(p, x[:], x[:], rs2[:], fg[:], ALU.mult, ALU.mult, [x, rs2, fg], [x])
        p.dma(out_dram[t * 128:(t + 1) * 128, :], x[:], reads=[x], writes=[out_dram])

    phase_moe(p, io, (io["w1b"], io["w3b"], io["w2b"]), modsc, h2T, gates, sc["x1b"], 16, out_cb, lambda t: 0)


def layer1(p, io, sc, xg, ctx1, ident, onehot, out_dram, dbg=None, mod_done=False):
    p.identb = p.alloc([128, 128], BF16)
    e_copy(p, "dve", p.identb[:], ident[:], [ident], [p.identb])
    if not mod_done:
        phase_mod(p, io["cT"], io["modw1"], io["modb1"], sc["modsc1"], ident)
    l1_proj(p, io, sc, xg, ctx1, ident)
    l1_conv(p, io, sc, p.identb)
    l1_ssd(p, io, sc)
    p.cc(lambda h: h.collective_compute("ReduceScatter", ALU.add, replica_groups=[[0, 1, 2, 3], [4, 5, 6, 7]],
                                        ins=[sc["rsrc"].t.opt()], outs=[sc["rdst"].t.opt()]),
         reads=[sc["rsrc"]], writes=[sc["rdst"]])
    l1_tail(p, io, sc, xg, onehot, ident, out_dram)


def host_inputs_l1(z):
    i = 1
    maps = []
    inw = z["ssd_in_w"][0]
    for k in range(8):
        bb = k // 4; j = k % 4
        cT = np.stack([z["c"][bb], z["c_ctx"]], axis=-1).reshape(8, 128, 2).transpose(1, 0, 2)
        wz = inw[:, 512 * j:512 * (j + 1)]
        wx = inw[:, 2048 + 512 * j:2048 + 512 * (j + 1)]
        wB = inw[:, 4096 + 256 * j:4096 + 256 * (j + 1)]
        wC = inw[:, 5120 + 256 * j:5120 + 256 * (j + 1)]
        wdt = np.concatenate([inw[:, 6144 + 8 * j:6144 + 8 * (j + 1)], inw[:, 6176 + 8 * j:6176 + 8 * (j + 1)]], 1)
        chs = np.concatenate([np.arange(512 * j, 512 * (j + 1)), 2048 + np.arange(256 * j, 256 * (j + 1)),
                              3072 + np.arange(256 * j, 256 * (j + 1))])
        cw = z["ssd_conv_w"][0][:, chs]
        convw = cw.T.reshape(8, 128, 5).transpose(1, 0, 2)
        convb = z["ssd_conv_b"][0][chs].reshape(8, 128).T
        hs = slice(8 * j, 8 * (j + 1))
        dtb = np.concatenate([z["ssd_dt_bias"][0, 0, hs], z["ssd_dt_bias"][0, 1, hs]])[None]
        alog = np.concatenate([z["ssd_a_log"][0, 0, hs], z["ssd_a_log"][0, 1, hs]])[None]
        oh = np.zeros((128, 4), np.float32); oh[:, j] = 1
        maps.append(dict(
            cT=np.ascontiguousarray(cT), modw1=z["mod_w"][i], modb1=z["mod_b"][i][None], n1g1=z["norm1_g"][i][None],
            n2g1=z["norm2_g"][i][None], fing=z["final_g"][None], wz=np.ascontiguousarray(wz),
            wxbc=np.ascontiguousarray(np.concatenate([wx, wB, wC], 1)), wdt=np.ascontiguousarray(wdt),
            convw=np.ascontiguousarray(convw), convb=np.ascontiguousarray(convb), dtb=np.ascontiguousarray(dtb),
            alog=np.ascontiguousarray(alog), dsk=z["ssd_d"][0][hs][None].copy(),
            sng=z["ssd_norm_g"][0][512 * j:512 * (j + 1)][None].copy(),
            outw=np.ascontiguousarray(z["ssd_out_w"][0][512 * j:512 * (j + 1), :]),
            triU=np.triu(np.ones((128, 128), np.float32)), triL=np.tril(np.ones((128, 128), np.float32)),
            ones=np.ones((128, 128), np.float32), rw=z["router_w"], rb=z["router_b"][None],
            w1b=z["moe_w1"][i], w3b=z["moe_w3"][i], w2b=z["moe_w2"][i],
            ident=np.eye(128, dtype=np.float32), onehot=oh))
    return maps


def colmajor(xb):
    return xb.reshape(128, 64, -1).transpose(1, 0, 2).reshape(8192, -1)


def build_full():
    p = Prog(); p.init_pool()
    io = {}
    for name, shape in L0_INPUTS + L1_INPUTS:
        if name not in io:
            io[name] = p.dram(name, shape)
    out = p.dram("out", [2048, D], kind="ExternalOutput")
    sc0 = dict(modsc=p.dram("modsc", [2, 6144], kind="Internal"),
               tabsc=p.dram("tabsc", [64, 2, 128, 2048], kind="Internal"),
               fsrc=p.dram("fsrc", [128, 128], kind="Internal"), fdst=p.dram("fdst", [512, 128], kind="Internal"),
               x1sc=p.dram("x1sc", [NT, D], kind="Internal"))
    xsrcs = [p.dram(f"xsrc{a}", [128, 2048], kind="Internal") for a in range(8)]
    xdsts = [p.dram(f"xdst{a}", [512, 2048], kind="Internal") for a in range(8)]
    xg = p.dram("xg", [8192, D], kind="Internal")
    ctx1 = p.dram("ctx1", [NCX, D], kind="Internal")
    sc1 = l1_scratch(p)
    ident = p.alloc([128, 128]); p.dma(ident[:], io["ident"][:], writes=[ident])
    onehot = p.alloc([128, 4]); p.dma(onehot[:], io["onehot"][:], writes=[onehot])
    m0 = p.mark()

    def out_cb(t, x):
        if t < 2:
            p.dma(ctx1[t * 128:(t + 1) * 128, :], x[:], reads=[x], writes=[ctx1])
        else:
            a = (t - 2) // 2; half = (t - 2) % 2
            dst = xsrcs[a][:].rearrange("i (b d) -> (i b) d", d=D)[half * 128:(half + 1) * 128, :]
            p.dma(dst, x[:], reads=[x], writes=[xsrcs[a]])

    phase_mod(p, io["cT"], io["modw1"], io["modb1"], sc1["modsc1"], ident)
    layer0(p, io, sc0, ident, onehot, out_cb)
    p.release(m0)
    xgv = xg[:].rearrange("(r a t) d -> a r t d", r=4, a=8)
    for a in range(8):
        p.cc(lambda h, a=a: h.collective_compute("AllGather", ALU.bypass, replica_groups=[[0, 1, 2, 3], [4, 5, 6, 7]],
                                                 ins=[xsrcs[a].t.opt()], outs=[xdsts[a].t.opt()]),
             reads=[xsrcs[a]], writes=[xdsts[a]])
        p.dma(xgv[a], xdsts[a][:].rearrange("(r i) (b d) -> r (i b) d", r=4, d=D), reads=[xdsts[a]], writes=[xg])
    layer1(p, io, sc1, xg, ctx1, ident, onehot, out, mod_done=True)
    p.wait_all()
    return p


def kernel(**inputs):
    z = {k: np.asarray(v) for k, v in inputs.items()}
    m0 = host_inputs_l0(z)
    m1 = host_inputs_l1(z)
    maps = []
    for k in range(8):
        d = dict(m0[k]); d.update(m1[k])
        maps.append({kk: np.ascontiguousarray(vv, dtype=np.float32) for kk, vv in d.items()})
    p = build_full()
    nc = p.finish()
    res = run_bass_kernel_spmd(nc, maps, core_ids=list(range(8))).results
    outp = np.zeros((2, 8192, D), np.float32)
    for bb in range(2):
        cm = np.concatenate([res[4 * bb + j]["out"] for j in range(4)], 0)
        outp[bb] = cm.reshape(64, 128, D).transpose(1, 0, 2).reshape(8192, D)
    return outp
```
